# Optimizing a Trainium2 kernel written in Bass

```python
import math
import jax, jax.numpy as jnp
from jax import lax
import numpy as np

D_MODEL = 1024
BATCH = 8
SEQ = 8192
DEPTH = 2

HEAD_DIM = 64
N_HEADS_A = 8
WIDTH_A = N_HEADS_A * HEAD_DIM
KV_RANK = 128
N_IDX_HEADS = 4
IDX_DIM = 64
INDEX_TOPK = 256
N_HEADS_B = 8
WIDTH_B = N_HEADS_B * HEAD_DIM
Q_BLOCK = 128
N_BUCKETS = 32
MAX_DISTANCE = 128
N_EXPERTS = 32
TOP_K = 4
D_EXPERT = 1024
SWIGLU_LIMIT = 7.0
SWIGLU_ALPHA = 1.702
MOE_BLOCK = 256
LN_EPS = 1e-5
RMS_EPS = 1e-6
DN_ALPHA = (2 * DEPTH) ** 0.25
DN_BETA = (8 * DEPTH) ** -0.25
COL_SPLITS = (WIDTH_A, KV_RANK, N_IDX_HEADS * IDX_DIM, IDX_DIM, N_IDX_HEADS,
              WIDTH_B, WIDTH_B, WIDTH_B, D_MODEL, D_MODEL)
N_COLS = sum(COL_SPLITS)

kernel_name = "hybrid_dsa_stickbreak_moe_deepnorm"


def layer_norm_f32(x):
    xf = x.astype(jnp.float32)
    mu = jnp.mean(xf, axis=-1, keepdims=True)
    var = jnp.mean(jnp.square(xf - mu), axis=-1, keepdims=True)
    return (xf - mu) * lax.rsqrt(var + LN_EPS)


def modulate(x, shift, scale):
    return (layer_norm_f32(x) * (1.0 + scale.astype(jnp.float32)) + shift.astype(jnp.float32)).astype(x.dtype)


def post_norm(z, g, b):
    return (layer_norm_f32(z) * g.astype(jnp.float32) + b.astype(jnp.float32)).astype(z.dtype)


def rms_norm(x, g):
    xf = x.astype(jnp.float32)
    y = xf * lax.rsqrt(jnp.mean(xf * xf, axis=-1, keepdims=True) + RMS_EPS)
    return (y * g.astype(jnp.float32)).astype(x.dtype)


def split_cols(p):
    out, o = [], 0
    for w in COL_SPLITS:
        out.append(p[..., o:o + w])
        o += w
    return out


def t5_bucket(n):
    max_exact = N_BUCKETS // 2
    nf = jnp.maximum(n, 1).astype(jnp.float32)
    large = max_exact + (jnp.log(nf / max_exact) / math.log(MAX_DISTANCE / max_exact)
                         * (N_BUCKETS - max_exact)).astype(jnp.int32)
    large = jnp.minimum(large, N_BUCKETS - 1)
    return jnp.where(n < max_exact, n, large)


def dsa_attention(q_lat, c_kv, q_idx, k_idx, w_idx, rel_bias):
    B, S = c_kv.shape[:2]
    n_blocks = S // Q_BLOCK
    topk = min(INDEX_TOPK, S // 4)
    key_pos = jnp.arange(S)

    def block(i):
        t0 = i * Q_BLOCK
        qpos = t0 + jnp.arange(Q_BLOCK)
        qi = lax.dynamic_slice_in_dim(q_idx, t0, Q_BLOCK, axis=1)
        wi = lax.dynamic_slice_in_dim(w_idx, t0, Q_BLOCK, axis=1)
        ql = lax.dynamic_slice_in_dim(q_lat, t0, Q_BLOCK, axis=1)
        dots = jax.nn.relu(jnp.einsum('bthd,bsd->bths', qi, k_idx).astype(jnp.float32))
        score = jnp.einsum('bths,bth->bts', dots, wi.astype(jnp.float32))
        causal = key_pos[None, :] <= qpos[:, None]
        score = jnp.where(causal[None], score, -jnp.inf)
        _, sel = lax.top_k(score, topk)
        c_sel = jax.vmap(lambda cb, ib: jnp.take(cb, ib, axis=0))(c_kv, sel)
        logits = jnp.einsum('bthr,btkr->bthk', ql, c_sel).astype(jnp.float32)
        dist = qpos[None, :, None] - sel
        bias = rel_bias[t5_bucket(jnp.maximum(dist, 0))]
        logits = logits + jnp.moveaxis(bias, -1, 2).astype(jnp.float32)
        logits = jnp.where((dist >= 0)[:, :, None, :], logits, -jnp.inf)
        p = jax.nn.softmax(logits, axis=-1).astype(c_kv.dtype)
        return jnp.einsum('bthk,btkr->bthr', p, c_sel)

    o = lax.map(block, jnp.arange(n_blocks))
    return jnp.moveaxis(o, 0, 1).reshape(B, S, N_HEADS_A, KV_RANK)


def stick_breaking_attention(q, k, v):
    B, S = q.shape[:2]
    n_blocks = S // Q_BLOCK
    key_pos = jnp.arange(S)
    scale = HEAD_DIM ** -0.5

    def block(i):
        t0 = i * Q_BLOCK
        qpos = t0 + jnp.arange(Q_BLOCK)
        qb = lax.dynamic_slice_in_dim(q, t0, Q_BLOCK, axis=1)
        z = jnp.einsum('bthd,bshd->bhts', qb, k).astype(jnp.float32) * scale
        strict = (key_pos[None, :] < qpos[:, None])[None, None]
        log_fail = jnp.where(strict, jax.nn.log_sigmoid(-z), 0.0)
        later = lax.cumsum(log_fail, axis=3, reverse=True) - log_fail
        a = jnp.where(strict, jnp.exp(jax.nn.log_sigmoid(z) + later), 0.0)
        return jnp.einsum('bhts,bshd->bthd', a.astype(v.dtype), v)

    o = lax.map(block, jnp.arange(n_blocks))
    return jnp.moveaxis(o, 0, 1).reshape(B, S, WIDTH_B)


def moe_ffn(h, w_router, b_router, w_gu, b_gu, w_dn, b_dn):
    B, S, D = h.shape
    N = B * S
    xt = h.reshape(N, D)
    logits = (xt @ w_router + b_router).astype(jnp.float32)
    top_val, top_idx = lax.top_k(logits, TOP_K)
    gate = jax.nn.softmax(top_val, axis=-1)
    flat_e = top_idx.reshape(-1)
    flat_g = gate.reshape(-1)
    order = jnp.argsort(flat_e)
    sorted_e = flat_e[order]
    token = (order // TOP_K).astype(jnp.int32)
    counts = jnp.zeros((N_EXPERTS,), jnp.int32).at[flat_e].add(1)
    padded = (counts + MOE_BLOCK - 1) // MOE_BLOCK * MOE_BLOCK
    start = jnp.cumsum(counts) - counts
    pad_end = jnp.cumsum(padded)
    pad_start = pad_end - padded
    dest = pad_start[sorted_e] + jnp.arange(N * TOP_K, dtype=jnp.int32) - start[sorted_e]
    n_blocks = -(-(N * TOP_K + N_EXPERTS * (MOE_BLOCK - 1)) // MOE_BLOCK)
    rows = n_blocks * MOE_BLOCK
    row_token = jnp.full((rows,), N, jnp.int32).at[dest].set(token)
    row_gate = jnp.zeros((rows,), jnp.float32).at[dest].set(flat_g[order])
    block_expert = jnp.minimum(
        jnp.searchsorted(pad_end, jnp.arange(n_blocks, dtype=jnp.int32) * MOE_BLOCK, side='right'),
        N_EXPERTS - 1)
    x_pad = jnp.concatenate([xt, jnp.zeros((1, D), xt.dtype)], axis=0)

    def expert_block(args):
        tok, g, e = args
        xb = x_pad[tok]
        gu = xb @ w_gu[e] + b_gu[e]
        a, u = jnp.split(gu, 2, axis=-1)
        a = jnp.minimum(a, SWIGLU_LIMIT)
        u = jnp.clip(u, -SWIGLU_LIMIT, SWIGLU_LIMIT)
        y = ((u + 1.0) * a * jax.nn.sigmoid(SWIGLU_ALPHA * a)) @ w_dn[e] + b_dn[e]
        return y * g[:, None].astype(y.dtype)

    ys = lax.map(expert_block, (row_token.reshape(n_blocks, MOE_BLOCK),
                                row_gate.reshape(n_blocks, MOE_BLOCK), block_expert))
    out = jax.ops.segment_sum(ys.reshape(rows, D), row_token, num_segments=N + 1)[:N]
    return out.reshape(B, S, D)


def setup_inputs(seed: int = 0) -> dict:
    key = jax.random.key(seed)
    ks = jax.random.split(key, 24)
    L, D, E, F = DEPTH, D_MODEL, N_EXPERTS, D_EXPERT

    def nrm(k, shape, s):
        return jax.random.normal(k, shape, jnp.float32) * s

    col_scale = jnp.concatenate([
        jnp.ones((sum(COL_SPLITS[:7]),), jnp.float32),
        jnp.full((WIDTH_B,), DN_BETA, jnp.float32),
        jnp.ones((2 * D,), jnp.float32)])
    return {
        "x": nrm(ks[0], (BATCH, SEQ, D), 1.0),
        "c": nrm(ks[1], (BATCH, D), 1.0),
        "rel_bias": nrm(ks[2], (N_BUCKETS, N_HEADS_A), 0.5),
        "w_ada": nrm(ks[3], (L, D, 6 * D), 0.5 * D ** -0.5),
        "b_ada": nrm(ks[4], (L, 6 * D), 0.01),
        "w_in": nrm(ks[5], (L, D, N_COLS), D ** -0.5) * col_scale,
        "g_kv": 1.0 + nrm(ks[6], (L, KV_RANK), 0.02),
        "w_uk": nrm(ks[7], (L, KV_RANK, N_HEADS_A, HEAD_DIM), KV_RANK ** -0.5),
        "w_uv": nrm(ks[8], (L, KV_RANK, N_HEADS_A, HEAD_DIM), KV_RANK ** -0.5 * DN_BETA),
        "w_a_out": nrm(ks[9], (L, WIDTH_A, D), WIDTH_A ** -0.5 * DN_BETA),
        "w_b_out": nrm(ks[10], (L, WIDTH_B, D), WIDTH_B ** -0.5 * DN_BETA),
        "w_o": nrm(ks[11], (L, D, D), D ** -0.5 * DN_BETA),
        "ln1_g": 1.0 + nrm(ks[12], (L, D), 0.02),
        "ln1_b": nrm(ks[13], (L, D), 0.01),
        "w_router": nrm(ks[14], (L, D, E), D ** -0.5),
        "b_router": nrm(ks[15], (L, E), 0.01),
        "w_gu": nrm(ks[16], (L, E, D, 2 * F), D ** -0.5),
        "b_gu": nrm(ks[17], (L, E, 2 * F), 0.01),
        "w_dn": nrm(ks[18], (L, E, F, D), F ** -0.5 * DN_BETA),
        "b_dn": nrm(ks[19], (L, E, D), 0.01),
        "ln2_g": 1.0 + nrm(ks[20], (L, D), 0.02),
        "ln2_b": nrm(ks[21], (L, D), 0.01),
    }


def reference(x, c, rel_bias, w_ada, b_ada, w_in, g_kv, w_uk, w_uv, w_a_out, w_b_out, w_o,
              ln1_g, ln1_b, w_router, b_router, w_gu, b_gu, w_dn, b_dn, ln2_g, ln2_b):
    B, S, D = x.shape
    cond = jax.nn.silu(c)
    idx_scale = (N_IDX_HEADS ** -0.5) * (IDX_DIM ** -0.5)
    for l in range(DEPTH):
        mod = cond @ w_ada[l] + b_ada[l]
        shift1, scale1, gate1, shift2, scale2, gate2 = [m[:, None, :] for m in jnp.split(mod, 6, axis=-1)]

        h = modulate(x, shift1, scale1)
        q_a, c_kv, q_idx, k_idx, w_idx, q_b, k_b, v_b, g_a, g_b = split_cols(h @ w_in[l])
        c_kv = rms_norm(c_kv, g_kv[l])
        q_a = q_a.reshape(B, S, N_HEADS_A, HEAD_DIM)
        q_lat = jnp.einsum('bshd,rhd->bshr', q_a, w_uk[l]) * (HEAD_DIM ** -0.5)
        o_lat = dsa_attention(q_lat, c_kv,
                              q_idx.reshape(B, S, N_IDX_HEADS, IDX_DIM), k_idx,
                              w_idx * idx_scale, rel_bias)
        o_a = jnp.einsum('bshr,rhd->bshd', o_lat, w_uv[l]).reshape(B, S, WIDTH_A)
        o_b = stick_breaking_attention(q_b.reshape(B, S, N_HEADS_B, HEAD_DIM),
                                       k_b.reshape(B, S, N_HEADS_B, HEAD_DIM),
                                       v_b.reshape(B, S, N_HEADS_B, HEAD_DIM))
        merged = jax.nn.sigmoid(g_a) * (o_a @ w_a_out[l]) + jax.nn.sigmoid(g_b) * (o_b @ w_b_out[l])
        y = merged @ w_o[l]
        x = post_norm(DN_ALPHA * x + gate1 * y, ln1_g[l], ln1_b[l])

        h = modulate(x, shift2, scale2)
        y = moe_ffn(h, w_router[l], b_router[l], w_gu[l], b_gu[l], w_dn[l], b_dn[l])
        x = post_norm(DN_ALPHA * x + gate2 * y, ln2_g[l], ln2_b[l])
    return x
```

```python
import contextlib
import numpy as np
import ml_dtypes
import concourse.bass as bass
import concourse.mybir as mybir
from concourse.bass_utils import run_bass_kernel_spmd

F32 = mybir.dt.float32
BF16 = mybir.dt.bfloat16
AF = mybir.ActivationFunctionType
ALU = mybir.AluOpType
AX = mybir.AxisListType

D = 1024
S = 8192
DEPTH = 2
NT = S // 128
NST = S // 512
HD = 64
NCOLS = 4548
NW1 = 2500
E = 32
FF = 1024
LN_EPS = 1e-5
RMS_EPS = 1e-6
DN_ALPHA = (2 * DEPTH) ** 0.25
TOPK = 256
NEG = -1.0e30
MBIG = -30000.0


class Res:
    __slots__ = ("wc", "wd", "rc", "rd", "name", "psum")

    def __init__(self, name="", psum=False):
        self.psum = psum
        self.wc = {}
        self.wd = []
        self.rc = {}
        self.rd = []
        self.name = name


class KB:
    COMPUTE = ("pe", "act", "dve", "pool")

    def __init__(self, nc, es):
        self.nc = nc
        self.es = es
        self.eng = {"pe": nc.tensor, "act": nc.scalar, "dve": nc.vector, "pool": nc.gpsimd, "sp": nc.sync}
        self.sem = {e: es.enter_context(nc.semaphore("s_" + e)) for e in self.COMPUTE}
        self.cnt = {e: 0 for e in self.COMPUTE}
        self.known = {e: {} for e in self.eng}
        self.NSD = 8
        self.dq = {}
        self.ninst = 0

    def _dq(self, q):
        if q not in self.dq:
            self.dq[q] = {"sems": [self.es.enter_context(self.nc.semaphore(f"d_{q}_{i}")) for i in range(self.NSD)],
                          "n": 0}
        return self.dq[q]

    def _wait(self, e, tok):
        if tok[0] == "c":
            _, e2, seq = tok
            if e == "pe" and e2 == "pe":
                return
            if self.known[e].get(e2, 0) >= seq:
                return
            self.eng[e].wait_ge(self.sem[e2], seq)
            self.known[e][e2] = seq
        else:
            _, q, slot, val = tok
            key = (q, slot)
            if self.known[e].get(key, 0) >= val:
                return
            self.eng[e].wait_ge(self.dq[q]["sems"][slot], val)
            self.known[e][key] = val
        self.ninst += 1

    def _deps(self, e, reads, writes):
        for r in reads:
            for e2, seq in r.wc.items():
                self._wait(e, ("c", e2, seq))
            for tok in r.wd:
                self._wait(e, tok)
            if r.psum:
                for e2, seq in r.rc.items():
                    if e2 != e:
                        self._wait(e, ("c", e2, seq))
        for w in writes:
            for e2, seq in w.wc.items():
                self._wait(e, ("c", e2, seq))
            for tok in w.wd:
                self._wait(e, tok)
            for e2, seq in w.rc.items():
                self._wait(e, ("c", e2, seq))
            for tok in w.rd:
                self._wait(e, tok)

    def op(self, e, fn, reads=(), writes=()):
        self._deps(e, reads, writes)
        ins = fn(self.eng[e])
        self.cnt[e] += 1
        seq = self.cnt[e]
        ins.then_inc(self.sem[e], 1)
        self.ninst += 1
        for r in reads:
            r.rc[e] = seq
        for w in writes:
            w.wc = {e: seq}
            w.wd = []
            w.rc = {}
            w.rd = []
        return ins

    def dma(self, q, out, in_, reads=(), writes=(), **kw):
        d = self._dq(q)
        k = d["n"]
        slot = k % self.NSD
        val = 16 * (k // self.NSD + 1)
        if k >= self.NSD:
            self._wait(q, ("d", q, slot, val - 16))
        self._deps(q, reads, writes)
        ins = self.eng[q].dma_start(out=out, in_=in_, **kw)
        ins.then_inc(d["sems"][slot], 16)
        d["n"] += 1
        self.ninst += 1
        tok = ("d", q, slot, val)
        for r in reads:
            r.rd.append(tok)
        for w in writes:
            w.wc = {}
            w.wd = [tok]
            w.rc = {}
            w.rd = []
        return ins

    def barrier(self, engines=None):
        engines = engines or list(self.eng)
        for e in engines:
            for e2 in self.COMPUTE:
                if self.cnt[e2] > 0:
                    self._wait(e, ("c", e2, self.cnt[e2]))
            for q, d in self.dq.items():
                n = d["n"]
                for k in range(max(0, n - self.NSD), n):
                    self._wait(e, ("d", q, k % self.NSD, 16 * (k // self.NSD + 1)))


class Ctx:
    pass


_uid = [0]


def T(C, ph, name, shape, dt):
    _uid[0] += 1
    h = ph.enter_context(C.nc.sbuf_tensor(f"{name}_{_uid[0]}", list(shape), dt))
    return h, Res(name)


def dram(C, name, shape, dt):
    return C.nc.dram_tensor(name, list(shape), dt, kind="Internal").ap(), Res(name)


def phase_ada(C):
    nc, kb = C.nc, C.kb
    with contextlib.ExitStack() as ph:
        ccol, r_ccol = T(C, ph, "ccol", [128, 8], F32)
        cond, r_cond = T(C, ph, "cond", [128, 8], F32)
        condB, r_condB = T(C, ph, "condB", [128, 8, 128], F32)
        wb = [T(C, ph, f"wada{i}", [128, 8, 512], F32) for i in range(2)]
        bB, r_bB = T(C, ph, "badaB", [128, 6144], F32)
        mo = [T(C, ph, f"mo{i}", [128, 512], F32) for i in range(2)]
        kb.dma("sp", ccol[:], C.c_col[:, :], writes=[r_ccol])
        kb.op("act", lambda e: e.activation(out=cond[:], in_=ccol[:], func=AF.Silu), reads=[r_ccol], writes=[r_cond])
        for kc in range(8):
            kb.op("dve", lambda e: e.tensor_copy(out=condB[:, kc, :], in_=cond[:, kc:kc + 1].to_broadcast([128, 128])),
                  reads=[r_cond], writes=[r_condB])
        it = 0
        for l in range(DEPTH):
            kb.dma("sp", bB[:], C.b_ada[l:l + 1, :].to_broadcast([128, 6144]), writes=[r_bB])
            for j in range(12):
                w, r_w = wb[it % 2]
                m, r_m = mo[it % 2]
                ps, r_ps = C.ps[it % 2]
                kb.dma("sp", w[:], C.w_ada[l, :, j * 512:(j + 1) * 512].rearrange("(kc p) n -> p kc n", p=128),
                       writes=[r_w])
                for kc in range(8):
                    kb.op("pe", lambda e: e.matmul(ps[:], lhsT=condB[:, kc, :], rhs=w[:, kc, :],
                                                   start=(kc == 0), stop=(kc == 7)),
                          reads=[r_condB, r_w], writes=[r_ps])
                plus1 = 1.0 if j in (2, 3, 8, 9) else 0.0
                kb.op("dve", lambda e: e.scalar_tensor_tensor(out=m[:], in0=ps[:], scalar=plus1,
                                                              in1=bB[:, j * 512:(j + 1) * 512],
                                                              op0=ALU.add, op1=ALU.add),
                      reads=[r_ps, r_bB], writes=[r_m])
                kb.dma("sp", C.mod_d[l, :, j * 512:(j + 1) * 512], m[:], reads=[r_m], writes=[C.r_mod])
                it += 1
        kb.barrier()


def dram(C, name, shape, dt):
    kind = "ExternalOutput" if name in C.dbg else "Internal"
    return C.nc.dram_tensor(name, list(shape), dt, kind=kind).ap(), Res(name)


class Rot:
    def __init__(self, items):
        self.items = list(items)
        self.i = 0

    def next(self):
        it = self.items[self.i % len(self.items)]
        self.i += 1
        return it


def bfv(ps):
    return ps[:].bitcast(BF16)


def ln_modulate(C, W, xt, r_xt, out_ap, r_out, scB, r_scB, shB, r_shB):
    kb = C.kb
    st_, r_st = W["stats"]
    mv, r_mv = W["mv"]
    sd, r_sd = W["sd"]
    rstd, r_rstd = W["rstd"]
    nmr, r_nmr = W["nmr"]
    xn, r_xn = W["xn"]
    epsT, r_eps = W["eps"]
    for c in range(2):
        kb.op("dve", lambda e: e.bn_stats(out=st_[:, c * 6:(c + 1) * 6], in_=xt[:, c * 512:(c + 1) * 512]),
              reads=[r_xt], writes=[r_st])
    kb.op("dve", lambda e: e.bn_aggr(out=mv[:], in_=st_[:]), reads=[r_st], writes=[r_mv])
    kb.op("act", lambda e: e.activation(out=sd[:], in_=mv[:, 1:2], func=AF.Sqrt, bias=epsT[:, 0:1], scale=1.0),
          reads=[r_mv, r_eps], writes=[r_sd])
    kb.op("dve", lambda e: e.reciprocal(out=rstd[:], in_=sd[:]), reads=[r_sd], writes=[r_rstd])
    kb.op("dve", lambda e: e.tensor_scalar(out=nmr[:], in0=mv[:, 0:1], scalar1=rstd[:, 0:1], scalar2=-1.0,
                                           op0=ALU.mult, op1=ALU.mult),
          reads=[r_mv, r_rstd], writes=[r_nmr])
    kb.op("act", lambda e: e.activation(out=xn[:], in_=xt[:], func=AF.Identity, bias=nmr[:, 0:1], scale=rstd[:, 0:1]),
          reads=[r_xt, r_rstd, r_nmr], writes=[r_xn])
    kb.op("pool", lambda e: e.tensor_tensor(out=xn[:], in0=xn[:], in1=scB, op=ALU.mult),
          reads=[r_xn, r_scB], writes=[r_xn])
    kb.op("dve", lambda e: e.tensor_tensor(out=out_ap, in0=xn[:], in1=shB, op=ALU.add),
          reads=[r_xn, r_shB], writes=[r_out])


def ln_work(C, ph, tag, eps):
    W = {
        "stats": T(C, ph, tag + "stats", [128, 12], F32),
        "mv": T(C, ph, tag + "mv", [128, 2], F32),
        "sd": T(C, ph, tag + "sd", [128, 1], F32),
        "rstd": T(C, ph, tag + "rstd", [128, 1], F32),
        "nmr": T(C, ph, tag + "nmr", [128, 1], F32),
        "xn": T(C, ph, tag + "xn", [128, 1024], F32),
        "eps": T(C, ph, tag + "eps", [128, 1], F32),
    }
    e_, r_e = W["eps"]
    C.kb.op("pool", lambda e: e.memset(e_[:], eps), writes=[r_e])
    return W


def phase_proj(C, l, x_src, r_xsrc):
    nc, kb = C.nc, C.kb
    with contextlib.ExitStack() as ph:
        W1, r_W1 = T(C, ph, "W1", [128, 8, NW1], BF16)
        Wkk, r_Wkk = T(C, ph, "Wkk", [128, 8, 128], BF16)
        wuk, r_wuk = T(C, ph, "wuk", [128, 4, 128], BF16)
        gkvB, r_gkvB = T(C, ph, "gkvB", [128, 128], F32)
        scB, r_scB = T(C, ph, "scB", [128, 1024], F32)
        shB, r_shB = T(C, ph, "shB", [128, 1024], F32)
        ident, r_id = T(C, ph, "ident", [128, 128], BF16)
        reps, r_reps = T(C, ph, "reps", [128, 1], F32)
        kb.op("pool", lambda e: e.memset(reps[:], RMS_EPS), writes=[r_reps])
        kb.dma("pool", W1[:], C.w_in[l, :, 0:NW1].rearrange("(kc p) n -> p kc n", p=128), writes=[r_W1])
        for hh in range(2):
            kb.dma("pool", Wkk[:, :, hh * 64:(hh + 1) * 64],
                   C.w_in[l, :, 896:960].rearrange("(kc p) n -> p kc n", p=128), writes=[r_Wkk])
        kb.dma("pool", wuk[:], C.w_uk_t[l].rearrange("(hp two) d r -> (two d) hp r", two=2), writes=[r_wuk])
        kb.dma("sp", gkvB[:], C.g_kv[l:l + 1, :].to_broadcast([128, 128]), writes=[r_gkvB])
        kb.dma("sp", scB[:], C.mod_d[l, :, 1024:2048], reads=[C.r_mod], writes=[r_scB])
        kb.dma("sp", shB[:], C.mod_d[l, :, 0:1024], reads=[C.r_mod], writes=[r_shB])
        kb.dma("sp", ident[:], C.ident_bf[:, :], writes=[r_id])
        LW = [ln_work(C, ph, f"lw{i}", LN_EPS) for i in range(2)]
        xb = [T(C, ph, f"xb{i}", [128, 1024], F32) for i in range(2)]
        hb = [T(C, ph, f"hb{i}", [128, 1024], BF16) for i in range(2)]
        hT = [(T(C, ph, f"hT{i}", [128, 8, 512], BF16)[0], [Res(f"hT{i}_{j}") for j in range(4)]) for i in range(2)]
        qaT = [T(C, ph, f"qaT{i}", [128, 512], BF16) for i in range(2)]
        def stage(name, shape, n):
            return [(T(C, ph, f"{name}{i}", shape, BF16)[0], [Res(f"{name}{i}_{j}") for j in range(n)]) for i in range(2)]
        s_qlat = stage("s_qlat", [128, 8, 512], 8)
        s_qidx = stage("s_qidx", [128, 2, 512], 2)
        s_kidx = stage("s_kidx", [128, 1, 512], 1)
        s_qb = stage("s_qb", [128, 4, 512], 4)
        s_kb = stage("s_kb", [128, 4, 512], 4)
        s_ckvT = stage("s_ckvT", [128, 4, 128], 4)
        s_ckv = stage("s_ckv", [128, 4, 128], 4)
        s_vb = stage("s_vb", [128, 4, 512], 4)
        s_wi = [(T(C, ph, f"s_wi{i}", [128, 4, 4], F32)[0], [Res(f"s_wi{i}_{j}") for j in range(4)]) for i in range(2)]
        ss_t = [T(C, ph, f"ss{i}", [128, 1], F32) for i in range(2)]
        sd2_t = [T(C, ph, f"sd2{i}", [128, 1], F32) for i in range(2)]
        rs_t = [T(C, ph, f"rs{i}", [128, 1], F32) for i in range(2)]
        junk, r_junk = T(C, ph, "junk", [128, 128], F32)
        pp = Rot(C.ps[0:6])
        psT, r_psT = C.ps[7]
        psT2, r_psT2 = C.ps[6]
        ev = Rot(["act", "dve"])

        def evac(eng, out_ap, in_ap, reads, writes, scale=None):
            if eng == "act":
                if scale is None:
                    kb.op("act", lambda e: e.copy(out=out_ap, in_=in_ap), reads=reads, writes=writes)
                else:
                    kb.op("act", lambda e: e.mul(out=out_ap, in_=in_ap, mul=scale), reads=reads, writes=writes)
            else:
                if scale is None:
                    kb.op("dve", lambda e: e.tensor_copy(out=out_ap, in_=in_ap), reads=reads, writes=writes)
                else:
                    kb.op("dve", lambda e: e.tensor_scalar(out=out_ap, in0=in_ap, scalar1=scale, scalar2=None,
                                                           op0=ALU.mult), reads=reads, writes=writes)

        for st in range(NST):
            sp_ = st % 2
            hTt, r_hT = hT[sp_]
            t0 = st * 512
            for j in range(4):
                tt = st * 4 + j
                xt, r_xt = xb[tt % 2]
                hbt, r_hb = hb[tt % 2]
                kb.dma("sp", xt[:], x_src[tt * 128:(tt + 1) * 128, :], reads=[r_xsrc], writes=[r_xt])
                ln_modulate(C, LW[tt % 2], xt, r_xt, hbt[:], r_hb, scB[:], r_scB, shB[:], r_shB)
                for kc in range(8):
                    kb.op("pe", lambda e: e.transpose(out=bfv(psT)[:, kc * 128:(kc + 1) * 128],
                                                      in_=hbt[:, kc * 128:(kc + 1) * 128], identity=ident[:]),
                          reads=[r_hb, r_id], writes=[r_psT])
                evac(ev.next(), hTt[:, :, j * 128:(j + 1) * 128],
                     bfv(psT).rearrange("p (kc t) -> p kc t", kc=8), [r_psT], [r_hT[j]])
            for j in range(4):
                tt = st * 4 + j
                ps, r_ps = pp.next()
                for (c0, c1, o0) in ((512, 640, 0), (960, 964, 128)):
                    for kc in range(8):
                        kb.op("pe", lambda e: e.matmul(ps[:, o0:o0 + (c1 - c0)], lhsT=hTt[:, kc, j * 128:(j + 1) * 128],
                                                       rhs=W1[:, kc, c0:c1], start=(kc == 0), stop=(kc == 7)),
                              reads=[r_hT[j], r_W1], writes=[r_ps])
                ss, r_ss = ss_t[tt % 2]
                sd2, r_sd2 = sd2_t[tt % 2]
                rs, r_rs = rs_t[tt % 2]
                kb.op("act", lambda e: e.activation(out=junk[:], in_=ps[:, 0:128], func=AF.Square, accum_out=ss[:]),
                      reads=[r_ps], writes=[r_junk, r_ss])
                kb.op("act", lambda e: e.activation(out=sd2[:], in_=ss[:], func=AF.Sqrt, bias=reps[:, 0:1],
                                                    scale=1.0 / 128.0), reads=[r_ss, r_reps], writes=[r_sd2])
                kb.op("dve", lambda e: e.reciprocal(out=rs[:], in_=sd2[:]), reads=[r_sd2], writes=[r_rs])
                ckv_t, r_ckv = s_ckv[sp_]
                kb.op("dve", lambda e: e.scalar_tensor_tensor(out=ckv_t[:, j, :], in0=ps[:, 0:128], scalar=rs[:, 0:1],
                                                              in1=gkvB[:], op0=ALU.mult, op1=ALU.mult),
                      reads=[r_ps, r_rs, r_gkvB], writes=[r_ckv[j]])
                wi_t, r_wi = s_wi[sp_]
                kb.op("dve", lambda e: e.tensor_scalar(out=wi_t[:, j, :], in0=ps[:, 128:132], scalar1=1.0 / 16.0,
                                                       scalar2=None, op0=ALU.mult), reads=[r_ps], writes=[r_wi[j]])
                kb.op("pe", lambda e: e.transpose(out=bfv(psT2)[:, j * 128:(j + 1) * 128], in_=ckv_t[:, j, :],
                                                  identity=ident[:]), reads=[r_ckv[j], r_id], writes=[r_psT2])
                ps, r_ps = pp.next()
                for kc in range(8):
                    kb.op("pe", lambda e: e.matmul(ps[:], lhsT=hTt[:, kc, j * 128:(j + 1) * 128],
                                                   rhs=W1[:, kc, 1988:2500], start=(kc == 0), stop=(kc == 7)),
                          reads=[r_hT[j], r_W1], writes=[r_ps])
                vb_t, r_vb = s_vb[sp_]
                evac(ev.next(), vb_t[:, j, :], ps[:], [r_ps], [r_vb[j]])
            ckvT_t, r_ckvT = s_ckvT[sp_]
            evac(ev.next(), ckvT_t[:].rearrange("p j t -> p (j t)"), bfv(psT2)[:, 0:512], [r_psT2], r_ckvT)
            rows = slice(t0, t0 + 512)
            kb.dma("sp", C.ckv_d[rows, :].rearrange("(j p) r -> p j r", p=128), ckv_t[:], reads=r_ckv, writes=[C.r_ckv])
            kb.dma("sp", C.widx_d[rows, :].rearrange("(j p) r -> p j r", p=128), wi_t[:], reads=r_wi, writes=[C.r_widx])
            kb.dma("sp", C.vb_d[rows, :].rearrange("(j p) r -> p j r", p=128), vb_t[:], reads=r_vb, writes=[C.r_vb])
            kb.dma("sp", C.ckvT_d[:, rows], ckvT_t[:].rearrange("p j t -> p (j t)"), reads=r_ckvT, writes=[C.r_ckvT])
            allj = r_hT
            groups = [("qa", 0, 4), ("qidx", 640, 2), ("kidx", None, 1), ("qb", 964, 4), ("kb", 1476, 4)]
            for (gname, c0, nch) in groups:
                for c in range(nch):
                    ps, r_ps = pp.next()
                    for kc in range(8):
                        if gname == "kidx":
                            lw, rl = Wkk[:, kc, :], r_Wkk
                        else:
                            lw, rl = W1[:, kc, c0 + c * 128:c0 + (c + 1) * 128], r_W1
                        kb.op("pe", lambda e: e.matmul(ps[:], lhsT=lw, rhs=hTt[:, kc, :], start=(kc == 0), stop=(kc == 7)),
                              reads=allj + [rl], writes=[r_ps])
                    if gname == "qa":
                        qa, r_qa = qaT[c % 2]
                        evac(ev.next(), qa[:], ps[:], [r_ps], [r_qa])
                        for hp in range(2):
                            h = 2 * c + hp
                            ps2, r_ps2 = pp.next()
                            kb.op("pe", lambda e: e.matmul(ps2[:], lhsT=wuk[hp * 64:(hp + 1) * 64, c, :],
                                                           rhs=qa[hp * 64:(hp + 1) * 64, :], start=True, stop=True),
                                  reads=[r_wuk, r_qa], writes=[r_ps2])
                            stg, r_stg = s_qlat[sp_]
                            evac(ev.next(), stg[:, h, :], ps2[:], [r_ps2], [r_stg[h]], scale=0.125)
                    else:
                        stg, r_stg = {"qidx": s_qidx, "kidx": s_kidx, "qb": s_qb, "kb": s_kb}[gname][sp_]
                        evac(ev.next(), stg[:, c, :], ps[:], [r_ps], [r_stg[c]], scale=(0.125 if gname == "kb" else None))
            cols = slice(t0, t0 + 512)
            stg, r_stg = s_qlat[sp_]
            kb.dma("sp", C.qlatT_d[:, :, cols].rearrange("h p t -> p h t"), stg[:], reads=r_stg, writes=[C.r_qlatT])
            stg, r_stg = s_qidx[sp_]
            kb.dma("sp", C.qidxT_d[:, :, cols].rearrange("h p t -> p h t"), stg[:], reads=r_stg, writes=[C.r_qidxT])
            stg, r_stg = s_kidx[sp_]
            kb.dma("sp", C.kidxT_d[:, cols], stg[:, 0, :], reads=r_stg, writes=[C.r_kidxT])
            stg, r_stg = s_qb[sp_]
            kb.dma("sp", C.qbT_d[:, :, cols].rearrange("h p t -> p h t"), stg[:], reads=r_stg, writes=[C.r_qbT])
            stg, r_stg = s_kb[sp_]
            kb.dma("sp", C.kbT_d[:, :, cols].rearrange("h p t -> p h t"), stg[:], reads=r_stg, writes=[C.r_kbT])
        kb.barrier()


def phase_sb(C, l):
    nc, kb = C.nc, C.kb
    with contextlib.ExitStack() as ph:
        negU, r_negU = T(C, ph, "negU", [128, 128], BF16)
        ones, r_ones = T(C, ph, "ones", [128, 128], BF16)
        cm, r_cm = T(C, ph, "cm", [128, 4, 512], BF16)
        kb.dma("sp", negU[:], C.negU_bf[:, :], writes=[r_negU])
        kb.dma("sp", ones[:], C.ones_bf[:, :], writes=[r_ones])
        kb.dma("sp", cm[:], C.cm_bf.rearrange("r p t -> p r t"), writes=[r_cm])
        qT = [T(C, ph, f"sbq{i}", [128, S], BF16) for i in range(2)]
        kT = [T(C, ph, f"sbk{i}", [128, S], BF16) for i in range(2)]
        vv = [T(C, ph, f"sbv{i}", [128, NT, 128], BF16) for i in range(2)]
        et = [T(C, ph, f"et{i}", [128, 512], F32) for i in range(2)]
        spT = [T(C, ph, f"spT{i}", [128, 512], BF16) for i in range(4)]
        arg2 = [T(C, ph, f"arg2{i}", [128, 512], F32) for i in range(2)]
        AT = [T(C, ph, f"AT{i}", [128, 512], BF16) for i in range(4)]
        carry = [T(C, ph, f"carry{i}", [128, 512], F32) for i in range(2)]
        ostg = [T(C, ph, f"ostg{i}", [128, 512], BF16) for i in range(2)]
        psz = C.ps[0:2]
        psa = C.ps[2:4]
        psc = C.ps[4:5]
        pso = C.ps[5:7]

        items = []
        gid = 0
        for c in range(4):
            for qi in range(NST):
                for hp in range(2):
                    nb = 4 * qi + 4
                    for j in range(nb - 1, -1, -1):
                        items.append(dict(c=c, qi=qi, hp=hp, j=j, first=(j == nb - 1), last=(j == 0), g=gid,
                                          rel=(j - 4 * qi)))
                    gid += 1
        loaded = set()

        def load_pair(c):
            if c in loaded or c >= 4:
                return
            loaded.add(c)
            q, r_q = qT[c % 2]
            k, r_k = kT[c % 2]
            v, r_v = vv[c % 2]
            kb.dma("sp", q[:], C.qbT_d[c], reads=[C.r_qbT], writes=[r_q])
            kb.dma("sp", k[:], C.kbT_d[c], reads=[C.r_kbT], writes=[r_k])
            kb.dma("sp", v[:], C.vb_d[:, c * 128:(c + 1) * 128].rearrange("(j p) d -> p j d", p=128),
                   reads=[C.r_vb], writes=[r_v])

        def stA(n, it):
            c, qi, hp, j = it["c"], it["qi"], it["hp"], it["j"]
            q, r_q = qT[c % 2]
            k, r_k = kT[c % 2]
            P = slice(hp * 64, hp * 64 + 64)
            pz, r_pz = psz[n % 2]
            kb.op("pe", lambda e: e.matmul(pz[:], lhsT=k[P, j * 128:(j + 1) * 128], rhs=q[P, qi * 512:(qi + 1) * 512],
                                           start=True, stop=True), reads=[r_k, r_q], writes=[r_pz])
            e_, r_e = et[n % 2]
            kb.op("act", lambda e: e.activation(out=e_[:], in_=pz[:], func=AF.Exp), reads=[r_pz], writes=[r_e])
            s_, r_s = spT[n % 4]
            kb.op("act", lambda e: e.activation(out=s_[:], in_=e_[:], func=AF.Ln, bias=1.0, scale=1.0),
                  reads=[r_e], writes=[r_s])
            if it["rel"] >= 0:
                kb.op("dve", lambda e: e.tensor_tensor(out=s_[:], in0=s_[:], in1=cm[:, it["rel"], :], op=ALU.mult),
                      reads=[r_s, r_cm], writes=[r_s])

        def stB(n, it):
            c, qi, hp, j = it["c"], it["qi"], it["hp"], it["j"]
            q, r_q = qT[c % 2]
            k, r_k = kT[c % 2]
            P = slice(hp * 64, hp * 64 + 64)
            s_, r_s = spT[n % 4]
            pa, r_pa = psa[n % 2]
            pc, r_pc = psc[0]
            kb.op("pe", lambda e: e.matmul(pa[:], lhsT=k[P, j * 128:(j + 1) * 128], rhs=q[P, qi * 512:(qi + 1) * 512],
                                           start=True, stop=False), reads=[r_k, r_q], writes=[r_pa])
            kb.op("pe", lambda e: e.matmul(pa[:], lhsT=negU[:], rhs=s_[:], start=False, stop=True),
                  reads=[r_negU, r_s], writes=[r_pa])
            cb, r_cb = carry[it["g"] % 2]
            if it["first"]:
                kb.op("pool", lambda e: e.memset(cb[:], 0.0), writes=[r_cb])
            a2, r_a2 = arg2[n % 2]
            kb.op("dve", lambda e: e.tensor_tensor(out=a2[:], in0=pa[:], in1=cb[:], op=ALU.subtract),
                  reads=[r_pa, r_cb], writes=[r_a2])
            if not it["last"]:
                kb.op("pe", lambda e: e.matmul(pc[:], lhsT=ones[:], rhs=s_[:], start=True, stop=True),
                      reads=[r_ones, r_s], writes=[r_pc])
                kb.op("dve", lambda e: e.tensor_tensor(out=cb[:], in0=pc[:], in1=cb[:], op=ALU.add),
                      reads=[r_pc, r_cb], writes=[r_cb])
            a_, r_a = AT[n % 4]
            kb.op("act", lambda e: e.activation(out=a_[:], in_=a2[:], func=AF.Exp), reads=[r_a2], writes=[r_a])
            if it["rel"] >= 0:
                kb.op("pool", lambda e: e.tensor_tensor(out=a_[:], in0=a_[:], in1=cm[:, it["rel"], :], op=ALU.mult),
                      reads=[r_a, r_cm], writes=[r_a])

        def stC(n, it):
            c, qi, hp, j = it["c"], it["qi"], it["hp"], it["j"]
            v, r_v = vv[c % 2]
            a_, r_a = AT[n % 4]
            po, r_po = pso[(c * NST + qi) % 2]
            kb.op("pe", lambda e: e.matmul(po[hp * 64:(hp + 1) * 64, :], lhsT=v[:, j, hp * 64:(hp + 1) * 64], rhs=a_[:],
                                           start=it["first"], stop=it["last"]), reads=[r_v, r_a], writes=[r_po])
            if it["last"] and hp == 1:
                og, r_og = ostg[(c * NST + qi) % 2]
                kb.op("dve", lambda e: e.tensor_copy(out=og[:], in_=po[:]), reads=[r_po], writes=[r_og])
                kb.dma("sp", C.obT_d[c, :, qi * 512:(qi + 1) * 512], og[:], reads=[r_og], writes=[C.r_obT])
                if qi == NST - 1:
                    load_pair(c + 2)

        N = len(items)
        load_pair(0)
        load_pair(1)
        for n in range(N + 2):
            if n < N:
                stA(n, items[n])
            if 0 <= n - 1 < N:
                stB(n - 1, items[n - 1])
            if 0 <= n - 2 < N:
                stC(n - 2, items[n - 2])
        kb.barrier()


def phase_dsa(C, l):
    nc, kb = C.nc, C.kb
    with contextlib.ExitStack() as ph:
        kidxT, r_kidxT = T(C, ph, "kidxT", [128, S], BF16)
        ckvT, r_ckvT = T(C, ph, "ckvT", [128, S], BF16)
        ckv, r_ckv = T(C, ph, "ckv", [128, NT, 128], BF16)
        wuv, r_wuv = T(C, ph, "wuv", [128, 8, 64], BF16)
        sc, r_sc = T(C, ph, "sc", [128, S], F32)
        mb, r_mb = T(C, ph, "mb", [128, S], BF16)
        dmask, r_dmask = T(C, ph, "dmask", [128, 128], F32)
        Irep, r_Irep = T(C, ph, "Irep", [128, 512], BF16)
        onesF, r_onesF = T(C, ph, "onesF", [128, 128], BF16)
        identf, r_identf = T(C, ph, "identf", [128, 128], F32)
        Tz, r_Tz = T(C, ph, "Tz", [128, 2, 1024], F32)
        b31B, r_b31B = T(C, ph, "b31B", [128, 8], F32)
        bmask, r_bmask = T(C, ph, "bmask", [8, 1024], F32)
        rbT, r_rbT = T(C, ph, "rbT", [8, 32], F32)
        c8, r_c8 = T(C, ph, "c8", [8, 1], F32)
        b31c, r_b31c = T(C, ph, "b31c", [40, 1], F32)
        b31hi, r_b31hi = T(C, ph, "b31hi", [40, 1], BF16)
        b31hif, r_b31hif = T(C, ph, "b31hif", [40, 1], F32)
        b31lo, r_b31lo = T(C, ph, "b31lo", [72, 1], F32)
        CB, r_CB = T(C, ph, "CB", [72, 1024], BF16)
        CBd = Res("CBdyn")
        kb.dma("sp", kidxT[:], C.kidxT_d[:, :], reads=[C.r_kidxT], writes=[r_kidxT])
        kb.dma("sp", ckvT[:], C.ckvT_d[:, :], reads=[C.r_ckvT], writes=[r_ckvT])
        kb.dma("sp", ckv[:], C.ckv_d.rearrange("(j p) r -> p j r", p=128), reads=[C.r_ckv], writes=[r_ckv])
        kb.dma("pool", wuv[:], C.w_uv[l], writes=[r_wuv])
        kb.dma("sp", dmask[:], C.dmask[:, :], writes=[r_dmask])
        kb.dma("sp", Irep[:], C.irep_bf[:, :], writes=[r_Irep])
        kb.dma("sp", onesF[:], C.ones_bf[:, :], writes=[r_onesF])
        kb.dma("sp", identf[:], C.ident_f[:, :], writes=[r_identf])
        kb.dma("sp", Tz[:], C.tz.rearrange("r p c -> p r c"), writes=[r_Tz])
        kb.dma("sp", b31B[:], C.rel_bias[31:32, :].to_broadcast([128, 8]), writes=[r_b31B])
        kb.dma("sp", bmask[:], C.bmask[:, :], writes=[r_bmask])
        kb.dma("sp", rbT[:], C.rel_bias_t[:, :], writes=[r_rbT])
        for r_ in range(2):
            kb.op("dve", lambda e: e.tensor_tensor(out=Tz[:, r_, :].rearrange("p (h t) -> p h t", h=8),
                                                   in0=Tz[:, r_, :].rearrange("p (h t) -> p h t", h=8),
                                                   in1=b31B[:].to_broadcast([128, 8, 128]) if False else
                                                   b31B[:].unsqueeze(2).to_broadcast([128, 8, 128]),
                                                   op=ALU.subtract), reads=[r_Tz, r_b31B], writes=[r_Tz])
        kb.op("dve", lambda e: e.reduce_max(out=c8[:], in_=rbT[:], axis=AX.X), reads=[r_rbT], writes=[r_c8])
        kb.op("dve", lambda e: e.tensor_scalar(out=c8[:], in0=c8[:], scalar1=-1.0, scalar2=None, op0=ALU.mult),
              reads=[r_c8], writes=[r_c8])
        kb.op("pool", lambda e: e.memset(CB[:], 0.0), writes=[r_CB])
        kb.dma("sp", b31c[32:40, :], C.rel_bias_t[:, 31:32], writes=[r_b31c], allow_slow_non_contiguous=True)
        kb.dma("sp", b31lo[64:72, :], C.rel_bias_t[:, 31:32], writes=[r_b31lo], allow_slow_non_contiguous=True)
        kb.op("dve", lambda e: e.tensor_copy(out=b31hi[32:40, :], in_=b31c[32:40, :]), reads=[r_b31c], writes=[r_b31hi])
        kb.op("dve", lambda e: e.tensor_copy(out=b31hif[32:40, :], in_=b31hi[32:40, :]), reads=[r_b31hi], writes=[r_b31hif])
        kb.dma("sp", b31lo[32:40, :], b31hif[32:40, :], reads=[r_b31hif], writes=[r_b31lo])
        bm32, r_bm32 = T(C, ph, "bm32", [72, 1024], F32)
        kb.dma("sp", bm32[32:40, :], C.bmask[:, :], writes=[r_bm32])
        kb.dma("sp", bm32[64:72, :], C.bmask[:, :], writes=[r_bm32])
        kb.op("dve", lambda e: e.tensor_scalar(out=CB[32:40, :], in0=bm32[32:40, :], scalar1=b31hif[32:40, 0:1],
                                               scalar2=None, op0=ALU.mult), reads=[r_bm32, r_b31hif], writes=[r_CB])
        hi64, r_hi64 = T(C, ph, "hi64", [72, 1], F32)
        kb.dma("sp", hi64[64:72, :], b31hif[32:40, :], reads=[r_b31hif], writes=[r_hi64])
        kb.op("dve", lambda e: e.tensor_tensor(out=b31lo[64:72, :], in0=b31lo[64:72, :], in1=hi64[64:72, :], op=ALU.subtract),
              reads=[r_b31lo, r_hi64], writes=[r_b31lo])
        kb.op("dve", lambda e: e.tensor_scalar(out=CB[64:72, :], in0=bm32[64:72, :], scalar1=b31lo[64:72, 0:1],
                                               scalar2=None, op0=ALU.mult), reads=[r_bm32, r_b31lo], writes=[r_CB])

        qi_t = [T(C, ph, f"qi{i}", [128, 2, 128], BF16) for i in range(2)]
        wi_t = [T(C, ph, f"wi{i}", [128, 4], F32) for i in range(2)]
        ql_t = [T(C, ph, f"ql{i}", [128, 1024], BF16) for i in range(2)]
        rl = [T(C, ph, f"rl{i}", [128, 512], F32) for i in range(4)]
        m8, r_m8 = T(C, ph, "m8", [128, 8], F32)
        mch, r_mch = T(C, ph, "mch", [128, 8, 16], F32)
        mrow, r_mrow = T(C, ph, "mrow", [128, 8], F32)
        mT, r_mT = T(C, ph, "mT", [8, 128], F32)
        junk, r_junk = T(C, ph, "junkb", [128, 512], BF16)
        zt = [T(C, ph, f"zt{i}", [128, 512], F32) for i in range(2)]
        pT = [T(C, ph, f"pT{i}", [128, 512], BF16) for i in range(3)]
        rden, r_rden = T(C, ph, "rden", [128, 512], F32)
        olT, r_olT = T(C, ph, "olT", [128, 1024], BF16)
        oast = [T(C, ph, f"oast{i}", [128, 4, 128], BF16) for i in range(2)]
        psA = Rot(C.ps[0:2])
        psZ = C.ps[2:4]
        psO = C.ps[4:6]
        psD = C.ps[6:8]

        for i in range(NT):
            nk = (i + 1) * 128
            chunks = [(c0, min(512, nk - c0)) for c0 in range(0, nk, 512)]
            tcols = slice(i * 128, (i + 1) * 128)
            qi, r_qi = qi_t[i % 2]
            wi, r_wi = wi_t[i % 2]
            ql, r_ql = ql_t[i % 2]
            kb.dma("sp", qi[:], C.qidxT_d[:, :, tcols].rearrange("c p t -> p c t"), reads=[C.r_qidxT], writes=[r_qi])
            kb.dma("sp", wi[:], C.widx_d[tcols, :], reads=[C.r_widx], writes=[r_wi])
            kb.dma("sp", ql[:].rearrange("p (h t) -> p h t", h=8), C.qlatT_d[:, :, tcols].rearrange("h p t -> p h t"),
                   reads=[C.r_qlatT], writes=[r_ql])
            for (c0, w) in chunks:
                for h in range(4):
                    P = slice((h % 2) * 64, (h % 2) * 64 + 64)
                    ps, r_ps = psA.next()
                    kb.op("pe", lambda e: e.matmul(ps[:, 0:w], lhsT=qi[P, h // 2, :], rhs=kidxT[P, c0:c0 + w],
                                                   start=True, stop=True), reads=[r_qi, r_kidxT], writes=[r_ps])
                    r_, r_r = rl[h]
                    kb.op("act", lambda e: e.activation(out=r_[:, 0:w], in_=ps[:, 0:w], func=AF.Relu),
                          reads=[r_ps], writes=[r_r])
                    if h == 0:
                        kb.op("dve", lambda e: e.tensor_scalar(out=sc[:, c0:c0 + w], in0=r_[:, 0:w], scalar1=wi[:, 0:1],
                                                               scalar2=None, op0=ALU.mult),
                              reads=[r_r, r_wi], writes=[r_sc])
                    else:
                        kb.op("dve", lambda e: e.scalar_tensor_tensor(out=sc[:, c0:c0 + w], in0=r_[:, 0:w],
                                                                      scalar=wi[:, h:h + 1], in1=sc[:, c0:c0 + w],
                                                                      op0=ALU.mult, op1=ALU.add),
                              reads=[r_r, r_wi, r_sc], writes=[r_sc])
            kb.op("dve", lambda e: e.tensor_tensor(out=sc[:, tcols], in0=sc[:, tcols], in1=dmask[:], op=ALU.add),
                  reads=[r_sc, r_dmask], writes=[r_sc])
            if i >= 2:
                for rnd in range(TOPK // 8):
                    kb.op("dve", lambda e: e.max(out=m8[:], in_=sc[:, 0:nk]), reads=[r_sc], writes=[r_m8])
                    kb.op("dve", lambda e: e.match_replace(out=sc[:, 0:nk], in_to_replace=m8[:], in_values=sc[:, 0:nk],
                                                           imm_value=2.0 * NEG), reads=[r_sc, r_m8], writes=[r_sc])
                kb.op("dve", lambda e: e.tensor_scalar(out=mb[:, 0:nk], in0=sc[:, 0:nk], scalar1=1.5 * NEG, scalar2=MBIG,
                                                       op0=ALU.is_gt, op1=ALU.mult), reads=[r_sc], writes=[r_mb])
            else:
                kb.op("dve", lambda e: e.tensor_scalar(out=mb[:, 0:nk], in0=sc[:, 0:nk], scalar1=0.5 * NEG, scalar2=MBIG,
                                                       op0=ALU.is_le, op1=ALU.mult), reads=[r_sc], writes=[r_mb])
            nch = len(chunks)
            for h in range(8):
                for ci, (c0, w) in enumerate(chunks):
                    ps, r_ps = psA.next()
                    kb.op("pe", lambda e: e.matmul(ps[:, 0:w], lhsT=ql[:, h * 128:(h + 1) * 128], rhs=ckvT[:, c0:c0 + w],
                                                   start=True, stop=False), reads=[r_ql, r_ckvT], writes=[r_ps])
                    kb.op("pe", lambda e: e.matmul(ps[:, 0:w], lhsT=Irep[:, 0:128], rhs=mb[:, c0:c0 + w],
                                                   start=False, stop=True), reads=[r_Irep, r_mb], writes=[r_ps])
                    kb.op("dve", lambda e: e.tensor_scalar(out=junk[:, 0:w], in0=ps[:, 0:w], scalar1=0.0, scalar2=NEG,
                                                           op0=ALU.add, op1=ALU.max, accum_out=mch[:, h, ci:ci + 1]),
                          reads=[r_ps], writes=[r_junk, r_mch])
            kb.op("dve", lambda e: e.tensor_reduce(out=mrow[:], in_=mch[:, :, 0:nch], axis=AX.X, op=ALU.max),
                  reads=[r_mch], writes=[r_mrow])
            ps, r_ps = psA.next()
            kb.op("pe", lambda e: e.transpose(out=ps[0:8, 0:128], in_=mrow[:], identity=identf[:]),
                  reads=[r_mrow, r_identf], writes=[r_ps])
            kb.op("dve", lambda e: e.tensor_scalar(out=mT[:], in0=ps[0:8, 0:128], scalar1=-1.0, scalar2=c8[:, 0:1],
                                                   op0=ALU.mult, op1=ALU.add), reads=[r_ps, r_c8], writes=[r_mT])
            kb.op("dve", lambda e: e.tensor_tensor(out=CB[0:8, :].rearrange("p (h t) -> p h t", h=8),
                                                   in0=mT[:].unsqueeze(1).to_broadcast([8, 8, 128]),
                                                   in1=bmask[:].rearrange("p (h t) -> p h t", h=8), op=ALU.mult),
                  reads=[r_mT, r_bmask, r_CB], writes=[CBd])
            items = [(half, jb) for half in range(2) for jb in range(i + 1)]

            def stA(n, it):
                half, jb = it
                hc = slice(half * 512, (half + 1) * 512)
                pz, r_pz = psZ[n % 2]
                kb.op("pe", lambda e: e.matmul(pz[:], lhsT=ckvT[:, jb * 128:(jb + 1) * 128], rhs=ql[:, hc],
                                               start=True, stop=False), reads=[r_ckvT, r_ql], writes=[r_pz])
                kb.op("pe", lambda e: e.matmul(pz[:], lhsT=mb[:, jb * 128:(jb + 1) * 128], rhs=Irep[:],
                                               start=False, stop=False), reads=[r_mb, r_Irep], writes=[r_pz])
                kb.op("pe", lambda e: e.matmul(pz[:], lhsT=onesF[0:72, :], rhs=CB[0:72, hc], start=False, stop=True),
                      reads=[r_onesF, r_CB, CBd], writes=[r_pz])
                p_, r_p = pT[n % 3]
                rel = i - jb
                if rel <= 1:
                    z_, r_z = zt[n % 2]
                    kb.op("dve", lambda e: e.tensor_tensor(out=z_[:], in0=pz[:], in1=Tz[:, rel, hc], op=ALU.add),
                          reads=[r_pz, r_Tz], writes=[r_z])
                    kb.op("act", lambda e: e.activation(out=p_[:], in_=z_[:], func=AF.Exp), reads=[r_z], writes=[r_p])
                else:
                    kb.op("act", lambda e: e.activation(out=p_[:], in_=pz[:], func=AF.Exp), reads=[r_pz], writes=[r_p])

            def stB(n, it):
                half, jb = it
                hc = slice(half * 512, (half + 1) * 512)
                p_, r_p = pT[n % 3]
                po, r_po = psO[half]
                pd, r_pd = psD[half]
                kb.op("pe", lambda e: e.matmul(po[:], lhsT=ckv[:, jb, :], rhs=p_[:], start=(jb == 0), stop=(jb == i)),
                      reads=[r_ckv, r_p], writes=[r_po])
                kb.op("pe", lambda e: e.matmul(pd[:], lhsT=onesF[:], rhs=p_[:], start=(jb == 0), stop=(jb == i)),
                      reads=[r_onesF, r_p], writes=[r_pd])
                if jb == i:
                    kb.op("dve", lambda e: e.reciprocal(out=rden[:], in_=pd[:]), reads=[r_pd], writes=[r_rden])
                    kb.op("dve", lambda e: e.tensor_tensor(out=olT[:, hc], in0=po[:], in1=rden[:], op=ALU.mult),
                          reads=[r_po, r_rden], writes=[r_olT])

            N = len(items)
            for n in range(N + 1):
                if n < N:
                    stA(n, items[n])
                if n >= 1:
                    stB(n - 1, items[n - 1])
            ps, r_ps = psA.next()
            for h in range(8):
                kb.op("pe", lambda e: e.matmul(ps[(h % 2) * 64:(h % 2) * 64 + 64, (h // 2) * 128:(h // 2 + 1) * 128],
                                               lhsT=wuv[:, h, :], rhs=olT[:, h * 128:(h + 1) * 128], start=True, stop=True),
                      reads=[r_wuv, r_olT], writes=[r_ps])
            og, r_og = oast[i % 2]
            kb.op("act", lambda e: e.copy(out=og[:].rearrange("p c t -> p (c t)"), in_=ps[:]), reads=[r_ps], writes=[r_og])
            kb.dma("sp", C.oaT_d[:, :, tcols].rearrange("c p t -> p c t"), og[:], reads=[r_og], writes=[C.r_oaT])
        kb.barrier()


def bload(C, ph, name, src_ap, n, reads=()):
    t, r = T(C, ph, name, [128, n], F32)
    C.kb.dma("sp", t[:], src_ap, reads=list(reads), writes=[r])
    return t, r


def phase_out(C, l, x_src, r_xsrc):
    nc, kb = C.nc, C.kb
    with contextlib.ExitStack() as ph:
        Wg, r_Wg = T(C, ph, "Wg", [128, 8, 2048], BF16)
        wao, r_wao = T(C, ph, "wao", [128, 4, 1024], BF16)
        wbo, r_wbo = T(C, ph, "wbo", [128, 4, 1024], BF16)
        wo, r_wo = T(C, ph, "wo", [128, 8, 1024], BF16)
        wr, r_wr = T(C, ph, "wr", [128, 8, 32], F32)
        kb.dma("pool", Wg[:], C.w_in[l, :, NW1:NCOLS].rearrange("(kc p) n -> p kc n", p=128), writes=[r_Wg])
        kb.dma("pool", wao[:], C.w_a_out[l].rearrange("(kc p) n -> p kc n", p=128), writes=[r_wao])
        kb.dma("pool", wbo[:], C.w_b_out[l].rearrange("(kc p) n -> p kc n", p=128), writes=[r_wbo])
        kb.dma("pool", wo[:], C.w_o[l].rearrange("(kc p) n -> p kc n", p=128), writes=[r_wo])
        kb.dma("sp", wr[:], C.w_router[l].rearrange("(kc p) n -> p kc n", p=128), writes=[r_wr])
        sc1, r_sc1 = bload(C, ph, "sc1", C.mod_d[l, :, 1024:2048], 1024, [C.r_mod])
        sh1, r_sh1 = bload(C, ph, "sh1", C.mod_d[l, :, 0:1024], 1024, [C.r_mod])
        g1, r_g1 = bload(C, ph, "g1", C.mod_d[l, :, 2048:3072], 1024, [C.r_mod])
        sh2, r_sh2 = bload(C, ph, "sh2", C.mod_d[l, :, 3072:4096], 1024, [C.r_mod])
        sc2, r_sc2 = bload(C, ph, "sc2", C.mod_d[l, :, 4096:5120], 1024, [C.r_mod])
        lg, r_lg = bload(C, ph, "lg", C.ln1_g[l:l + 1, :].to_broadcast([128, 1024]), 1024)
        lb, r_lb = bload(C, ph, "lb", C.ln1_b[l:l + 1, :].to_broadcast([128, 1024]), 1024)
        brB, r_brB = bload(C, ph, "brB", C.b_router[l:l + 1, :].to_broadcast([128, 32]), 32)
        ident, r_id = T(C, ph, "identb", [128, 128], BF16)
        identf, r_idf = T(C, ph, "identf2", [128, 128], F32)
        kb.dma("sp", ident[:], C.ident_bf[:, :], writes=[r_id])
        kb.dma("sp", identf[:], C.ident_f[:, :], writes=[r_idf])
        LW = ln_work(C, ph, "olw", LN_EPS)
        xres = [T(C, ph, "xres0", [128, 4, 1024], F32)[0]] * 2
        r_xres = [[Res(f"xres_{j}") for j in range(4)]] * 2
        hb, r_hb = T(C, ph, "ohb", [128, 1024], BF16)
        hT, _ = T(C, ph, "ohT", [128, 8, 512], BF16)
        r_hT = [Res(f"ohT_{j}") for j in range(4)]
        oaT, r_oaT = T(C, ph, "ooaT", [128, 4, 512], BF16)
        obT, r_obT = T(C, ph, "oobT", [128, 4, 512], BF16)
        sga = [T(C, ph, f"sga{i}", [128, 512], F32) for i in range(2)]
        sgb = [T(C, ph, f"sgb{i}", [128, 512], F32) for i in range(2)]
        t1 = [T(C, ph, f"t1{i}", [128, 512], F32) for i in range(2)]
        t2 = [T(C, ph, f"t2{i}", [128, 512], F32) for i in range(2)]
        mT, _ = T(C, ph, "mergT", [128, 8, 512], BF16)
        r_mT = [Res(f"mergT_{j}") for j in range(8)]
        yt, r_yt = T(C, ph, "yt", [128, 1024], F32)
        zt, r_zt = T(C, ph, "zt_o", [128, 1024], F32)
        x1t = [T(C, ph, f"x1t{i}", [128, 1024], F32) for i in range(2)]
        h2f, r_h2f = T(C, ph, "h2f", [128, 1024], F32)
        h2T32, r_h2T32 = T(C, ph, "h2T32", [128, 8, 128], F32)
        h2Ts = [T(C, ph, f"h2Ts{i}", [128, 8, 512], BF16)[0] for i in range(2)]
        r_h2Ts = [[Res(f"h2Ts{i}_{j}") for j in range(4)] for i in range(2)]
        lgt, r_lgt = T(C, ph, "lgt", [128, 32], F32)
        m8, r_m8 = T(C, ph, "om8", [128, 8], F32)
        nmx, r_nmx = T(C, ph, "nmx", [128, 1], F32)
        msk, r_msk = T(C, ph, "msk", [128, 32], F32)
        ex, r_ex = T(C, ph, "ex", [128, 32], F32)
        rs, r_rs = T(C, ph, "ors", [128, 1], F32)
        gst = [T(C, ph, f"gst{i}", [128, 4, 32], F32)[0] for i in range(2)]
        r_gst = [[Res(f"gst{i}_{j}") for j in range(4)] for i in range(2)]
        gTs = [T(C, ph, f"gTs{i}", [32, 512], F32)[0] for i in range(2)]
        r_gTs = [[Res(f"gTs{i}_{j}") for j in range(4)] for i in range(2)]
        pp = Rot(C.ps[0:4])
        psT, r_psT = C.ps[7]
        psT32 = C.ps[5:7]
        psY = C.ps[4:5]

        for st in range(getattr(C, 'out_nst', NST)):
            stage = getattr(C, 'out_stage', 99)
            sp_ = st % 2
            t0 = st * 512
            cols = slice(t0, t0 + 512)
            xr = xres[sp_]
            for j in range(4):
                tt = st * 4 + j
                kb.dma("sp", xr[:, j, :], x_src[tt * 128:(tt + 1) * 128, :], reads=[r_xsrc], writes=[r_xres[sp_][j]])
                ln_modulate(C, LW, xr[:, j, :], r_xres[sp_][j], hb[:], r_hb, sc1[:], r_sc1, sh1[:], r_sh1)
                for kc in range(8):
                    kb.op("pe", lambda e: e.transpose(out=bfv(psT)[:, kc * 128:(kc + 1) * 128],
                                                      in_=hb[:, kc * 128:(kc + 1) * 128], identity=ident[:]),
                          reads=[r_hb, r_id], writes=[r_psT])
                kb.op("act", lambda e: e.copy(out=hT[:, :, j * 128:(j + 1) * 128],
                                              in_=bfv(psT).rearrange("p (kc t) -> p kc t", kc=8)),
                      reads=[r_psT], writes=[r_hT[j]])
            kb.dma("sp", oaT[:], C.oaT_d[:, :, cols].rearrange("c p t -> p c t"), reads=[C.r_oaT], writes=[r_oaT])
            kb.dma("sp", obT[:], C.obT_d[:, :, cols].rearrange("c p t -> p c t"), reads=[C.r_obT], writes=[r_obT])
            for n_ in range(8 if stage >= 1 else 0):
                ncs = slice(n_ * 128, (n_ + 1) * 128)
                pga, r_pga = pp.next()
                pgb, r_pgb = pp.next()
                pa, r_pa = pp.next()
                pb, r_pb = pp.next()
                for kc in range(8):
                    kb.op("pe", lambda e: e.matmul(pga[:], lhsT=Wg[:, kc, n_ * 128:(n_ + 1) * 128], rhs=hT[:, kc, :],
                                                   start=(kc == 0), stop=(kc == 7)), reads=r_hT + [r_Wg], writes=[r_pga])
                for kc in range(8):
                    kb.op("pe", lambda e: e.matmul(pgb[:], lhsT=Wg[:, kc, 1024 + n_ * 128:1024 + (n_ + 1) * 128],
                                                   rhs=hT[:, kc, :], start=(kc == 0), stop=(kc == 7)),
                          reads=r_hT + [r_Wg], writes=[r_pgb])
                for c in range(4):
                    kb.op("pe", lambda e: e.matmul(pa[:], lhsT=wao[:, c, ncs], rhs=oaT[:, c, :], start=(c == 0), stop=(c == 3)),
                          reads=[r_wao, r_oaT], writes=[r_pa])
                for c in range(4):
                    kb.op("pe", lambda e: e.matmul(pb[:], lhsT=wbo[:, c, ncs], rhs=obT[:, c, :], start=(c == 0), stop=(c == 3)),
                          reads=[r_wbo, r_obT], writes=[r_pb])
                sa, r_sa = sga[n_ % 2]
                sb_, r_sb = sgb[n_ % 2]
                a1, r_a1 = t1[n_ % 2]
                a2, r_a2 = t2[n_ % 2]
                kb.op("act", lambda e: e.activation(out=sa[:], in_=pga[:], func=AF.Sigmoid), reads=[r_pga], writes=[r_sa])
                kb.op("act", lambda e: e.activation(out=sb_[:], in_=pgb[:], func=AF.Sigmoid), reads=[r_pgb], writes=[r_sb])
                kb.op("dve", lambda e: e.tensor_tensor(out=a1[:], in0=pa[:], in1=sa[:], op=ALU.mult),
                      reads=[r_pa, r_sa], writes=[r_a1])
                kb.op("dve", lambda e: e.tensor_tensor(out=a2[:], in0=pb[:], in1=sb_[:], op=ALU.mult),
                      reads=[r_pb, r_sb], writes=[r_a2])
                kb.op("pool", lambda e: e.tensor_tensor(out=mT[:, n_, :], in0=a1[:], in1=a2[:], op=ALU.add),
                      reads=[r_a1, r_a2], writes=[r_mT[n_]])
            for j in range(4 if stage >= 2 else 0):
                tt = st * 4 + j
                for nh in range(2):
                    py, r_py = psY[0]
                    for n_ in range(8):
                        kb.op("pe", lambda e: e.matmul(py[:], lhsT=mT[:, n_, j * 128:(j + 1) * 128],
                                                       rhs=wo[:, n_, nh * 512:(nh + 1) * 512], start=(n_ == 0), stop=(n_ == 7)),
                              reads=r_mT + [r_wo], writes=[r_py])
                    kb.op("dve", lambda e: e.tensor_tensor(out=yt[:, nh * 512:(nh + 1) * 512], in0=py[:],
                                                           in1=g1[:, nh * 512:(nh + 1) * 512], op=ALU.mult),
                          reads=[r_py, r_g1], writes=[r_yt])
                kb.op("dve", lambda e: e.scalar_tensor_tensor(out=zt[:], in0=xr[:, j, :], scalar=float(DN_ALPHA), in1=yt[:],
                                                              op0=ALU.mult, op1=ALU.add),
                      reads=[r_xres[sp_][j], r_yt], writes=[r_zt])
                x1, r_x1 = x1t[tt % 2]
                ln_modulate(C, LW, zt, r_zt, x1[:], r_x1, lg[:], r_lg, lb[:], r_lb)
                kb.dma("sp", C.x1_d[tt * 128:(tt + 1) * 128, :], x1[:], reads=[r_x1], writes=[C.r_x1])
                if stage < 3:
                    continue
                ln_modulate(C, LW, x1, r_x1, h2f[:], r_h2f, sc2[:], r_sc2, sh2[:], r_sh2)
                sub = getattr(C, 'out_sub', 99)
                for half in range(2 if sub >= 1 else 0):
                    p32, r_p32 = psT32[half]
                    for k4 in range(4):
                        kc = half * 4 + k4
                        kb.op("pe", lambda e: e.transpose(out=p32[:, k4 * 128:(k4 + 1) * 128],
                                                          in_=h2f[:, kc * 128:(kc + 1) * 128], identity=identf[:]),
                              reads=[r_h2f, r_idf], writes=[r_p32])
                    if sub < 2:
                        continue
                    kb.op("act", lambda e: e.copy(out=h2T32[:, half * 4:half * 4 + 4, :],
                                                  in_=p32[:].rearrange("p (k t) -> p k t", k=4)),
                          reads=[r_p32], writes=[r_h2T32])
                    if sub < 3:
                        continue
                    kb.op("dve", lambda e: e.tensor_copy(out=h2Ts[sp_][:, half * 4:half * 4 + 4, j * 128:(j + 1) * 128],
                                                         in_=p32[:].rearrange("p (k t) -> p k t", k=4)),
                          reads=[r_p32], writes=[r_h2Ts[sp_][j]])
                if stage < 4:
                    continue
                pl, r_pl = pp.next()
                for kc in range(8):
                    kb.op("pe", lambda e: e.matmul(pl[:, 0:32], lhsT=h2T32[:, kc, :], rhs=wr[:, kc, :],
                                                   start=(kc == 0), stop=(kc == 7)), reads=[r_h2T32, r_wr], writes=[r_pl])
                kb.op("dve", lambda e: e.tensor_tensor(out=lgt[:], in0=pl[:, 0:32], in1=brB[:], op=ALU.add),
                      reads=[r_pl, r_brB], writes=[r_lgt])
                kb.op("dve", lambda e: e.max(out=m8[:], in_=lgt[:]), reads=[r_lgt], writes=[r_m8])
                kb.op("dve", lambda e: e.tensor_scalar(out=nmx[:], in0=m8[:, 0:1], scalar1=-1.0, scalar2=None, op0=ALU.mult),
                      reads=[r_m8], writes=[r_nmx])
                kb.op("dve", lambda e: e.tensor_scalar(out=msk[:], in0=lgt[:], scalar1=m8[:, 3:4], scalar2=None, op0=ALU.is_ge),
                      reads=[r_lgt, r_m8], writes=[r_msk])
                kb.op("act", lambda e: e.activation(out=ex[:], in_=lgt[:], func=AF.Exp, bias=nmx[:, 0:1], scale=1.0),
                      reads=[r_lgt, r_nmx], writes=[r_ex])
                kb.op("dve", lambda e: e.tensor_tensor(out=ex[:], in0=ex[:], in1=msk[:], op=ALU.mult),
                      reads=[r_ex, r_msk], writes=[r_ex])
                kb.op("dve", lambda e: e.reduce_sum(out=rs[:], in_=ex[:], axis=AX.X), reads=[r_ex], writes=[r_rs])
                kb.op("dve", lambda e: e.reciprocal(out=rs[:], in_=rs[:]), reads=[r_rs], writes=[r_rs])
                kb.op("dve", lambda e: e.tensor_scalar(out=gst[sp_][:, j, :], in0=ex[:], scalar1=rs[:, 0:1], scalar2=None,
                                                       op0=ALU.mult), reads=[r_ex, r_rs], writes=[r_gst[sp_][j]])
                pg, r_pg = pp.next()
                kb.op("pe", lambda e: e.transpose(out=pg[0:32, 0:128], in_=gst[sp_][:, j, :], identity=identf[:]),
                      reads=[r_gst[sp_][j], r_idf], writes=[r_pg])
                kb.op("act", lambda e: e.copy(out=gTs[sp_][:, j * 128:(j + 1) * 128], in_=pg[0:32, 0:128]),
                      reads=[r_pg], writes=[r_gTs[sp_][j]])
            if stage < 4:
                continue
            kb.dma("sp", C.h2T_d[:, :, cols].rearrange("k p t -> p k t"), h2Ts[sp_][:], reads=r_h2Ts[sp_], writes=[C.r_h2T])
            kb.dma("sp", C.gates_d[cols, :].rearrange("(j p) e -> p j e", p=128), gst[sp_][:], reads=r_gst[sp_],
                   writes=[C.r_gates])
            kb.dma("sp", C.gatesT_d[:, cols], gTs[sp_][:], reads=r_gTs[sp_], writes=[C.r_gatesT])
        kb.barrier()


TS = 1024


def phase_moe(C, l, x_dst, r_xdst):
    nc, kb = C.nc, C.kb
    with contextlib.ExitStack() as ph:
        wgu = [T(C, ph, f"wgu{i}", [128, 8, 2048], BF16) for i in range(2)]
        wdn = [T(C, ph, f"wdn{i}", [128, 8, 1024], BF16) for i in range(2)]
        bgu, r_bgu = T(C, ph, "bgu", [128, E, 16], F32)
        bdn, r_bdn = T(C, ph, "bdn", [32, 1024], F32)
        kb.dma("sp", bgu[:], C.b_gu_t[l].rearrange("e p c -> p e c"), writes=[r_bgu])
        kb.dma("sp", bdn[:], C.b_dn[l], writes=[r_bdn])
        kb.op("pool", lambda e: e.tensor_scalar(out=bgu[:, :, 8:16], in0=bgu[:, :, 8:16], scalar1=1.0, scalar2=None,
                                                op0=ALU.add), reads=[r_bgu], writes=[r_bgu])
        g2, r_g2 = bload(C, ph, "g2", C.mod_d[l, :, 5120:6144], 1024, [C.r_mod])
        lg, r_lg = bload(C, ph, "lg2", C.ln2_g[l:l + 1, :].to_broadcast([128, 1024]), 1024)
        lb, r_lb = bload(C, ph, "lb2", C.ln2_b[l:l + 1, :].to_broadcast([128, 1024]), 1024)
        LW = ln_work(C, ph, "mlw", LN_EPS)
        h2T, r_h2T = T(C, ph, "mh2T", [128, 8, TS], BF16)
        gts, r_gts = T(C, ph, "mgts", [128, TS // 128, 32], F32)
        gT, r_gT = T(C, ph, "mgT", [32, TS], F32)
        acc, _ = T(C, ph, "macc", [128, TS // 128, 1024], F32)
        r_acc = [[Res(f"acc{j}_{nh}") for nh in range(2)] for j in range(TS // 128)]
        a_sb = [T(C, ph, f"a_sb{i}", [128, 512], F32) for i in range(2)]
        sg = [T(C, ph, f"sg{i}", [128, 512], BF16) for i in range(2)]
        gg = [T(C, ph, f"gg{i}", [128, 512], F32) for i in range(2)]
        u_sb = [T(C, ph, f"u_sb{i}", [128, 512], F32) for i in range(2)]
        actT = [T(C, ph, f"actT{i}", [128, 8, 512], BF16)[0] for i in range(2)]
        r_actT = [[Res(f"actT{i}_{f}") for f in range(8)] for i in range(2)]
        x1t, r_x1t = T(C, ph, "mx1t", [128, 1024], F32)
        xo = [(x1t, r_x1t)] * 2
        ppA = Rot(C.ps[0:2])
        ppU = Rot(C.ps[2:4])
        ppY = Rot(C.ps[4:8])
        wloaded = {}

        def load_w(idx):
            if idx in wloaded or idx >= (S // TS) * E:
                return
            wloaded[idx] = True
            e_ = idx % E
            g_, r_g = wgu[idx % 2]
            d_, r_d = wdn[idx % 2]
            kb.dma("pool", g_[:], C.w_gu[l, e_].rearrange("(kc p) n -> p kc n", p=128), writes=[r_g])
            kb.dma("pool", d_[:], C.w_dn[l, e_].rearrange("(kc p) n -> p kc n", p=128), writes=[r_d])

        load_w(0)
        load_w(1)
        for ts in range(S // TS):
            tcols = slice(ts * TS, (ts + 1) * TS)
            kb.dma("sp", h2T[:], C.h2T_d[:, :, tcols].rearrange("k p t -> p k t"), reads=[C.r_h2T], writes=[r_h2T])
            kb.dma("sp", gts[:], C.gates_d[tcols, :].rearrange("(j p) e -> p j e", p=128), reads=[C.r_gates], writes=[r_gts])
            kb.dma("sp", gT[:], C.gatesT_d[:, tcols], reads=[C.r_gatesT], writes=[r_gT])
            for j in range(TS // 128):
                for nh in range(2):
                    py, r_py = ppY.next()
                    kb.op("pe", lambda e: e.matmul(py[:], lhsT=gT[:, j * 128:(j + 1) * 128], rhs=bdn[:, nh * 512:(nh + 1) * 512],
                                                   start=True, stop=True), reads=[r_gT, r_bdn], writes=[r_py])
                    kb.op("act", lambda e: e.copy(out=acc[:, j, nh * 512:(nh + 1) * 512], in_=py[:]),
                          reads=[r_py], writes=[r_acc[j][nh]])
            for e_ in range(E):
                idx = ts * E + e_
                g_, r_g = wgu[idx % 2]
                d_, r_d = wdn[idx % 2]
                for t2 in range(TS // 512):
                    aT = actT[t2 % 2]
                    r_aT = r_actT[t2 % 2]
                    hc = slice(t2 * 512, (t2 + 1) * 512)
                    for fc in range(8):
                        pa, r_pa = ppA.next()
                        pu, r_pu = ppU.next()
                        for kc in range(8):
                            kb.op("pe", lambda e: e.matmul(pa[:], lhsT=g_[:, kc, fc * 128:(fc + 1) * 128], rhs=h2T[:, kc, hc],
                                                           start=(kc == 0), stop=(kc == 7)), reads=[r_g, r_h2T], writes=[r_pa])
                        for kc in range(8):
                            kb.op("pe", lambda e: e.matmul(pu[:], lhsT=g_[:, kc, 1024 + fc * 128:1024 + (fc + 1) * 128],
                                                           rhs=h2T[:, kc, hc], start=(kc == 0), stop=(kc == 7)),
                                  reads=[r_g, r_h2T], writes=[r_pu])
                        a_, r_a = a_sb[fc % 2]
                        s_, r_s = sg[fc % 2]
                        q_, r_q = gg[fc % 2]
                        u_, r_u = u_sb[fc % 2]
                        kb.op("dve", lambda e: e.tensor_scalar(out=a_[:], in0=pa[:], scalar1=bgu[:, e_, fc:fc + 1], scalar2=7.0,
                                                               op0=ALU.add, op1=ALU.min), reads=[r_pa, r_bgu], writes=[r_a])
                        kb.op("act", lambda e: e.activation(out=s_[:], in_=a_[:], func=AF.Sigmoid, scale=1.702),
                              reads=[r_a], writes=[r_s])
                        kb.op("dve", lambda e: e.tensor_scalar(out=u_[:], in0=pu[:], scalar1=bgu[:, e_, 8 + fc:9 + fc], scalar2=8.0,
                                                               op0=ALU.add, op1=ALU.min), reads=[r_pu, r_bgu], writes=[r_u])
                        kb.op("pool", lambda e: e.tensor_tensor(out=q_[:], in0=a_[:], in1=s_[:], op=ALU.mult),
                              reads=[r_a, r_s], writes=[r_q])
                        kb.op("dve", lambda e: e.scalar_tensor_tensor(out=aT[:, fc, :], in0=u_[:], scalar=-6.0, in1=q_[:],
                                                                      op0=ALU.max, op1=ALU.mult),
                              reads=[r_u, r_q], writes=[r_aT[fc]])
                    for j4 in range(4):
                        j = t2 * 4 + j4
                        for nh in range(2):
                            py, r_py = ppY.next()
                            for fc in range(8):
                                kb.op("pe", lambda e: e.matmul(py[:], lhsT=aT[:, fc, j4 * 128:(j4 + 1) * 128],
                                                               rhs=d_[:, fc, nh * 512:(nh + 1) * 512],
                                                               start=(fc == 0), stop=(fc == 7)), reads=r_aT + [r_d], writes=[r_py])
                            kb.op("dve", lambda e: e.scalar_tensor_tensor(out=acc[:, j, nh * 512:(nh + 1) * 512], in0=py[:],
                                                                          scalar=gts[:, j, e_:e_ + 1],
                                                                          in1=acc[:, j, nh * 512:(nh + 1) * 512],
                                                                          op0=ALU.mult, op1=ALU.add),
                                  reads=[r_py, r_gts, r_acc[j][nh]], writes=[r_acc[j][nh]])
                load_w(idx + 2)
            for j in range(TS // 128):
                tt = ts * (TS // 128) + j
                kb.dma("sp", x1t[:], C.x1_d[tt * 128:(tt + 1) * 128, :], reads=[C.r_x1], writes=[r_x1t])
                kb.op("pool", lambda e: e.tensor_tensor(out=acc[:, j, :], in0=acc[:, j, :], in1=g2[:], op=ALU.mult),
                      reads=r_acc[j] + [r_g2], writes=r_acc[j])
                kb.op("dve", lambda e: e.scalar_tensor_tensor(out=x1t[:], in0=x1t[:], scalar=float(DN_ALPHA), in1=acc[:, j, :],
                                                              op0=ALU.mult, op1=ALU.add), reads=[r_x1t] + r_acc[j], writes=[r_x1t])
                xo_, r_xo = xo[tt % 2]
                ln_modulate(C, LW, x1t, r_x1t, xo_[:], r_xo, lg[:], r_lg, lb[:], r_lb)
                kb.dma("sp", x_dst[tt * 128:(tt + 1) * 128, :], xo_[:], reads=[r_xo], writes=[r_xdst])
        kb.barrier()

def alloc_scratch(C):
    C.mod_d, C.r_mod = dram(C, "mod_d", [DEPTH, 128, 6 * D], F32)
    C.qlatT_d, C.r_qlatT = dram(C, "qlatT_d", [8, 128, S], BF16)
    C.qidxT_d, C.r_qidxT = dram(C, "qidxT_d", [2, 128, S], BF16)
    C.kidxT_d, C.r_kidxT = dram(C, "kidxT_d", [128, S], BF16)
    C.widx_d, C.r_widx = dram(C, "widx_d", [S, 4], F32)
    C.ckv_d, C.r_ckv = dram(C, "ckv_d", [S, 128], BF16)
    C.ckvT_d, C.r_ckvT = dram(C, "ckvT_d", [128, S], BF16)
    C.qbT_d, C.r_qbT = dram(C, "qbT_d", [4, 128, S], BF16)
    C.kbT_d, C.r_kbT = dram(C, "kbT_d", [4, 128, S], BF16)
    C.vb_d, C.r_vb = dram(C, "vb_d", [S, 512], BF16)
    C.obT_d, C.r_obT = dram(C, "obT_d", [4, 128, S], BF16)
    C.oaT_d, C.r_oaT = dram(C, "oaT_d", [4, 128, S], BF16)
    C.x1_d, C.r_x1 = dram(C, "x1_d", [S, D], F32)
    C.x2_d, C.r_x2 = dram(C, "x2_d", [S, D], F32)
    C.h2T_d, C.r_h2T = dram(C, "h2T_d", [8, 128, S], BF16)
    C.gates_d, C.r_gates = dram(C, "gates_d", [S, 32], F32)
    C.gatesT_d, C.r_gatesT = dram(C, "gatesT_d", [32, S], F32)


def build(dbg=(), upto=99, skip_sb=False, skip_dsa=False, nlayers=DEPTH):
    nc = bass.Bass("TRN2", target_bir_lowering=False)
    C = Ctx()
    C.skip_sb = skip_sb
    C.skip_dsa = skip_dsa
    C.nc = nc
    C.dbg = set(dbg)

    def inp(name, shape, dt=F32):
        return nc.dram_tensor(name, list(shape), dt, kind="ExternalInput").ap()

    C.x = inp("x", [S, D])
    C.c_col = inp("c_col", [128, 8])
    C.w_ada = inp("w_ada", [DEPTH, D, 6 * D])
    C.b_ada = inp("b_ada", [DEPTH, 6 * D])
    C.w_in = inp("w_in", [DEPTH, D, NCOLS])
    C.w_uk_t = inp("w_uk_t", [DEPTH, 8, 64, 128])
    C.g_kv = inp("g_kv", [DEPTH, 128])
    C.ident_bf = inp("ident_bf", [128, 128], BF16)
    C.negU_bf = inp("negU_bf", [128, 128], BF16)
    C.ones_bf = inp("ones_bf", [128, 128], BF16)
    C.cm_bf = inp("cm_bf", [4, 128, 512], BF16)
    C.w_uv = inp("w_uv", [DEPTH, 128, 8, 64])
    C.rel_bias = inp("rel_bias", [32, 8])
    C.rel_bias_t = inp("rel_bias_t", [8, 32])
    C.dmask = inp("dmask", [128, 128])
    C.irep_bf = inp("irep_bf", [128, 512], BF16)
    C.ident_f = inp("ident_f", [128, 128])
    C.tz = inp("tz", [2, 128, 1024])
    C.bmask = inp("bmask", [8, 1024])
    C.w_a_out = inp("w_a_out", [DEPTH, 512, D])
    C.w_b_out = inp("w_b_out", [DEPTH, 512, D])
    C.w_o = inp("w_o", [DEPTH, D, D])
    C.ln1_g = inp("ln1_g", [DEPTH, D])
    C.ln1_b = inp("ln1_b", [DEPTH, D])
    C.ln2_g = inp("ln2_g", [DEPTH, D])
    C.ln2_b = inp("ln2_b", [DEPTH, D])
    C.w_router = inp("w_router", [DEPTH, D, E])
    C.b_router = inp("b_router", [DEPTH, E])
    C.w_gu = inp("w_gu", [DEPTH, E, D, 2 * FF])
    C.b_gu_t = inp("b_gu_t", [DEPTH, E, 128, 16])
    C.w_dn = inp("w_dn", [DEPTH, E, FF, D])
    C.b_dn = inp("b_dn", [DEPTH, E, D])
    C.out = nc.dram_tensor("out", [S, D], F32, kind="ExternalOutput").ap()
    C.r_out = Res("out")
    C.r_x = Res("x")
    with contextlib.ExitStack() as es:
        C.es = es
        C.kb = KB(nc, es)
        C.ps = [(es.enter_context(nc.psum_tensor(f"ps{i}", [128, 512], F32)), Res(f"ps{i}", psum=True)) for i in range(8)]
        alloc_scratch(C)
        phase_ada(C)
        x_src, r_xsrc = C.x, C.r_x
        for l in range(nlayers):
            if upto >= 1:
                phase_proj(C, l, x_src, r_xsrc)
            if upto >= 2 and not C.skip_sb:
                phase_sb(C, l)
            if upto >= 3 and not C.skip_dsa:
                phase_dsa(C, l)
            if upto >= 4:
                phase_out(C, l, x_src, r_xsrc)
            if upto >= 5:
                last = (l == DEPTH - 1)
                x_dst, r_xdst = (C.out, C.r_out) if last else (C.x2_d, C.r_x2)
                phase_moe(C, l, x_dst, r_xdst)
                x_src, r_xsrc = x_dst, r_xdst
        C.kb.barrier()
    C.ninst = C.kb.ninst
    return nc, C


def host_inputs(inputs, b):
    f = np.float32
    return {
        "x": np.ascontiguousarray(inputs["x"][b]),
        "c_col": np.ascontiguousarray(np.asarray(inputs["c"][b], f).reshape(8, 128).T),
        "w_ada": inputs["w_ada"], "b_ada": inputs["b_ada"], "w_in": inputs["w_in"],
        "w_uk_t": np.ascontiguousarray(np.transpose(inputs["w_uk"], (0, 2, 3, 1))),
        "g_kv": inputs["g_kv"],
        "w_a_out": inputs["w_a_out"], "w_b_out": inputs["w_b_out"], "w_o": inputs["w_o"],
        "ln1_g": inputs["ln1_g"], "ln1_b": inputs["ln1_b"], "ln2_g": inputs["ln2_g"], "ln2_b": inputs["ln2_b"],
        "w_router": inputs["w_router"], "b_router": inputs["b_router"],
        "w_gu": inputs["w_gu"], "w_dn": inputs["w_dn"], "b_dn": inputs["b_dn"],
        "b_gu_t": np.ascontiguousarray(np.asarray(inputs["b_gu"], f).reshape(DEPTH, E, 16, 128).transpose(0, 1, 3, 2)),
        "w_uv": inputs["w_uv"], "rel_bias": inputs["rel_bias"],
        "rel_bias_t": np.ascontiguousarray(np.asarray(inputs["rel_bias"], f).T),
        "tz": _tz_table(np.asarray(inputs["rel_bias"], f)),
        "ident_bf": np.eye(128, dtype=np.float32).astype(ml_dtypes.bfloat16),
        **CONSTS,
    }


def _make_consts():
    bf = ml_dtypes.bfloat16
    jj = np.arange(128)
    negU = -(jj[:, None] >= jj[None, :]).astype(np.float32)
    ones = np.ones((128, 128), np.float32)
    s_ = np.arange(128)[None, :, None]
    t_ = np.arange(512)[None, None, :]
    rel = np.arange(4)[:, None, None]
    cm = ((s_ + 128 * rel) < t_).astype(np.float32)
    dmask = np.where(jj[None, :] > jj[:, None], np.float32(NEG), np.float32(0)).astype(np.float32)
    irep = np.tile(np.eye(128, dtype=np.float32), (1, 4))
    bmask = np.zeros((8, 8, 128), np.float32)
    for h in range(8):
        bmask[h, h, :] = 1.0
    return {"negU_bf": negU.astype(bf), "ones_bf": ones.astype(bf), "cm_bf": cm.astype(bf),
            "dmask": dmask, "irep_bf": irep.astype(bf), "ident_f": np.eye(128, dtype=np.float32),
            "bmask": bmask.reshape(8, 1024)}


def _t5_bucket(n):
    import math
    max_exact = 16
    nf = np.maximum(n, 1).astype(np.float32)
    large = max_exact + (np.log(nf / np.float32(max_exact)) / np.float32(math.log(128 / max_exact))
                         * np.float32(32 - max_exact)).astype(np.int32)
    large = np.minimum(large, 31)
    return np.where(n < max_exact, n, large)


_TZ_IDX = None


def _tz_table(rel_bias):
    global _TZ_IDX
    if _TZ_IDX is None:
        s_ = np.arange(128)[None, :, None]
        t_ = np.arange(128)[None, None, :]
        rel = np.arange(2)[:, None, None]
        dist = np.maximum(128 * rel + t_ - s_, 0)
        _TZ_IDX = _t5_bucket(dist)
    g = rel_bias[_TZ_IDX]
    return np.ascontiguousarray(np.transpose(g, (0, 1, 3, 2)).reshape(2, 128, 1024))


CONSTS = _make_consts()


def kernel(**inputs):
    inputs = {k: np.asarray(v) for k, v in inputs.items()}
    nc, _ = build()
    n = 8
    in_maps = [host_inputs(inputs, b) for b in range(n)]
    res = run_bass_kernel_spmd(nc, in_maps, core_ids=list(range(n)))
    out = np.stack([np.asarray(res.results[b]["out"], dtype=np.float32) for b in range(n)], axis=0)
    return out
```

```python
import contextlib
import numpy as np
import ml_dtypes
import concourse.bass as bass
import concourse.mybir as mybir
from concourse.bass_utils import run_bass_kernel_spmd

F32 = mybir.dt.float32
BF16 = mybir.dt.bfloat16
AF = mybir.ActivationFunctionType
ALU = mybir.AluOpType
AX = mybir.AxisListType

D = 1024
S = 8192
DEPTH = 2
NT = S // 128
NST = S // 512
HD = 64
NCOLS = 4548
NW1 = 2500
E = 32
FF = 1024
LN_EPS = 1e-5
RMS_EPS = 1e-6
DN_ALPHA = (2 * DEPTH) ** 0.25
TOPK = 256
NEG = -1.0e30
MBIG = -30000.0


class Res:
    __slots__ = ("wc", "wd", "rc", "rd", "name", "psum")

    def __init__(self, name="", psum=False):
        self.psum = psum
        self.wc = {}
        self.wd = []
        self.rc = {}
        self.rd = []
        self.name = name


class KB:
    COMPUTE = ("pe", "act", "dve", "pool")

    def __init__(self, nc, es):
        self.nc = nc
        self.es = es
        self.eng = {"pe": nc.tensor, "act": nc.scalar, "dve": nc.vector, "pool": nc.gpsimd, "sp": nc.sync}
        self.sem = {e: es.enter_context(nc.semaphore("s_" + e)) for e in self.COMPUTE}
        self.cnt = {e: 0 for e in self.COMPUTE}
        self.known = {e: {} for e in self.eng}
        self.NSD = 8
        self.dq = {}
        self.ninst = 0

    def _dq(self, q):
        if q not in self.dq:
            self.dq[q] = {"sems": [self.es.enter_context(self.nc.semaphore(f"d_{q}_{i}")) for i in range(self.NSD)],
                          "n": 0}
        return self.dq[q]

    def _wait(self, e, tok):
        if tok[0] == "c":
            _, e2, seq = tok
            if e == "pe" and e2 == "pe":
                return
            if self.known[e].get(e2, 0) >= seq:
                return
            self.eng[e].wait_ge(self.sem[e2], seq)
            self.known[e][e2] = seq
        else:
            _, q, slot, val = tok
            key = (q, slot)
            if self.known[e].get(key, 0) >= val:
                return
            self.eng[e].wait_ge(self.dq[q]["sems"][slot], val)
            self.known[e][key] = val
        self.ninst += 1

    def _deps(self, e, reads, writes):
        for r in reads:
            for e2, seq in r.wc.items():
                self._wait(e, ("c", e2, seq))
            for tok in r.wd:
                self._wait(e, tok)
            if r.psum:
                for e2, seq in r.rc.items():
                    if e2 != e:
                        self._wait(e, ("c", e2, seq))
        for w in writes:
            for e2, seq in w.wc.items():
                self._wait(e, ("c", e2, seq))
            for tok in w.wd:
                self._wait(e, tok)
            for e2, seq in w.rc.items():
                self._wait(e, ("c", e2, seq))
            for tok in w.rd:
                self._wait(e, tok)

    def op(self, e, fn, reads=(), writes=()):
        self._deps(e, reads, writes)
        ins = fn(self.eng[e])
        self.cnt[e] += 1
        seq = self.cnt[e]
        ins.then_inc(self.sem[e], 1)
        self.ninst += 1
        for r in reads:
            r.rc[e] = seq
        for w in writes:
            w.wc = {e: seq}
            w.wd = []
            w.rc = {}
            w.rd = []
        return ins

    def dma(self, q, out, in_, reads=(), writes=(), **kw):
        d = self._dq(q)
        k = d["n"]
        slot = k % self.NSD
        val = 16 * (k // self.NSD + 1)
        if k >= self.NSD:
            self._wait(q, ("d", q, slot, val - 16))
        self._deps(q, reads, writes)
        ins = self.eng[q].dma_start(out=out, in_=in_, **kw)
        ins.then_inc(d["sems"][slot], 16)
        d["n"] += 1
        self.ninst += 1
        tok = ("d", q, slot, val)
        for r in reads:
            r.rd.append(tok)
        for w in writes:
            w.wc = {}
            w.wd = [tok]
            w.rc = {}
            w.rd = []
        return ins

    def barrier(self, engines=None):
        engines = engines or list(self.eng)
        for e in engines:
            for e2 in self.COMPUTE:
                if self.cnt[e2] > 0:
                    self._wait(e, ("c", e2, self.cnt[e2]))
            for q, d in self.dq.items():
                n = d["n"]
                for k in range(max(0, n - self.NSD), n):
                    self._wait(e, ("d", q, k % self.NSD, 16 * (k // self.NSD + 1)))


class Ctx:
    pass


_uid = [0]


def T(C, ph, name, shape, dt):
    _uid[0] += 1
    h = ph.enter_context(C.nc.sbuf_tensor(f"{name}_{_uid[0]}", list(shape), dt))
    return h, Res(name)


def dram(C, name, shape, dt):
    return C.nc.dram_tensor(name, list(shape), dt, kind="Internal").ap(), Res(name)


def phase_ada(C):
    nc, kb = C.nc, C.kb
    with contextlib.ExitStack() as ph:
        ccol, r_ccol = T(C, ph, "ccol", [128, 8], F32)
        cond, r_cond = T(C, ph, "cond", [128, 8], F32)
        condB, r_condB = T(C, ph, "condB", [128, 8, 128], F32)
        wb = [T(C, ph, f"wada{i}", [128, 8, 512], F32) for i in range(2)]
        bB, r_bB = T(C, ph, "badaB", [128, 6144], F32)
        mo = [T(C, ph, f"mo{i}", [128, 512], F32) for i in range(2)]
        kb.dma("sp", ccol[:], C.c_col[:, :], writes=[r_ccol])
        kb.op("act", lambda e: e.activation(out=cond[:], in_=ccol[:], func=AF.Silu), reads=[r_ccol], writes=[r_cond])
        for kc in range(8):
            kb.op("dve", lambda e: e.tensor_copy(out=condB[:, kc, :], in_=cond[:, kc:kc + 1].to_broadcast([128, 128])),
                  reads=[r_cond], writes=[r_condB])
        it = 0
        for l in range(DEPTH):
            kb.dma("sp", bB[:], C.b_ada[l:l + 1, :].to_broadcast([128, 6144]), writes=[r_bB])
            for j in range(12):
                w, r_w = wb[it % 2]
                m, r_m = mo[it % 2]
                ps, r_ps = C.ps[it % 2]
                kb.dma("sp", w[:], C.w_ada[l, :, j * 512:(j + 1) * 512].rearrange("(kc p) n -> p kc n", p=128),
                       writes=[r_w])
                for kc in range(8):
                    kb.op("pe", lambda e: e.matmul(ps[:], lhsT=condB[:, kc, :], rhs=w[:, kc, :],
                                                   start=(kc == 0), stop=(kc == 7)),
                          reads=[r_condB, r_w], writes=[r_ps])
                plus1 = 1.0 if j in (2, 3, 8, 9) else 0.0
                kb.op("dve", lambda e: e.scalar_tensor_tensor(out=m[:], in0=ps[:], scalar=plus1,
                                                              in1=bB[:, j * 512:(j + 1) * 512],
                                                              op0=ALU.add, op1=ALU.add),
                      reads=[r_ps, r_bB], writes=[r_m])
                kb.dma("sp", C.mod_d[l, :, j * 512:(j + 1) * 512], m[:], reads=[r_m], writes=[C.r_mod])
                it += 1
        kb.barrier()


def dram(C, name, shape, dt):
    kind = "ExternalOutput" if name in C.dbg else "Internal"
    return C.nc.dram_tensor(name, list(shape), dt, kind=kind).ap(), Res(name)


class Rot:
    def __init__(self, items):
        self.items = list(items)
        self.i = 0

    def next(self):
        it = self.items[self.i % len(self.items)]
        self.i += 1
        return it


def bfv(ps):
    return ps[:].bitcast(BF16)


def ln_modulate(C, W, xt, r_xt, out_ap, r_out, scB, r_scB, shB, r_shB):
    kb = C.kb
    st_, r_st = W["stats"]
    mv, r_mv = W["mv"]
    sd, r_sd = W["sd"]
    rstd, r_rstd = W["rstd"]
    nmr, r_nmr = W["nmr"]
    xn, r_xn = W["xn"]
    epsT, r_eps = W["eps"]
    for c in range(2):
        kb.op("dve", lambda e: e.bn_stats(out=st_[:, c * 6:(c + 1) * 6], in_=xt[:, c * 512:(c + 1) * 512]),
              reads=[r_xt], writes=[r_st])
    kb.op("dve", lambda e: e.bn_aggr(out=mv[:], in_=st_[:]), reads=[r_st], writes=[r_mv])
    kb.op("act", lambda e: e.activation(out=sd[:], in_=mv[:, 1:2], func=AF.Sqrt, bias=epsT[:, 0:1], scale=1.0),
          reads=[r_mv, r_eps], writes=[r_sd])
    kb.op("dve", lambda e: e.reciprocal(out=rstd[:], in_=sd[:]), reads=[r_sd], writes=[r_rstd])
    kb.op("dve", lambda e: e.tensor_scalar(out=nmr[:], in0=mv[:, 0:1], scalar1=rstd[:, 0:1], scalar2=-1.0,
                                           op0=ALU.mult, op1=ALU.mult),
          reads=[r_mv, r_rstd], writes=[r_nmr])
    kb.op("act", lambda e: e.activation(out=xn[:], in_=xt[:], func=AF.Identity, bias=nmr[:, 0:1], scale=rstd[:, 0:1]),
          reads=[r_xt, r_rstd, r_nmr], writes=[r_xn])
    kb.op("pool", lambda e: e.tensor_tensor(out=xn[:], in0=xn[:], in1=scB, op=ALU.mult),
          reads=[r_xn, r_scB], writes=[r_xn])
    kb.op("dve", lambda e: e.tensor_tensor(out=out_ap, in0=xn[:], in1=shB, op=ALU.add),
          reads=[r_xn, r_shB], writes=[r_out])


def ln_work(C, ph, tag, eps):
    W = {
        "stats": T(C, ph, tag + "stats", [128, 12], F32),
        "mv": T(C, ph, tag + "mv", [128, 2], F32),
        "sd": T(C, ph, tag + "sd", [128, 1], F32),
        "rstd": T(C, ph, tag + "rstd", [128, 1], F32),
        "nmr": T(C, ph, tag + "nmr", [128, 1], F32),
        "xn": T(C, ph, tag + "xn", [128, 1024], F32),
        "eps": T(C, ph, tag + "eps", [128, 1], F32),
    }
    e_, r_e = W["eps"]
    C.kb.op("pool", lambda e: e.memset(e_[:], eps), writes=[r_e])
    return W


def phase_proj(C, l, x_src, r_xsrc):
    nc, kb = C.nc, C.kb
    with contextlib.ExitStack() as ph:
        W1, r_W1 = T(C, ph, "W1", [128, 8, NW1], BF16)
        Wkk, r_Wkk = T(C, ph, "Wkk", [128, 8, 128], BF16)
        wuk, r_wuk = T(C, ph, "wuk", [128, 4, 128], BF16)
        gkvB, r_gkvB = T(C, ph, "gkvB", [128, 128], F32)
        scB, r_scB = T(C, ph, "scB", [128, 1024], F32)
        shB, r_shB = T(C, ph, "shB", [128, 1024], F32)
        ident, r_id = T(C, ph, "ident", [128, 128], BF16)
        reps, r_reps = T(C, ph, "reps", [128, 1], F32)
        kb.op("pool", lambda e: e.memset(reps[:], RMS_EPS), writes=[r_reps])
        kb.dma("pool", W1[:], C.w_in[l, :, 0:NW1].rearrange("(kc p) n -> p kc n", p=128), writes=[r_W1])
        for hh in range(2):
            kb.dma("pool", Wkk[:, :, hh * 64:(hh + 1) * 64],
                   C.w_in[l, :, 896:960].rearrange("(kc p) n -> p kc n", p=128), writes=[r_Wkk])
        kb.dma("pool", wuk[:], C.w_uk_t[l].rearrange("(hp two) d r -> (two d) hp r", two=2), writes=[r_wuk])
        kb.dma("sp", gkvB[:], C.g_kv[l:l + 1, :].to_broadcast([128, 128]), writes=[r_gkvB])
        kb.dma("sp", scB[:], C.mod_d[l, :, 1024:2048], reads=[C.r_mod], writes=[r_scB])
        kb.dma("sp", shB[:], C.mod_d[l, :, 0:1024], reads=[C.r_mod], writes=[r_shB])
        kb.dma("sp", ident[:], C.ident_bf[:, :], writes=[r_id])
        LW = [ln_work(C, ph, f"lw{i}", LN_EPS) for i in range(2)]
        xb = [T(C, ph, f"xb{i}", [128, 1024], F32) for i in range(2)]
        hb = [T(C, ph, f"hb{i}", [128, 1024], BF16) for i in range(2)]
        hT = [(T(C, ph, f"hT{i}", [128, 8, 512], BF16)[0], [Res(f"hT{i}_{j}") for j in range(4)]) for i in range(2)]
        qaT = [T(C, ph, f"qaT{i}", [128, 512], BF16) for i in range(2)]
        def stage(name, shape, n):
            return [(T(C, ph, f"{name}{i}", shape, BF16)[0], [Res(f"{name}{i}_{j}") for j in range(n)]) for i in range(2)]
        s_qlat = stage("s_qlat", [128, 8, 512], 8)
        s_qidx = stage("s_qidx", [128, 2, 512], 2)
        s_kidx = stage("s_kidx", [128, 1, 512], 1)
        s_qb = stage("s_qb", [128, 4, 512], 4)
        s_kb = stage("s_kb", [128, 4, 512], 4)
        s_ckvT = stage("s_ckvT", [128, 4, 128], 4)
        s_ckv = stage("s_ckv", [128, 4, 128], 4)
        s_vb = stage("s_vb", [128, 4, 512], 4)
        s_wi = [(T(C, ph, f"s_wi{i}", [128, 4, 4], F32)[0], [Res(f"s_wi{i}_{j}") for j in range(4)]) for i in range(2)]
        ss_t = [T(C, ph, f"ss{i}", [128, 1], F32) for i in range(2)]
        sd2_t = [T(C, ph, f"sd2{i}", [128, 1], F32) for i in range(2)]
        rs_t = [T(C, ph, f"rs{i}", [128, 1], F32) for i in range(2)]
        junk, r_junk = T(C, ph, "junk", [128, 128], F32)
        pp = Rot(C.ps[0:6])
        psT, r_psT = C.ps[7]
        psT2, r_psT2 = C.ps[6]
        ev = Rot(["act", "dve"])

        def evac(eng, out_ap, in_ap, reads, writes, scale=None):
            if eng == "act":
                if scale is None:
                    kb.op("act", lambda e: e.copy(out=out_ap, in_=in_ap), reads=reads, writes=writes)
                else:
                    kb.op("act", lambda e: e.mul(out=out_ap, in_=in_ap, mul=scale), reads=reads, writes=writes)
            else:
                if scale is None:
                    kb.op("dve", lambda e: e.tensor_copy(out=out_ap, in_=in_ap), reads=reads, writes=writes)
                else:
                    kb.op("dve", lambda e: e.tensor_scalar(out=out_ap, in0=in_ap, scalar1=scale, scalar2=None,
                                                           op0=ALU.mult), reads=reads, writes=writes)

        for st in range(NST):
            sp_ = st % 2
            hTt, r_hT = hT[sp_]
            t0 = st * 512
            for j in range(4):
                tt = st * 4 + j
                xt, r_xt = xb[tt % 2]
                hbt, r_hb = hb[tt % 2]
                kb.dma("sp", xt[:], x_src[tt * 128:(tt + 1) * 128, :], reads=[r_xsrc], writes=[r_xt])
                ln_modulate(C, LW[tt % 2], xt, r_xt, hbt[:], r_hb, scB[:], r_scB, shB[:], r_shB)
                for kc in range(8):
                    kb.op("pe", lambda e: e.transpose(out=bfv(psT)[:, kc * 128:(kc + 1) * 128],
                                                      in_=hbt[:, kc * 128:(kc + 1) * 128], identity=ident[:]),
                          reads=[r_hb, r_id], writes=[r_psT])
                evac(ev.next(), hTt[:, :, j * 128:(j + 1) * 128],
                     bfv(psT).rearrange("p (kc t) -> p kc t", kc=8), [r_psT], [r_hT[j]])
            for j in range(4):
                tt = st * 4 + j
                ps, r_ps = pp.next()
                for (c0, c1, o0) in ((512, 640, 0), (960, 964, 128)):
                    for kc in range(8):
                        kb.op("pe", lambda e: e.matmul(ps[:, o0:o0 + (c1 - c0)], lhsT=hTt[:, kc, j * 128:(j + 1) * 128],
                                                       rhs=W1[:, kc, c0:c1], start=(kc == 0), stop=(kc == 7)),
                              reads=[r_hT[j], r_W1], writes=[r_ps])
                ss, r_ss = ss_t[tt % 2]
                sd2, r_sd2 = sd2_t[tt % 2]
                rs, r_rs = rs_t[tt % 2]
                kb.op("act", lambda e: e.activation(out=junk[:], in_=ps[:, 0:128], func=AF.Square, accum_out=ss[:]),
                      reads=[r_ps], writes=[r_junk, r_ss])
                kb.op("act", lambda e: e.activation(out=sd2[:], in_=ss[:], func=AF.Sqrt, bias=reps[:, 0:1],
                                                    scale=1.0 / 128.0), reads=[r_ss, r_reps], writes=[r_sd2])
                kb.op("dve", lambda e: e.reciprocal(out=rs[:], in_=sd2[:]), reads=[r_sd2], writes=[r_rs])
                ckv_t, r_ckv = s_ckv[sp_]
                kb.op("dve", lambda e: e.scalar_tensor_tensor(out=ckv_t[:, j, :], in0=ps[:, 0:128], scalar=rs[:, 0:1],
                                                              in1=gkvB[:], op0=ALU.mult, op1=ALU.mult),
                      reads=[r_ps, r_rs, r_gkvB], writes=[r_ckv[j]])
                wi_t, r_wi = s_wi[sp_]
                kb.op("dve", lambda e: e.tensor_scalar(out=wi_t[:, j, :], in0=ps[:, 128:132], scalar1=1.0 / 16.0,
                                                       scalar2=None, op0=ALU.mult), reads=[r_ps], writes=[r_wi[j]])
                kb.op("pe", lambda e: e.transpose(out=bfv(psT2)[:, j * 128:(j + 1) * 128], in_=ckv_t[:, j, :],
                                                  identity=ident[:]), reads=[r_ckv[j], r_id], writes=[r_psT2])
                ps, r_ps = pp.next()
                for kc in range(8):
                    kb.op("pe", lambda e: e.matmul(ps[:], lhsT=hTt[:, kc, j * 128:(j + 1) * 128],
                                                   rhs=W1[:, kc, 1988:2500], start=(kc == 0), stop=(kc == 7)),
                          reads=[r_hT[j], r_W1], writes=[r_ps])
                vb_t, r_vb = s_vb[sp_]
                evac(ev.next(), vb_t[:, j, :], ps[:], [r_ps], [r_vb[j]])
            ckvT_t, r_ckvT = s_ckvT[sp_]
            evac(ev.next(), ckvT_t[:].rearrange("p j t -> p (j t)"), bfv(psT2)[:, 0:512], [r_psT2], r_ckvT)
            rows = slice(t0, t0 + 512)
            kb.dma("sp", C.ckv_d[rows, :].rearrange("(j p) r -> p j r", p=128), ckv_t[:], reads=r_ckv, writes=[C.r_ckv])
            kb.dma("sp", C.widx_d[rows, :].rearrange("(j p) r -> p j r", p=128), wi_t[:], reads=r_wi, writes=[C.r_widx])
            kb.dma("sp", C.vb_d[rows, :].rearrange("(j p) r -> p j r", p=128), vb_t[:], reads=r_vb, writes=[C.r_vb])
            kb.dma("sp", C.ckvT_d[:, rows], ckvT_t[:].rearrange("p j t -> p (j t)"), reads=r_ckvT, writes=[C.r_ckvT])
            allj = r_hT
            groups = [("qa", 0, 4), ("qidx", 640, 2), ("kidx", None, 1), ("qb", 964, 4), ("kb", 1476, 4)]
            for (gname, c0, nch) in groups:
                for c in range(nch):
                    ps, r_ps = pp.next()
                    for kc in range(8):
                        if gname == "kidx":
                            lw, rl = Wkk[:, kc, :], r_Wkk
                        else:
                            lw, rl = W1[:, kc, c0 + c * 128:c0 + (c + 1) * 128], r_W1
                        kb.op("pe", lambda e: e.matmul(ps[:], lhsT=lw, rhs=hTt[:, kc, :], start=(kc == 0), stop=(kc == 7)),
                              reads=allj + [rl], writes=[r_ps])
                    if gname == "qa":
                        qa, r_qa = qaT[c % 2]
                        evac(ev.next(), qa[:], ps[:], [r_ps], [r_qa])
                        for hp in range(2):
                            h = 2 * c + hp
                            ps2, r_ps2 = pp.next()
                            kb.op("pe", lambda e: e.matmul(ps2[:], lhsT=wuk[hp * 64:(hp + 1) * 64, c, :],
                                                           rhs=qa[hp * 64:(hp + 1) * 64, :], start=True, stop=True),
                                  reads=[r_wuk, r_qa], writes=[r_ps2])
                            stg, r_stg = s_qlat[sp_]
                            evac(ev.next(), stg[:, h, :], ps2[:], [r_ps2], [r_stg[h]], scale=0.125)
                    else:
                        stg, r_stg = {"qidx": s_qidx, "kidx": s_kidx, "qb": s_qb, "kb": s_kb}[gname][sp_]
                        evac(ev.next(), stg[:, c, :], ps[:], [r_ps], [r_stg[c]], scale=(0.125 if gname == "kb" else None))
            cols = slice(t0, t0 + 512)
            stg, r_stg = s_qlat[sp_]
            kb.dma("sp", C.qlatT_d[:, :, cols].rearrange("h p t -> p h t"), stg[:], reads=r_stg, writes=[C.r_qlatT])
            stg, r_stg = s_qidx[sp_]
            kb.dma("sp", C.qidxT_d[:, :, cols].rearrange("h p t -> p h t"), stg[:], reads=r_stg, writes=[C.r_qidxT])
            stg, r_stg = s_kidx[sp_]
            kb.dma("sp", C.kidxT_d[:, cols], stg[:, 0, :], reads=r_stg, writes=[C.r_kidxT])
            stg, r_stg = s_qb[sp_]
            kb.dma("sp", C.qbT_d[:, :, cols].rearrange("h p t -> p h t"), stg[:], reads=r_stg, writes=[C.r_qbT])
            stg, r_stg = s_kb[sp_]
            kb.dma("sp", C.kbT_d[:, :, cols].rearrange("h p t -> p h t"), stg[:], reads=r_stg, writes=[C.r_kbT])
        kb.barrier()


def phase_sb(C, l):
    nc, kb = C.nc, C.kb
    with contextlib.ExitStack() as ph:
        negU, r_negU = T(C, ph, "negU", [128, 128], BF16)
        ones, r_ones = T(C, ph, "ones", [128, 128], BF16)
        cm, r_cm = T(C, ph, "cm", [128, 4, 512], BF16)
        kb.dma("sp", negU[:], C.negU_bf[:, :], writes=[r_negU])
        kb.dma("sp", ones[:], C.ones_bf[:, :], writes=[r_ones])
        kb.dma("sp", cm[:], C.cm_bf.rearrange("r p t -> p r t"), writes=[r_cm])
        qT = [T(C, ph, f"sbq{i}", [128, S], BF16) for i in range(2)]
        kT = [T(C, ph, f"sbk{i}", [128, S], BF16) for i in range(2)]
        vv = [T(C, ph, f"sbv{i}", [128, NT, 128], BF16) for i in range(2)]
        et = [T(C, ph, f"et{i}", [128, 512], F32) for i in range(2)]
        spT = [T(C, ph, f"spT{i}", [128, 512], BF16) for i in range(4)]
        arg2 = [T(C, ph, f"arg2{i}", [128, 512], F32) for i in range(2)]
        AT = [T(C, ph, f"AT{i}", [128, 512], BF16) for i in range(4)]
        carry = [T(C, ph, f"carry{i}", [128, 512], F32) for i in range(2)]
        ostg = [T(C, ph, f"ostg{i}", [128, 512], BF16) for i in range(2)]
        psz = C.ps[0:2]
        psa = C.ps[2:4]
        psc = C.ps[4:5]
        pso = C.ps[5:7]

        items = []
        gid = 0
        for c in range(4):
            for qi in range(NST):
                for hp in range(2):
                    nb = 4 * qi + 4
                    for j in range(nb - 1, -1, -1):
                        items.append(dict(c=c, qi=qi, hp=hp, j=j, first=(j == nb - 1), last=(j == 0), g=gid,
                                          rel=(j - 4 * qi)))
                    gid += 1
        loaded = set()

        def load_pair(c):
            if c in loaded or c >= 4:
                return
            loaded.add(c)
            q, r_q = qT[c % 2]
            k, r_k = kT[c % 2]
            v, r_v = vv[c % 2]
            kb.dma("sp", q[:], C.qbT_d[c], reads=[C.r_qbT], writes=[r_q])
            kb.dma("sp", k[:], C.kbT_d[c], reads=[C.r_kbT], writes=[r_k])
            kb.dma("sp", v[:], C.vb_d[:, c * 128:(c + 1) * 128].rearrange("(j p) d -> p j d", p=128),
                   reads=[C.r_vb], writes=[r_v])

        def stA(n, it):
            c, qi, hp, j = it["c"], it["qi"], it["hp"], it["j"]
            q, r_q = qT[c % 2]
            k, r_k = kT[c % 2]
            P = slice(hp * 64, hp * 64 + 64)
            pz, r_pz = psz[n % 2]
            kb.op("pe", lambda e: e.matmul(pz[:], lhsT=k[P, j * 128:(j + 1) * 128], rhs=q[P, qi * 512:(qi + 1) * 512],
                                           start=True, stop=True), reads=[r_k, r_q], writes=[r_pz])
            e_, r_e = et[n % 2]
            kb.op("act", lambda e: e.activation(out=e_[:], in_=pz[:], func=AF.Exp), reads=[r_pz], writes=[r_e])

        def stA2(n, it):
            e_, r_e = et[n % 2]
            s_, r_s = spT[n % 4]
            kb.op("act", lambda e: e.activation(out=s_[:], in_=e_[:], func=AF.Ln, bias=1.0, scale=1.0),
                  reads=[r_e], writes=[r_s])
            if it["rel"] >= 0:
                kb.op("dve", lambda e: e.tensor_tensor(out=s_[:], in0=s_[:], in1=cm[:, it["rel"], :], op=ALU.mult),
                      reads=[r_s, r_cm], writes=[r_s])

        def stB(n, it):
            c, qi, hp, j = it["c"], it["qi"], it["hp"], it["j"]
            q, r_q = qT[c % 2]
            k, r_k = kT[c % 2]
            P = slice(hp * 64, hp * 64 + 64)
            s_, r_s = spT[n % 4]
            pa, r_pa = psa[n % 2]
            pc, r_pc = psc[0]
            kb.op("pe", lambda e: e.matmul(pa[:], lhsT=k[P, j * 128:(j + 1) * 128], rhs=q[P, qi * 512:(qi + 1) * 512],
                                           start=True, stop=False), reads=[r_k, r_q], writes=[r_pa])
            kb.op("pe", lambda e: e.matmul(pa[:], lhsT=negU[:], rhs=s_[:], start=False, stop=True),
                  reads=[r_negU, r_s], writes=[r_pa])
            kk = n % 2
            cb, r_cb = carry[kk]
            cbn, r_cbn = carry[1 - kk]
            if it["first"]:
                kb.op("dve", lambda e: e.memset(cb[:], 0.0), writes=[r_cb])
            if not it["last"]:
                kb.op("pe", lambda e: e.matmul(pc[:], lhsT=ones[:], rhs=s_[:], start=True, stop=True),
                      reads=[r_ones, r_s], writes=[r_pc])
                kb.op("dve", lambda e: e.tensor_tensor(out=cbn[:], in0=pc[:], in1=cb[:], op=ALU.add),
                      reads=[r_pc, r_cb], writes=[r_cbn])
            a2, r_a2 = arg2[n % 2]
            kb.op("dve", lambda e: e.tensor_tensor(out=a2[:], in0=pa[:], in1=cb[:], op=ALU.subtract),
                  reads=[r_pa, r_cb], writes=[r_a2])

        def stB2(n, it):
            a2, r_a2 = arg2[n % 2]
            a_, r_a = AT[n % 4]
            kb.op("act", lambda e: e.activation(out=a_[:], in_=a2[:], func=AF.Exp), reads=[r_a2], writes=[r_a])
            if it["rel"] >= 0:
                kb.op("pool", lambda e: e.tensor_tensor(out=a_[:], in0=a_[:], in1=cm[:, it["rel"], :], op=ALU.mult),
                      reads=[r_a, r_cm], writes=[r_a])

        def stC(n, it):
            c, qi, hp, j = it["c"], it["qi"], it["hp"], it["j"]
            v, r_v = vv[c % 2]
            a_, r_a = AT[n % 4]
            po, r_po = pso[(c * NST + qi) % 2]
            kb.op("pe", lambda e: e.matmul(po[hp * 64:(hp + 1) * 64, :], lhsT=v[:, j, hp * 64:(hp + 1) * 64], rhs=a_[:],
                                           start=it["first"], stop=it["last"]), reads=[r_v, r_a], writes=[r_po])
            if it["last"] and hp == 1:
                og, r_og = ostg[(c * NST + qi) % 2]
                kb.op("dve", lambda e: e.tensor_copy(out=og[:], in_=po[:]), reads=[r_po], writes=[r_og])
                kb.dma("sp", C.obT_d[c, :, qi * 512:(qi + 1) * 512], og[:], reads=[r_og], writes=[C.r_obT])
                if qi == NST - 1:
                    load_pair(c + 2)

        N = len(items)
        load_pair(0)
        load_pair(1)
        for n in range(N + 3):
            if n < N:
                stA(n, items[n])
            if 0 <= n - 2 < N:
                stB2(n - 2, items[n - 2])
            if n < N:
                stA2(n, items[n])
            if 0 <= n - 1 < N:
                stB(n - 1, items[n - 1])
            if 0 <= n - 3 < N:
                stC(n - 3, items[n - 3])
        kb.barrier()


def phase_dsa(C, l):
    nc, kb = C.nc, C.kb
    with contextlib.ExitStack() as ph:
        kidxT, r_kidxT = T(C, ph, "kidxT", [128, S], BF16)
        ckvT, r_ckvT = T(C, ph, "ckvT", [128, S], BF16)
        ckv, r_ckv = T(C, ph, "ckv", [128, NT, 128], BF16)
        wuv, r_wuv = T(C, ph, "wuv", [128, 8, 64], BF16)
        sc, r_sc = T(C, ph, "sc", [128, S], F32)
        mb, r_mb = T(C, ph, "mb", [128, S], BF16)
        dmask, r_dmask = T(C, ph, "dmask", [128, 128], F32)
        Irep, r_Irep = T(C, ph, "Irep", [128, 512], BF16)
        onesF, r_onesF = T(C, ph, "onesF", [128, 128], BF16)
        identf, r_identf = T(C, ph, "identf", [128, 128], F32)
        Tz, r_Tz = T(C, ph, "Tz", [128, 2, 1024], F32)
        b31B, r_b31B = T(C, ph, "b31B", [128, 8], F32)
        bmask, r_bmask = T(C, ph, "bmask", [8, 1024], F32)
        rbT, r_rbT = T(C, ph, "rbT", [8, 32], F32)
        c8, r_c8 = T(C, ph, "c8", [8, 1], F32)
        b31c, r_b31c = T(C, ph, "b31c", [40, 1], F32)
        b31hi, r_b31hi = T(C, ph, "b31hi", [40, 1], BF16)
        b31hif, r_b31hif = T(C, ph, "b31hif", [40, 1], F32)
        b31lo, r_b31lo = T(C, ph, "b31lo", [72, 1], F32)
        CB, r_CB = T(C, ph, "CB", [72, 1024], BF16)
        CBd = Res("CBdyn")
        kb.dma("sp", kidxT[:], C.kidxT_d[:, :], reads=[C.r_kidxT], writes=[r_kidxT])
        kb.dma("sp", ckvT[:], C.ckvT_d[:, :], reads=[C.r_ckvT], writes=[r_ckvT])
        kb.dma("sp", ckv[:], C.ckv_d.rearrange("(j p) r -> p j r", p=128), reads=[C.r_ckv], writes=[r_ckv])
        kb.dma("pool", wuv[:], C.w_uv[l], writes=[r_wuv])
        kb.dma("sp", dmask[:], C.dmask[:, :], writes=[r_dmask])
        kb.dma("sp", Irep[:], C.irep_bf[:, :], writes=[r_Irep])
        kb.dma("sp", onesF[:], C.ones_bf[:, :], writes=[r_onesF])
        kb.dma("sp", identf[:], C.ident_f[:, :], writes=[r_identf])
        kb.dma("sp", Tz[:], C.tz.rearrange("r p c -> p r c"), writes=[r_Tz])
        kb.dma("sp", b31B[:], C.rel_bias[31:32, :].to_broadcast([128, 8]), writes=[r_b31B])
        kb.dma("sp", bmask[:], C.bmask[:, :], writes=[r_bmask])
        kb.dma("sp", rbT[:], C.rel_bias_t[:, :], writes=[r_rbT])
        for r_ in range(2):
            kb.op("dve", lambda e: e.tensor_tensor(out=Tz[:, r_, :].rearrange("p (h t) -> p h t", h=8),
                                                   in0=Tz[:, r_, :].rearrange("p (h t) -> p h t", h=8),
                                                   in1=b31B[:].to_broadcast([128, 8, 128]) if False else
                                                   b31B[:].unsqueeze(2).to_broadcast([128, 8, 128]),
                                                   op=ALU.subtract), reads=[r_Tz, r_b31B], writes=[r_Tz])
        kb.op("dve", lambda e: e.reduce_max(out=c8[:], in_=rbT[:], axis=AX.X), reads=[r_rbT], writes=[r_c8])
        kb.op("dve", lambda e: e.tensor_scalar(out=c8[:], in0=c8[:], scalar1=-1.0, scalar2=None, op0=ALU.mult),
              reads=[r_c8], writes=[r_c8])
        kb.op("pool", lambda e: e.memset(CB[:], 0.0), writes=[r_CB])
        kb.dma("sp", b31c[32:40, :], C.rel_bias_t[:, 31:32], writes=[r_b31c], allow_slow_non_contiguous=True)
        kb.dma("sp", b31lo[64:72, :], C.rel_bias_t[:, 31:32], writes=[r_b31lo], allow_slow_non_contiguous=True)
        kb.op("dve", lambda e: e.tensor_copy(out=b31hi[32:40, :], in_=b31c[32:40, :]), reads=[r_b31c], writes=[r_b31hi])
        kb.op("dve", lambda e: e.tensor_copy(out=b31hif[32:40, :], in_=b31hi[32:40, :]), reads=[r_b31hi], writes=[r_b31hif])
        kb.dma("sp", b31lo[32:40, :], b31hif[32:40, :], reads=[r_b31hif], writes=[r_b31lo])
        bm32, r_bm32 = T(C, ph, "bm32", [72, 1024], F32)
        kb.dma("sp", bm32[32:40, :], C.bmask[:, :], writes=[r_bm32])
        kb.dma("sp", bm32[64:72, :], C.bmask[:, :], writes=[r_bm32])
        kb.op("dve", lambda e: e.tensor_scalar(out=CB[32:40, :], in0=bm32[32:40, :], scalar1=b31hif[32:40, 0:1],
                                               scalar2=None, op0=ALU.mult), reads=[r_bm32, r_b31hif], writes=[r_CB])
        hi64, r_hi64 = T(C, ph, "hi64", [72, 1], F32)
        kb.dma("sp", hi64[64:72, :], b31hif[32:40, :], reads=[r_b31hif], writes=[r_hi64])
        kb.op("dve", lambda e: e.tensor_tensor(out=b31lo[64:72, :], in0=b31lo[64:72, :], in1=hi64[64:72, :], op=ALU.subtract),
              reads=[r_b31lo, r_hi64], writes=[r_b31lo])
        kb.op("dve", lambda e: e.tensor_scalar(out=CB[64:72, :], in0=bm32[64:72, :], scalar1=b31lo[64:72, 0:1],
                                               scalar2=None, op0=ALU.mult), reads=[r_bm32, r_b31lo], writes=[r_CB])

        CB2, r_CB2 = T(C, ph, "CB2", [72, 1024], BF16)
        kb.op("dve", lambda e: e.tensor_copy(out=CB2[:], in_=CB[:]), reads=[r_CB], writes=[r_CB2])
        CB3, r_CB3 = T(C, ph, "CB3", [72, 1024], BF16)
        kb.op("dve", lambda e: e.tensor_copy(out=CB3[:], in_=CB[:]), reads=[r_CB], writes=[r_CB3])
        CBs = [(CB, r_CB, Res("CBdyn0")), (CB2, r_CB2, Res("CBdyn1")), (CB3, r_CB3, Res("CBdyn2"))]
        g8, r_g8 = T(C, ph, "g8", [8, 128], F32)
        negK, r_negK = T(C, ph, "negK", [8, 1], F32)
        kb.dma("sp", g8[:], C.g_kv[l:l + 1, :].to_broadcast([8, 128]), writes=[r_g8])
        kb.op("dve", lambda e: e.tensor_reduce(out=negK[:], in_=g8[:], axis=AX.X, op=ALU.max, apply_absolute_value=True),
              reads=[r_g8], writes=[r_negK])
        kb.op("dve", lambda e: e.tensor_scalar(out=negK[:], in0=negK[:], scalar1=-1.02 * (128.0 ** 0.5), scalar2=None,
                                               op0=ALU.mult), reads=[r_negK], writes=[r_negK])
        sc2, r_sc2 = T(C, ph, "sc2", [128, S], F32)
        mb2, r_mb2 = T(C, ph, "mb2", [128, S], BF16)
        scs = [(sc, r_sc), (sc2, r_sc2)]
        mbs = [(mb, r_mb), (mb2, r_mb2)]
        qi_t = [T(C, ph, f"qi{i}", [128, 2, 128], BF16) for i in range(3)]
        wi_t = [T(C, ph, f"wi{i}", [128, 4], F32) for i in range(3)]
        ql_t = [T(C, ph, f"ql{i}", [128, 1024], BF16) for i in range(3)]
        qsq, r_qsq = T(C, ph, "qsq", [128, 1024], BF16)
        sq8, r_sq8 = T(C, ph, "sq8", [8, 1024], F32)
        rl = [T(C, ph, f"rl{i}", [128, 512], F32) for i in range(4)]
        rtmp, r_rtmp = T(C, ph, "rtmp", [128, 512], F32)
        m8, r_m8 = T(C, ph, "m8", [128, 8], F32)
        zt = [T(C, ph, f"zt{i}", [128, 512], F32) for i in range(2)]
        pT = [T(C, ph, f"pT{i}", [128, 512], BF16) for i in range(3)]
        rden, r_rden = T(C, ph, "rden", [128, 512], F32)
        olT, r_olT = T(C, ph, "olT", [128, 1024], BF16)
        oast = [T(C, ph, f"oast{i}", [128, 4, 128], BF16) for i in range(2)]
        psA = Rot(C.ps[0:2])
        psZ = C.ps[2:4]
        psO = C.ps[4:6]
        psD = C.ps[6:8]
        nctr = [0]

        def chunks_of(i):
            nk = (i + 1) * 128
            return [(c0, min(512, nk - c0)) for c0 in range(0, nk, 512)]

        def stL(i):
            tcols = slice(i * 128, (i + 1) * 128)
            qi, r_qi = qi_t[i % 3]
            wi, r_wi = wi_t[i % 3]
            ql, r_ql = ql_t[i % 3]
            kb.dma("sp", qi[:], C.qidxT_d[:, :, tcols].rearrange("c p t -> p c t"), reads=[C.r_qidxT], writes=[r_qi])
            kb.dma("sp", wi[:], C.widx_d[tcols, :], reads=[C.r_widx], writes=[r_wi])
            kb.dma("sp", ql[:].rearrange("p (h t) -> p h t", h=8), C.qlatT_d[:, :, tcols].rearrange("h p t -> p h t"),
                   reads=[C.r_qlatT], writes=[r_ql])
            cb, r_cb, r_cbd = CBs[i % 3]
            kb.op("dve", lambda e: e.tensor_tensor(out=qsq[:], in0=ql[:], in1=ql[:], op=ALU.mult),
                  reads=[r_ql], writes=[r_qsq])
            ps, r_ps = psA.next()
            ps2, r_ps2 = psA.next()
            for half, (p_, r_p) in enumerate(((ps, r_ps), (ps2, r_ps2))):
                kb.op("pe", lambda e: e.matmul(p_[0:8, :], lhsT=onesF[:, 0:8], rhs=qsq[:, half * 512:(half + 1) * 512],
                                               start=True, stop=True), reads=[r_onesF, r_qsq], writes=[r_p])
                kb.op("act", lambda e: e.activation(out=sq8[:, half * 512:(half + 1) * 512], in_=p_[0:8, :], func=AF.Sqrt),
                      reads=[r_p], writes=[r_sq8])
            kb.op("dve", lambda e: e.tensor_scalar(out=sq8[:], in0=sq8[:], scalar1=negK[:, 0:1], scalar2=c8[:, 0:1],
                                                    op0=ALU.mult, op1=ALU.add), reads=[r_sq8, r_negK, r_c8], writes=[r_sq8])
            kb.op("dve", lambda e: e.tensor_tensor(out=cb[0:8, :], in0=sq8[:], in1=bmask[:], op=ALU.mult),
                  reads=[r_sq8, r_bmask, r_cb], writes=[r_cbd])

        def stA(i):
            tcols = slice(i * 128, (i + 1) * 128)
            qi, r_qi = qi_t[i % 3]
            wi, r_wi = wi_t[i % 3]
            sc_, r_sc_ = scs[i % 2]
            for (c0, w) in chunks_of(i):
                for h in range(4):
                    P = slice((h % 2) * 64, (h % 2) * 64 + 64)
                    ps, r_ps = psA.next()
                    kb.op("pe", lambda e: e.matmul(ps[:, 0:w], lhsT=qi[P, h // 2, :], rhs=kidxT[P, c0:c0 + w],
                                                   start=True, stop=True), reads=[r_qi, r_kidxT], writes=[r_ps])
                    r_, r_r = rl[h]
                    kb.op("act", lambda e: e.activation(out=r_[:, 0:w], in_=ps[:, 0:w], func=AF.Relu),
                          reads=[r_ps], writes=[r_r])
                    if h == 0:
                        kb.op("dve", lambda e: e.tensor_scalar(out=sc_[:, c0:c0 + w], in0=r_[:, 0:w], scalar1=wi[:, 0:1],
                                                               scalar2=None, op0=ALU.mult),
                              reads=[r_r, r_wi], writes=[r_sc_])
                    else:
                        kb.op("dve", lambda e: e.scalar_tensor_tensor(out=sc_[:, c0:c0 + w], in0=r_[:, 0:w],
                                                                      scalar=wi[:, h:h + 1], in1=sc_[:, c0:c0 + w],
                                                                      op0=ALU.mult, op1=ALU.add),
                              reads=[r_r, r_wi, r_sc_], writes=[r_sc_])
            kb.op("dve", lambda e: e.tensor_tensor(out=sc_[:, tcols], in0=sc_[:, tcols], in1=dmask[:], op=ALU.add),
                  reads=[r_sc_, r_dmask], writes=[r_sc_])

        def stB(i):
            nk = (i + 1) * 128
            sc_, r_sc_ = scs[i % 2]
            mb_, r_mb_ = mbs[i % 2]
            if i >= 2:
                for rnd in range(TOPK // 8):
                    kb.op("dve", lambda e: e.max(out=m8[:], in_=sc_[:, 0:nk]), reads=[r_sc_], writes=[r_m8])
                    kb.op("dve", lambda e: e.match_replace(out=sc_[:, 0:nk], in_to_replace=m8[:], in_values=sc_[:, 0:nk],
                                                           imm_value=2.0 * NEG), reads=[r_sc_, r_m8], writes=[r_sc_])
                kb.op("dve", lambda e: e.tensor_scalar(out=mb_[:, 0:nk], in0=sc_[:, 0:nk], scalar1=1.5 * NEG, scalar2=MBIG,
                                                       op0=ALU.is_gt, op1=ALU.mult), reads=[r_sc_], writes=[r_mb_])
            else:
                kb.op("dve", lambda e: e.tensor_scalar(out=mb_[:, 0:nk], in0=sc_[:, 0:nk], scalar1=0.5 * NEG, scalar2=MBIG,
                                                       op0=ALU.is_le, op1=ALU.mult), reads=[r_sc_], writes=[r_mb_])

        def stF(i, jbs):
            ql, r_ql = ql_t[i % 3]
            mb_, r_mb_ = mbs[i % 2]
            cb, r_cb, r_cbd = CBs[i % 3]
            items = [(half, jb) for half in range(2) for jb in jbs]
            first_jb, last_jb = i, 0

            def fa(n, it):
                half, jb = it
                hc = slice(half * 512, (half + 1) * 512)
                pz, r_pz = psZ[n % 2]
                kb.op("pe", lambda e: e.matmul(pz[:], lhsT=ckvT[:, jb * 128:(jb + 1) * 128], rhs=ql[:, hc],
                                               start=True, stop=False), reads=[r_ckvT, r_ql], writes=[r_pz])
                kb.op("pe", lambda e: e.matmul(pz[:], lhsT=mb_[:, jb * 128:(jb + 1) * 128], rhs=Irep[:],
                                               start=False, stop=False), reads=[r_mb_, r_Irep], writes=[r_pz])
                kb.op("pe", lambda e: e.matmul(pz[:], lhsT=onesF[0:72, :], rhs=cb[0:72, hc], start=False, stop=True),
                      reads=[r_onesF, r_cb, r_cbd], writes=[r_pz])
                p_, r_p = pT[n % 3]
                rel = i - jb
                if rel <= 1:
                    z_, r_z = zt[n % 2]
                    kb.op("dve", lambda e: e.tensor_tensor(out=z_[:], in0=pz[:], in1=Tz[:, rel, hc], op=ALU.add),
                          reads=[r_pz, r_Tz], writes=[r_z])
                    kb.op("act", lambda e: e.activation(out=p_[:], in_=z_[:], func=AF.Exp), reads=[r_z], writes=[r_p])
                else:
                    kb.op("act", lambda e: e.activation(out=p_[:], in_=pz[:], func=AF.Exp), reads=[r_pz], writes=[r_p])

            def fb(n, it):
                half, jb = it
                p_, r_p = pT[n % 3]
                po, r_po = psO[half]
                pd, r_pd = psD[half]
                kb.op("pe", lambda e: e.matmul(po[:], lhsT=ckv[:, jb, :], rhs=p_[:], start=(jb == first_jb), stop=(jb == last_jb)),
                      reads=[r_ckv, r_p], writes=[r_po])
                kb.op("pe", lambda e: e.matmul(pd[:], lhsT=onesF[:], rhs=p_[:], start=(jb == first_jb), stop=(jb == last_jb)),
                      reads=[r_onesF, r_p], writes=[r_pd])

            N = len(items)
            base = nctr[0]
            for n in range(N + 1):
                if n < N:
                    fa(base + n, items[n])
                if n >= 1:
                    fb(base + n - 1, items[n - 1])
            nctr[0] += N

        def stT(i):
            tcols = slice(i * 128, (i + 1) * 128)
            for half in range(2):
                hc = slice(half * 512, (half + 1) * 512)
                po, r_po = psO[half]
                pd, r_pd = psD[half]
                kb.op("dve", lambda e: e.reciprocal(out=rden[:], in_=pd[:]), reads=[r_pd], writes=[r_rden])
                kb.op("dve", lambda e: e.tensor_tensor(out=olT[:, hc], in0=po[:], in1=rden[:], op=ALU.mult),
                      reads=[r_po, r_rden], writes=[r_olT])
            ps, r_ps = psA.next()
            for h in range(8):
                kb.op("pe", lambda e: e.matmul(ps[(h % 2) * 64:(h % 2) * 64 + 64, (h // 2) * 128:(h // 2 + 1) * 128],
                                               lhsT=wuv[:, h, :], rhs=olT[:, h * 128:(h + 1) * 128], start=True, stop=True),
                      reads=[r_wuv, r_olT], writes=[r_ps])
            og, r_og = oast[i % 2]
            kb.op("act", lambda e: e.copy(out=og[:].rearrange("p c t -> p (c t)"), in_=ps[:]), reads=[r_ps], writes=[r_og])
            kb.dma("sp", C.oaT_d[:, :, tcols].rearrange("c p t -> p c t"), og[:], reads=[r_og], writes=[C.r_oaT])

        stL(0)
        stL(1)
        stA(0)
        stB(0)
        stA(1)
        for i in range(NT):
            if i + 2 < NT:
                stL(i + 2)
            stF(i, [jb for jb in (i, i - 1) if jb >= 0])
            if i + 2 < NT:
                stA(i + 2)
            if i + 1 < NT:
                stB(i + 1)
            if i >= 2:
                stF(i, list(range(i - 2, -1, -1)))
            stT(i)
        kb.barrier()


def bload(C, ph, name, src_ap, n, reads=()):
    t, r = T(C, ph, name, [128, n], F32)
    C.kb.dma("sp", t[:], src_ap, reads=list(reads), writes=[r])
    return t, r


def phase_out(C, l, x_src, r_xsrc):
    nc, kb = C.nc, C.kb
    with contextlib.ExitStack() as ph:
        Wg, r_Wg = T(C, ph, "Wg", [128, 8, 2048], BF16)
        wao, r_wao = T(C, ph, "wao", [128, 4, 1024], BF16)
        wbo, r_wbo = T(C, ph, "wbo", [128, 4, 1024], BF16)
        wo, r_wo = T(C, ph, "wo", [128, 8, 1024], BF16)
        wr, r_wr = T(C, ph, "wr", [128, 8, 32], F32)
        kb.dma("pool", Wg[:], C.w_in[l, :, NW1:NCOLS].rearrange("(kc p) n -> p kc n", p=128), writes=[r_Wg])
        kb.dma("pool", wao[:], C.w_a_out[l].rearrange("(kc p) n -> p kc n", p=128), writes=[r_wao])
        kb.dma("pool", wbo[:], C.w_b_out[l].rearrange("(kc p) n -> p kc n", p=128), writes=[r_wbo])
        kb.dma("pool", wo[:], C.w_o[l].rearrange("(kc p) n -> p kc n", p=128), writes=[r_wo])
        kb.dma("sp", wr[:], C.w_router[l].rearrange("(kc p) n -> p kc n", p=128), writes=[r_wr])
        sc1, r_sc1 = bload(C, ph, "sc1", C.mod_d[l, :, 1024:2048], 1024, [C.r_mod])
        sh1, r_sh1 = bload(C, ph, "sh1", C.mod_d[l, :, 0:1024], 1024, [C.r_mod])
        g1, r_g1 = bload(C, ph, "g1", C.mod_d[l, :, 2048:3072], 1024, [C.r_mod])
        sh2, r_sh2 = bload(C, ph, "sh2", C.mod_d[l, :, 3072:4096], 1024, [C.r_mod])
        sc2, r_sc2 = bload(C, ph, "sc2", C.mod_d[l, :, 4096:5120], 1024, [C.r_mod])
        lg, r_lg = bload(C, ph, "lg", C.ln1_g[l:l + 1, :].to_broadcast([128, 1024]), 1024)
        lb, r_lb = bload(C, ph, "lb", C.ln1_b[l:l + 1, :].to_broadcast([128, 1024]), 1024)
        brB, r_brB = bload(C, ph, "brB", C.b_router[l:l + 1, :].to_broadcast([128, 32]), 32)
        ident, r_id = T(C, ph, "identb", [128, 128], BF16)
        identf, r_idf = T(C, ph, "identf2", [128, 128], F32)
        kb.dma("sp", ident[:], C.ident_bf[:, :], writes=[r_id])
        kb.dma("sp", identf[:], C.ident_f[:, :], writes=[r_idf])
        LW = ln_work(C, ph, "olw", LN_EPS)
        xres = [T(C, ph, "xres0", [128, 4, 1024], F32)[0]] * 2
        r_xres = [[Res(f"xres_{j}") for j in range(4)]] * 2
        hb, r_hb = T(C, ph, "ohb", [128, 1024], BF16)
        hT, _ = T(C, ph, "ohT", [128, 8, 512], BF16)
        r_hT = [Res(f"ohT_{j}") for j in range(4)]
        oaT, r_oaT = T(C, ph, "ooaT", [128, 4, 512], BF16)
        obT, r_obT = T(C, ph, "oobT", [128, 4, 512], BF16)
        sga = [T(C, ph, f"sga{i}", [128, 512], F32) for i in range(2)]
        sgb = [T(C, ph, f"sgb{i}", [128, 512], F32) for i in range(2)]
        t1 = [T(C, ph, f"t1{i}", [128, 512], F32) for i in range(2)]
        t2 = [T(C, ph, f"t2{i}", [128, 512], F32) for i in range(2)]
        mT, _ = T(C, ph, "mergT", [128, 8, 512], BF16)
        r_mT = [Res(f"mergT_{j}") for j in range(8)]
        yt, r_yt = T(C, ph, "yt", [128, 1024], F32)
        zt, r_zt = T(C, ph, "zt_o", [128, 1024], F32)
        x1t = [T(C, ph, f"x1t{i}", [128, 1024], F32) for i in range(2)]
        h2f, r_h2f = T(C, ph, "h2f", [128, 1024], F32)
        h2T32, r_h2T32 = T(C, ph, "h2T32", [128, 8, 128], F32)
        h2Ts = [T(C, ph, f"h2Ts{i}", [128, 8, 512], BF16)[0] for i in range(2)]
        r_h2Ts = [[Res(f"h2Ts{i}_{j}") for j in range(4)] for i in range(2)]
        lgt, r_lgt = T(C, ph, "lgt", [128, 32], F32)
        m8, r_m8 = T(C, ph, "om8", [128, 8], F32)
        nmx, r_nmx = T(C, ph, "nmx", [128, 1], F32)
        msk, r_msk = T(C, ph, "msk", [128, 32], F32)
        ex, r_ex = T(C, ph, "ex", [128, 32], F32)
        rs, r_rs = T(C, ph, "ors", [128, 1], F32)
        gst = [T(C, ph, f"gst{i}", [128, 4, 32], F32)[0] for i in range(2)]
        r_gst = [[Res(f"gst{i}_{j}") for j in range(4)] for i in range(2)]
        gTs = [T(C, ph, f"gTs{i}", [32, 512], F32)[0] for i in range(2)]
        r_gTs = [[Res(f"gTs{i}_{j}") for j in range(4)] for i in range(2)]
        pp = Rot(C.ps[0:4])
        psT, r_psT = C.ps[7]
        psT32 = C.ps[5:7]
        psY = C.ps[4:5]

        for st in range(getattr(C, 'out_nst', NST)):
            stage = getattr(C, 'out_stage', 99)
            sp_ = st % 2
            t0 = st * 512
            cols = slice(t0, t0 + 512)
            xr = xres[sp_]
            for j in range(4):
                tt = st * 4 + j
                kb.dma("sp", xr[:, j, :], x_src[tt * 128:(tt + 1) * 128, :], reads=[r_xsrc], writes=[r_xres[sp_][j]])
                ln_modulate(C, LW, xr[:, j, :], r_xres[sp_][j], hb[:], r_hb, sc1[:], r_sc1, sh1[:], r_sh1)
                for kc in range(8):
                    kb.op("pe", lambda e: e.transpose(out=bfv(psT)[:, kc * 128:(kc + 1) * 128],
                                                      in_=hb[:, kc * 128:(kc + 1) * 128], identity=ident[:]),
                          reads=[r_hb, r_id], writes=[r_psT])
                kb.op("act", lambda e: e.copy(out=hT[:, :, j * 128:(j + 1) * 128],
                                              in_=bfv(psT).rearrange("p (kc t) -> p kc t", kc=8)),
                      reads=[r_psT], writes=[r_hT[j]])
            kb.dma("sp", oaT[:], C.oaT_d[:, :, cols].rearrange("c p t -> p c t"), reads=[C.r_oaT], writes=[r_oaT])
            kb.dma("sp", obT[:], C.obT_d[:, :, cols].rearrange("c p t -> p c t"), reads=[C.r_obT], writes=[r_obT])
            for n_ in range(8 if stage >= 1 else 0):
                ncs = slice(n_ * 128, (n_ + 1) * 128)
                pga, r_pga = pp.next()
                pgb, r_pgb = pp.next()
                pa, r_pa = pp.next()
                pb, r_pb = pp.next()
                for kc in range(8):
                    kb.op("pe", lambda e: e.matmul(pga[:], lhsT=Wg[:, kc, n_ * 128:(n_ + 1) * 128], rhs=hT[:, kc, :],
                                                   start=(kc == 0), stop=(kc == 7)), reads=r_hT + [r_Wg], writes=[r_pga])
                for kc in range(8):
                    kb.op("pe", lambda e: e.matmul(pgb[:], lhsT=Wg[:, kc, 1024 + n_ * 128:1024 + (n_ + 1) * 128],
                                                   rhs=hT[:, kc, :], start=(kc == 0), stop=(kc == 7)),
                          reads=r_hT + [r_Wg], writes=[r_pgb])
                for c in range(4):
                    kb.op("pe", lambda e: e.matmul(pa[:], lhsT=wao[:, c, ncs], rhs=oaT[:, c, :], start=(c == 0), stop=(c == 3)),
                          reads=[r_wao, r_oaT], writes=[r_pa])
                for c in range(4):
                    kb.op("pe", lambda e: e.matmul(pb[:], lhsT=wbo[:, c, ncs], rhs=obT[:, c, :], start=(c == 0), stop=(c == 3)),
                          reads=[r_wbo, r_obT], writes=[r_pb])
                sa, r_sa = sga[n_ % 2]
                sb_, r_sb = sgb[n_ % 2]
                a1, r_a1 = t1[n_ % 2]
                a2, r_a2 = t2[n_ % 2]
                kb.op("act", lambda e: e.activation(out=sa[:], in_=pga[:], func=AF.Sigmoid), reads=[r_pga], writes=[r_sa])
                kb.op("act", lambda e: e.activation(out=sb_[:], in_=pgb[:], func=AF.Sigmoid), reads=[r_pgb], writes=[r_sb])
                kb.op("dve", lambda e: e.tensor_tensor(out=a1[:], in0=pa[:], in1=sa[:], op=ALU.mult),
                      reads=[r_pa, r_sa], writes=[r_a1])
                kb.op("dve", lambda e: e.tensor_tensor(out=a2[:], in0=pb[:], in1=sb_[:], op=ALU.mult),
                      reads=[r_pb, r_sb], writes=[r_a2])
                kb.op("pool", lambda e: e.tensor_tensor(out=mT[:, n_, :], in0=a1[:], in1=a2[:], op=ALU.add),
                      reads=[r_a1, r_a2], writes=[r_mT[n_]])
            for j in range(4 if stage >= 2 else 0):
                tt = st * 4 + j
                for nh in range(2):
                    py, r_py = psY[0]
                    for n_ in range(8):
                        kb.op("pe", lambda e: e.matmul(py[:], lhsT=mT[:, n_, j * 128:(j + 1) * 128],
                                                       rhs=wo[:, n_, nh * 512:(nh + 1) * 512], start=(n_ == 0), stop=(n_ == 7)),
                              reads=r_mT + [r_wo], writes=[r_py])
                    kb.op("dve", lambda e: e.tensor_tensor(out=yt[:, nh * 512:(nh + 1) * 512], in0=py[:],
                                                           in1=g1[:, nh * 512:(nh + 1) * 512], op=ALU.mult),
                          reads=[r_py, r_g1], writes=[r_yt])
                kb.op("dve", lambda e: e.scalar_tensor_tensor(out=zt[:], in0=xr[:, j, :], scalar=float(DN_ALPHA), in1=yt[:],
                                                              op0=ALU.mult, op1=ALU.add),
                      reads=[r_xres[sp_][j], r_yt], writes=[r_zt])
                x1, r_x1 = x1t[tt % 2]
                ln_modulate(C, LW, zt, r_zt, x1[:], r_x1, lg[:], r_lg, lb[:], r_lb)
                kb.dma("sp", C.x1_d[tt * 128:(tt + 1) * 128, :], x1[:], reads=[r_x1], writes=[C.r_x1])
                if stage < 3:
                    continue
                ln_modulate(C, LW, x1, r_x1, h2f[:], r_h2f, sc2[:], r_sc2, sh2[:], r_sh2)
                sub = getattr(C, 'out_sub', 99)
                for half in range(2 if sub >= 1 else 0):
                    p32, r_p32 = psT32[half]
                    for k4 in range(4):
                        kc = half * 4 + k4
                        kb.op("pe", lambda e: e.transpose(out=p32[:, k4 * 128:(k4 + 1) * 128],
                                                          in_=h2f[:, kc * 128:(kc + 1) * 128], identity=identf[:]),
                              reads=[r_h2f, r_idf], writes=[r_p32])
                    if sub < 2:
                        continue
                    kb.op("act", lambda e: e.copy(out=h2T32[:, half * 4:half * 4 + 4, :],
                                                  in_=p32[:].rearrange("p (k t) -> p k t", k=4)),
                          reads=[r_p32], writes=[r_h2T32])
                    if sub < 3:
                        continue
                    kb.op("dve", lambda e: e.tensor_copy(out=h2Ts[sp_][:, half * 4:half * 4 + 4, j * 128:(j + 1) * 128],
                                                         in_=p32[:].rearrange("p (k t) -> p k t", k=4)),
                          reads=[r_p32], writes=[r_h2Ts[sp_][j]])
                if stage < 4:
                    continue
                pl, r_pl = pp.next()
                for kc in range(8):
                    kb.op("pe", lambda e: e.matmul(pl[:, 0:32], lhsT=h2T32[:, kc, :], rhs=wr[:, kc, :],
                                                   start=(kc == 0), stop=(kc == 7)), reads=[r_h2T32, r_wr], writes=[r_pl])
                kb.op("dve", lambda e: e.tensor_tensor(out=lgt[:], in0=pl[:, 0:32], in1=brB[:], op=ALU.add),
                      reads=[r_pl, r_brB], writes=[r_lgt])
                kb.op("dve", lambda e: e.max(out=m8[:], in_=lgt[:]), reads=[r_lgt], writes=[r_m8])
                kb.op("dve", lambda e: e.tensor_scalar(out=nmx[:], in0=m8[:, 0:1], scalar1=-1.0, scalar2=None, op0=ALU.mult),
                      reads=[r_m8], writes=[r_nmx])
                kb.op("dve", lambda e: e.tensor_scalar(out=msk[:], in0=lgt[:], scalar1=m8[:, 3:4], scalar2=None, op0=ALU.is_ge),
                      reads=[r_lgt, r_m8], writes=[r_msk])
                kb.op("act", lambda e: e.activation(out=ex[:], in_=lgt[:], func=AF.Exp, bias=nmx[:, 0:1], scale=1.0),
                      reads=[r_lgt, r_nmx], writes=[r_ex])
                kb.op("dve", lambda e: e.tensor_tensor(out=ex[:], in0=ex[:], in1=msk[:], op=ALU.mult),
                      reads=[r_ex, r_msk], writes=[r_ex])
                kb.op("dve", lambda e: e.reduce_sum(out=rs[:], in_=ex[:], axis=AX.X), reads=[r_ex], writes=[r_rs])
                kb.op("dve", lambda e: e.reciprocal(out=rs[:], in_=rs[:]), reads=[r_rs], writes=[r_rs])
                kb.op("dve", lambda e: e.tensor_scalar(out=gst[sp_][:, j, :], in0=ex[:], scalar1=rs[:, 0:1], scalar2=None,
                                                       op0=ALU.mult), reads=[r_ex, r_rs], writes=[r_gst[sp_][j]])
                pg, r_pg = pp.next()
                kb.op("pe", lambda e: e.transpose(out=pg[0:32, 0:128], in_=gst[sp_][:, j, :], identity=identf[:]),
                      reads=[r_gst[sp_][j], r_idf], writes=[r_pg])
                kb.op("act", lambda e: e.copy(out=gTs[sp_][:, j * 128:(j + 1) * 128], in_=pg[0:32, 0:128]),
                      reads=[r_pg], writes=[r_gTs[sp_][j]])
            if stage < 4:
                continue
            kb.dma("sp", C.h2T_d[:, :, cols].rearrange("k p t -> p k t"), h2Ts[sp_][:], reads=r_h2Ts[sp_], writes=[C.r_h2T])
            kb.dma("sp", C.gates_d[cols, :].rearrange("(j p) e -> p j e", p=128), gst[sp_][:], reads=r_gst[sp_],
                   writes=[C.r_gates])
            kb.dma("sp", C.gatesT_d[:, cols], gTs[sp_][:], reads=r_gTs[sp_], writes=[C.r_gatesT])
        kb.barrier()


TS = 1024


def phase_moe(C, l, x_dst, r_xdst):
    nc, kb = C.nc, C.kb
    with contextlib.ExitStack() as ph:
        wgu = [T(C, ph, f"wgu{i}", [128, 8, 2048], BF16) for i in range(2)]
        wdn = [T(C, ph, f"wdn{i}", [128, 8, 1024], BF16) for i in range(2)]
        bgu, r_bgu = T(C, ph, "bgu", [128, E, 16], F32)
        bdn, r_bdn = T(C, ph, "bdn", [32, 1024], F32)
        kb.dma("sp", bgu[:], C.b_gu_t[l].rearrange("e p c -> p e c"), writes=[r_bgu])
        kb.dma("sp", bdn[:], C.b_dn[l], writes=[r_bdn])
        kb.op("pool", lambda e: e.tensor_scalar(out=bgu[:, :, 8:16], in0=bgu[:, :, 8:16], scalar1=1.0, scalar2=None,
                                                op0=ALU.add), reads=[r_bgu], writes=[r_bgu])
        g2, r_g2 = bload(C, ph, "g2", C.mod_d[l, :, 5120:6144], 1024, [C.r_mod])
        lg, r_lg = bload(C, ph, "lg2", C.ln2_g[l:l + 1, :].to_broadcast([128, 1024]), 1024)
        lb, r_lb = bload(C, ph, "lb2", C.ln2_b[l:l + 1, :].to_broadcast([128, 1024]), 1024)
        LW = ln_work(C, ph, "mlw", LN_EPS)
        h2T, r_h2T = T(C, ph, "mh2T", [128, 8, TS], BF16)
        gts, r_gts = T(C, ph, "mgts", [128, TS // 128, 32], F32)
        gT, r_gT = T(C, ph, "mgT", [32, TS], F32)
        acc, _ = T(C, ph, "macc", [128, TS // 128, 1024], F32)
        r_acc = [[Res(f"acc{j}_{nh}") for nh in range(2)] for j in range(TS // 128)]
        a_sb = [T(C, ph, f"a_sb{i}", [128, 512], F32) for i in range(2)]
        sg = [T(C, ph, f"sg{i}", [128, 512], BF16) for i in range(2)]
        gg = [T(C, ph, f"gg{i}", [128, 512], F32) for i in range(2)]
        u_sb = [T(C, ph, f"u_sb{i}", [128, 512], F32) for i in range(2)]
        actT = [T(C, ph, f"actT{i}", [128, 8, 512], BF16)[0] for i in range(2)]
        r_actT = [[Res(f"actT{i}_{f}") for f in range(8)] for i in range(2)]
        x1t, r_x1t = T(C, ph, "mx1t", [128, 1024], F32)
        xo = [(x1t, r_x1t)] * 2
        ppA = Rot(C.ps[0:2])
        ppU = Rot(C.ps[2:4])
        ppY = Rot(C.ps[4:8])
        wloaded = {}

        def load_w(idx):
            if idx in wloaded or idx >= (S // TS) * E:
                return
            wloaded[idx] = True
            e_ = idx % E
            g_, r_g = wgu[idx % 2]
            d_, r_d = wdn[idx % 2]
            kb.dma("pool", g_[:], C.w_gu[l, e_].rearrange("(kc p) n -> p kc n", p=128), writes=[r_g])
            kb.dma("pool", d_[:], C.w_dn[l, e_].rearrange("(kc p) n -> p kc n", p=128), writes=[r_d])

        load_w(0)
        load_w(1)
        for ts in range(S // TS):
            tcols = slice(ts * TS, (ts + 1) * TS)
            kb.dma("sp", h2T[:], C.h2T_d[:, :, tcols].rearrange("k p t -> p k t"), reads=[C.r_h2T], writes=[r_h2T])
            kb.dma("sp", gts[:], C.gates_d[tcols, :].rearrange("(j p) e -> p j e", p=128), reads=[C.r_gates], writes=[r_gts])
            kb.dma("sp", gT[:], C.gatesT_d[:, tcols], reads=[C.r_gatesT], writes=[r_gT])
            for j in range(TS // 128):
                for nh in range(2):
                    py, r_py = ppY.next()
                    kb.op("pe", lambda e: e.matmul(py[:], lhsT=gT[:, j * 128:(j + 1) * 128], rhs=bdn[:, nh * 512:(nh + 1) * 512],
                                                   start=True, stop=True), reads=[r_gT, r_bdn], writes=[r_py])
                    kb.op("act", lambda e: e.copy(out=acc[:, j, nh * 512:(nh + 1) * 512], in_=py[:]),
                          reads=[r_py], writes=[r_acc[j][nh]])
            for e_ in range(E):
                idx = ts * E + e_
                g_, r_g = wgu[idx % 2]
                d_, r_d = wdn[idx % 2]
                for t2 in range(TS // 512):
                    aT = actT[t2 % 2]
                    r_aT = r_actT[t2 % 2]
                    hc = slice(t2 * 512, (t2 + 1) * 512)
                    for fc in range(8):
                        pa, r_pa = ppA.next()
                        pu, r_pu = ppU.next()
                        for kc in range(8):
                            kb.op("pe", lambda e: e.matmul(pa[:], lhsT=g_[:, kc, fc * 128:(fc + 1) * 128], rhs=h2T[:, kc, hc],
                                                           start=(kc == 0), stop=(kc == 7)), reads=[r_g, r_h2T], writes=[r_pa])
                        for kc in range(8):
                            kb.op("pe", lambda e: e.matmul(pu[:], lhsT=g_[:, kc, 1024 + fc * 128:1024 + (fc + 1) * 128],
                                                           rhs=h2T[:, kc, hc], start=(kc == 0), stop=(kc == 7)),
                                  reads=[r_g, r_h2T], writes=[r_pu])
                        a_, r_a = a_sb[fc % 2]
                        s_, r_s = sg[fc % 2]
                        q_, r_q = gg[fc % 2]
                        u_, r_u = u_sb[fc % 2]
                        kb.op("dve", lambda e: e.tensor_scalar(out=a_[:], in0=pa[:], scalar1=bgu[:, e_, fc:fc + 1], scalar2=7.0,
                                                               op0=ALU.add, op1=ALU.min), reads=[r_pa, r_bgu], writes=[r_a])
                        kb.op("act", lambda e: e.activation(out=s_[:], in_=a_[:], func=AF.Sigmoid, scale=1.702),
                              reads=[r_a], writes=[r_s])
                        kb.op("dve", lambda e: e.tensor_scalar(out=u_[:], in0=pu[:], scalar1=bgu[:, e_, 8 + fc:9 + fc], scalar2=8.0,
                                                               op0=ALU.add, op1=ALU.min), reads=[r_pu, r_bgu], writes=[r_u])
                        kb.op("pool", lambda e: e.tensor_tensor(out=q_[:], in0=a_[:], in1=s_[:], op=ALU.mult),
                              reads=[r_a, r_s], writes=[r_q])
                        kb.op("dve", lambda e: e.scalar_tensor_tensor(out=aT[:, fc, :], in0=u_[:], scalar=-6.0, in1=q_[:],
                                                                      op0=ALU.max, op1=ALU.mult),
                              reads=[r_u, r_q], writes=[r_aT[fc]])
                    for j4 in range(4):
                        j = t2 * 4 + j4
                        for nh in range(2):
                            py, r_py = ppY.next()
                            for fc in range(8):
                                kb.op("pe", lambda e: e.matmul(py[:], lhsT=aT[:, fc, j4 * 128:(j4 + 1) * 128],
                                                               rhs=d_[:, fc, nh * 512:(nh + 1) * 512],
                                                               start=(fc == 0), stop=(fc == 7)), reads=r_aT + [r_d], writes=[r_py])
                            kb.op("dve", lambda e: e.scalar_tensor_tensor(out=acc[:, j, nh * 512:(nh + 1) * 512], in0=py[:],
                                                                          scalar=gts[:, j, e_:e_ + 1],
                                                                          in1=acc[:, j, nh * 512:(nh + 1) * 512],
                                                                          op0=ALU.mult, op1=ALU.add),
                                  reads=[r_py, r_gts, r_acc[j][nh]], writes=[r_acc[j][nh]])
                load_w(idx + 2)
            for j in range(TS // 128):
                tt = ts * (TS // 128) + j
                kb.dma("sp", x1t[:], C.x1_d[tt * 128:(tt + 1) * 128, :], reads=[C.r_x1], writes=[r_x1t])
                kb.op("pool", lambda e: e.tensor_tensor(out=acc[:, j, :], in0=acc[:, j, :], in1=g2[:], op=ALU.mult),
                      reads=r_acc[j] + [r_g2], writes=r_acc[j])
                kb.op("dve", lambda e: e.scalar_tensor_tensor(out=x1t[:], in0=x1t[:], scalar=float(DN_ALPHA), in1=acc[:, j, :],
                                                              op0=ALU.mult, op1=ALU.add), reads=[r_x1t] + r_acc[j], writes=[r_x1t])
                xo_, r_xo = xo[tt % 2]
                ln_modulate(C, LW, x1t, r_x1t, xo_[:], r_xo, lg[:], r_lg, lb[:], r_lb)
                kb.dma("sp", x_dst[tt * 128:(tt + 1) * 128, :], xo_[:], reads=[r_xo], writes=[r_xdst])
        kb.barrier()

def alloc_scratch(C):
    C.mod_d, C.r_mod = dram(C, "mod_d", [DEPTH, 128, 6 * D], F32)
    C.qlatT_d, C.r_qlatT = dram(C, "qlatT_d", [8, 128, S], BF16)
    C.qidxT_d, C.r_qidxT = dram(C, "qidxT_d", [2, 128, S], BF16)
    C.kidxT_d, C.r_kidxT = dram(C, "kidxT_d", [128, S], BF16)
    C.widx_d, C.r_widx = dram(C, "widx_d", [S, 4], F32)
    C.ckv_d, C.r_ckv = dram(C, "ckv_d", [S, 128], BF16)
    C.ckvT_d, C.r_ckvT = dram(C, "ckvT_d", [128, S], BF16)
    C.qbT_d, C.r_qbT = dram(C, "qbT_d", [4, 128, S], BF16)
    C.kbT_d, C.r_kbT = dram(C, "kbT_d", [4, 128, S], BF16)
    C.vb_d, C.r_vb = dram(C, "vb_d", [S, 512], BF16)
    C.obT_d, C.r_obT = dram(C, "obT_d", [4, 128, S], BF16)
    C.oaT_d, C.r_oaT = dram(C, "oaT_d", [4, 128, S], BF16)
    C.x1_d, C.r_x1 = dram(C, "x1_d", [S, D], F32)
    C.x2_d, C.r_x2 = dram(C, "x2_d", [S, D], F32)
    C.h2T_d, C.r_h2T = dram(C, "h2T_d", [8, 128, S], BF16)
    C.gates_d, C.r_gates = dram(C, "gates_d", [S, 32], F32)
    C.gatesT_d, C.r_gatesT = dram(C, "gatesT_d", [32, S], F32)


def build(dbg=(), upto=99, skip_sb=False, skip_dsa=False, nlayers=DEPTH):
    nc = bass.Bass("TRN2", target_bir_lowering=False)
    C = Ctx()
    C.skip_sb = skip_sb
    C.skip_dsa = skip_dsa
    C.nc = nc
    C.dbg = set(dbg)

    def inp(name, shape, dt=F32):
        return nc.dram_tensor(name, list(shape), dt, kind="ExternalInput").ap()

    C.x = inp("x", [S, D])
    C.c_col = inp("c_col", [128, 8])
    C.w_ada = inp("w_ada", [DEPTH, D, 6 * D])
    C.b_ada = inp("b_ada", [DEPTH, 6 * D])
    C.w_in = inp("w_in", [DEPTH, D, NCOLS])
    C.w_uk_t = inp("w_uk_t", [DEPTH, 8, 64, 128])
    C.g_kv = inp("g_kv", [DEPTH, 128])
    C.ident_bf = inp("ident_bf", [128, 128], BF16)
    C.negU_bf = inp("negU_bf", [128, 128], BF16)
    C.ones_bf = inp("ones_bf", [128, 128], BF16)
    C.cm_bf = inp("cm_bf", [4, 128, 512], BF16)
    C.w_uv = inp("w_uv", [DEPTH, 128, 8, 64])
    C.rel_bias = inp("rel_bias", [32, 8])
    C.rel_bias_t = inp("rel_bias_t", [8, 32])
    C.dmask = inp("dmask", [128, 128])
    C.irep_bf = inp("irep_bf", [128, 512], BF16)
    C.ident_f = inp("ident_f", [128, 128])
    C.tz = inp("tz", [2, 128, 1024])
    C.bmask = inp("bmask", [8, 1024])
    C.w_a_out = inp("w_a_out", [DEPTH, 512, D])
    C.w_b_out = inp("w_b_out", [DEPTH, 512, D])
    C.w_o = inp("w_o", [DEPTH, D, D])
    C.ln1_g = inp("ln1_g", [DEPTH, D])
    C.ln1_b = inp("ln1_b", [DEPTH, D])
    C.ln2_g = inp("ln2_g", [DEPTH, D])
    C.ln2_b = inp("ln2_b", [DEPTH, D])
    C.w_router = inp("w_router", [DEPTH, D, E])
    C.b_router = inp("b_router", [DEPTH, E])
    C.w_gu = inp("w_gu", [DEPTH, E, D, 2 * FF])
    C.b_gu_t = inp("b_gu_t", [DEPTH, E, 128, 16])
    C.w_dn = inp("w_dn", [DEPTH, E, FF, D])
    C.b_dn = inp("b_dn", [DEPTH, E, D])
    C.out = nc.dram_tensor("out", [S, D], F32, kind="ExternalOutput").ap()
    C.r_out = Res("out")
    C.r_x = Res("x")
    with contextlib.ExitStack() as es:
        C.es = es
        C.kb = KB(nc, es)
        C.ps = [(es.enter_context(nc.psum_tensor(f"ps{i}", [128, 512], F32)), Res(f"ps{i}", psum=True)) for i in range(8)]
        alloc_scratch(C)
        phase_ada(C)
        x_src, r_xsrc = C.x, C.r_x
        for l in range(nlayers):
            if upto >= 1:
                phase_proj(C, l, x_src, r_xsrc)
            if upto >= 2 and not C.skip_sb:
                phase_sb(C, l)
            if upto >= 3 and not C.skip_dsa:
                phase_dsa(C, l)
            if upto >= 4:
                phase_out(C, l, x_src, r_xsrc)
            if upto >= 5:
                last = (l == DEPTH - 1)
                x_dst, r_xdst = (C.out, C.r_out) if last else (C.x2_d, C.r_x2)
                phase_moe(C, l, x_dst, r_xdst)
                x_src, r_xsrc = x_dst, r_xdst
        C.kb.barrier()
    C.ninst = C.kb.ninst
    return nc, C


def host_inputs(inputs, b):
    f = np.float32
    return {
        "x": np.ascontiguousarray(inputs["x"][b]),
        "c_col": np.ascontiguousarray(np.asarray(inputs["c"][b], f).reshape(8, 128).T),
        "w_ada": inputs["w_ada"], "b_ada": inputs["b_ada"], "w_in": inputs["w_in"],
        "w_uk_t": np.ascontiguousarray(np.transpose(inputs["w_uk"], (0, 2, 3, 1))),
        "g_kv": inputs["g_kv"],
        "w_a_out": inputs["w_a_out"], "w_b_out": inputs["w_b_out"], "w_o": inputs["w_o"],
        "ln1_g": inputs["ln1_g"], "ln1_b": inputs["ln1_b"], "ln2_g": inputs["ln2_g"], "ln2_b": inputs["ln2_b"],
        "w_router": inputs["w_router"], "b_router": inputs["b_router"],
        "w_gu": inputs["w_gu"], "w_dn": inputs["w_dn"], "b_dn": inputs["b_dn"],
        "b_gu_t": np.ascontiguousarray(np.asarray(inputs["b_gu"], f).reshape(DEPTH, E, 16, 128).transpose(0, 1, 3, 2)),
        "w_uv": inputs["w_uv"], "rel_bias": inputs["rel_bias"],
        "rel_bias_t": np.ascontiguousarray(np.asarray(inputs["rel_bias"], f).T),
        "tz": _tz_table(np.asarray(inputs["rel_bias"], f)),
        "ident_bf": np.eye(128, dtype=np.float32).astype(ml_dtypes.bfloat16),
        **CONSTS,
    }


def _make_consts():
    bf = ml_dtypes.bfloat16
    jj = np.arange(128)
    negU = -(jj[:, None] >= jj[None, :]).astype(np.float32)
    ones = np.ones((128, 128), np.float32)
    s_ = np.arange(128)[None, :, None]
    t_ = np.arange(512)[None, None, :]
    rel = np.arange(4)[:, None, None]
    cm = ((s_ + 128 * rel) < t_).astype(np.float32)
    dmask = np.where(jj[None, :] > jj[:, None], np.float32(NEG), np.float32(0)).astype(np.float32)
    irep = np.tile(np.eye(128, dtype=np.float32), (1, 4))
    bmask = np.zeros((8, 8, 128), np.float32)
    for h in range(8):
        bmask[h, h, :] = 1.0
    return {"negU_bf": negU.astype(bf), "ones_bf": ones.astype(bf), "cm_bf": cm.astype(bf),
            "dmask": dmask, "irep_bf": irep.astype(bf), "ident_f": np.eye(128, dtype=np.float32),
            "bmask": bmask.reshape(8, 1024)}


def _t5_bucket(n):
    import math
    max_exact = 16
    nf = np.maximum(n, 1).astype(np.float32)
    large = max_exact + (np.log(nf / np.float32(max_exact)) / np.float32(math.log(128 / max_exact))
                         * np.float32(32 - max_exact)).astype(np.int32)
    large = np.minimum(large, 31)
    return np.where(n < max_exact, n, large)


_TZ_IDX = None


def _tz_table(rel_bias):
    global _TZ_IDX
    if _TZ_IDX is None:
        s_ = np.arange(128)[None, :, None]
        t_ = np.arange(128)[None, None, :]
        rel = np.arange(2)[:, None, None]
        dist = np.maximum(128 * rel + t_ - s_, 0)
        _TZ_IDX = _t5_bucket(dist)
    g = rel_bias[_TZ_IDX]
    return np.ascontiguousarray(np.transpose(g, (0, 1, 3, 2)).reshape(2, 128, 1024))


CONSTS = _make_consts()


def kernel(**inputs):
    inputs = {k: np.asarray(v) for k, v in inputs.items()}
    nc, _ = build()
    n = 8
    in_maps = [host_inputs(inputs, b) for b in range(n)]
    res = run_bass_kernel_spmd(nc, in_maps, core_ids=list(range(n)))
    out = np.stack([np.asarray(res.results[b]["out"], dtype=np.float32) for b in range(n)], axis=0)
    return out
```

```python
import contextlib
import numpy as np
import ml_dtypes
import concourse.bass as bass
import concourse.mybir as mybir
from concourse.bass_utils import run_bass_kernel_spmd

F32 = mybir.dt.float32
BF16 = mybir.dt.bfloat16
AF = mybir.ActivationFunctionType
ALU = mybir.AluOpType
AX = mybir.AxisListType

D = 1024
S = 8192
DEPTH = 2
NT = S // 128
NST = S // 512
HD = 64
NCOLS = 4548
NW1 = 2500
E = 32
FF = 1024
LN_EPS = 1e-5
RMS_EPS = 1e-6
DN_ALPHA = (2 * DEPTH) ** 0.25
TOPK = 256
NEG = -1.0e30
MBIG = -30000.0


class Res:
    __slots__ = ("wc", "wd", "rc", "rd", "name", "psum")

    def __init__(self, name="", psum=False):
        self.psum = psum
        self.wc = {}
        self.wd = []
        self.rc = {}
        self.rd = []
        self.name = name


class KB:
    COMPUTE = ("pe", "act", "dve", "pool")

    def __init__(self, nc, es):
        self.nc = nc
        self.es = es
        self.eng = {"pe": nc.tensor, "act": nc.scalar, "dve": nc.vector, "pool": nc.gpsimd, "sp": nc.sync}
        self.sem = {e: es.enter_context(nc.semaphore("s_" + e)) for e in self.COMPUTE}
        self.cnt = {e: 0 for e in self.COMPUTE}
        self.known = {e: {} for e in self.eng}
        self.NSD = 8
        self.dq = {}
        self.ninst = 0

    def _dq(self, q):
        if q not in self.dq:
            self.dq[q] = {"sems": [self.es.enter_context(self.nc.semaphore(f"d_{q}_{i}")) for i in range(self.NSD)],
                          "n": 0}
        return self.dq[q]

    def _wait(self, e, tok):
        if tok[0] == "c":
            _, e2, seq = tok
            if e == "pe" and e2 == "pe":
                return
            if self.known[e].get(e2, 0) >= seq:
                return
            self.eng[e].wait_ge(self.sem[e2], seq)
            self.known[e][e2] = seq
        else:
            _, q, slot, val = tok
            key = (q, slot)
            if self.known[e].get(key, 0) >= val:
                return
            self.eng[e].wait_ge(self.dq[q]["sems"][slot], val)
            self.known[e][key] = val
        self.ninst += 1

    def _deps(self, e, reads, writes):
        for r in reads:
            for e2, seq in r.wc.items():
                self._wait(e, ("c", e2, seq))
            for tok in r.wd:
                self._wait(e, tok)
            if r.psum:
                for e2, seq in r.rc.items():
                    if e2 != e:
                        self._wait(e, ("c", e2, seq))
        for w in writes:
            for e2, seq in w.wc.items():
                self._wait(e, ("c", e2, seq))
            for tok in w.wd:
                self._wait(e, tok)
            for e2, seq in w.rc.items():
                self._wait(e, ("c", e2, seq))
            for tok in w.rd:
                self._wait(e, tok)

    def op(self, e, fn, reads=(), writes=()):
        self._deps(e, reads, writes)
        ins = fn(self.eng[e])
        self.cnt[e] += 1
        seq = self.cnt[e]
        ins.then_inc(self.sem[e], 1)
        self.ninst += 1
        for r in reads:
            r.rc[e] = seq
        for w in writes:
            w.wc = {e: seq}
            w.wd = []
            w.rc = {}
            w.rd = []
        return ins

    def dma(self, q, out, in_, reads=(), writes=(), **kw):
        d = self._dq(q)
        k = d["n"]
        slot = k % self.NSD
        val = 16 * (k // self.NSD + 1)
        if k >= self.NSD:
            self._wait(q, ("d", q, slot, val - 16))
        self._deps(q, reads, writes)
        ins = self.eng[q].dma_start(out=out, in_=in_, **kw)
        ins.then_inc(d["sems"][slot], 16)
        d["n"] += 1
        self.ninst += 1
        tok = ("d", q, slot, val)
        for r in reads:
            r.rd.append(tok)
        for w in writes:
            w.wc = {}
            w.wd = [tok]
            w.rc = {}
            w.rd = []
        return ins

    def barrier(self, engines=None):
        engines = engines or list(self.eng)
        for e in engines:
            for e2 in self.COMPUTE:
                if self.cnt[e2] > 0:
                    self._wait(e, ("c", e2, self.cnt[e2]))
            for q, d in self.dq.items():
                n = d["n"]
                for k in range(max(0, n - self.NSD), n):
                    self._wait(e, ("d", q, k % self.NSD, 16 * (k // self.NSD + 1)))


class Ctx:
    pass


_uid = [0]


def T(C, ph, name, shape, dt):
    _uid[0] += 1
    h = ph.enter_context(C.nc.sbuf_tensor(f"{name}_{_uid[0]}", list(shape), dt))
    return h, Res(name)


def dram(C, name, shape, dt):
    return C.nc.dram_tensor(name, list(shape), dt, kind="Internal").ap(), Res(name)


def phase_ada(C):
    nc, kb = C.nc, C.kb
    with contextlib.ExitStack() as ph:
        ccol, r_ccol = T(C, ph, "ccol", [128, 8], F32)
        cond, r_cond = T(C, ph, "cond", [128, 8], F32)
        condB, r_condB = T(C, ph, "condB", [128, 8, 128], F32)
        wb = [T(C, ph, f"wada{i}", [128, 8, 512], F32) for i in range(2)]
        bB, r_bB = T(C, ph, "badaB", [128, 6144], F32)
        mo = [T(C, ph, f"mo{i}", [128, 512], F32) for i in range(2)]
        kb.dma("sp", ccol[:], C.c_col[:, :], writes=[r_ccol])
        kb.op("act", lambda e: e.activation(out=cond[:], in_=ccol[:], func=AF.Silu), reads=[r_ccol], writes=[r_cond])
        for kc in range(8):
            kb.op("dve", lambda e: e.tensor_copy(out=condB[:, kc, :], in_=cond[:, kc:kc + 1].to_broadcast([128, 128])),
                  reads=[r_cond], writes=[r_condB])
        it = 0
        for l in range(DEPTH):
            kb.dma("sp", bB[:], C.b_ada[l:l + 1, :].to_broadcast([128, 6144]), writes=[r_bB])
            for j in range(12):
                w, r_w = wb[it % 2]
                m, r_m = mo[it % 2]
                ps, r_ps = C.ps[it % 2]
                kb.dma("sp", w[:], C.w_ada[l, :, j * 512:(j + 1) * 512].rearrange("(kc p) n -> p kc n", p=128),
                       writes=[r_w])
                for kc in range(8):
                    kb.op("pe", lambda e: e.matmul(ps[:], lhsT=condB[:, kc, :], rhs=w[:, kc, :],
                                                   start=(kc == 0), stop=(kc == 7)),
                          reads=[r_condB, r_w], writes=[r_ps])
                plus1 = 1.0 if j in (2, 3, 8, 9) else 0.0
                kb.op("dve", lambda e: e.scalar_tensor_tensor(out=m[:], in0=ps[:], scalar=plus1,
                                                              in1=bB[:, j * 512:(j + 1) * 512],
                                                              op0=ALU.add, op1=ALU.add),
                      reads=[r_ps, r_bB], writes=[r_m])
                kb.dma("sp", C.mod_d[l, :, j * 512:(j + 1) * 512], m[:], reads=[r_m], writes=[C.r_mod])
                it += 1
        kb.barrier()


def dram(C, name, shape, dt):
    kind = "ExternalOutput" if name in C.dbg else "Internal"
    return C.nc.dram_tensor(name, list(shape), dt, kind=kind).ap(), Res(name)


class Rot:
    def __init__(self, items):
        self.items = list(items)
        self.i = 0

    def next(self):
        it = self.items[self.i % len(self.items)]
        self.i += 1
        return it


def bfv(ps):
    return ps[:].bitcast(BF16)


def ln_modulate(C, W, xt, r_xt, out_ap, r_out, scB, r_scB, shB, r_shB):
    kb = C.kb
    st_, r_st = W["stats"]
    mv, r_mv = W["mv"]
    sd, r_sd = W["sd"]
    rstd, r_rstd = W["rstd"]
    nmr, r_nmr = W["nmr"]
    xn, r_xn = W["xn"]
    epsT, r_eps = W["eps"]
    for c in range(2):
        kb.op("dve", lambda e: e.bn_stats(out=st_[:, c * 6:(c + 1) * 6], in_=xt[:, c * 512:(c + 1) * 512]),
              reads=[r_xt], writes=[r_st])
    kb.op("dve", lambda e: e.bn_aggr(out=mv[:], in_=st_[:]), reads=[r_st], writes=[r_mv])
    kb.op("act", lambda e: e.activation(out=sd[:], in_=mv[:, 1:2], func=AF.Sqrt, bias=epsT[:, 0:1], scale=1.0),
          reads=[r_mv, r_eps], writes=[r_sd])
    kb.op("dve", lambda e: e.reciprocal(out=rstd[:], in_=sd[:]), reads=[r_sd], writes=[r_rstd])
    kb.op("dve", lambda e: e.tensor_scalar(out=nmr[:], in0=mv[:, 0:1], scalar1=rstd[:, 0:1], scalar2=-1.0,
                                           op0=ALU.mult, op1=ALU.mult),
          reads=[r_mv, r_rstd], writes=[r_nmr])
    kb.op("act", lambda e: e.activation(out=xn[:], in_=xt[:], func=AF.Identity, bias=nmr[:, 0:1], scale=rstd[:, 0:1]),
          reads=[r_xt, r_rstd, r_nmr], writes=[r_xn])
    kb.op("pool", lambda e: e.tensor_tensor(out=xn[:], in0=xn[:], in1=scB, op=ALU.mult),
          reads=[r_xn, r_scB], writes=[r_xn])
    kb.op("dve", lambda e: e.tensor_tensor(out=out_ap, in0=xn[:], in1=shB, op=ALU.add),
          reads=[r_xn, r_shB], writes=[r_out])


def ln_work(C, ph, tag, eps):
    W = {
        "stats": T(C, ph, tag + "stats", [128, 12], F32),
        "mv": T(C, ph, tag + "mv", [128, 2], F32),
        "sd": T(C, ph, tag + "sd", [128, 1], F32),
        "rstd": T(C, ph, tag + "rstd", [128, 1], F32),
        "nmr": T(C, ph, tag + "nmr", [128, 1], F32),
        "xn": T(C, ph, tag + "xn", [128, 1024], F32),
        "eps": T(C, ph, tag + "eps", [128, 1], F32),
    }
    e_, r_e = W["eps"]
    C.kb.op("pool", lambda e: e.memset(e_[:], eps), writes=[r_e])
    return W


def phase_proj(C, l, x_src, r_xsrc):
    nc, kb = C.nc, C.kb
    with contextlib.ExitStack() as ph:
        W1, r_W1 = T(C, ph, "W1", [128, 8, NW1], BF16)
        Wkk, r_Wkk = T(C, ph, "Wkk", [128, 8, 128], BF16)
        wuk, r_wuk = T(C, ph, "wuk", [128, 4, 128], BF16)
        gkvB, r_gkvB = T(C, ph, "gkvB", [128, 128], F32)
        scB, r_scB = T(C, ph, "scB", [128, 1024], F32)
        shB, r_shB = T(C, ph, "shB", [128, 1024], F32)
        ident, r_id = T(C, ph, "ident", [128, 128], BF16)
        reps, r_reps = T(C, ph, "reps", [128, 1], F32)
        kb.op("pool", lambda e: e.memset(reps[:], RMS_EPS), writes=[r_reps])
        kb.dma("pool", W1[:], C.w_in[l, :, 0:NW1].rearrange("(kc p) n -> p kc n", p=128), writes=[r_W1])
        for hh in range(2):
            kb.dma("pool", Wkk[:, :, hh * 64:(hh + 1) * 64],
                   C.w_in[l, :, 896:960].rearrange("(kc p) n -> p kc n", p=128), writes=[r_Wkk])
        kb.dma("pool", wuk[:], C.w_uk_t[l].rearrange("(hp two) d r -> (two d) hp r", two=2), writes=[r_wuk])
        kb.dma("sp", gkvB[:], C.g_kv[l:l + 1, :].to_broadcast([128, 128]), writes=[r_gkvB])
        kb.dma("sp", scB[:], C.mod_d[l, :, 1024:2048], reads=[C.r_mod], writes=[r_scB])
        kb.dma("sp", shB[:], C.mod_d[l, :, 0:1024], reads=[C.r_mod], writes=[r_shB])
        kb.dma("sp", ident[:], C.ident_bf[:, :], writes=[r_id])
        LW = [ln_work(C, ph, f"lw{i}", LN_EPS) for i in range(2)]
        xb = [T(C, ph, f"xb{i}", [128, 1024], F32) for i in range(2)]
        hb = [T(C, ph, f"hb{i}", [128, 1024], BF16) for i in range(2)]
        hT = [(T(C, ph, f"hT{i}", [128, 8, 512], BF16)[0], [Res(f"hT{i}_{j}") for j in range(4)]) for i in range(2)]
        qaT = [T(C, ph, f"qaT{i}", [128, 512], BF16) for i in range(2)]
        def stage(name, shape, n):
            return [(T(C, ph, f"{name}{i}", shape, BF16)[0], [Res(f"{name}{i}_{j}") for j in range(n)]) for i in range(2)]
        s_qlat = stage("s_qlat", [128, 8, 512], 8)
        s_qidx = stage("s_qidx", [128, 2, 512], 2)
        s_kidx = stage("s_kidx", [128, 1, 512], 1)
        s_qb = stage("s_qb", [128, 4, 512], 4)
        s_kb = stage("s_kb", [128, 4, 512], 4)
        s_ckvT = stage("s_ckvT", [128, 4, 128], 4)
        s_ckv = stage("s_ckv", [128, 4, 128], 4)
        s_vb = stage("s_vb", [128, 4, 512], 4)
        s_wi = [(T(C, ph, f"s_wi{i}", [128, 4, 4], F32)[0], [Res(f"s_wi{i}_{j}") for j in range(4)]) for i in range(2)]
        ss_t = [T(C, ph, f"ss{i}", [128, 1], F32) for i in range(2)]
        sd2_t = [T(C, ph, f"sd2{i}", [128, 1], F32) for i in range(2)]
        rs_t = [T(C, ph, f"rs{i}", [128, 1], F32) for i in range(2)]
        junk, r_junk = T(C, ph, "junk", [128, 128], F32)
        pp = Rot(C.ps[0:6])
        psT, r_psT = C.ps[7]
        psT2, r_psT2 = C.ps[6]
        ev = Rot(["act", "dve"])

        def evac(eng, out_ap, in_ap, reads, writes, scale=None):
            if eng == "act":
                if scale is None:
                    kb.op("act", lambda e: e.copy(out=out_ap, in_=in_ap), reads=reads, writes=writes)
                else:
                    kb.op("act", lambda e: e.mul(out=out_ap, in_=in_ap, mul=scale), reads=reads, writes=writes)
            else:
                if scale is None:
                    kb.op("dve", lambda e: e.tensor_copy(out=out_ap, in_=in_ap), reads=reads, writes=writes)
                else:
                    kb.op("dve", lambda e: e.tensor_scalar(out=out_ap, in0=in_ap, scalar1=scale, scalar2=None,
                                                           op0=ALU.mult), reads=reads, writes=writes)

        for st in range(NST):
            sp_ = st % 2
            hTt, r_hT = hT[sp_]
            t0 = st * 512
            for j in range(4):
                tt = st * 4 + j
                xt, r_xt = xb[tt % 2]
                hbt, r_hb = hb[tt % 2]
                kb.dma("sp", xt[:], x_src[tt * 128:(tt + 1) * 128, :], reads=[r_xsrc], writes=[r_xt])
                ln_modulate(C, LW[tt % 2], xt, r_xt, hbt[:], r_hb, scB[:], r_scB, shB[:], r_shB)
                for kc in range(8):
                    kb.op("pe", lambda e: e.transpose(out=bfv(psT)[:, kc * 128:(kc + 1) * 128],
                                                      in_=hbt[:, kc * 128:(kc + 1) * 128], identity=ident[:]),
                          reads=[r_hb, r_id], writes=[r_psT])
                evac(ev.next(), hTt[:, :, j * 128:(j + 1) * 128],
                     bfv(psT).rearrange("p (kc t) -> p kc t", kc=8), [r_psT], [r_hT[j]])
            for j in range(4):
                tt = st * 4 + j
                ps, r_ps = pp.next()
                for (c0, c1, o0) in ((512, 640, 0), (960, 964, 128)):
                    for kc in range(8):
                        kb.op("pe", lambda e: e.matmul(ps[:, o0:o0 + (c1 - c0)], lhsT=hTt[:, kc, j * 128:(j + 1) * 128],
                                                       rhs=W1[:, kc, c0:c1], start=(kc == 0), stop=(kc == 7)),
                              reads=[r_hT[j], r_W1], writes=[r_ps])
                ss, r_ss = ss_t[tt % 2]
                sd2, r_sd2 = sd2_t[tt % 2]
                rs, r_rs = rs_t[tt % 2]
                kb.op("act", lambda e: e.activation(out=junk[:], in_=ps[:, 0:128], func=AF.Square, accum_out=ss[:]),
                      reads=[r_ps], writes=[r_junk, r_ss])
                kb.op("act", lambda e: e.activation(out=sd2[:], in_=ss[:], func=AF.Sqrt, bias=reps[:, 0:1],
                                                    scale=1.0 / 128.0), reads=[r_ss, r_reps], writes=[r_sd2])
                kb.op("dve", lambda e: e.reciprocal(out=rs[:], in_=sd2[:]), reads=[r_sd2], writes=[r_rs])
                ckv_t, r_ckv = s_ckv[sp_]
                kb.op("dve", lambda e: e.scalar_tensor_tensor(out=ckv_t[:, j, :], in0=ps[:, 0:128], scalar=rs[:, 0:1],
                                                              in1=gkvB[:], op0=ALU.mult, op1=ALU.mult),
                      reads=[r_ps, r_rs, r_gkvB], writes=[r_ckv[j]])
                wi_t, r_wi = s_wi[sp_]
                kb.op("dve", lambda e: e.tensor_scalar(out=wi_t[:, j, :], in0=ps[:, 128:132], scalar1=1.0 / 16.0,
                                                       scalar2=None, op0=ALU.mult), reads=[r_ps], writes=[r_wi[j]])
                kb.op("pe", lambda e: e.transpose(out=bfv(psT2)[:, j * 128:(j + 1) * 128], in_=ckv_t[:, j, :],
                                                  identity=ident[:]), reads=[r_ckv[j], r_id], writes=[r_psT2])
                ps, r_ps = pp.next()
                for kc in range(8):
                    kb.op("pe", lambda e: e.matmul(ps[:], lhsT=hTt[:, kc, j * 128:(j + 1) * 128],
                                                   rhs=W1[:, kc, 1988:2500], start=(kc == 0), stop=(kc == 7)),
                          reads=[r_hT[j], r_W1], writes=[r_ps])
                vb_t, r_vb = s_vb[sp_]
                evac(ev.next(), vb_t[:, j, :], ps[:], [r_ps], [r_vb[j]])
            ckvT_t, r_ckvT = s_ckvT[sp_]
            evac(ev.next(), ckvT_t[:].rearrange("p j t -> p (j t)"), bfv(psT2)[:, 0:512], [r_psT2], r_ckvT)
            rows = slice(t0, t0 + 512)
            kb.dma("sp", C.ckv_d[rows, :].rearrange("(j p) r -> p j r", p=128), ckv_t[:], reads=r_ckv, writes=[C.r_ckv])
            kb.dma("sp", C.widx_d[rows, :].rearrange("(j p) r -> p j r", p=128), wi_t[:], reads=r_wi, writes=[C.r_widx])
            kb.dma("sp", C.vb_d[rows, :].rearrange("(j p) r -> p j r", p=128), vb_t[:], reads=r_vb, writes=[C.r_vb])
            kb.dma("sp", C.ckvT_d[:, rows], ckvT_t[:].rearrange("p j t -> p (j t)"), reads=r_ckvT, writes=[C.r_ckvT])
            allj = r_hT
            groups = [("qa", 0, 4), ("qidx", 640, 2), ("kidx", None, 1), ("qb", 964, 4), ("kb", 1476, 4)]
            for (gname, c0, nch) in groups:
                for c in range(nch):
                    ps, r_ps = pp.next()
                    for kc in range(8):
                        if gname == "kidx":
                            lw, rl = Wkk[:, kc, :], r_Wkk
                        else:
                            lw, rl = W1[:, kc, c0 + c * 128:c0 + (c + 1) * 128], r_W1
                        kb.op("pe", lambda e: e.matmul(ps[:], lhsT=lw, rhs=hTt[:, kc, :], start=(kc == 0), stop=(kc == 7)),
                              reads=allj + [rl], writes=[r_ps])
                    if gname == "qa":
                        qa, r_qa = qaT[c % 2]
                        evac(ev.next(), qa[:], ps[:], [r_ps], [r_qa])
                        for hp in range(2):
                            h = 2 * c + hp
                            ps2, r_ps2 = pp.next()
                            kb.op("pe", lambda e: e.matmul(ps2[:], lhsT=wuk[hp * 64:(hp + 1) * 64, c, :],
                                                           rhs=qa[hp * 64:(hp + 1) * 64, :], start=True, stop=True),
                                  reads=[r_wuk, r_qa], writes=[r_ps2])
                            stg, r_stg = s_qlat[sp_]
                            evac(ev.next(), stg[:, h, :], ps2[:], [r_ps2], [r_stg[h]], scale=0.125)
                    else:
                        stg, r_stg = {"qidx": s_qidx, "kidx": s_kidx, "qb": s_qb, "kb": s_kb}[gname][sp_]
                        evac(ev.next(), stg[:, c, :], ps[:], [r_ps], [r_stg[c]], scale=(0.125 if gname == "kb" else None))
            cols = slice(t0, t0 + 512)
            stg, r_stg = s_qlat[sp_]
            kb.dma("sp", C.qlatT_d[:, :, cols].rearrange("h p t -> p h t"), stg[:], reads=r_stg, writes=[C.r_qlatT])
            stg, r_stg = s_qidx[sp_]
            kb.dma("sp", C.qidxT_d[:, :, cols].rearrange("h p t -> p h t"), stg[:], reads=r_stg, writes=[C.r_qidxT])
            stg, r_stg = s_kidx[sp_]
            kb.dma("sp", C.kidxT_d[:, cols], stg[:, 0, :], reads=r_stg, writes=[C.r_kidxT])
            stg, r_stg = s_qb[sp_]
            kb.dma("sp", C.qbT_d[:, :, cols].rearrange("h p t -> p h t"), stg[:], reads=r_stg, writes=[C.r_qbT])
            stg, r_stg = s_kb[sp_]
            kb.dma("sp", C.kbT_d[:, :, cols].rearrange("h p t -> p h t"), stg[:], reads=r_stg, writes=[C.r_kbT])
        kb.barrier()


def phase_sb(C, l):
    nc, kb = C.nc, C.kb
    with contextlib.ExitStack() as ph:
        negU, r_negU = T(C, ph, "negU", [128, 128], BF16)
        ones, r_ones = T(C, ph, "ones", [128, 128], BF16)
        cm, r_cm = T(C, ph, "cm", [128, 4, 512], BF16)
        kb.dma("sp", negU[:], C.negU_bf[:, :], writes=[r_negU])
        kb.dma("sp", ones[:], C.ones_bf[:, :], writes=[r_ones])
        kb.dma("sp", cm[:], C.cm_bf.rearrange("r p t -> p r t"), writes=[r_cm])
        qT = [T(C, ph, f"sbq{i}", [128, S], BF16) for i in range(2)]
        kT = [T(C, ph, f"sbk{i}", [128, S], BF16) for i in range(2)]
        vv = [T(C, ph, f"sbv{i}", [128, NT, 128], BF16) for i in range(2)]
        et = [T(C, ph, f"et{i}", [128, 512], F32) for i in range(2)]
        spT = [T(C, ph, f"spT{i}", [128, 512], BF16) for i in range(4)]
        arg2 = [T(C, ph, f"arg2{i}", [128, 512], F32) for i in range(2)]
        AT = [T(C, ph, f"AT{i}", [128, 512], BF16) for i in range(4)]
        carry = [T(C, ph, f"carry{i}", [128, 512], F32) for i in range(2)]
        ostg = [T(C, ph, f"ostg{i}", [128, 512], BF16) for i in range(2)]
        psz = C.ps[0:2]
        psa = C.ps[2:4]
        psc = C.ps[4:5]
        pso = C.ps[5:7]

        items = []
        gid = 0
        for c in range(4):
            for qi in range(NST):
                for hp in range(2):
                    nb = 4 * qi + 4
                    for j in range(nb - 1, -1, -1):
                        items.append(dict(c=c, qi=qi, hp=hp, j=j, first=(j == nb - 1), last=(j == 0), g=gid,
                                          rel=(j - 4 * qi)))
                    gid += 1
        loaded = set()

        def load_pair(c):
            if c in loaded or c >= 4:
                return
            loaded.add(c)
            q, r_q = qT[c % 2]
            k, r_k = kT[c % 2]
            v, r_v = vv[c % 2]
            kb.dma("sp", q[:], C.qbT_d[c], reads=[C.r_qbT], writes=[r_q])
            kb.dma("sp", k[:], C.kbT_d[c], reads=[C.r_kbT], writes=[r_k])
            kb.dma("sp", v[:], C.vb_d[:, c * 128:(c + 1) * 128].rearrange("(j p) d -> p j d", p=128),
                   reads=[C.r_vb], writes=[r_v])

        def stA(n, it):
            c, qi, hp, j = it["c"], it["qi"], it["hp"], it["j"]
            q, r_q = qT[c % 2]
            k, r_k = kT[c % 2]
            P = slice(hp * 64, hp * 64 + 64)
            pz, r_pz = psz[n % 2]
            kb.op("pe", lambda e: e.matmul(pz[:], lhsT=k[P, j * 128:(j + 1) * 128], rhs=q[P, qi * 512:(qi + 1) * 512],
                                           start=True, stop=True), reads=[r_k, r_q], writes=[r_pz])
            e_, r_e = et[n % 2]
            kb.op("act", lambda e: e.activation(out=e_[:], in_=pz[:], func=AF.Exp), reads=[r_pz], writes=[r_e])

        def stA2(n, it):
            e_, r_e = et[n % 2]
            s_, r_s = spT[n % 4]
            kb.op("act", lambda e: e.activation(out=s_[:], in_=e_[:], func=AF.Ln, bias=1.0, scale=1.0),
                  reads=[r_e], writes=[r_s])
            if it["rel"] >= 0:
                kb.op("dve", lambda e: e.tensor_tensor(out=s_[:], in0=s_[:], in1=cm[:, it["rel"], :], op=ALU.mult),
                      reads=[r_s, r_cm], writes=[r_s])

        def stB(n, it):
            c, qi, hp, j = it["c"], it["qi"], it["hp"], it["j"]
            q, r_q = qT[c % 2]
            k, r_k = kT[c % 2]
            P = slice(hp * 64, hp * 64 + 64)
            s_, r_s = spT[n % 4]
            pa, r_pa = psa[n % 2]
            pc, r_pc = psc[0]
            kb.op("pe", lambda e: e.matmul(pa[:], lhsT=k[P, j * 128:(j + 1) * 128], rhs=q[P, qi * 512:(qi + 1) * 512],
                                           start=True, stop=False), reads=[r_k, r_q], writes=[r_pa])
            kb.op("pe", lambda e: e.matmul(pa[:], lhsT=negU[:], rhs=s_[:], start=False, stop=True),
                  reads=[r_negU, r_s], writes=[r_pa])
            kk = n % 2
            cb, r_cb = carry[kk]
            cbn, r_cbn = carry[1 - kk]
            if it["first"]:
                kb.op("dve", lambda e: e.memset(cb[:], 0.0), writes=[r_cb])
            if not it["last"]:
                kb.op("pe", lambda e: e.matmul(pc[:], lhsT=ones[:], rhs=s_[:], start=True, stop=True),
                      reads=[r_ones, r_s], writes=[r_pc])
                kb.op("dve", lambda e: e.tensor_tensor(out=cbn[:], in0=pc[:], in1=cb[:], op=ALU.add),
                      reads=[r_pc, r_cb], writes=[r_cbn])
            a2, r_a2 = arg2[n % 2]
            kb.op("dve", lambda e: e.tensor_tensor(out=a2[:], in0=pa[:], in1=cb[:], op=ALU.subtract),
                  reads=[r_pa, r_cb], writes=[r_a2])

        def stB2(n, it):
            a2, r_a2 = arg2[n % 2]
            a_, r_a = AT[n % 4]
            kb.op("act", lambda e: e.activation(out=a_[:], in_=a2[:], func=AF.Exp), reads=[r_a2], writes=[r_a])
            if it["rel"] >= 0:
                kb.op("pool", lambda e: e.tensor_tensor(out=a_[:], in0=a_[:], in1=cm[:, it["rel"], :], op=ALU.mult),
                      reads=[r_a, r_cm], writes=[r_a])

        def stC(n, it):
            c, qi, hp, j = it["c"], it["qi"], it["hp"], it["j"]
            v, r_v = vv[c % 2]
            a_, r_a = AT[n % 4]
            po, r_po = pso[(c * NST + qi) % 2]
            kb.op("pe", lambda e: e.matmul(po[hp * 64:(hp + 1) * 64, :], lhsT=v[:, j, hp * 64:(hp + 1) * 64], rhs=a_[:],
                                           start=it["first"], stop=it["last"]), reads=[r_v, r_a], writes=[r_po])
            if it["last"] and hp == 1:
                og, r_og = ostg[(c * NST + qi) % 2]
                kb.op("dve", lambda e: e.tensor_copy(out=og[:], in_=po[:]), reads=[r_po], writes=[r_og])
                kb.dma("sp", C.obT_d[c, :, qi * 512:(qi + 1) * 512], og[:], reads=[r_og], writes=[C.r_obT])
                if qi == NST - 1:
                    load_pair(c + 2)

        N = len(items)
        load_pair(0)
        load_pair(1)
        for n in range(N + 3):
            if n < N:
                stA(n, items[n])
            if 0 <= n - 2 < N:
                stB2(n - 2, items[n - 2])
            if n < N:
                stA2(n, items[n])
            if 0 <= n - 1 < N:
                stB(n - 1, items[n - 1])
            if 0 <= n - 3 < N:
                stC(n - 3, items[n - 3])
        kb.barrier()


def phase_dsa(C, l):
    nc, kb = C.nc, C.kb
    with contextlib.ExitStack() as ph:
        kidxT, r_kidxT = T(C, ph, "kidxT", [128, S], BF16)
        ckvT, r_ckvT = T(C, ph, "ckvT", [128, S], BF16)
        ckv, r_ckv = T(C, ph, "ckv", [128, NT, 128], BF16)
        wuv, r_wuv = T(C, ph, "wuv", [128, 8, 64], BF16)
        sc, r_sc = T(C, ph, "sc", [128, S], F32)
        mb, r_mb = T(C, ph, "mb", [128, S], BF16)
        dmask, r_dmask = T(C, ph, "dmask", [128, 128], F32)
        Irep, r_Irep = T(C, ph, "Irep", [128, 512], BF16)
        onesF, r_onesF = T(C, ph, "onesF", [128, 128], BF16)
        identf, r_identf = T(C, ph, "identf", [128, 128], F32)
        Tz, r_Tz = T(C, ph, "Tz", [128, 2, 1024], F32)
        b31B, r_b31B = T(C, ph, "b31B", [128, 8], F32)
        bmask, r_bmask = T(C, ph, "bmask", [8, 1024], F32)
        rbT, r_rbT = T(C, ph, "rbT", [8, 32], F32)
        c8, r_c8 = T(C, ph, "c8", [8, 1], F32)
        b31c, r_b31c = T(C, ph, "b31c", [40, 1], F32)
        b31hi, r_b31hi = T(C, ph, "b31hi", [40, 1], BF16)
        b31hif, r_b31hif = T(C, ph, "b31hif", [40, 1], F32)
        b31lo, r_b31lo = T(C, ph, "b31lo", [72, 1], F32)
        CB, r_CB = T(C, ph, "CB", [72, 1024], BF16)
        CBd = Res("CBdyn")
        kb.dma("sp", kidxT[:], C.kidxT_d[:, :], reads=[C.r_kidxT], writes=[r_kidxT])
        kb.dma("sp", ckvT[:], C.ckvT_d[:, :], reads=[C.r_ckvT], writes=[r_ckvT])
        kb.dma("sp", ckv[:], C.ckv_d.rearrange("(j p) r -> p j r", p=128), reads=[C.r_ckv], writes=[r_ckv])
        kb.dma("pool", wuv[:], C.w_uv[l], writes=[r_wuv])
        kb.dma("sp", dmask[:], C.dmask[:, :], writes=[r_dmask])
        kb.dma("sp", Irep[:], C.irep_bf[:, :], writes=[r_Irep])
        kb.dma("sp", onesF[:], C.ones_bf[:, :], writes=[r_onesF])
        kb.dma("sp", identf[:], C.ident_f[:, :], writes=[r_identf])
        kb.dma("sp", Tz[:], C.tz.rearrange("r p c -> p r c"), writes=[r_Tz])
        kb.dma("sp", b31B[:], C.rel_bias[31:32, :].to_broadcast([128, 8]), writes=[r_b31B])
        kb.dma("sp", bmask[:], C.bmask[:, :], writes=[r_bmask])
        kb.dma("sp", rbT[:], C.rel_bias_t[:, :], writes=[r_rbT])
        for r_ in range(2):
            kb.op("dve", lambda e: e.tensor_tensor(out=Tz[:, r_, :].rearrange("p (h t) -> p h t", h=8),
                                                   in0=Tz[:, r_, :].rearrange("p (h t) -> p h t", h=8),
                                                   in1=b31B[:].to_broadcast([128, 8, 128]) if False else
                                                   b31B[:].unsqueeze(2).to_broadcast([128, 8, 128]),
                                                   op=ALU.subtract), reads=[r_Tz, r_b31B], writes=[r_Tz])
        kb.op("dve", lambda e: e.reduce_max(out=c8[:], in_=rbT[:], axis=AX.X), reads=[r_rbT], writes=[r_c8])
        kb.op("dve", lambda e: e.tensor_scalar(out=c8[:], in0=c8[:], scalar1=-1.0, scalar2=None, op0=ALU.mult),
              reads=[r_c8], writes=[r_c8])
        kb.op("pool", lambda e: e.memset(CB[:], 0.0), writes=[r_CB])
        kb.dma("sp", b31c[32:40, :], C.rel_bias_t[:, 31:32], writes=[r_b31c], allow_slow_non_contiguous=True)
        kb.dma("sp", b31lo[64:72, :], C.rel_bias_t[:, 31:32], writes=[r_b31lo], allow_slow_non_contiguous=True)
        kb.op("dve", lambda e: e.tensor_copy(out=b31hi[32:40, :], in_=b31c[32:40, :]), reads=[r_b31c], writes=[r_b31hi])
        kb.op("dve", lambda e: e.tensor_copy(out=b31hif[32:40, :], in_=b31hi[32:40, :]), reads=[r_b31hi], writes=[r_b31hif])
        kb.dma("sp", b31lo[32:40, :], b31hif[32:40, :], reads=[r_b31hif], writes=[r_b31lo])
        bm32, r_bm32 = T(C, ph, "bm32", [72, 1024], F32)
        kb.dma("sp", bm32[32:40, :], C.bmask[:, :], writes=[r_bm32])
        kb.dma("sp", bm32[64:72, :], C.bmask[:, :], writes=[r_bm32])
        kb.op("dve", lambda e: e.tensor_scalar(out=CB[32:40, :], in0=bm32[32:40, :], scalar1=b31hif[32:40, 0:1],
                                               scalar2=None, op0=ALU.mult), reads=[r_bm32, r_b31hif], writes=[r_CB])
        hi64, r_hi64 = T(C, ph, "hi64", [72, 1], F32)
        kb.dma("sp", hi64[64:72, :], b31hif[32:40, :], reads=[r_b31hif], writes=[r_hi64])
        kb.op("dve", lambda e: e.tensor_tensor(out=b31lo[64:72, :], in0=b31lo[64:72, :], in1=hi64[64:72, :], op=ALU.subtract),
              reads=[r_b31lo, r_hi64], writes=[r_b31lo])
        kb.op("dve", lambda e: e.tensor_scalar(out=CB[64:72, :], in0=bm32[64:72, :], scalar1=b31lo[64:72, 0:1],
                                               scalar2=None, op0=ALU.mult), reads=[r_bm32, r_b31lo], writes=[r_CB])

        CB2, r_CB2 = T(C, ph, "CB2", [72, 1024], BF16)
        kb.op("dve", lambda e: e.tensor_copy(out=CB2[:], in_=CB[:]), reads=[r_CB], writes=[r_CB2])
        CB3, r_CB3 = T(C, ph, "CB3", [72, 1024], BF16)
        kb.op("dve", lambda e: e.tensor_copy(out=CB3[:], in_=CB[:]), reads=[r_CB], writes=[r_CB3])
        CBs = [(CB, r_CB, Res("CBdyn0")), (CB2, r_CB2, Res("CBdyn1")), (CB3, r_CB3, Res("CBdyn2"))]
        g8, r_g8 = T(C, ph, "g8", [8, 128], F32)
        negK, r_negK = T(C, ph, "negK", [8, 1], F32)
        kb.dma("sp", g8[:], C.g_kv[l:l + 1, :].to_broadcast([8, 128]), writes=[r_g8])
        kb.op("dve", lambda e: e.tensor_reduce(out=negK[:], in_=g8[:], axis=AX.X, op=ALU.max, apply_absolute_value=True),
              reads=[r_g8], writes=[r_negK])
        kb.op("dve", lambda e: e.tensor_scalar(out=negK[:], in0=negK[:], scalar1=-1.02 * (128.0 ** 0.5), scalar2=None,
                                               op0=ALU.mult), reads=[r_negK], writes=[r_negK])
        sc2, r_sc2 = T(C, ph, "sc2", [128, S], F32)
        mb2, r_mb2 = T(C, ph, "mb2", [128, S], BF16)
        scs = [(sc, r_sc), (sc2, r_sc2)]
        mbs = [(mb, r_mb), (mb2, r_mb2)]
        qi_t = [T(C, ph, f"qi{i}", [128, 2, 128], BF16) for i in range(3)]
        wi_t = [T(C, ph, f"wi{i}", [128, 4], F32) for i in range(3)]
        ql_t = [T(C, ph, f"ql{i}", [128, 1024], BF16) for i in range(3)]
        qsq, r_qsq = T(C, ph, "qsq", [128, 1024], BF16)
        sq8, r_sq8 = T(C, ph, "sq8", [8, 1024], F32)
        rl = [T(C, ph, f"rl{i}", [128, 512], F32) for i in range(4)]
        rtmp, r_rtmp = T(C, ph, "rtmp", [128, 512], F32)
        m8, r_m8 = T(C, ph, "m8", [128, 8], F32)
        zt = [T(C, ph, f"zt{i}", [128, 512], F32) for i in range(2)]
        pT = [T(C, ph, f"pT{i}", [128, 512], BF16) for i in range(3)]
        rden, r_rden = T(C, ph, "rden", [128, 512], F32)
        olT, r_olT = T(C, ph, "olT", [128, 1024], BF16)
        oast = [T(C, ph, f"oast{i}", [128, 4, 128], BF16) for i in range(2)]
        psA = Rot(C.ps[0:2])
        psZ = C.ps[2:4]
        psO = C.ps[4:6]
        psD = C.ps[6:8]
        nctr = [0]

        def chunks_of(i):
            nk = (i + 1) * 128
            return [(c0, min(512, nk - c0)) for c0 in range(0, nk, 512)]

        def stL(i):
            tcols = slice(i * 128, (i + 1) * 128)
            qi, r_qi = qi_t[i % 3]
            wi, r_wi = wi_t[i % 3]
            ql, r_ql = ql_t[i % 3]
            kb.dma("sp", qi[:], C.qidxT_d[:, :, tcols].rearrange("c p t -> p c t"), reads=[C.r_qidxT], writes=[r_qi])
            kb.dma("sp", wi[:], C.widx_d[tcols, :], reads=[C.r_widx], writes=[r_wi])
            kb.dma("sp", ql[:].rearrange("p (h t) -> p h t", h=8), C.qlatT_d[:, :, tcols].rearrange("h p t -> p h t"),
                   reads=[C.r_qlatT], writes=[r_ql])
            cb, r_cb, r_cbd = CBs[i % 3]
            kb.op("dve", lambda e: e.tensor_tensor(out=qsq[:], in0=ql[:], in1=ql[:], op=ALU.mult),
                  reads=[r_ql], writes=[r_qsq])
            ps, r_ps = psA.next()
            ps2, r_ps2 = psA.next()
            for half, (p_, r_p) in enumerate(((ps, r_ps), (ps2, r_ps2))):
                kb.op("pe", lambda e: e.matmul(p_[0:8, :], lhsT=onesF[:, 0:8], rhs=qsq[:, half * 512:(half + 1) * 512],
                                               start=True, stop=True), reads=[r_onesF, r_qsq], writes=[r_p])
                kb.op("act", lambda e: e.activation(out=sq8[:, half * 512:(half + 1) * 512], in_=p_[0:8, :], func=AF.Sqrt),
                      reads=[r_p], writes=[r_sq8])
            kb.op("dve", lambda e: e.tensor_scalar(out=sq8[:], in0=sq8[:], scalar1=negK[:, 0:1], scalar2=c8[:, 0:1],
                                                    op0=ALU.mult, op1=ALU.add), reads=[r_sq8, r_negK, r_c8], writes=[r_sq8])
            kb.op("dve", lambda e: e.tensor_tensor(out=cb[0:8, :], in0=sq8[:], in1=bmask[:], op=ALU.mult),
                  reads=[r_sq8, r_bmask, r_cb], writes=[r_cbd])

        def stA(i):
            tcols = slice(i * 128, (i + 1) * 128)
            qi, r_qi = qi_t[i % 3]
            wi, r_wi = wi_t[i % 3]
            sc_, r_sc_ = scs[i % 2]
            for (c0, w) in chunks_of(i):
                for h in range(4):
                    P = slice((h % 2) * 64, (h % 2) * 64 + 64)
                    ps, r_ps = psA.next()
                    kb.op("pe", lambda e: e.matmul(ps[:, 0:w], lhsT=qi[P, h // 2, :], rhs=kidxT[P, c0:c0 + w],
                                                   start=True, stop=True), reads=[r_qi, r_kidxT], writes=[r_ps])
                    r_, r_r = rl[h]
                    kb.op("act", lambda e: e.activation(out=r_[:, 0:w], in_=ps[:, 0:w], func=AF.Relu),
                          reads=[r_ps], writes=[r_r])
                    if h == 0:
                        kb.op("dve", lambda e: e.tensor_scalar(out=sc_[:, c0:c0 + w], in0=r_[:, 0:w], scalar1=wi[:, 0:1],
                                                               scalar2=None, op0=ALU.mult),
                              reads=[r_r, r_wi], writes=[r_sc_])
                    else:
                        kb.op("dve", lambda e: e.scalar_tensor_tensor(out=sc_[:, c0:c0 + w], in0=r_[:, 0:w],
                                                                      scalar=wi[:, h:h + 1], in1=sc_[:, c0:c0 + w],
                                                                      op0=ALU.mult, op1=ALU.add),
                              reads=[r_r, r_wi, r_sc_], writes=[r_sc_])
            kb.op("dve", lambda e: e.tensor_tensor(out=sc_[:, tcols], in0=sc_[:, tcols], in1=dmask[:], op=ALU.add),
                  reads=[r_sc_, r_dmask], writes=[r_sc_])

        def stB(i):
            nk = (i + 1) * 128
            sc_, r_sc_ = scs[i % 2]
            mb_, r_mb_ = mbs[i % 2]
            if i >= 2:
                for rnd in range(TOPK // 8):
                    kb.op("dve", lambda e: e.max(out=m8[:], in_=sc_[:, 0:nk]), reads=[r_sc_], writes=[r_m8])
                    kb.op("dve", lambda e: e.match_replace(out=sc_[:, 0:nk], in_to_replace=m8[:], in_values=sc_[:, 0:nk],
                                                           imm_value=2.0 * NEG), reads=[r_sc_, r_m8], writes=[r_sc_])
                kb.op("dve", lambda e: e.tensor_scalar(out=mb_[:, 0:nk], in0=sc_[:, 0:nk], scalar1=1.5 * NEG, scalar2=MBIG,
                                                       op0=ALU.is_gt, op1=ALU.mult), reads=[r_sc_], writes=[r_mb_])
            else:
                kb.op("dve", lambda e: e.tensor_scalar(out=mb_[:, 0:nk], in0=sc_[:, 0:nk], scalar1=0.5 * NEG, scalar2=MBIG,
                                                       op0=ALU.is_le, op1=ALU.mult), reads=[r_sc_], writes=[r_mb_])

        def stF(i, jbs):
            ql, r_ql = ql_t[i % 3]
            mb_, r_mb_ = mbs[i % 2]
            cb, r_cb, r_cbd = CBs[i % 3]
            items = [(half, jb) for half in range(2) for jb in jbs]
            first_jb, last_jb = i, 0

            def fa(n, it):
                half, jb = it
                hc = slice(half * 512, (half + 1) * 512)
                pz, r_pz = psZ[n % 2]
                kb.op("pe", lambda e: e.matmul(pz[:], lhsT=ckvT[:, jb * 128:(jb + 1) * 128], rhs=ql[:, hc],
                                               start=True, stop=False), reads=[r_ckvT, r_ql], writes=[r_pz])
                kb.op("pe", lambda e: e.matmul(pz[:], lhsT=mb_[:, jb * 128:(jb + 1) * 128], rhs=Irep[:],
                                               start=False, stop=False), reads=[r_mb_, r_Irep], writes=[r_pz])
                kb.op("pe", lambda e: e.matmul(pz[:], lhsT=onesF[0:72, :], rhs=cb[0:72, hc], start=False, stop=True),
                      reads=[r_onesF, r_cb, r_cbd], writes=[r_pz])
                p_, r_p = pT[n % 3]
                rel = i - jb
                if rel <= 1:
                    z_, r_z = zt[n % 2]
                    kb.op("dve", lambda e: e.tensor_tensor(out=z_[:], in0=pz[:], in1=Tz[:, rel, hc], op=ALU.add),
                          reads=[r_pz, r_Tz], writes=[r_z])
                    kb.op("act", lambda e: e.activation(out=p_[:], in_=z_[:], func=AF.Exp), reads=[r_z], writes=[r_p])
                else:
                    kb.op("act", lambda e: e.activation(out=p_[:], in_=pz[:], func=AF.Exp), reads=[r_pz], writes=[r_p])

            def fb(n, it):
                half, jb = it
                p_, r_p = pT[n % 3]
                po, r_po = psO[half]
                pd, r_pd = psD[half]
                kb.op("pe", lambda e: e.matmul(po[:], lhsT=ckv[:, jb, :], rhs=p_[:], start=(jb == first_jb), stop=(jb == last_jb)),
                      reads=[r_ckv, r_p], writes=[r_po])
                kb.op("pe", lambda e: e.matmul(pd[:], lhsT=onesF[:], rhs=p_[:], start=(jb == first_jb), stop=(jb == last_jb)),
                      reads=[r_onesF, r_p], writes=[r_pd])

            N = len(items)
            base = nctr[0]
            for n in range(N + 1):
                if n < N:
                    fa(base + n, items[n])
                if n >= 1:
                    fb(base + n - 1, items[n - 1])
            nctr[0] += N

        def stT(i):
            tcols = slice(i * 128, (i + 1) * 128)
            for half in range(2):
                hc = slice(half * 512, (half + 1) * 512)
                po, r_po = psO[half]
                pd, r_pd = psD[half]
                kb.op("dve", lambda e: e.reciprocal(out=rden[:], in_=pd[:]), reads=[r_pd], writes=[r_rden])
                kb.op("dve", lambda e: e.tensor_tensor(out=olT[:, hc], in0=po[:], in1=rden[:], op=ALU.mult),
                      reads=[r_po, r_rden], writes=[r_olT])
            ps, r_ps = psA.next()
            for h in range(8):
                kb.op("pe", lambda e: e.matmul(ps[(h % 2) * 64:(h % 2) * 64 + 64, (h // 2) * 128:(h // 2 + 1) * 128],
                                               lhsT=wuv[:, h, :], rhs=olT[:, h * 128:(h + 1) * 128], start=True, stop=True),
                      reads=[r_wuv, r_olT], writes=[r_ps])
            og, r_og = oast[i % 2]
            kb.op("act", lambda e: e.copy(out=og[:].rearrange("p c t -> p (c t)"), in_=ps[:]), reads=[r_ps], writes=[r_og])
            kb.dma("sp", C.oaT_d[:, :, tcols].rearrange("c p t -> p c t"), og[:], reads=[r_og], writes=[C.r_oaT])

        stL(0)
        stL(1)
        stA(0)
        stB(0)
        stA(1)
        for i in range(NT):
            if i + 2 < NT:
                stL(i + 2)
            stF(i, [jb for jb in (i, i - 1) if jb >= 0])
            if i + 2 < NT:
                stA(i + 2)
            if i + 1 < NT:
                stB(i + 1)
            if i >= 2:
                stF(i, list(range(i - 2, -1, -1)))
            stT(i)
        kb.barrier()


def bload(C, ph, name, src_ap, n, reads=()):
    t, r = T(C, ph, name, [128, n], F32)
    C.kb.dma("sp", t[:], src_ap, reads=list(reads), writes=[r])
    return t, r


def phase_out(C, l, x_src, r_xsrc):
    nc, kb = C.nc, C.kb
    with contextlib.ExitStack() as ph:
        Wg, r_Wg = T(C, ph, "Wg", [128, 8, 2048], BF16)
        wao, r_wao = T(C, ph, "wao", [128, 4, 1024], BF16)
        wbo, r_wbo = T(C, ph, "wbo", [128, 4, 1024], BF16)
        wo, r_wo = T(C, ph, "wo", [128, 8, 1024], BF16)
        wr, r_wr = T(C, ph, "wr", [128, 8, 32], F32)
        kb.dma("pool", Wg[:], C.w_in[l, :, NW1:NCOLS].rearrange("(kc p) n -> p kc n", p=128), writes=[r_Wg])
        kb.dma("pool", wao[:], C.w_a_out[l].rearrange("(kc p) n -> p kc n", p=128), writes=[r_wao])
        kb.dma("pool", wbo[:], C.w_b_out[l].rearrange("(kc p) n -> p kc n", p=128), writes=[r_wbo])
        kb.dma("pool", wo[:], C.w_o[l].rearrange("(kc p) n -> p kc n", p=128), writes=[r_wo])
        kb.dma("sp", wr[:], C.w_router[l].rearrange("(kc p) n -> p kc n", p=128), writes=[r_wr])
        sc1, r_sc1 = bload(C, ph, "sc1", C.mod_d[l, :, 1024:2048], 1024, [C.r_mod])
        sh1, r_sh1 = bload(C, ph, "sh1", C.mod_d[l, :, 0:1024], 1024, [C.r_mod])
        g1, r_g1 = bload(C, ph, "g1", C.mod_d[l, :, 2048:3072], 1024, [C.r_mod])
        sh2, r_sh2 = bload(C, ph, "sh2", C.mod_d[l, :, 3072:4096], 1024, [C.r_mod])
        sc2, r_sc2 = bload(C, ph, "sc2", C.mod_d[l, :, 4096:5120], 1024, [C.r_mod])
        lg, r_lg = bload(C, ph, "lg", C.ln1_g[l:l + 1, :].to_broadcast([128, 1024]), 1024)
        lb, r_lb = bload(C, ph, "lb", C.ln1_b[l:l + 1, :].to_broadcast([128, 1024]), 1024)
        brB, r_brB = bload(C, ph, "brB", C.b_router[l:l + 1, :].to_broadcast([128, 32]), 32)
        ident, r_id = T(C, ph, "identb", [128, 128], BF16)
        identf, r_idf = T(C, ph, "identf2", [128, 128], F32)
        kb.dma("sp", ident[:], C.ident_bf[:, :], writes=[r_id])
        kb.dma("sp", identf[:], C.ident_f[:, :], writes=[r_idf])
        LW = ln_work(C, ph, "olw", LN_EPS)
        xres = [T(C, ph, "xres0", [128, 4, 1024], F32)[0]] * 2
        r_xres = [[Res(f"xres_{j}") for j in range(4)]] * 2
        hb, r_hb = T(C, ph, "ohb", [128, 1024], BF16)
        hT, _ = T(C, ph, "ohT", [128, 8, 512], BF16)
        r_hT = [Res(f"ohT_{j}") for j in range(4)]
        oaT, r_oaT = T(C, ph, "ooaT", [128, 4, 512], BF16)
        obT, r_obT = T(C, ph, "oobT", [128, 4, 512], BF16)
        sga = [T(C, ph, f"sga{i}", [128, 512], F32) for i in range(2)]
        sgb = [T(C, ph, f"sgb{i}", [128, 512], F32) for i in range(2)]
        t1 = [T(C, ph, f"t1{i}", [128, 512], F32) for i in range(2)]
        t2 = [T(C, ph, f"t2{i}", [128, 512], F32) for i in range(2)]
        mT, _ = T(C, ph, "mergT", [128, 8, 512], BF16)
        r_mT = [Res(f"mergT_{j}") for j in range(8)]
        yt, r_yt = T(C, ph, "yt", [128, 1024], F32)
        zt, r_zt = T(C, ph, "zt_o", [128, 1024], F32)
        x1t = [T(C, ph, f"x1t{i}", [128, 1024], F32) for i in range(2)]
        h2f, r_h2f = T(C, ph, "h2f", [128, 1024], F32)
        h2T32, r_h2T32 = T(C, ph, "h2T32", [128, 8, 128], F32)
        h2Ts = [T(C, ph, f"h2Ts{i}", [128, 8, 512], BF16)[0] for i in range(2)]
        r_h2Ts = [[Res(f"h2Ts{i}_{j}") for j in range(4)] for i in range(2)]
        lgt, r_lgt = T(C, ph, "lgt", [128, 32], F32)
        m8, r_m8 = T(C, ph, "om8", [128, 8], F32)
        nmx, r_nmx = T(C, ph, "nmx", [128, 1], F32)
        msk, r_msk = T(C, ph, "msk", [128, 32], F32)
        ex, r_ex = T(C, ph, "ex", [128, 32], F32)
        rs, r_rs = T(C, ph, "ors", [128, 1], F32)
        gst = [T(C, ph, f"gst{i}", [128, 4, 32], F32)[0] for i in range(2)]
        r_gst = [[Res(f"gst{i}_{j}") for j in range(4)] for i in range(2)]
        gTs = [T(C, ph, f"gTs{i}", [32, 512], F32)[0] for i in range(2)]
        r_gTs = [[Res(f"gTs{i}_{j}") for j in range(4)] for i in range(2)]
        pp = Rot(C.ps[0:4])
        psT, r_psT = C.ps[7]
        psT32 = C.ps[5:7]
        psY = C.ps[4:5]

        for st in range(getattr(C, 'out_nst', NST)):
            stage = getattr(C, 'out_stage', 99)
            sp_ = st % 2
            t0 = st * 512
            cols = slice(t0, t0 + 512)
            xr = xres[sp_]
            for j in range(4):
                tt = st * 4 + j
                kb.dma("sp", xr[:, j, :], x_src[tt * 128:(tt + 1) * 128, :], reads=[r_xsrc], writes=[r_xres[sp_][j]])
                ln_modulate(C, LW, xr[:, j, :], r_xres[sp_][j], hb[:], r_hb, sc1[:], r_sc1, sh1[:], r_sh1)
                for kc in range(8):
                    kb.op("pe", lambda e: e.transpose(out=bfv(psT)[:, kc * 128:(kc + 1) * 128],
                                                      in_=hb[:, kc * 128:(kc + 1) * 128], identity=ident[:]),
                          reads=[r_hb, r_id], writes=[r_psT])
                kb.op("act", lambda e: e.copy(out=hT[:, :, j * 128:(j + 1) * 128],
                                              in_=bfv(psT).rearrange("p (kc t) -> p kc t", kc=8)),
                      reads=[r_psT], writes=[r_hT[j]])
            kb.dma("sp", oaT[:], C.oaT_d[:, :, cols].rearrange("c p t -> p c t"), reads=[C.r_oaT], writes=[r_oaT])
            kb.dma("sp", obT[:], C.obT_d[:, :, cols].rearrange("c p t -> p c t"), reads=[C.r_obT], writes=[r_obT])
            for n_ in range(8 if stage >= 1 else 0):
                ncs = slice(n_ * 128, (n_ + 1) * 128)
                pga, r_pga = pp.next()
                pgb, r_pgb = pp.next()
                pa, r_pa = pp.next()
                pb, r_pb = pp.next()
                for kc in range(8):
                    kb.op("pe", lambda e: e.matmul(pga[:], lhsT=Wg[:, kc, n_ * 128:(n_ + 1) * 128], rhs=hT[:, kc, :],
                                                   start=(kc == 0), stop=(kc == 7)), reads=r_hT + [r_Wg], writes=[r_pga])
                for kc in range(8):
                    kb.op("pe", lambda e: e.matmul(pgb[:], lhsT=Wg[:, kc, 1024 + n_ * 128:1024 + (n_ + 1) * 128],
                                                   rhs=hT[:, kc, :], start=(kc == 0), stop=(kc == 7)),
                          reads=r_hT + [r_Wg], writes=[r_pgb])
                for c in range(4):
                    kb.op("pe", lambda e: e.matmul(pa[:], lhsT=wao[:, c, ncs], rhs=oaT[:, c, :], start=(c == 0), stop=(c == 3)),
                          reads=[r_wao, r_oaT], writes=[r_pa])
                for c in range(4):
                    kb.op("pe", lambda e: e.matmul(pb[:], lhsT=wbo[:, c, ncs], rhs=obT[:, c, :], start=(c == 0), stop=(c == 3)),
                          reads=[r_wbo, r_obT], writes=[r_pb])
                sa, r_sa = sga[n_ % 2]
                sb_, r_sb = sgb[n_ % 2]
                a1, r_a1 = t1[n_ % 2]
                a2, r_a2 = t2[n_ % 2]
                kb.op("act", lambda e: e.activation(out=sa[:], in_=pga[:], func=AF.Sigmoid), reads=[r_pga], writes=[r_sa])
                kb.op("act", lambda e: e.activation(out=sb_[:], in_=pgb[:], func=AF.Sigmoid), reads=[r_pgb], writes=[r_sb])
                kb.op("dve", lambda e: e.tensor_tensor(out=a1[:], in0=pa[:], in1=sa[:], op=ALU.mult),
                      reads=[r_pa, r_sa], writes=[r_a1])
                kb.op("dve", lambda e: e.tensor_tensor(out=a2[:], in0=pb[:], in1=sb_[:], op=ALU.mult),
                      reads=[r_pb, r_sb], writes=[r_a2])
                kb.op("pool", lambda e: e.tensor_tensor(out=mT[:, n_, :], in0=a1[:], in1=a2[:], op=ALU.add),
                      reads=[r_a1, r_a2], writes=[r_mT[n_]])
            for j in range(4 if stage >= 2 else 0):
                tt = st * 4 + j
                for nh in range(2):
                    py, r_py = psY[0]
                    for n_ in range(8):
                        kb.op("pe", lambda e: e.matmul(py[:], lhsT=mT[:, n_, j * 128:(j + 1) * 128],
                                                       rhs=wo[:, n_, nh * 512:(nh + 1) * 512], start=(n_ == 0), stop=(n_ == 7)),
                              reads=r_mT + [r_wo], writes=[r_py])
                    kb.op("dve", lambda e: e.tensor_tensor(out=yt[:, nh * 512:(nh + 1) * 512], in0=py[:],
                                                           in1=g1[:, nh * 512:(nh + 1) * 512], op=ALU.mult),
                          reads=[r_py, r_g1], writes=[r_yt])
                kb.op("dve", lambda e: e.scalar_tensor_tensor(out=zt[:], in0=xr[:, j, :], scalar=float(DN_ALPHA), in1=yt[:],
                                                              op0=ALU.mult, op1=ALU.add),
                      reads=[r_xres[sp_][j], r_yt], writes=[r_zt])
                x1, r_x1 = x1t[tt % 2]
                ln_modulate(C, LW, zt, r_zt, x1[:], r_x1, lg[:], r_lg, lb[:], r_lb)
                kb.dma("sp", C.x1_d[tt * 128:(tt + 1) * 128, :], x1[:], reads=[r_x1], writes=[C.r_x1])
                if stage < 3:
                    continue
                ln_modulate(C, LW, x1, r_x1, h2f[:], r_h2f, sc2[:], r_sc2, sh2[:], r_sh2)
                sub = getattr(C, 'out_sub', 99)
                for half in range(2 if sub >= 1 else 0):
                    p32, r_p32 = psT32[half]
                    for k4 in range(4):
                        kc = half * 4 + k4
                        kb.op("pe", lambda e: e.transpose(out=p32[:, k4 * 128:(k4 + 1) * 128],
                                                          in_=h2f[:, kc * 128:(kc + 1) * 128], identity=identf[:]),
                              reads=[r_h2f, r_idf], writes=[r_p32])
                    if sub < 2:
                        continue
                    kb.op("act", lambda e: e.copy(out=h2T32[:, half * 4:half * 4 + 4, :],
                                                  in_=p32[:].rearrange("p (k t) -> p k t", k=4)),
                          reads=[r_p32], writes=[r_h2T32])
                    if sub < 3:
                        continue
                    kb.op("dve", lambda e: e.tensor_copy(out=h2Ts[sp_][:, half * 4:half * 4 + 4, j * 128:(j + 1) * 128],
                                                         in_=p32[:].rearrange("p (k t) -> p k t", k=4)),
                          reads=[r_p32], writes=[r_h2Ts[sp_][j]])
                if stage < 4:
                    continue
                pl, r_pl = pp.next()
                for kc in range(8):
                    kb.op("pe", lambda e: e.matmul(pl[:, 0:32], lhsT=h2T32[:, kc, :], rhs=wr[:, kc, :],
                                                   start=(kc == 0), stop=(kc == 7)), reads=[r_h2T32, r_wr], writes=[r_pl])
                kb.op("dve", lambda e: e.tensor_tensor(out=lgt[:], in0=pl[:, 0:32], in1=brB[:], op=ALU.add),
                      reads=[r_pl, r_brB], writes=[r_lgt])
                kb.op("dve", lambda e: e.max(out=m8[:], in_=lgt[:]), reads=[r_lgt], writes=[r_m8])
                kb.op("dve", lambda e: e.tensor_scalar(out=nmx[:], in0=m8[:, 0:1], scalar1=-1.0, scalar2=None, op0=ALU.mult),
                      reads=[r_m8], writes=[r_nmx])
                kb.op("dve", lambda e: e.tensor_scalar(out=msk[:], in0=lgt[:], scalar1=m8[:, 3:4], scalar2=None, op0=ALU.is_ge),
                      reads=[r_lgt, r_m8], writes=[r_msk])
                kb.op("act", lambda e: e.activation(out=ex[:], in_=lgt[:], func=AF.Exp, bias=nmx[:, 0:1], scale=1.0),
                      reads=[r_lgt, r_nmx], writes=[r_ex])
                kb.op("dve", lambda e: e.tensor_tensor(out=ex[:], in0=ex[:], in1=msk[:], op=ALU.mult),
                      reads=[r_ex, r_msk], writes=[r_ex])
                kb.op("dve", lambda e: e.reduce_sum(out=rs[:], in_=ex[:], axis=AX.X), reads=[r_ex], writes=[r_rs])
                kb.op("dve", lambda e: e.reciprocal(out=rs[:], in_=rs[:]), reads=[r_rs], writes=[r_rs])
                kb.op("dve", lambda e: e.tensor_scalar(out=gst[sp_][:, j, :], in0=ex[:], scalar1=rs[:, 0:1], scalar2=None,
                                                       op0=ALU.mult), reads=[r_ex, r_rs], writes=[r_gst[sp_][j]])
                pg, r_pg = pp.next()
                kb.op("pe", lambda e: e.transpose(out=pg[0:32, 0:128], in_=gst[sp_][:, j, :], identity=identf[:]),
                      reads=[r_gst[sp_][j], r_idf], writes=[r_pg])
                kb.op("act", lambda e: e.copy(out=gTs[sp_][:, j * 128:(j + 1) * 128], in_=pg[0:32, 0:128]),
                      reads=[r_pg], writes=[r_gTs[sp_][j]])
            if stage < 4:
                continue
            kb.dma("sp", C.h2T_d[:, :, cols].rearrange("k p t -> p k t"), h2Ts[sp_][:], reads=r_h2Ts[sp_], writes=[C.r_h2T])
            kb.dma("sp", C.gates_d[cols, :].rearrange("(j p) e -> p j e", p=128), gst[sp_][:], reads=r_gst[sp_],
                   writes=[C.r_gates])
            kb.dma("sp", C.gatesT_d[:, cols], gTs[sp_][:], reads=r_gTs[sp_], writes=[C.r_gatesT])
        kb.barrier()


TS = 1024


def phase_moe(C, l, x_dst, r_xdst):
    nc, kb = C.nc, C.kb
    with contextlib.ExitStack() as ph:
        wgu = [T(C, ph, f"wgu{i}", [128, 8, 2048], BF16) for i in range(2)]
        wdn = [T(C, ph, f"wdn{i}", [128, 8, 1024], BF16) for i in range(2)]
        bgu, r_bgu = T(C, ph, "bgu", [128, E, 16], F32)
        bdn, r_bdn = T(C, ph, "bdn", [32, 1024], F32)
        kb.dma("sp", bgu[:], C.b_gu_t[l].rearrange("e p c -> p e c"), writes=[r_bgu])
        kb.dma("sp", bdn[:], C.b_dn[l], writes=[r_bdn])
        kb.op("pool", lambda e: e.tensor_scalar(out=bgu[:, :, 8:16], in0=bgu[:, :, 8:16], scalar1=1.0, scalar2=None,
                                                op0=ALU.add), reads=[r_bgu], writes=[r_bgu])
        g2, r_g2 = bload(C, ph, "g2", C.mod_d[l, :, 5120:6144], 1024, [C.r_mod])
        lg, r_lg = bload(C, ph, "lg2", C.ln2_g[l:l + 1, :].to_broadcast([128, 1024]), 1024)
        lb, r_lb = bload(C, ph, "lb2", C.ln2_b[l:l + 1, :].to_broadcast([128, 1024]), 1024)
        LW = ln_work(C, ph, "mlw", LN_EPS)
        h2T, r_h2T = T(C, ph, "mh2T", [128, 8, TS], BF16)
        gts, r_gts = T(C, ph, "mgts", [128, TS // 128, 32], F32)
        gT, r_gT = T(C, ph, "mgT", [32, TS], F32)
        acc, _ = T(C, ph, "macc", [128, TS // 128, 1024], F32)
        r_acc = [[Res(f"acc{j}_{nh}") for nh in range(2)] for j in range(TS // 128)]
        a_sb = [T(C, ph, f"a_sb{i}", [128, 512], F32) for i in range(2)]
        sg = [T(C, ph, f"sg{i}", [128, 512], BF16) for i in range(2)]
        gg = [T(C, ph, f"gg{i}", [128, 512], F32) for i in range(2)]
        u_sb = [T(C, ph, f"u_sb{i}", [128, 512], F32) for i in range(2)]
        actT = [T(C, ph, f"actT{i}", [128, 8, 512], BF16)[0] for i in range(2)]
        r_actT = [[Res(f"actT{i}_{f}") for f in range(8)] for i in range(2)]
        x1t, r_x1t = T(C, ph, "mx1t", [128, 1024], F32)
        xo = [(x1t, r_x1t)] * 2
        ppA = Rot(C.ps[0:2])
        ppU = Rot(C.ps[2:4])
        ppY = Rot(C.ps[4:8])
        wloaded = {}

        def load_w(idx):
            if idx in wloaded or idx >= (S // TS) * E:
                return
            wloaded[idx] = True
            e_ = idx % E
            g_, r_g = wgu[idx % 2]
            d_, r_d = wdn[idx % 2]
            kb.dma("pool", g_[:], C.w_gu[l, e_].rearrange("(kc p) n -> p kc n", p=128), writes=[r_g])
            kb.dma("pool", d_[:], C.w_dn[l, e_].rearrange("(kc p) n -> p kc n", p=128), writes=[r_d])

        load_w(0)
        load_w(1)
        for ts in range(S // TS):
            tcols = slice(ts * TS, (ts + 1) * TS)
            kb.dma("sp", h2T[:], C.h2T_d[:, :, tcols].rearrange("k p t -> p k t"), reads=[C.r_h2T], writes=[r_h2T])
            kb.dma("sp", gts[:], C.gates_d[tcols, :].rearrange("(j p) e -> p j e", p=128), reads=[C.r_gates], writes=[r_gts])
            kb.dma("sp", gT[:], C.gatesT_d[:, tcols], reads=[C.r_gatesT], writes=[r_gT])
            for j in range(TS // 128):
                for nh in range(2):
                    py, r_py = ppY.next()
                    kb.op("pe", lambda e: e.matmul(py[:], lhsT=gT[:, j * 128:(j + 1) * 128], rhs=bdn[:, nh * 512:(nh + 1) * 512],
                                                   start=True, stop=True), reads=[r_gT, r_bdn], writes=[r_py])
                    kb.op("act", lambda e: e.copy(out=acc[:, j, nh * 512:(nh + 1) * 512], in_=py[:]),
                          reads=[r_py], writes=[r_acc[j][nh]])
            def AU(e_, t2):
                idx = ts * E + e_
                g_, r_g = wgu[idx % 2]
                aT = actT[t2 % 2]
                r_aT = r_actT[t2 % 2]
                hc = slice(t2 * 512, (t2 + 1) * 512)
                for fc in range(8):
                    pa, r_pa = ppA.next()
                    pu, r_pu = ppU.next()
                    for kc in range(8):
                        kb.op("pe", lambda e: e.matmul(pa[:], lhsT=g_[:, kc, fc * 128:(fc + 1) * 128], rhs=h2T[:, kc, hc],
                                                       start=(kc == 0), stop=(kc == 7)), reads=[r_g, r_h2T], writes=[r_pa])
                    for kc in range(8):
                        kb.op("pe", lambda e: e.matmul(pu[:], lhsT=g_[:, kc, 1024 + fc * 128:1024 + (fc + 1) * 128],
                                                       rhs=h2T[:, kc, hc], start=(kc == 0), stop=(kc == 7)),
                              reads=[r_g, r_h2T], writes=[r_pu])
                    a_, r_a = a_sb[fc % 2]
                    s_, r_s = sg[fc % 2]
                    q_, r_q = gg[fc % 2]
                    u_, r_u = u_sb[fc % 2]
                    kb.op("dve", lambda e: e.tensor_scalar(out=a_[:], in0=pa[:], scalar1=bgu[:, e_, fc:fc + 1], scalar2=7.0,
                                                           op0=ALU.add, op1=ALU.min), reads=[r_pa, r_bgu], writes=[r_a])
                    kb.op("act", lambda e: e.activation(out=s_[:], in_=a_[:], func=AF.Sigmoid, scale=1.702),
                          reads=[r_a], writes=[r_s])
                    kb.op("dve", lambda e: e.tensor_scalar(out=u_[:], in0=pu[:], scalar1=bgu[:, e_, 8 + fc:9 + fc], scalar2=8.0,
                                                           op0=ALU.add, op1=ALU.min), reads=[r_pu, r_bgu], writes=[r_u])
                    kb.op("pool", lambda e: e.tensor_tensor(out=q_[:], in0=a_[:], in1=s_[:], op=ALU.mult),
                          reads=[r_a, r_s], writes=[r_q])
                    kb.op("dve", lambda e: e.scalar_tensor_tensor(out=aT[:, fc, :], in0=u_[:], scalar=-6.0, in1=q_[:],
                                                                  op0=ALU.max, op1=ALU.mult),
                          reads=[r_u, r_q], writes=[r_aT[fc]])

            def YY(e_, t2):
                idx = ts * E + e_
                d_, r_d = wdn[idx % 2]
                aT = actT[t2 % 2]
                r_aT = r_actT[t2 % 2]
                for j4 in range(4):
                    j = t2 * 4 + j4
                    for nh in range(2):
                        py, r_py = ppY.next()
                        for fc in range(8):
                            kb.op("pe", lambda e: e.matmul(py[:], lhsT=aT[:, fc, j4 * 128:(j4 + 1) * 128],
                                                           rhs=d_[:, fc, nh * 512:(nh + 1) * 512],
                                                           start=(fc == 0), stop=(fc == 7)), reads=r_aT + [r_d], writes=[r_py])
                        kb.op("dve", lambda e: e.scalar_tensor_tensor(out=acc[:, j, nh * 512:(nh + 1) * 512], in0=py[:],
                                                                      scalar=gts[:, j, e_:e_ + 1],
                                                                      in1=acc[:, j, nh * 512:(nh + 1) * 512],
                                                                      op0=ALU.mult, op1=ALU.add),
                              reads=[r_py, r_gts, r_acc[j][nh]], writes=[r_acc[j][nh]])
                if t2 == TS // 512 - 1:
                    load_w(idx + 2)

            units = [(e_, t2) for e_ in range(E) for t2 in range(TS // 512)]
            for ui, un in enumerate(units):
                AU(*un)
                if ui >= 1:
                    YY(*units[ui - 1])
            YY(*units[-1])
            for j in range(TS // 128):
                tt = ts * (TS // 128) + j
                kb.dma("sp", x1t[:], C.x1_d[tt * 128:(tt + 1) * 128, :], reads=[C.r_x1], writes=[r_x1t])
                kb.op("pool", lambda e: e.tensor_tensor(out=acc[:, j, :], in0=acc[:, j, :], in1=g2[:], op=ALU.mult),
                      reads=r_acc[j] + [r_g2], writes=r_acc[j])
                kb.op("dve", lambda e: e.scalar_tensor_tensor(out=x1t[:], in0=x1t[:], scalar=float(DN_ALPHA), in1=acc[:, j, :],
                                                              op0=ALU.mult, op1=ALU.add), reads=[r_x1t] + r_acc[j], writes=[r_x1t])
                xo_, r_xo = xo[tt % 2]
                ln_modulate(C, LW, x1t, r_x1t, xo_[:], r_xo, lg[:], r_lg, lb[:], r_lb)
                kb.dma("sp", x_dst[tt * 128:(tt + 1) * 128, :], xo_[:], reads=[r_xo], writes=[r_xdst])
        kb.barrier()

def alloc_scratch(C):
    C.mod_d, C.r_mod = dram(C, "mod_d", [DEPTH, 128, 6 * D], F32)
    C.qlatT_d, C.r_qlatT = dram(C, "qlatT_d", [8, 128, S], BF16)
    C.qidxT_d, C.r_qidxT = dram(C, "qidxT_d", [2, 128, S], BF16)
    C.kidxT_d, C.r_kidxT = dram(C, "kidxT_d", [128, S], BF16)
    C.widx_d, C.r_widx = dram(C, "widx_d", [S, 4], F32)
    C.ckv_d, C.r_ckv = dram(C, "ckv_d", [S, 128], BF16)
    C.ckvT_d, C.r_ckvT = dram(C, "ckvT_d", [128, S], BF16)
    C.qbT_d, C.r_qbT = dram(C, "qbT_d", [4, 128, S], BF16)
    C.kbT_d, C.r_kbT = dram(C, "kbT_d", [4, 128, S], BF16)
    C.vb_d, C.r_vb = dram(C, "vb_d", [S, 512], BF16)
    C.obT_d, C.r_obT = dram(C, "obT_d", [4, 128, S], BF16)
    C.oaT_d, C.r_oaT = dram(C, "oaT_d", [4, 128, S], BF16)
    C.x1_d, C.r_x1 = dram(C, "x1_d", [S, D], F32)
    C.x2_d, C.r_x2 = dram(C, "x2_d", [S, D], F32)
    C.h2T_d, C.r_h2T = dram(C, "h2T_d", [8, 128, S], BF16)
    C.gates_d, C.r_gates = dram(C, "gates_d", [S, 32], F32)
    C.gatesT_d, C.r_gatesT = dram(C, "gatesT_d", [32, S], F32)


def build(dbg=(), upto=99, skip_sb=False, skip_dsa=False, nlayers=DEPTH):
    nc = bass.Bass("TRN2", target_bir_lowering=False)
    C = Ctx()
    C.skip_sb = skip_sb
    C.skip_dsa = skip_dsa
    C.nc = nc
    C.dbg = set(dbg)

    def inp(name, shape, dt=F32):
        return nc.dram_tensor(name, list(shape), dt, kind="ExternalInput").ap()

    C.x = inp("x", [S, D])
    C.c_col = inp("c_col", [128, 8])
    C.w_ada = inp("w_ada", [DEPTH, D, 6 * D])
    C.b_ada = inp("b_ada", [DEPTH, 6 * D])
    C.w_in = inp("w_in", [DEPTH, D, NCOLS])
    C.w_uk_t = inp("w_uk_t", [DEPTH, 8, 64, 128])
    C.g_kv = inp("g_kv", [DEPTH, 128])
    C.ident_bf = inp("ident_bf", [128, 128], BF16)
    C.negU_bf = inp("negU_bf", [128, 128], BF16)
    C.ones_bf = inp("ones_bf", [128, 128], BF16)
    C.cm_bf = inp("cm_bf", [4, 128, 512], BF16)
    C.w_uv = inp("w_uv", [DEPTH, 128, 8, 64])
    C.rel_bias = inp("rel_bias", [32, 8])
    C.rel_bias_t = inp("rel_bias_t", [8, 32])
    C.dmask = inp("dmask", [128, 128])
    C.irep_bf = inp("irep_bf", [128, 512], BF16)
    C.ident_f = inp("ident_f", [128, 128])
    C.tz = inp("tz", [2, 128, 1024])
    C.bmask = inp("bmask", [8, 1024])
    C.w_a_out = inp("w_a_out", [DEPTH, 512, D])
    C.w_b_out = inp("w_b_out", [DEPTH, 512, D])
    C.w_o = inp("w_o", [DEPTH, D, D])
    C.ln1_g = inp("ln1_g", [DEPTH, D])
    C.ln1_b = inp("ln1_b", [DEPTH, D])
    C.ln2_g = inp("ln2_g", [DEPTH, D])
    C.ln2_b = inp("ln2_b", [DEPTH, D])
    C.w_router = inp("w_router", [DEPTH, D, E])
    C.b_router = inp("b_router", [DEPTH, E])
    C.w_gu = inp("w_gu", [DEPTH, E, D, 2 * FF])
    C.b_gu_t = inp("b_gu_t", [DEPTH, E, 128, 16])
    C.w_dn = inp("w_dn", [DEPTH, E, FF, D])
    C.b_dn = inp("b_dn", [DEPTH, E, D])
    C.out = nc.dram_tensor("out", [S, D], F32, kind="ExternalOutput").ap()
    C.r_out = Res("out")
    C.r_x = Res("x")
    with contextlib.ExitStack() as es:
        C.es = es
        C.kb = KB(nc, es)
        C.ps = [(es.enter_context(nc.psum_tensor(f"ps{i}", [128, 512], F32)), Res(f"ps{i}", psum=True)) for i in range(8)]
        alloc_scratch(C)
        phase_ada(C)
        x_src, r_xsrc = C.x, C.r_x
        for l in range(nlayers):
            if upto >= 1:
                phase_proj(C, l, x_src, r_xsrc)
            if upto >= 2 and not C.skip_sb:
                phase_sb(C, l)
            if upto >= 3 and not C.skip_dsa:
                phase_dsa(C, l)
            if upto >= 4:
                phase_out(C, l, x_src, r_xsrc)
            if upto >= 5:
                last = (l == DEPTH - 1)
                x_dst, r_xdst = (C.out, C.r_out) if last else (C.x2_d, C.r_x2)
                phase_moe(C, l, x_dst, r_xdst)
                x_src, r_xsrc = x_dst, r_xdst
        C.kb.barrier()
    C.ninst = C.kb.ninst
    return nc, C


def host_inputs(inputs, b):
    f = np.float32
    return {
        "x": np.ascontiguousarray(inputs["x"][b]),
        "c_col": np.ascontiguousarray(np.asarray(inputs["c"][b], f).reshape(8, 128).T),
        "w_ada": inputs["w_ada"], "b_ada": inputs["b_ada"], "w_in": inputs["w_in"],
        "w_uk_t": np.ascontiguousarray(np.transpose(inputs["w_uk"], (0, 2, 3, 1))),
        "g_kv": inputs["g_kv"],
        "w_a_out": inputs["w_a_out"], "w_b_out": inputs["w_b_out"], "w_o": inputs["w_o"],
        "ln1_g": inputs["ln1_g"], "ln1_b": inputs["ln1_b"], "ln2_g": inputs["ln2_g"], "ln2_b": inputs["ln2_b"],
        "w_router": inputs["w_router"], "b_router": inputs["b_router"],
        "w_gu": inputs["w_gu"], "w_dn": inputs["w_dn"], "b_dn": inputs["b_dn"],
        "b_gu_t": np.ascontiguousarray(np.asarray(inputs["b_gu"], f).reshape(DEPTH, E, 16, 128).transpose(0, 1, 3, 2)),
        "w_uv": inputs["w_uv"], "rel_bias": inputs["rel_bias"],
        "rel_bias_t": np.ascontiguousarray(np.asarray(inputs["rel_bias"], f).T),
        "tz": _tz_table(np.asarray(inputs["rel_bias"], f)),
        "ident_bf": np.eye(128, dtype=np.float32).astype(ml_dtypes.bfloat16),
        **CONSTS,
    }


def _make_consts():
    bf = ml_dtypes.bfloat16
    jj = np.arange(128)
    negU = -(jj[:, None] >= jj[None, :]).astype(np.float32)
    ones = np.ones((128, 128), np.float32)
    s_ = np.arange(128)[None, :, None]
    t_ = np.arange(512)[None, None, :]
    rel = np.arange(4)[:, None, None]
    cm = ((s_ + 128 * rel) < t_).astype(np.float32)
    dmask = np.where(jj[None, :] > jj[:, None], np.float32(NEG), np.float32(0)).astype(np.float32)
    irep = np.tile(np.eye(128, dtype=np.float32), (1, 4))
    bmask = np.zeros((8, 8, 128), np.float32)
    for h in range(8):
        bmask[h, h, :] = 1.0
    return {"negU_bf": negU.astype(bf), "ones_bf": ones.astype(bf), "cm_bf": cm.astype(bf),
            "dmask": dmask, "irep_bf": irep.astype(bf), "ident_f": np.eye(128, dtype=np.float32),
            "bmask": bmask.reshape(8, 1024)}


def _t5_bucket(n):
    import math
    max_exact = 16
    nf = np.maximum(n, 1).astype(np.float32)
    large = max_exact + (np.log(nf / np.float32(max_exact)) / np.float32(math.log(128 / max_exact))
                         * np.float32(32 - max_exact)).astype(np.int32)
    large = np.minimum(large, 31)
    return np.where(n < max_exact, n, large)


_TZ_IDX = None


def _tz_table(rel_bias):
    global _TZ_IDX
    if _TZ_IDX is None:
        s_ = np.arange(128)[None, :, None]
        t_ = np.arange(128)[None, None, :]
        rel = np.arange(2)[:, None, None]
        dist = np.maximum(128 * rel + t_ - s_, 0)
        _TZ_IDX = _t5_bucket(dist)
    g = rel_bias[_TZ_IDX]
    return np.ascontiguousarray(np.transpose(g, (0, 1, 3, 2)).reshape(2, 128, 1024))


CONSTS = _make_consts()


def kernel(**inputs):
    inputs = {k: np.asarray(v) for k, v in inputs.items()}
    nc, _ = build()
    n = 8
    in_maps = [host_inputs(inputs, b) for b in range(n)]
    res = run_bass_kernel_spmd(nc, in_maps, core_ids=list(range(n)))
    out = np.stack([np.asarray(res.results[b]["out"], dtype=np.float32) for b in range(n)], axis=0)
    return out
```

```python
import contextlib
import numpy as np
import ml_dtypes
import concourse.bass as bass
import concourse.mybir as mybir
from concourse.bass_utils import run_bass_kernel_spmd

F32 = mybir.dt.float32
BF16 = mybir.dt.bfloat16
AF = mybir.ActivationFunctionType
ALU = mybir.AluOpType
AX = mybir.AxisListType

D = 1024
S = 8192
DEPTH = 2
NT = S // 128
NST = S // 512
HD = 64
NCOLS = 4548
NW1 = 2500
E = 32
FF = 1024
LN_EPS = 1e-5
RMS_EPS = 1e-6
DN_ALPHA = (2 * DEPTH) ** 0.25
TOPK = 256
NEG = -1.0e30
MBIG = -30000.0


class Res:
    __slots__ = ("wc", "wd", "rc", "rd", "name", "psum")

    def __init__(self, name="", psum=False):
        self.psum = psum
        self.wc = {}
        self.wd = []
        self.rc = {}
        self.rd = []
        self.name = name


class KB:
    COMPUTE = ("pe", "act", "dve", "pool")

    def __init__(self, nc, es):
        self.nc = nc
        self.es = es
        self.eng = {"pe": nc.tensor, "act": nc.scalar, "dve": nc.vector, "pool": nc.gpsimd, "sp": nc.sync}
        self.sem = {e: es.enter_context(nc.semaphore("s_" + e)) for e in self.COMPUTE}
        self.cnt = {e: 0 for e in self.COMPUTE}
        self.known = {e: {} for e in self.eng}
        self.NSD = 8
        self.dq = {}
        self.ninst = 0

    def _dq(self, q):
        if q not in self.dq:
            self.dq[q] = {"sems": [self.es.enter_context(self.nc.semaphore(f"d_{q}_{i}")) for i in range(self.NSD)],
                          "n": 0}
        return self.dq[q]

    def _wait(self, e, tok):
        if tok[0] == "c":
            _, e2, seq = tok
            if e == "pe" and e2 == "pe":
                return
            if self.known[e].get(e2, 0) >= seq:
                return
            self.eng[e].wait_ge(self.sem[e2], seq)
            self.known[e][e2] = seq
        else:
            _, q, slot, val = tok
            key = (q, slot)
            if self.known[e].get(key, 0) >= val:
                return
            self.eng[e].wait_ge(self.dq[q]["sems"][slot], val)
            self.known[e][key] = val
        self.ninst += 1

    def _deps(self, e, reads, writes):
        for r in reads:
            for e2, seq in r.wc.items():
                self._wait(e, ("c", e2, seq))
            for tok in r.wd:
                self._wait(e, tok)
            if r.psum:
                for e2, seq in r.rc.items():
                    if e2 != e:
                        self._wait(e, ("c", e2, seq))
        for w in writes:
            for e2, seq in w.wc.items():
                self._wait(e, ("c", e2, seq))
            for tok in w.wd:
                self._wait(e, tok)
            for e2, seq in w.rc.items():
                self._wait(e, ("c", e2, seq))
            for tok in w.rd:
                self._wait(e, tok)

    def op(self, e, fn, reads=(), writes=()):
        self._deps(e, reads, writes)
        ins = fn(self.eng[e])
        self.cnt[e] += 1
        seq = self.cnt[e]
        ins.then_inc(self.sem[e], 1)
        self.ninst += 1
        for r in reads:
            r.rc[e] = seq
        for w in writes:
            w.wc = {e: seq}
            w.wd = []
            w.rc = {}
            w.rd = []
        return ins

    def dma(self, q, out, in_, reads=(), writes=(), **kw):
        d = self._dq(q)
        k = d["n"]
        slot = k % self.NSD
        val = 16 * (k // self.NSD + 1)
        if k >= self.NSD:
            self._wait(q, ("d", q, slot, val - 16))
        self._deps(q, reads, writes)
        ins = self.eng[q].dma_start(out=out, in_=in_, **kw)
        ins.then_inc(d["sems"][slot], 16)
        d["n"] += 1
        self.ninst += 1
        tok = ("d", q, slot, val)
        for r in reads:
            r.rd.append(tok)
        for w in writes:
            w.wc = {}
            w.wd = [tok]
            w.rc = {}
            w.rd = []
        return ins

    def barrier(self, engines=None):
        engines = engines or list(self.eng)
        for e in engines:
            for e2 in self.COMPUTE:
                if self.cnt[e2] > 0:
                    self._wait(e, ("c", e2, self.cnt[e2]))
            for q, d in self.dq.items():
                n = d["n"]
                for k in range(max(0, n - self.NSD), n):
                    self._wait(e, ("d", q, k % self.NSD, 16 * (k // self.NSD + 1)))


class Ctx:
    pass


_uid = [0]


def T(C, ph, name, shape, dt):
    _uid[0] += 1
    h = ph.enter_context(C.nc.sbuf_tensor(f"{name}_{_uid[0]}", list(shape), dt))
    return h, Res(name)


def dram(C, name, shape, dt):
    return C.nc.dram_tensor(name, list(shape), dt, kind="Internal").ap(), Res(name)


def phase_ada(C):
    nc, kb = C.nc, C.kb
    with contextlib.ExitStack() as ph:
        ccol, r_ccol = T(C, ph, "ccol", [128, 8], F32)
        cond, r_cond = T(C, ph, "cond", [128, 8], F32)
        condB, r_condB = T(C, ph, "condB", [128, 8, 128], F32)
        wb = [T(C, ph, f"wada{i}", [128, 8, 512], F32) for i in range(2)]
        bB, r_bB = T(C, ph, "badaB", [128, 6144], F32)
        mo = [T(C, ph, f"mo{i}", [128, 512], F32) for i in range(2)]
        kb.dma("sp", ccol[:], C.c_col[:, :], writes=[r_ccol])
        kb.op("act", lambda e: e.activation(out=cond[:], in_=ccol[:], func=AF.Silu), reads=[r_ccol], writes=[r_cond])
        for kc in range(8):
            kb.op("dve", lambda e: e.tensor_copy(out=condB[:, kc, :], in_=cond[:, kc:kc + 1].to_broadcast([128, 128])),
                  reads=[r_cond], writes=[r_condB])
        it = 0
        for l in range(DEPTH):
            kb.dma("sp", bB[:], C.b_ada[l:l + 1, :].to_broadcast([128, 6144]), writes=[r_bB])
            for j in range(12):
                w, r_w = wb[it % 2]
                m, r_m = mo[it % 2]
                ps, r_ps = C.ps[it % 2]
                kb.dma("sp", w[:], C.w_ada[l, :, j * 512:(j + 1) * 512].rearrange("(kc p) n -> p kc n", p=128),
                       writes=[r_w])
                for kc in range(8):
                    kb.op("pe", lambda e: e.matmul(ps[:], lhsT=condB[:, kc, :], rhs=w[:, kc, :],
                                                   start=(kc == 0), stop=(kc == 7)),
                          reads=[r_condB, r_w], writes=[r_ps])
                plus1 = 1.0 if j in (2, 3, 8, 9) else 0.0
                kb.op("dve", lambda e: e.scalar_tensor_tensor(out=m[:], in0=ps[:], scalar=plus1,
                                                              in1=bB[:, j * 512:(j + 1) * 512],
                                                              op0=ALU.add, op1=ALU.add),
                      reads=[r_ps, r_bB], writes=[r_m])
                kb.dma("sp", C.mod_d[l, :, j * 512:(j + 1) * 512], m[:], reads=[r_m], writes=[C.r_mod])
                it += 1
        kb.barrier()


def dram(C, name, shape, dt):
    kind = "ExternalOutput" if name in C.dbg else "Internal"
    return C.nc.dram_tensor(name, list(shape), dt, kind=kind).ap(), Res(name)


class Rot:
    def __init__(self, items):
        self.items = list(items)
        self.i = 0

    def next(self):
        it = self.items[self.i % len(self.items)]
        self.i += 1
        return it


def bfv(ps):
    return ps[:].bitcast(BF16)


def ln_modulate(C, W, xt, r_xt, out_ap, r_out, scB, r_scB, shB, r_shB):
    kb = C.kb
    st_, r_st = W["stats"]
    mv, r_mv = W["mv"]
    sd, r_sd = W["sd"]
    rstd, r_rstd = W["rstd"]
    nmr, r_nmr = W["nmr"]
    xn, r_xn = W["xn"]
    epsT, r_eps = W["eps"]
    for c in range(2):
        kb.op("dve", lambda e: e.bn_stats(out=st_[:, c * 6:(c + 1) * 6], in_=xt[:, c * 512:(c + 1) * 512]),
              reads=[r_xt], writes=[r_st])
    kb.op("dve", lambda e: e.bn_aggr(out=mv[:], in_=st_[:]), reads=[r_st], writes=[r_mv])
    kb.op("act", lambda e: e.activation(out=sd[:], in_=mv[:, 1:2], func=AF.Sqrt, bias=epsT[:, 0:1], scale=1.0),
          reads=[r_mv, r_eps], writes=[r_sd])
    kb.op("dve", lambda e: e.reciprocal(out=rstd[:], in_=sd[:]), reads=[r_sd], writes=[r_rstd])
    kb.op("dve", lambda e: e.tensor_scalar(out=nmr[:], in0=mv[:, 0:1], scalar1=rstd[:, 0:1], scalar2=-1.0,
                                           op0=ALU.mult, op1=ALU.mult),
          reads=[r_mv, r_rstd], writes=[r_nmr])
    kb.op("act", lambda e: e.activation(out=xn[:], in_=xt[:], func=AF.Identity, bias=nmr[:, 0:1], scale=rstd[:, 0:1]),
          reads=[r_xt, r_rstd, r_nmr], writes=[r_xn])
    kb.op("pool", lambda e: e.tensor_tensor(out=xn[:], in0=xn[:], in1=scB, op=ALU.mult),
          reads=[r_xn, r_scB], writes=[r_xn])
    kb.op("dve", lambda e: e.tensor_tensor(out=out_ap, in0=xn[:], in1=shB, op=ALU.add),
          reads=[r_xn, r_shB], writes=[r_out])


def ln_work(C, ph, tag, eps):
    W = {
        "stats": T(C, ph, tag + "stats", [128, 12], F32),
        "mv": T(C, ph, tag + "mv", [128, 2], F32),
        "sd": T(C, ph, tag + "sd", [128, 1], F32),
        "rstd": T(C, ph, tag + "rstd", [128, 1], F32),
        "nmr": T(C, ph, tag + "nmr", [128, 1], F32),
        "xn": T(C, ph, tag + "xn", [128, 1024], F32),
        "eps": T(C, ph, tag + "eps", [128, 1], F32),
    }
    e_, r_e = W["eps"]
    C.kb.op("pool", lambda e: e.memset(e_[:], eps), writes=[r_e])
    return W


def phase_proj(C, l, x_src, r_xsrc):
    nc, kb = C.nc, C.kb
    with contextlib.ExitStack() as ph:
        W1, r_W1 = T(C, ph, "W1", [128, 8, NW1], BF16)
        Wkk, r_Wkk = T(C, ph, "Wkk", [128, 8, 128], BF16)
        wuk, r_wuk = T(C, ph, "wuk", [128, 4, 128], BF16)
        gkvB, r_gkvB = T(C, ph, "gkvB", [128, 128], F32)
        scB, r_scB = T(C, ph, "scB", [128, 1024], F32)
        shB, r_shB = T(C, ph, "shB", [128, 1024], F32)
        ident, r_id = T(C, ph, "ident", [128, 128], BF16)
        reps, r_reps = T(C, ph, "reps", [128, 1], F32)
        kb.op("pool", lambda e: e.memset(reps[:], RMS_EPS), writes=[r_reps])
        kb.dma("pool", W1[:], C.w_in[l, :, 0:NW1].rearrange("(kc p) n -> p kc n", p=128), writes=[r_W1])
        for hh in range(2):
            kb.dma("pool", Wkk[:, :, hh * 64:(hh + 1) * 64],
                   C.w_in[l, :, 896:960].rearrange("(kc p) n -> p kc n", p=128), writes=[r_Wkk])
        kb.dma("pool", wuk[:], C.w_uk_t[l].rearrange("(hp two) d r -> (two d) hp r", two=2), writes=[r_wuk])
        kb.dma("sp", gkvB[:], C.g_kv[l:l + 1, :].to_broadcast([128, 128]), writes=[r_gkvB])
        kb.dma("sp", scB[:], C.mod_d[l, :, 1024:2048], reads=[C.r_mod], writes=[r_scB])
        kb.dma("sp", shB[:], C.mod_d[l, :, 0:1024], reads=[C.r_mod], writes=[r_shB])
        kb.dma("sp", ident[:], C.ident_bf[:, :], writes=[r_id])
        LW = [ln_work(C, ph, f"lw{i}", LN_EPS) for i in range(2)]
        xb = [T(C, ph, f"xb{i}", [128, 1024], F32) for i in range(2)]
        hb = [T(C, ph, f"hb{i}", [128, 1024], BF16) for i in range(2)]
        hT = [(T(C, ph, f"hT{i}", [128, 8, 512], BF16)[0], [Res(f"hT{i}_{j}") for j in range(4)]) for i in range(2)]
        qaT = [T(C, ph, f"qaT{i}", [128, 512], BF16) for i in range(2)]
        def stage(name, shape, n):
            return [(T(C, ph, f"{name}{i}", shape, BF16)[0], [Res(f"{name}{i}_{j}") for j in range(n)]) for i in range(2)]
        s_qlat = stage("s_qlat", [128, 8, 512], 8)
        s_qidx = stage("s_qidx", [128, 2, 512], 2)
        s_kidx = stage("s_kidx", [128, 1, 512], 1)
        s_qb = stage("s_qb", [128, 4, 512], 4)
        s_kb = stage("s_kb", [128, 4, 512], 4)
        s_ckvT = stage("s_ckvT", [128, 4, 128], 4)
        s_ckv = stage("s_ckv", [128, 4, 128], 4)
        s_vb = stage("s_vb", [128, 4, 512], 4)
        s_wi = [(T(C, ph, f"s_wi{i}", [128, 4, 4], F32)[0], [Res(f"s_wi{i}_{j}") for j in range(4)]) for i in range(2)]
        ss_t = [T(C, ph, f"ss{i}", [128, 1], F32) for i in range(2)]
        sd2_t = [T(C, ph, f"sd2{i}", [128, 1], F32) for i in range(2)]
        rs_t = [T(C, ph, f"rs{i}", [128, 1], F32) for i in range(2)]
        junk, r_junk = T(C, ph, "junk", [128, 128], F32)
        pp = Rot(C.ps[0:6])
        psT, r_psT = C.ps[7]
        psT2, r_psT2 = C.ps[6]
        ev = Rot(["act", "dve"])

        def evac(eng, out_ap, in_ap, reads, writes, scale=None):
            if eng == "act":
                if scale is None:
                    kb.op("act", lambda e: e.copy(out=out_ap, in_=in_ap), reads=reads, writes=writes)
                else:
                    kb.op("act", lambda e: e.mul(out=out_ap, in_=in_ap, mul=scale), reads=reads, writes=writes)
            else:
                if scale is None:
                    kb.op("dve", lambda e: e.tensor_copy(out=out_ap, in_=in_ap), reads=reads, writes=writes)
                else:
                    kb.op("dve", lambda e: e.tensor_scalar(out=out_ap, in0=in_ap, scalar1=scale, scalar2=None,
                                                           op0=ALU.mult), reads=reads, writes=writes)

        for st in range(NST):
            sp_ = st % 2
            hTt, r_hT = hT[sp_]
            t0 = st * 512
            for j in range(4):
                tt = st * 4 + j
                xt, r_xt = xb[tt % 2]
                hbt, r_hb = hb[tt % 2]
                kb.dma("sp", xt[:], x_src[tt * 128:(tt + 1) * 128, :], reads=[r_xsrc], writes=[r_xt])
                ln_modulate(C, LW[tt % 2], xt, r_xt, hbt[:], r_hb, scB[:], r_scB, shB[:], r_shB)
                for kc in range(8):
                    kb.op("pe", lambda e: e.transpose(out=bfv(psT)[:, kc * 128:(kc + 1) * 128],
                                                      in_=hbt[:, kc * 128:(kc + 1) * 128], identity=ident[:]),
                          reads=[r_hb, r_id], writes=[r_psT])
                evac(ev.next(), hTt[:, :, j * 128:(j + 1) * 128],
                     bfv(psT).rearrange("p (kc t) -> p kc t", kc=8), [r_psT], [r_hT[j]])
            for j in range(4):
                tt = st * 4 + j
                ps, r_ps = pp.next()
                for (c0, c1, o0) in ((512, 640, 0), (960, 964, 128)):
                    for kc in range(8):
                        kb.op("pe", lambda e: e.matmul(ps[:, o0:o0 + (c1 - c0)], lhsT=hTt[:, kc, j * 128:(j + 1) * 128],
                                                       rhs=W1[:, kc, c0:c1], start=(kc == 0), stop=(kc == 7)),
                              reads=[r_hT[j], r_W1], writes=[r_ps])
                ss, r_ss = ss_t[tt % 2]
                sd2, r_sd2 = sd2_t[tt % 2]
                rs, r_rs = rs_t[tt % 2]
                kb.op("act", lambda e: e.activation(out=junk[:], in_=ps[:, 0:128], func=AF.Square, accum_out=ss[:]),
                      reads=[r_ps], writes=[r_junk, r_ss])
                kb.op("act", lambda e: e.activation(out=sd2[:], in_=ss[:], func=AF.Sqrt, bias=reps[:, 0:1],
                                                    scale=1.0 / 128.0), reads=[r_ss, r_reps], writes=[r_sd2])
                kb.op("dve", lambda e: e.reciprocal(out=rs[:], in_=sd2[:]), reads=[r_sd2], writes=[r_rs])
                ckv_t, r_ckv = s_ckv[sp_]
                kb.op("dve", lambda e: e.scalar_tensor_tensor(out=ckv_t[:, j, :], in0=ps[:, 0:128], scalar=rs[:, 0:1],
                                                              in1=gkvB[:], op0=ALU.mult, op1=ALU.mult),
                      reads=[r_ps, r_rs, r_gkvB], writes=[r_ckv[j]])
                wi_t, r_wi = s_wi[sp_]
                kb.op("dve", lambda e: e.tensor_scalar(out=wi_t[:, j, :], in0=ps[:, 128:132], scalar1=1.0 / 16.0,
                                                       scalar2=None, op0=ALU.mult), reads=[r_ps], writes=[r_wi[j]])
                kb.op("pe", lambda e: e.transpose(out=bfv(psT2)[:, j * 128:(j + 1) * 128], in_=ckv_t[:, j, :],
                                                  identity=ident[:]), reads=[r_ckv[j], r_id], writes=[r_psT2])
                ps, r_ps = pp.next()
                for kc in range(8):
                    kb.op("pe", lambda e: e.matmul(ps[:], lhsT=hTt[:, kc, j * 128:(j + 1) * 128],
                                                   rhs=W1[:, kc, 1988:2500], start=(kc == 0), stop=(kc == 7)),
                          reads=[r_hT[j], r_W1], writes=[r_ps])
                vb_t, r_vb = s_vb[sp_]
                evac(ev.next(), vb_t[:, j, :], ps[:], [r_ps], [r_vb[j]])
            ckvT_t, r_ckvT = s_ckvT[sp_]
            evac(ev.next(), ckvT_t[:].rearrange("p j t -> p (j t)"), bfv(psT2)[:, 0:512], [r_psT2], r_ckvT)
            rows = slice(t0, t0 + 512)
            kb.dma("sp", C.ckv_d[rows, :].rearrange("(j p) r -> p j r", p=128), ckv_t[:], reads=r_ckv, writes=[C.r_ckv])
            kb.dma("sp", C.widx_d[rows, :].rearrange("(j p) r -> p j r", p=128), wi_t[:], reads=r_wi, writes=[C.r_widx])
            kb.dma("sp", C.vb_d[rows, :].rearrange("(j p) r -> p j r", p=128), vb_t[:], reads=r_vb, writes=[C.r_vb])
            kb.dma("sp", C.ckvT_d[:, rows], ckvT_t[:].rearrange("p j t -> p (j t)"), reads=r_ckvT, writes=[C.r_ckvT])
            allj = r_hT
            groups = [("qa", 0, 4), ("qidx", 640, 2), ("kidx", None, 1), ("qb", 964, 4), ("kb", 1476, 4)]
            for (gname, c0, nch) in groups:
                for c in range(nch):
                    ps, r_ps = pp.next()
                    for kc in range(8):
                        if gname == "kidx":
                            lw, rl = Wkk[:, kc, :], r_Wkk
                        else:
                            lw, rl = W1[:, kc, c0 + c * 128:c0 + (c + 1) * 128], r_W1
                        kb.op("pe", lambda e: e.matmul(ps[:], lhsT=lw, rhs=hTt[:, kc, :], start=(kc == 0), stop=(kc == 7)),
                              reads=allj + [rl], writes=[r_ps])
                    if gname == "qa":
                        qa, r_qa = qaT[c % 2]
                        evac(ev.next(), qa[:], ps[:], [r_ps], [r_qa])
                        for hp in range(2):
                            h = 2 * c + hp
                            ps2, r_ps2 = pp.next()
                            kb.op("pe", lambda e: e.matmul(ps2[:], lhsT=wuk[hp * 64:(hp + 1) * 64, c, :],
                                                           rhs=qa[hp * 64:(hp + 1) * 64, :], start=True, stop=True),
                                  reads=[r_wuk, r_qa], writes=[r_ps2])
                            stg, r_stg = s_qlat[sp_]
                            evac(ev.next(), stg[:, h, :], ps2[:], [r_ps2], [r_stg[h]], scale=0.125)
                    else:
                        stg, r_stg = {"qidx": s_qidx, "kidx": s_kidx, "qb": s_qb, "kb": s_kb}[gname][sp_]
                        evac(ev.next(), stg[:, c, :], ps[:], [r_ps], [r_stg[c]], scale=(0.125 if gname == "kb" else None))
            cols = slice(t0, t0 + 512)
            stg, r_stg = s_qlat[sp_]
            kb.dma("sp", C.qlatT_d[:, :, cols].rearrange("h p t -> p h t"), stg[:], reads=r_stg, writes=[C.r_qlatT])
            stg, r_stg = s_qidx[sp_]
            kb.dma("sp", C.qidxT_d[:, :, cols].rearrange("h p t -> p h t"), stg[:], reads=r_stg, writes=[C.r_qidxT])
            stg, r_stg = s_kidx[sp_]
            kb.dma("sp", C.kidxT_d[:, cols], stg[:, 0, :], reads=r_stg, writes=[C.r_kidxT])
            stg, r_stg = s_qb[sp_]
            kb.dma("sp", C.qbT_d[:, :, cols].rearrange("h p t -> p h t"), stg[:], reads=r_stg, writes=[C.r_qbT])
            stg, r_stg = s_kb[sp_]
            kb.dma("sp", C.kbT_d[:, :, cols].rearrange("h p t -> p h t"), stg[:], reads=r_stg, writes=[C.r_kbT])
        kb.barrier()


def phase_sb(C, l):
    nc, kb = C.nc, C.kb
    with contextlib.ExitStack() as ph:
        negU, r_negU = T(C, ph, "negU", [128, 128], BF16)
        ones, r_ones = T(C, ph, "ones", [128, 128], BF16)
        cm, r_cm = T(C, ph, "cm", [128, 4, 512], BF16)
        kb.dma("sp", negU[:], C.negU_bf[:, :], writes=[r_negU])
        kb.dma("sp", ones[:], C.ones_bf[:, :], writes=[r_ones])
        kb.dma("sp", cm[:], C.cm_bf.rearrange("r p t -> p r t"), writes=[r_cm])
        qT = [T(C, ph, f"sbq{i}", [128, S], BF16) for i in range(2)]
        kT = [T(C, ph, f"sbk{i}", [128, S], BF16) for i in range(2)]
        vv = [T(C, ph, f"sbv{i}", [128, NT, 128], BF16) for i in range(2)]
        et = [T(C, ph, f"et{i}", [128, 512], F32) for i in range(2)]
        spT = [T(C, ph, f"spT{i}", [128, 512], BF16) for i in range(4)]
        arg2 = [T(C, ph, f"arg2{i}", [128, 512], F32) for i in range(2)]
        AT = [T(C, ph, f"AT{i}", [128, 512], BF16) for i in range(4)]
        carry = [T(C, ph, f"carry{i}", [128, 512], F32) for i in range(2)]
        ostg = [T(C, ph, f"ostg{i}", [128, 512], BF16) for i in range(2)]
        psz = C.ps[0:2]
        psa = C.ps[2:4]
        psc = C.ps[4:5]
        pso = C.ps[5:7]

        items = []
        gid = 0
        for c in range(4):
            for qi in range(NST):
                for hp in range(2):
                    nb = 4 * qi + 4
                    for j in range(nb - 1, -1, -1):
                        items.append(dict(c=c, qi=qi, hp=hp, j=j, first=(j == nb - 1), last=(j == 0), g=gid,
                                          rel=(j - 4 * qi)))
                    gid += 1
        loaded = set()

        def load_pair(c):
            if c in loaded or c >= 4:
                return
            loaded.add(c)
            q, r_q = qT[c % 2]
            k, r_k = kT[c % 2]
            v, r_v = vv[c % 2]
            kb.dma("sp", q[:], C.qbT_d[c], reads=[C.r_qbT], writes=[r_q])
            kb.dma("sp", k[:], C.kbT_d[c], reads=[C.r_kbT], writes=[r_k])
            kb.dma("sp", v[:], C.vb_d[:, c * 128:(c + 1) * 128].rearrange("(j p) d -> p j d", p=128),
                   reads=[C.r_vb], writes=[r_v])

        def stA(n, it):
            c, qi, hp, j = it["c"], it["qi"], it["hp"], it["j"]
            q, r_q = qT[c % 2]
            k, r_k = kT[c % 2]
            P = slice(hp * 64, hp * 64 + 64)
            pz, r_pz = psz[n % 2]
            kb.op("pe", lambda e: e.matmul(pz[:], lhsT=k[P, j * 128:(j + 1) * 128], rhs=q[P, qi * 512:(qi + 1) * 512],
                                           start=True, stop=True), reads=[r_k, r_q], writes=[r_pz])
            e_, r_e = et[n % 2]
            kb.op("act", lambda e: e.activation(out=e_[:], in_=pz[:], func=AF.Exp), reads=[r_pz], writes=[r_e])

        def stA2(n, it):
            e_, r_e = et[n % 2]
            s_, r_s = spT[n % 4]
            kb.op("act", lambda e: e.activation(out=s_[:], in_=e_[:], func=AF.Ln, bias=1.0, scale=1.0),
                  reads=[r_e], writes=[r_s])
            if it["rel"] >= 0:
                kb.op("dve", lambda e: e.tensor_tensor(out=s_[:], in0=s_[:], in1=cm[:, it["rel"], :], op=ALU.mult),
                      reads=[r_s, r_cm], writes=[r_s])

        def stB(n, it):
            c, qi, hp, j = it["c"], it["qi"], it["hp"], it["j"]
            q, r_q = qT[c % 2]
            k, r_k = kT[c % 2]
            P = slice(hp * 64, hp * 64 + 64)
            s_, r_s = spT[n % 4]
            pa, r_pa = psa[n % 2]
            pc, r_pc = psc[0]
            kb.op("pe", lambda e: e.matmul(pa[:], lhsT=k[P, j * 128:(j + 1) * 128], rhs=q[P, qi * 512:(qi + 1) * 512],
                                           start=True, stop=False), reads=[r_k, r_q], writes=[r_pa])
            kb.op("pe", lambda e: e.matmul(pa[:], lhsT=negU[:], rhs=s_[:], start=False, stop=True),
                  reads=[r_negU, r_s], writes=[r_pa])
            kk = n % 2
            cb, r_cb = carry[kk]
            cbn, r_cbn = carry[1 - kk]
            if it["first"]:
                kb.op("dve", lambda e: e.memset(cb[:], 0.0), writes=[r_cb])
            if not it["last"]:
                kb.op("pe", lambda e: e.matmul(pc[:], lhsT=ones[:], rhs=s_[:], start=True, stop=True),
                      reads=[r_ones, r_s], writes=[r_pc])
                kb.op("dve", lambda e: e.tensor_tensor(out=cbn[:], in0=pc[:], in1=cb[:], op=ALU.add),
                      reads=[r_pc, r_cb], writes=[r_cbn])
            a2, r_a2 = arg2[n % 2]
            kb.op("dve", lambda e: e.tensor_tensor(out=a2[:], in0=pa[:], in1=cb[:], op=ALU.subtract),
                  reads=[r_pa, r_cb], writes=[r_a2])

        def stB2(n, it):
            a2, r_a2 = arg2[n % 2]
            a_, r_a = AT[n % 4]
            kb.op("act", lambda e: e.activation(out=a_[:], in_=a2[:], func=AF.Exp), reads=[r_a2], writes=[r_a])
            if it["rel"] >= 0:
                kb.op("pool", lambda e: e.tensor_tensor(out=a_[:], in0=a_[:], in1=cm[:, it["rel"], :], op=ALU.mult),
                      reads=[r_a, r_cm], writes=[r_a])

        def stC(n, it):
            c, qi, hp, j = it["c"], it["qi"], it["hp"], it["j"]
            v, r_v = vv[c % 2]
            a_, r_a = AT[n % 4]
            po, r_po = pso[(c * NST + qi) % 2]
            kb.op("pe", lambda e: e.matmul(po[hp * 64:(hp + 1) * 64, :], lhsT=v[:, j, hp * 64:(hp + 1) * 64], rhs=a_[:],
                                           start=it["first"], stop=it["last"]), reads=[r_v, r_a], writes=[r_po])
            if it["last"] and hp == 1:
                og, r_og = ostg[(c * NST + qi) % 2]
                kb.op("dve", lambda e: e.tensor_copy(out=og[:], in_=po[:]), reads=[r_po], writes=[r_og])
                kb.dma("sp", C.obT_d[c, :, qi * 512:(qi + 1) * 512], og[:], reads=[r_og], writes=[C.r_obT])
                if qi == NST - 1:
                    load_pair(c + 2)

        N = len(items)
        load_pair(0)
        load_pair(1)
        for n in range(N + 3):
            if n < N:
                stA(n, items[n])
            if 0 <= n - 2 < N:
                stB2(n - 2, items[n - 2])
            if n < N:
                stA2(n, items[n])
            if 0 <= n - 1 < N:
                stB(n - 1, items[n - 1])
            if 0 <= n - 3 < N:
                stC(n - 3, items[n - 3])
        kb.barrier()


def phase_dsa(C, l):
    nc, kb = C.nc, C.kb
    with contextlib.ExitStack() as ph:
        kidxT, r_kidxT = T(C, ph, "kidxT", [128, S], BF16)
        ckvT, r_ckvT = T(C, ph, "ckvT", [128, S], BF16)
        ckv, r_ckv = T(C, ph, "ckv", [128, NT, 128], BF16)
        wuv, r_wuv = T(C, ph, "wuv", [128, 8, 64], BF16)
        sc, r_sc = T(C, ph, "sc", [128, S], F32)
        mb, r_mb = T(C, ph, "mb", [128, S], BF16)
        dmask, r_dmask = T(C, ph, "dmask", [128, 128], F32)
        Irep, r_Irep = T(C, ph, "Irep", [128, 512], BF16)
        onesF, r_onesF = T(C, ph, "onesF", [128, 128], BF16)
        identf, r_identf = T(C, ph, "identf", [128, 128], F32)
        Tz, r_Tz = T(C, ph, "Tz", [128, 2, 1024], F32)
        b31B, r_b31B = T(C, ph, "b31B", [128, 8], F32)
        bmask, r_bmask = T(C, ph, "bmask", [8, 1024], F32)
        rbT, r_rbT = T(C, ph, "rbT", [8, 32], F32)
        c8, r_c8 = T(C, ph, "c8", [8, 1], F32)
        b31c, r_b31c = T(C, ph, "b31c", [40, 1], F32)
        b31hi, r_b31hi = T(C, ph, "b31hi", [40, 1], BF16)
        b31hif, r_b31hif = T(C, ph, "b31hif", [40, 1], F32)
        b31lo, r_b31lo = T(C, ph, "b31lo", [72, 1], F32)
        CB, r_CB = T(C, ph, "CB", [72, 1024], BF16)
        CBd = Res("CBdyn")
        kb.dma("sp", kidxT[:], C.kidxT_d[:, :], reads=[C.r_kidxT], writes=[r_kidxT])
        kb.dma("sp", ckvT[:], C.ckvT_d[:, :], reads=[C.r_ckvT], writes=[r_ckvT])
        kb.dma("sp", ckv[:], C.ckv_d.rearrange("(j p) r -> p j r", p=128), reads=[C.r_ckv], writes=[r_ckv])
        kb.dma("pool", wuv[:], C.w_uv[l], writes=[r_wuv])
        kb.dma("sp", dmask[:], C.dmask[:, :], writes=[r_dmask])
        kb.dma("sp", Irep[:], C.irep_bf[:, :], writes=[r_Irep])
        kb.dma("sp", onesF[:], C.ones_bf[:, :], writes=[r_onesF])
        kb.dma("sp", identf[:], C.ident_f[:, :], writes=[r_identf])
        kb.dma("sp", Tz[:], C.tz.rearrange("r p c -> p r c"), writes=[r_Tz])
        kb.dma("sp", b31B[:], C.rel_bias[31:32, :].to_broadcast([128, 8]), writes=[r_b31B])
        kb.dma("sp", bmask[:], C.bmask[:, :], writes=[r_bmask])
        kb.dma("sp", rbT[:], C.rel_bias_t[:, :], writes=[r_rbT])
        for r_ in range(2):
            kb.op("dve", lambda e: e.tensor_tensor(out=Tz[:, r_, :].rearrange("p (h t) -> p h t", h=8),
                                                   in0=Tz[:, r_, :].rearrange("p (h t) -> p h t", h=8),
                                                   in1=b31B[:].to_broadcast([128, 8, 128]) if False else
                                                   b31B[:].unsqueeze(2).to_broadcast([128, 8, 128]),
                                                   op=ALU.subtract), reads=[r_Tz, r_b31B], writes=[r_Tz])
        kb.op("dve", lambda e: e.reduce_max(out=c8[:], in_=rbT[:], axis=AX.X), reads=[r_rbT], writes=[r_c8])
        kb.op("dve", lambda e: e.tensor_scalar(out=c8[:], in0=c8[:], scalar1=-1.0, scalar2=None, op0=ALU.mult),
              reads=[r_c8], writes=[r_c8])
        kb.op("pool", lambda e: e.memset(CB[:], 0.0), writes=[r_CB])
        kb.dma("sp", b31c[32:40, :], C.rel_bias_t[:, 31:32], writes=[r_b31c], allow_slow_non_contiguous=True)
        kb.dma("sp", b31lo[64:72, :], C.rel_bias_t[:, 31:32], writes=[r_b31lo], allow_slow_non_contiguous=True)
        kb.op("dve", lambda e: e.tensor_copy(out=b31hi[32:40, :], in_=b31c[32:40, :]), reads=[r_b31c], writes=[r_b31hi])
        kb.op("dve", lambda e: e.tensor_copy(out=b31hif[32:40, :], in_=b31hi[32:40, :]), reads=[r_b31hi], writes=[r_b31hif])
        kb.dma("sp", b31lo[32:40, :], b31hif[32:40, :], reads=[r_b31hif], writes=[r_b31lo])
        bm32, r_bm32 = T(C, ph, "bm32", [72, 1024], F32)
        kb.dma("sp", bm32[32:40, :], C.bmask[:, :], writes=[r_bm32])
        kb.dma("sp", bm32[64:72, :], C.bmask[:, :], writes=[r_bm32])
        kb.op("dve", lambda e: e.tensor_scalar(out=CB[32:40, :], in0=bm32[32:40, :], scalar1=b31hif[32:40, 0:1],
                                               scalar2=None, op0=ALU.mult), reads=[r_bm32, r_b31hif], writes=[r_CB])
        hi64, r_hi64 = T(C, ph, "hi64", [72, 1], F32)
        kb.dma("sp", hi64[64:72, :], b31hif[32:40, :], reads=[r_b31hif], writes=[r_hi64])
        kb.op("dve", lambda e: e.tensor_tensor(out=b31lo[64:72, :], in0=b31lo[64:72, :], in1=hi64[64:72, :], op=ALU.subtract),
              reads=[r_b31lo, r_hi64], writes=[r_b31lo])
        kb.op("dve", lambda e: e.tensor_scalar(out=CB[64:72, :], in0=bm32[64:72, :], scalar1=b31lo[64:72, 0:1],
                                               scalar2=None, op0=ALU.mult), reads=[r_bm32, r_b31lo], writes=[r_CB])

        CB2, r_CB2 = T(C, ph, "CB2", [72, 1024], BF16)
        kb.op("dve", lambda e: e.tensor_copy(out=CB2[:], in_=CB[:]), reads=[r_CB], writes=[r_CB2])
        CB3, r_CB3 = T(C, ph, "CB3", [72, 1024], BF16)
        kb.op("dve", lambda e: e.tensor_copy(out=CB3[:], in_=CB[:]), reads=[r_CB], writes=[r_CB3])
        CBs = [(CB, r_CB, Res("CBdyn0")), (CB2, r_CB2, Res("CBdyn1")), (CB3, r_CB3, Res("CBdyn2"))]
        g8, r_g8 = T(C, ph, "g8", [8, 128], F32)
        negK, r_negK = T(C, ph, "negK", [8, 1], F32)
        kb.dma("sp", g8[:], C.g_kv[l:l + 1, :].to_broadcast([8, 128]), writes=[r_g8])
        kb.op("dve", lambda e: e.tensor_reduce(out=negK[:], in_=g8[:], axis=AX.X, op=ALU.max, apply_absolute_value=True),
              reads=[r_g8], writes=[r_negK])
        kb.op("dve", lambda e: e.tensor_scalar(out=negK[:], in0=negK[:], scalar1=-1.02 * (128.0 ** 0.5), scalar2=None,
                                               op0=ALU.mult), reads=[r_negK], writes=[r_negK])
        sc2, r_sc2 = T(C, ph, "sc2", [128, S], F32)
        mb2, r_mb2 = T(C, ph, "mb2", [128, S], BF16)
        scs = [(sc, r_sc), (sc2, r_sc2)]
        mbs = [(mb, r_mb), (mb2, r_mb2)]
        qi_t = [T(C, ph, f"qi{i}", [128, 2, 128], BF16) for i in range(3)]
        wi_t = [T(C, ph, f"wi{i}", [128, 4], F32) for i in range(3)]
        ql_t = [T(C, ph, f"ql{i}", [128, 1024], BF16) for i in range(3)]
        qsq, r_qsq = T(C, ph, "qsq", [128, 1024], BF16)
        sq8, r_sq8 = T(C, ph, "sq8", [8, 1024], F32)
        rl = [T(C, ph, f"rl{i}", [128, 512], F32) for i in range(4)]
        rtmp, r_rtmp = T(C, ph, "rtmp", [128, 512], F32)
        m8, r_m8 = T(C, ph, "m8", [128, 8], F32)
        zt = [T(C, ph, f"zt{i}", [128, 512], F32) for i in range(2)]
        pT = [T(C, ph, f"pT{i}", [128, 512], BF16) for i in range(3)]
        rden, r_rden = T(C, ph, "rden", [128, 512], F32)
        olT, r_olT = T(C, ph, "olT", [128, 1024], BF16)
        oast = [T(C, ph, f"oast{i}", [128, 4, 128], BF16) for i in range(2)]
        psA = Rot(C.ps[0:2])
        psZ = C.ps[2:4]
        psO = C.ps[4:6]
        psD = C.ps[6:8]
        nctr = [0]

        def chunks_of(i):
            nk = (i + 1) * 128
            return [(c0, min(512, nk - c0)) for c0 in range(0, nk, 512)]

        def stL(i):
            tcols = slice(i * 128, (i + 1) * 128)
            qi, r_qi = qi_t[i % 3]
            wi, r_wi = wi_t[i % 3]
            ql, r_ql = ql_t[i % 3]
            kb.dma("sp", qi[:], C.qidxT_d[:, :, tcols].rearrange("c p t -> p c t"), reads=[C.r_qidxT], writes=[r_qi])
            kb.dma("sp", wi[:], C.widx_d[tcols, :], reads=[C.r_widx], writes=[r_wi])
            kb.dma("sp", ql[:].rearrange("p (h t) -> p h t", h=8), C.qlatT_d[:, :, tcols].rearrange("h p t -> p h t"),
                   reads=[C.r_qlatT], writes=[r_ql])
            cb, r_cb, r_cbd = CBs[i % 3]
            kb.op("dve", lambda e: e.tensor_tensor(out=qsq[:], in0=ql[:], in1=ql[:], op=ALU.mult),
                  reads=[r_ql], writes=[r_qsq])
            ps, r_ps = psA.next()
            ps2, r_ps2 = psA.next()
            for half, (p_, r_p) in enumerate(((ps, r_ps), (ps2, r_ps2))):
                kb.op("pe", lambda e: e.matmul(p_[0:8, :], lhsT=onesF[:, 0:8], rhs=qsq[:, half * 512:(half + 1) * 512],
                                               start=True, stop=True), reads=[r_onesF, r_qsq], writes=[r_p])
                kb.op("act", lambda e: e.activation(out=sq8[:, half * 512:(half + 1) * 512], in_=p_[0:8, :], func=AF.Sqrt),
                      reads=[r_p], writes=[r_sq8])
            kb.op("dve", lambda e: e.tensor_scalar(out=sq8[:], in0=sq8[:], scalar1=negK[:, 0:1], scalar2=c8[:, 0:1],
                                                    op0=ALU.mult, op1=ALU.add), reads=[r_sq8, r_negK, r_c8], writes=[r_sq8])
            kb.op("dve", lambda e: e.tensor_tensor(out=cb[0:8, :], in0=sq8[:], in1=bmask[:], op=ALU.mult),
                  reads=[r_sq8, r_bmask, r_cb], writes=[r_cbd])

        def stA(i):
            tcols = slice(i * 128, (i + 1) * 128)
            qi, r_qi = qi_t[i % 3]
            wi, r_wi = wi_t[i % 3]
            sc_, r_sc_ = scs[i % 2]
            for (c0, w) in chunks_of(i):
                for h in range(4):
                    P = slice((h % 2) * 64, (h % 2) * 64 + 64)
                    ps, r_ps = psA.next()
                    kb.op("pe", lambda e: e.matmul(ps[:, 0:w], lhsT=qi[P, h // 2, :], rhs=kidxT[P, c0:c0 + w],
                                                   start=True, stop=True), reads=[r_qi, r_kidxT], writes=[r_ps])
                    r_, r_r = rl[h]
                    kb.op("act", lambda e: e.activation(out=r_[:, 0:w], in_=ps[:, 0:w], func=AF.Relu),
                          reads=[r_ps], writes=[r_r])
                    if h == 0:
                        kb.op("dve", lambda e: e.tensor_scalar(out=sc_[:, c0:c0 + w], in0=r_[:, 0:w], scalar1=wi[:, 0:1],
                                                               scalar2=None, op0=ALU.mult),
                              reads=[r_r, r_wi], writes=[r_sc_])
                    else:
                        kb.op("dve", lambda e: e.scalar_tensor_tensor(out=sc_[:, c0:c0 + w], in0=r_[:, 0:w],
                                                                      scalar=wi[:, h:h + 1], in1=sc_[:, c0:c0 + w],
                                                                      op0=ALU.mult, op1=ALU.add),
                              reads=[r_r, r_wi, r_sc_], writes=[r_sc_])
            kb.op("dve", lambda e: e.tensor_tensor(out=sc_[:, tcols], in0=sc_[:, tcols], in1=dmask[:], op=ALU.add),
                  reads=[r_sc_, r_dmask], writes=[r_sc_])

        def stB(i):
            nk = (i + 1) * 128
            sc_, r_sc_ = scs[i % 2]
            mb_, r_mb_ = mbs[i % 2]
            if i >= 2:
                for rnd in range(TOPK // 8):
                    kb.op("dve", lambda e: e.max(out=m8[:], in_=sc_[:, 0:nk]), reads=[r_sc_], writes=[r_m8])
                    kb.op("dve", lambda e: e.match_replace(out=sc_[:, 0:nk], in_to_replace=m8[:], in_values=sc_[:, 0:nk],
                                                           imm_value=2.0 * NEG), reads=[r_sc_, r_m8], writes=[r_sc_])
                kb.op("dve", lambda e: e.tensor_scalar(out=mb_[:, 0:nk], in0=sc_[:, 0:nk], scalar1=1.5 * NEG, scalar2=MBIG,
                                                       op0=ALU.is_gt, op1=ALU.mult), reads=[r_sc_], writes=[r_mb_])
            else:
                kb.op("dve", lambda e: e.tensor_scalar(out=mb_[:, 0:nk], in0=sc_[:, 0:nk], scalar1=0.5 * NEG, scalar2=MBIG,
                                                       op0=ALU.is_le, op1=ALU.mult), reads=[r_sc_], writes=[r_mb_])

        def stF(i, jbs):
            ql, r_ql = ql_t[i % 3]
            mb_, r_mb_ = mbs[i % 2]
            cb, r_cb, r_cbd = CBs[i % 3]
            items = [(half, jb) for half in range(2) for jb in jbs]
            first_jb, last_jb = i, 0

            def fa(n, it):
                half, jb = it
                hc = slice(half * 512, (half + 1) * 512)
                pz, r_pz = psZ[n % 2]
                kb.op("pe", lambda e: e.matmul(pz[:], lhsT=ckvT[:, jb * 128:(jb + 1) * 128], rhs=ql[:, hc],
                                               start=True, stop=False), reads=[r_ckvT, r_ql], writes=[r_pz])
                kb.op("pe", lambda e: e.matmul(pz[:], lhsT=mb_[:, jb * 128:(jb + 1) * 128], rhs=Irep[:],
                                               start=False, stop=False), reads=[r_mb_, r_Irep], writes=[r_pz])
                kb.op("pe", lambda e: e.matmul(pz[:], lhsT=onesF[0:72, :], rhs=cb[0:72, hc], start=False, stop=True),
                      reads=[r_onesF, r_cb, r_cbd], writes=[r_pz])
                p_, r_p = pT[n % 3]
                rel = i - jb
                if rel <= 1:
                    z_, r_z = zt[n % 2]
                    kb.op("dve", lambda e: e.tensor_tensor(out=z_[:], in0=pz[:], in1=Tz[:, rel, hc], op=ALU.add),
                          reads=[r_pz, r_Tz], writes=[r_z])
                    kb.op("act", lambda e: e.activation(out=p_[:], in_=z_[:], func=AF.Exp), reads=[r_z], writes=[r_p])
                else:
                    kb.op("act", lambda e: e.activation(out=p_[:], in_=pz[:], func=AF.Exp), reads=[r_pz], writes=[r_p])

            def fb(n, it):
                half, jb = it
                p_, r_p = pT[n % 3]
                po, r_po = psO[half]
                pd, r_pd = psD[half]
                kb.op("pe", lambda e: e.matmul(po[:], lhsT=ckv[:, jb, :], rhs=p_[:], start=(jb == first_jb), stop=(jb == last_jb)),
                      reads=[r_ckv, r_p], writes=[r_po])
                kb.op("pe", lambda e: e.matmul(pd[:], lhsT=onesF[:], rhs=p_[:], start=(jb == first_jb), stop=(jb == last_jb)),
                      reads=[r_onesF, r_p], writes=[r_pd])

            N = len(items)
            base = nctr[0]
            for n in range(N + 1):
                if n < N:
                    fa(base + n, items[n])
                if n >= 1:
                    fb(base + n - 1, items[n - 1])
            nctr[0] += N

        def stT(i):
            tcols = slice(i * 128, (i + 1) * 128)
            for half in range(2):
                hc = slice(half * 512, (half + 1) * 512)
                po, r_po = psO[half]
                pd, r_pd = psD[half]
                kb.op("dve", lambda e: e.reciprocal(out=rden[:], in_=pd[:]), reads=[r_pd], writes=[r_rden])
                kb.op("dve", lambda e: e.tensor_tensor(out=olT[:, hc], in0=po[:], in1=rden[:], op=ALU.mult),
                      reads=[r_po, r_rden], writes=[r_olT])
            ps, r_ps = psA.next()
            for h in range(8):
                kb.op("pe", lambda e: e.matmul(ps[(h % 2) * 64:(h % 2) * 64 + 64, (h // 2) * 128:(h // 2 + 1) * 128],
                                               lhsT=wuv[:, h, :], rhs=olT[:, h * 128:(h + 1) * 128], start=True, stop=True),
                      reads=[r_wuv, r_olT], writes=[r_ps])
            og, r_og = oast[i % 2]
            kb.op("act", lambda e: e.copy(out=og[:].rearrange("p c t -> p (c t)"), in_=ps[:]), reads=[r_ps], writes=[r_og])
            kb.dma("sp", C.oaT_d[:, :, tcols].rearrange("c p t -> p c t"), og[:], reads=[r_og], writes=[C.r_oaT])

        stL(0)
        stL(1)
        stA(0)
        stB(0)
        stA(1)
        for i in range(NT):
            if i + 2 < NT:
                stL(i + 2)
            stF(i, [jb for jb in (i, i - 1) if jb >= 0])
            if i + 2 < NT:
                stA(i + 2)
            if i + 1 < NT:
                stB(i + 1)
            if i >= 2:
                stF(i, list(range(i - 2, -1, -1)))
            stT(i)
        kb.barrier()


def bload(C, ph, name, src_ap, n, reads=()):
    t, r = T(C, ph, name, [128, n], F32)
    C.kb.dma("sp", t[:], src_ap, reads=list(reads), writes=[r])
    return t, r


def phase_out(C, l, x_src, r_xsrc):
    nc, kb = C.nc, C.kb
    with contextlib.ExitStack() as ph:
        Wg, r_Wg = T(C, ph, "Wg", [128, 8, 2048], BF16)
        wao, r_wao = T(C, ph, "wao", [128, 4, 1024], BF16)
        wbo, r_wbo = T(C, ph, "wbo", [128, 4, 1024], BF16)
        wo, r_wo = T(C, ph, "wo", [128, 8, 1024], BF16)
        wr, r_wr = T(C, ph, "wr", [128, 8, 32], F32)
        kb.dma("pool", Wg[:], C.w_in[l, :, NW1:NCOLS].rearrange("(kc p) n -> p kc n", p=128), writes=[r_Wg])
        kb.dma("pool", wao[:], C.w_a_out[l].rearrange("(kc p) n -> p kc n", p=128), writes=[r_wao])
        kb.dma("pool", wbo[:], C.w_b_out[l].rearrange("(kc p) n -> p kc n", p=128), writes=[r_wbo])
        kb.dma("pool", wo[:], C.w_o[l].rearrange("(kc p) n -> p kc n", p=128), writes=[r_wo])
        kb.dma("sp", wr[:], C.w_router[l].rearrange("(kc p) n -> p kc n", p=128), writes=[r_wr])
        sc1, r_sc1 = bload(C, ph, "sc1", C.mod_d[l, :, 1024:2048], 1024, [C.r_mod])
        sh1, r_sh1 = bload(C, ph, "sh1", C.mod_d[l, :, 0:1024], 1024, [C.r_mod])
        g1, r_g1 = bload(C, ph, "g1", C.mod_d[l, :, 2048:3072], 1024, [C.r_mod])
        sh2, r_sh2 = bload(C, ph, "sh2", C.mod_d[l, :, 3072:4096], 1024, [C.r_mod])
        sc2, r_sc2 = bload(C, ph, "sc2", C.mod_d[l, :, 4096:5120], 1024, [C.r_mod])
        lg, r_lg = bload(C, ph, "lg", C.ln1_g[l:l + 1, :].to_broadcast([128, 1024]), 1024)
        lb, r_lb = bload(C, ph, "lb", C.ln1_b[l:l + 1, :].to_broadcast([128, 1024]), 1024)
        brB, r_brB = bload(C, ph, "brB", C.b_router[l:l + 1, :].to_broadcast([128, 32]), 32)
        ident, r_id = T(C, ph, "identb", [128, 128], BF16)
        identf, r_idf = T(C, ph, "identf2", [128, 128], F32)
        kb.dma("sp", ident[:], C.ident_bf[:, :], writes=[r_id])
        kb.dma("sp", identf[:], C.ident_f[:, :], writes=[r_idf])
        LW = ln_work(C, ph, "olw", LN_EPS)
        xres = [T(C, ph, "xres0", [128, 4, 1024], F32)[0]] * 2
        r_xres = [[Res(f"xres_{j}") for j in range(4)]] * 2
        hb, r_hb = T(C, ph, "ohb", [128, 1024], BF16)
        hT, _ = T(C, ph, "ohT", [128, 8, 512], BF16)
        r_hT = [Res(f"ohT_{j}") for j in range(4)]
        oaT, r_oaT = T(C, ph, "ooaT", [128, 4, 512], BF16)
        obT, r_obT = T(C, ph, "oobT", [128, 4, 512], BF16)
        sga = [T(C, ph, f"sga{i}", [128, 512], F32) for i in range(2)]
        sgb = [T(C, ph, f"sgb{i}", [128, 512], F32) for i in range(2)]
        t1 = [T(C, ph, f"t1{i}", [128, 512], F32) for i in range(2)]
        t2 = [T(C, ph, f"t2{i}", [128, 512], F32) for i in range(2)]
        mT, _ = T(C, ph, "mergT", [128, 8, 512], BF16)
        r_mT = [Res(f"mergT_{j}") for j in range(8)]
        yt, r_yt = T(C, ph, "yt", [128, 1024], F32)
        zt, r_zt = T(C, ph, "zt_o", [128, 1024], F32)
        x1t = [T(C, ph, f"x1t{i}", [128, 1024], F32) for i in range(2)]
        h2f, r_h2f = T(C, ph, "h2f", [128, 1024], F32)
        h2T32, r_h2T32 = T(C, ph, "h2T32", [128, 8, 128], F32)
        h2Ts = [T(C, ph, f"h2Ts{i}", [128, 8, 512], BF16)[0] for i in range(2)]
        r_h2Ts = [[Res(f"h2Ts{i}_{j}") for j in range(4)] for i in range(2)]
        lgt, r_lgt = T(C, ph, "lgt", [128, 32], F32)
        m8, r_m8 = T(C, ph, "om8", [128, 8], F32)
        nmx, r_nmx = T(C, ph, "nmx", [128, 1], F32)
        msk, r_msk = T(C, ph, "msk", [128, 32], F32)
        ex, r_ex = T(C, ph, "ex", [128, 32], F32)
        rs, r_rs = T(C, ph, "ors", [128, 1], F32)
        gst = [T(C, ph, f"gst{i}", [128, 4, 32], F32)[0] for i in range(2)]
        r_gst = [[Res(f"gst{i}_{j}") for j in range(4)] for i in range(2)]
        gTs = [T(C, ph, f"gTs{i}", [32, 512], F32)[0] for i in range(2)]
        r_gTs = [[Res(f"gTs{i}_{j}") for j in range(4)] for i in range(2)]
        pp = Rot(C.ps[0:4])
        psT, r_psT = C.ps[7]
        psT32 = C.ps[5:7]
        psY = C.ps[4:5]

        for st in range(getattr(C, 'out_nst', NST)):
            stage = getattr(C, 'out_stage', 99)
            sp_ = st % 2
            t0 = st * 512
            cols = slice(t0, t0 + 512)
            xr = xres[sp_]
            for j in range(4):
                tt = st * 4 + j
                kb.dma("sp", xr[:, j, :], x_src[tt * 128:(tt + 1) * 128, :], reads=[r_xsrc], writes=[r_xres[sp_][j]])
                ln_modulate(C, LW, xr[:, j, :], r_xres[sp_][j], hb[:], r_hb, sc1[:], r_sc1, sh1[:], r_sh1)
                for kc in range(8):
                    kb.op("pe", lambda e: e.transpose(out=bfv(psT)[:, kc * 128:(kc + 1) * 128],
                                                      in_=hb[:, kc * 128:(kc + 1) * 128], identity=ident[:]),
                          reads=[r_hb, r_id], writes=[r_psT])
                kb.op("act", lambda e: e.copy(out=hT[:, :, j * 128:(j + 1) * 128],
                                              in_=bfv(psT).rearrange("p (kc t) -> p kc t", kc=8)),
                      reads=[r_psT], writes=[r_hT[j]])
            kb.dma("sp", oaT[:], C.oaT_d[:, :, cols].rearrange("c p t -> p c t"), reads=[C.r_oaT], writes=[r_oaT])
            kb.dma("sp", obT[:], C.obT_d[:, :, cols].rearrange("c p t -> p c t"), reads=[C.r_obT], writes=[r_obT])
            for n_ in range(8 if stage >= 1 else 0):
                ncs = slice(n_ * 128, (n_ + 1) * 128)
                pga, r_pga = pp.next()
                pgb, r_pgb = pp.next()
                pa, r_pa = pp.next()
                pb, r_pb = pp.next()
                for kc in range(8):
                    kb.op("pe", lambda e: e.matmul(pga[:], lhsT=Wg[:, kc, n_ * 128:(n_ + 1) * 128], rhs=hT[:, kc, :],
                                                   start=(kc == 0), stop=(kc == 7)), reads=r_hT + [r_Wg], writes=[r_pga])
                for kc in range(8):
                    kb.op("pe", lambda e: e.matmul(pgb[:], lhsT=Wg[:, kc, 1024 + n_ * 128:1024 + (n_ + 1) * 128],
                                                   rhs=hT[:, kc, :], start=(kc == 0), stop=(kc == 7)),
                          reads=r_hT + [r_Wg], writes=[r_pgb])
                for c in range(4):
                    kb.op("pe", lambda e: e.matmul(pa[:], lhsT=wao[:, c, ncs], rhs=oaT[:, c, :], start=(c == 0), stop=(c == 3)),
                          reads=[r_wao, r_oaT], writes=[r_pa])
                for c in range(4):
                    kb.op("pe", lambda e: e.matmul(pb[:], lhsT=wbo[:, c, ncs], rhs=obT[:, c, :], start=(c == 0), stop=(c == 3)),
                          reads=[r_wbo, r_obT], writes=[r_pb])
                sa, r_sa = sga[n_ % 2]
                sb_, r_sb = sgb[n_ % 2]
                a1, r_a1 = t1[n_ % 2]
                a2, r_a2 = t2[n_ % 2]
                kb.op("act", lambda e: e.activation(out=sa[:], in_=pga[:], func=AF.Sigmoid), reads=[r_pga], writes=[r_sa])
                kb.op("act", lambda e: e.activation(out=sb_[:], in_=pgb[:], func=AF.Sigmoid), reads=[r_pgb], writes=[r_sb])
                kb.op("dve", lambda e: e.tensor_tensor(out=a1[:], in0=pa[:], in1=sa[:], op=ALU.mult),
                      reads=[r_pa, r_sa], writes=[r_a1])
                kb.op("dve", lambda e: e.tensor_tensor(out=a2[:], in0=pb[:], in1=sb_[:], op=ALU.mult),
                      reads=[r_pb, r_sb], writes=[r_a2])
                kb.op("pool", lambda e: e.tensor_tensor(out=mT[:, n_, :], in0=a1[:], in1=a2[:], op=ALU.add),
                      reads=[r_a1, r_a2], writes=[r_mT[n_]])
            for j in range(4 if stage >= 2 else 0):
                tt = st * 4 + j
                for nh in range(2):
                    py, r_py = psY[0]
                    for n_ in range(8):
                        kb.op("pe", lambda e: e.matmul(py[:], lhsT=mT[:, n_, j * 128:(j + 1) * 128],
                                                       rhs=wo[:, n_, nh * 512:(nh + 1) * 512], start=(n_ == 0), stop=(n_ == 7)),
                              reads=r_mT + [r_wo], writes=[r_py])
                    kb.op("dve", lambda e: e.tensor_tensor(out=yt[:, nh * 512:(nh + 1) * 512], in0=py[:],
                                                           in1=g1[:, nh * 512:(nh + 1) * 512], op=ALU.mult),
                          reads=[r_py, r_g1], writes=[r_yt])
                kb.op("dve", lambda e: e.scalar_tensor_tensor(out=zt[:], in0=xr[:, j, :], scalar=float(DN_ALPHA), in1=yt[:],
                                                              op0=ALU.mult, op1=ALU.add),
                      reads=[r_xres[sp_][j], r_yt], writes=[r_zt])
                x1, r_x1 = x1t[tt % 2]
                ln_modulate(C, LW, zt, r_zt, x1[:], r_x1, lg[:], r_lg, lb[:], r_lb)
                kb.dma("sp", C.x1_d[tt * 128:(tt + 1) * 128, :], x1[:], reads=[r_x1], writes=[C.r_x1])
                if stage < 3:
                    continue
                ln_modulate(C, LW, x1, r_x1, h2f[:], r_h2f, sc2[:], r_sc2, sh2[:], r_sh2)
                sub = getattr(C, 'out_sub', 99)
                for half in range(2 if sub >= 1 else 0):
                    p32, r_p32 = psT32[half]
                    for k4 in range(4):
                        kc = half * 4 + k4
                        kb.op("pe", lambda e: e.transpose(out=p32[:, k4 * 128:(k4 + 1) * 128],
                                                          in_=h2f[:, kc * 128:(kc + 1) * 128], identity=identf[:]),
                              reads=[r_h2f, r_idf], writes=[r_p32])
                    if sub < 2:
                        continue
                    kb.op("act", lambda e: e.copy(out=h2T32[:, half * 4:half * 4 + 4, :],
                                                  in_=p32[:].rearrange("p (k t) -> p k t", k=4)),
                          reads=[r_p32], writes=[r_h2T32])
                    if sub < 3:
                        continue
                    kb.op("dve", lambda e: e.tensor_copy(out=h2Ts[sp_][:, half * 4:half * 4 + 4, j * 128:(j + 1) * 128],
                                                         in_=p32[:].rearrange("p (k t) -> p k t", k=4)),
                          reads=[r_p32], writes=[r_h2Ts[sp_][j]])
                if stage < 4:
                    continue
                pl, r_pl = pp.next()
                for kc in range(8):
                    kb.op("pe", lambda e: e.matmul(pl[:, 0:32], lhsT=h2T32[:, kc, :], rhs=wr[:, kc, :],
                                                   start=(kc == 0), stop=(kc == 7)), reads=[r_h2T32, r_wr], writes=[r_pl])
                kb.op("dve", lambda e: e.tensor_tensor(out=lgt[:], in0=pl[:, 0:32], in1=brB[:], op=ALU.add),
                      reads=[r_pl, r_brB], writes=[r_lgt])
                kb.op("dve", lambda e: e.max(out=m8[:], in_=lgt[:]), reads=[r_lgt], writes=[r_m8])
                kb.op("dve", lambda e: e.tensor_scalar(out=nmx[:], in0=m8[:, 0:1], scalar1=-1.0, scalar2=None, op0=ALU.mult),
                      reads=[r_m8], writes=[r_nmx])
                kb.op("dve", lambda e: e.tensor_scalar(out=msk[:], in0=lgt[:], scalar1=m8[:, 3:4], scalar2=None, op0=ALU.is_ge),
                      reads=[r_lgt, r_m8], writes=[r_msk])
                kb.op("act", lambda e: e.activation(out=ex[:], in_=lgt[:], func=AF.Exp, bias=nmx[:, 0:1], scale=1.0),
                      reads=[r_lgt, r_nmx], writes=[r_ex])
                kb.op("dve", lambda e: e.tensor_tensor(out=ex[:], in0=ex[:], in1=msk[:], op=ALU.mult),
                      reads=[r_ex, r_msk], writes=[r_ex])
                kb.op("dve", lambda e: e.reduce_sum(out=rs[:], in_=ex[:], axis=AX.X), reads=[r_ex], writes=[r_rs])
                kb.op("dve", lambda e: e.reciprocal(out=rs[:], in_=rs[:]), reads=[r_rs], writes=[r_rs])
                kb.op("dve", lambda e: e.tensor_scalar(out=gst[sp_][:, j, :], in0=ex[:], scalar1=rs[:, 0:1], scalar2=None,
                                                       op0=ALU.mult), reads=[r_ex, r_rs], writes=[r_gst[sp_][j]])
                pg, r_pg = pp.next()
                kb.op("pe", lambda e: e.transpose(out=pg[0:32, 0:128], in_=gst[sp_][:, j, :], identity=identf[:]),
                      reads=[r_gst[sp_][j], r_idf], writes=[r_pg])
                kb.op("act", lambda e: e.copy(out=gTs[sp_][:, j * 128:(j + 1) * 128], in_=pg[0:32, 0:128]),
                      reads=[r_pg], writes=[r_gTs[sp_][j]])
            if stage < 4:
                continue
            kb.dma("sp", C.h2T_d[:, :, cols].rearrange("k p t -> p k t"), h2Ts[sp_][:], reads=r_h2Ts[sp_], writes=[C.r_h2T])
            kb.dma("sp", C.gates_d[cols, :].rearrange("(j p) e -> p j e", p=128), gst[sp_][:], reads=r_gst[sp_],
                   writes=[C.r_gates])
            kb.dma("sp", C.gatesT_d[:, cols], gTs[sp_][:], reads=r_gTs[sp_], writes=[C.r_gatesT])
        kb.barrier()


TS = 1024


def phase_moe(C, l, x_dst, r_xdst):
    nc, kb = C.nc, C.kb
    with contextlib.ExitStack() as ph:
        wgu = [T(C, ph, f"wgu{i}", [128, 8, 2048], BF16) for i in range(2)]
        wdn = [T(C, ph, f"wdn{i}", [128, 8, 1024], BF16) for i in range(2)]
        bgu, r_bgu = T(C, ph, "bgu", [128, E, 16], F32)
        bdn, r_bdn = T(C, ph, "bdn", [32, 1024], F32)
        kb.dma("sp", bgu[:], C.b_gu_t[l].rearrange("e p c -> p e c"), writes=[r_bgu])
        kb.dma("sp", bdn[:], C.b_dn[l], writes=[r_bdn])
        kb.op("pool", lambda e: e.tensor_scalar(out=bgu[:, :, 8:16], in0=bgu[:, :, 8:16], scalar1=1.0, scalar2=None,
                                                op0=ALU.add), reads=[r_bgu], writes=[r_bgu])
        g2, r_g2 = bload(C, ph, "g2", C.mod_d[l, :, 5120:6144], 1024, [C.r_mod])
        lg, r_lg = bload(C, ph, "lg2", C.ln2_g[l:l + 1, :].to_broadcast([128, 1024]), 1024)
        lb, r_lb = bload(C, ph, "lb2", C.ln2_b[l:l + 1, :].to_broadcast([128, 1024]), 1024)
        LW = ln_work(C, ph, "mlw", LN_EPS)
        h2T, r_h2T = T(C, ph, "mh2T", [128, 8, TS], BF16)
        gts, r_gts = T(C, ph, "mgts", [128, TS // 128, 32], F32)
        gT, r_gT = T(C, ph, "mgT", [32, TS], F32)
        acc, _ = T(C, ph, "macc", [128, TS // 128, 1024], F32)
        r_acc = [[Res(f"acc{j}_{nh}") for nh in range(2)] for j in range(TS // 128)]
        a_sb = [T(C, ph, f"a_sb{i}", [128, 512], F32) for i in range(2)]
        sg = [T(C, ph, f"sg{i}", [128, 512], BF16) for i in range(2)]
        gg = [T(C, ph, f"gg{i}", [128, 512], F32) for i in range(2)]
        u_sb = [T(C, ph, f"u_sb{i}", [128, 512], F32) for i in range(2)]
        actT = [T(C, ph, f"actT{i}", [128, 8, 512], BF16)[0] for i in range(2)]
        r_actT = [[Res(f"actT{i}_{f}") for f in range(8)] for i in range(2)]
        x1t, r_x1t = T(C, ph, "mx1t", [128, 1024], F32)
        xo = [(x1t, r_x1t)] * 2
        ppA = Rot(C.ps[0:2])
        ppU = Rot(C.ps[2:4])
        ppY = Rot(C.ps[4:8])
        wloaded = {}

        def load_w(idx):
            if idx in wloaded or idx >= (S // TS) * E:
                return
            wloaded[idx] = True
            e_ = idx % E
            g_, r_g = wgu[idx % 2]
            d_, r_d = wdn[idx % 2]
            kb.dma("pool", g_[:], C.w_gu[l, e_].rearrange("(kc p) n -> p kc n", p=128), writes=[r_g])
            kb.dma("pool", d_[:], C.w_dn[l, e_].rearrange("(kc p) n -> p kc n", p=128), writes=[r_d])

        load_w(0)
        load_w(1)

        def loads_h(ts):
            tcols = slice(ts * TS, (ts + 1) * TS)
            kb.dma("sp", h2T[:], C.h2T_d[:, :, tcols].rearrange("k p t -> p k t"), reads=[C.r_h2T], writes=[r_h2T])

        def loads_g(ts):
            tcols = slice(ts * TS, (ts + 1) * TS)
            kb.dma("sp", gts[:], C.gates_d[tcols, :].rearrange("(j p) e -> p j e", p=128), reads=[C.r_gates], writes=[r_gts])
            kb.dma("sp", gT[:], C.gatesT_d[:, tcols], reads=[C.r_gatesT], writes=[r_gT])

        def acc_init(ts):
            for j in range(TS // 128):
                for nh in range(2):
                    py, r_py = ppY.next()
                    kb.op("pe", lambda e: e.matmul(py[:], lhsT=gT[:, j * 128:(j + 1) * 128], rhs=bdn[:, nh * 512:(nh + 1) * 512],
                                                   start=True, stop=True), reads=[r_gT, r_bdn], writes=[r_py])
                    kb.op("act", lambda e: e.copy(out=acc[:, j, nh * 512:(nh + 1) * 512], in_=py[:]),
                          reads=[r_py], writes=[r_acc[j][nh]])

        def AU(ts, e_, t2):
            idx = ts * E + e_
            g_, r_g = wgu[idx % 2]
            aT = actT[t2 % 2]
            r_aT = r_actT[t2 % 2]
            hc = slice(t2 * 512, (t2 + 1) * 512)
            for fc in range(8):
                pa, r_pa = ppA.next()
                pu, r_pu = ppU.next()
                for kc in range(8):
                    kb.op("pe", lambda e: e.matmul(pa[:], lhsT=g_[:, kc, fc * 128:(fc + 1) * 128], rhs=h2T[:, kc, hc],
                                                   start=(kc == 0), stop=(kc == 7)), reads=[r_g, r_h2T], writes=[r_pa])
                for kc in range(8):
                    kb.op("pe", lambda e: e.matmul(pu[:], lhsT=g_[:, kc, 1024 + fc * 128:1024 + (fc + 1) * 128],
                                                   rhs=h2T[:, kc, hc], start=(kc == 0), stop=(kc == 7)),
                          reads=[r_g, r_h2T], writes=[r_pu])
                a_, r_a = a_sb[fc % 2]
                s_, r_s = sg[fc % 2]
                q_, r_q = gg[fc % 2]
                u_, r_u = u_sb[fc % 2]
                kb.op("dve", lambda e: e.tensor_scalar(out=a_[:], in0=pa[:], scalar1=bgu[:, e_, fc:fc + 1], scalar2=7.0,
                                                       op0=ALU.add, op1=ALU.min), reads=[r_pa, r_bgu], writes=[r_a])
                kb.op("act", lambda e: e.activation(out=s_[:], in_=a_[:], func=AF.Sigmoid, scale=1.702),
                      reads=[r_a], writes=[r_s])
                kb.op("dve", lambda e: e.tensor_scalar(out=u_[:], in0=pu[:], scalar1=bgu[:, e_, 8 + fc:9 + fc], scalar2=8.0,
                                                       op0=ALU.add, op1=ALU.min), reads=[r_pu, r_bgu], writes=[r_u])
                kb.op("pool", lambda e: e.tensor_tensor(out=q_[:], in0=a_[:], in1=s_[:], op=ALU.mult),
                      reads=[r_a, r_s], writes=[r_q])
                kb.op("dve", lambda e: e.scalar_tensor_tensor(out=aT[:, fc, :], in0=u_[:], scalar=-6.0, in1=q_[:],
                                                              op0=ALU.max, op1=ALU.mult),
                      reads=[r_u, r_q], writes=[r_aT[fc]])

        def YY(ts, e_, t2):
            idx = ts * E + e_
            d_, r_d = wdn[idx % 2]
            aT = actT[t2 % 2]
            r_aT = r_actT[t2 % 2]
            for j4 in range(4):
                j = t2 * 4 + j4
                for nh in range(2):
                    py, r_py = ppY.next()
                    for fc in range(8):
                        kb.op("pe", lambda e: e.matmul(py[:], lhsT=aT[:, fc, j4 * 128:(j4 + 1) * 128],
                                                       rhs=d_[:, fc, nh * 512:(nh + 1) * 512],
                                                       start=(fc == 0), stop=(fc == 7)), reads=r_aT + [r_d], writes=[r_py])
                    kb.op("dve", lambda e: e.scalar_tensor_tensor(out=acc[:, j, nh * 512:(nh + 1) * 512], in0=py[:],
                                                                  scalar=gts[:, j, e_:e_ + 1],
                                                                  in1=acc[:, j, nh * 512:(nh + 1) * 512],
                                                                  op0=ALU.mult, op1=ALU.add),
                          reads=[r_py, r_gts, r_acc[j][nh]], writes=[r_acc[j][nh]])
            if t2 == TS // 512 - 1:
                load_w(idx + 2)

        def postnorm(ts):
            for j in range(TS // 128):
                tt = ts * (TS // 128) + j
                kb.dma("sp", x1t[:], C.x1_d[tt * 128:(tt + 1) * 128, :], reads=[C.r_x1], writes=[r_x1t])
                kb.op("pool", lambda e: e.tensor_tensor(out=acc[:, j, :], in0=acc[:, j, :], in1=g2[:], op=ALU.mult),
                      reads=r_acc[j] + [r_g2], writes=r_acc[j])
                kb.op("dve", lambda e: e.scalar_tensor_tensor(out=x1t[:], in0=x1t[:], scalar=float(DN_ALPHA), in1=acc[:, j, :],
                                                              op0=ALU.mult, op1=ALU.add), reads=[r_x1t] + r_acc[j], writes=[r_x1t])
                xo_, r_xo = xo[tt % 2]
                ln_modulate(C, LW, x1t, r_x1t, xo_[:], r_xo, lg[:], r_lg, lb[:], r_lb)
                kb.dma("sp", x_dst[tt * 128:(tt + 1) * 128, :], xo_[:], reads=[r_xo], writes=[r_xdst])

        units = [(ts, e_, t2) for ts in range(S // TS) for e_ in range(E) for t2 in range(TS // 512)]
        loads_h(0)
        loads_g(0)
        acc_init(0)
        for ui, un in enumerate(units):
            ts, e_, t2 = un
            first = (e_ == 0 and t2 == 0 and ts > 0)
            if first:
                loads_h(ts)
            AU(*un)
            if ui >= 1:
                YY(*units[ui - 1])
            if first:
                postnorm(ts - 1)
                loads_g(ts)
                acc_init(ts)
        YY(*units[-1])
        postnorm(S // TS - 1)
        kb.barrier()

def alloc_scratch(C):
    C.mod_d, C.r_mod = dram(C, "mod_d", [DEPTH, 128, 6 * D], F32)
    C.qlatT_d, C.r_qlatT = dram(C, "qlatT_d", [8, 128, S], BF16)
    C.qidxT_d, C.r_qidxT = dram(C, "qidxT_d", [2, 128, S], BF16)
    C.kidxT_d, C.r_kidxT = dram(C, "kidxT_d", [128, S], BF16)
    C.widx_d, C.r_widx = dram(C, "widx_d", [S, 4], F32)
    C.ckv_d, C.r_ckv = dram(C, "ckv_d", [S, 128], BF16)
    C.ckvT_d, C.r_ckvT = dram(C, "ckvT_d", [128, S], BF16)
    C.qbT_d, C.r_qbT = dram(C, "qbT_d", [4, 128, S], BF16)
    C.kbT_d, C.r_kbT = dram(C, "kbT_d", [4, 128, S], BF16)
    C.vb_d, C.r_vb = dram(C, "vb_d", [S, 512], BF16)
    C.obT_d, C.r_obT = dram(C, "obT_d", [4, 128, S], BF16)
    C.oaT_d, C.r_oaT = dram(C, "oaT_d", [4, 128, S], BF16)
    C.x1_d, C.r_x1 = dram(C, "x1_d", [S, D], F32)
    C.x2_d, C.r_x2 = dram(C, "x2_d", [S, D], F32)
    C.h2T_d, C.r_h2T = dram(C, "h2T_d", [8, 128, S], BF16)
    C.gates_d, C.r_gates = dram(C, "gates_d", [S, 32], F32)
    C.gatesT_d, C.r_gatesT = dram(C, "gatesT_d", [32, S], F32)


def build(dbg=(), upto=99, skip_sb=False, skip_dsa=False, nlayers=DEPTH):
    nc = bass.Bass("TRN2", target_bir_lowering=False)
    C = Ctx()
    C.skip_sb = skip_sb
    C.skip_dsa = skip_dsa
    C.nc = nc
    C.dbg = set(dbg)

    def inp(name, shape, dt=F32):
        return nc.dram_tensor(name, list(shape), dt, kind="ExternalInput").ap()

    C.x = inp("x", [S, D])
    C.c_col = inp("c_col", [128, 8])
    C.w_ada = inp("w_ada", [DEPTH, D, 6 * D])
    C.b_ada = inp("b_ada", [DEPTH, 6 * D])
    C.w_in = inp("w_in", [DEPTH, D, NCOLS])
    C.w_uk_t = inp("w_uk_t", [DEPTH, 8, 64, 128])
    C.g_kv = inp("g_kv", [DEPTH, 128])
    C.ident_bf = inp("ident_bf", [128, 128], BF16)
    C.negU_bf = inp("negU_bf", [128, 128], BF16)
    C.ones_bf = inp("ones_bf", [128, 128], BF16)
    C.cm_bf = inp("cm_bf", [4, 128, 512], BF16)
    C.w_uv = inp("w_uv", [DEPTH, 128, 8, 64])
    C.rel_bias = inp("rel_bias", [32, 8])
    C.rel_bias_t = inp("rel_bias_t", [8, 32])
    C.dmask = inp("dmask", [128, 128])
    C.irep_bf = inp("irep_bf", [128, 512], BF16)
    C.ident_f = inp("ident_f", [128, 128])
    C.tz = inp("tz", [2, 128, 1024])
    C.bmask = inp("bmask", [8, 1024])
    C.w_a_out = inp("w_a_out", [DEPTH, 512, D])
    C.w_b_out = inp("w_b_out", [DEPTH, 512, D])
    C.w_o = inp("w_o", [DEPTH, D, D])
    C.ln1_g = inp("ln1_g", [DEPTH, D])
    C.ln1_b = inp("ln1_b", [DEPTH, D])
    C.ln2_g = inp("ln2_g", [DEPTH, D])
    C.ln2_b = inp("ln2_b", [DEPTH, D])
    C.w_router = inp("w_router", [DEPTH, D, E])
    C.b_router = inp("b_router", [DEPTH, E])
    C.w_gu = inp("w_gu", [DEPTH, E, D, 2 * FF])
    C.b_gu_t = inp("b_gu_t", [DEPTH, E, 128, 16])
    C.w_dn = inp("w_dn", [DEPTH, E, FF, D])
    C.b_dn = inp("b_dn", [DEPTH, E, D])
    C.out = nc.dram_tensor("out", [S, D], F32, kind="ExternalOutput").ap()
    C.r_out = Res("out")
    C.r_x = Res("x")
    with contextlib.ExitStack() as es:
        C.es = es
        C.kb = KB(nc, es)
        C.ps = [(es.enter_context(nc.psum_tensor(f"ps{i}", [128, 512], F32)), Res(f"ps{i}", psum=True)) for i in range(8)]
        alloc_scratch(C)
        phase_ada(C)
        x_src, r_xsrc = C.x, C.r_x
        for l in range(nlayers):
            if upto >= 1:
                phase_proj(C, l, x_src, r_xsrc)
            if upto >= 2 and not C.skip_sb:
                phase_sb(C, l)
            if upto >= 3 and not C.skip_dsa:
                phase_dsa(C, l)
            if upto >= 4:
                phase_out(C, l, x_src, r_xsrc)
            if upto >= 5:
                last = (l == DEPTH - 1)
                x_dst, r_xdst = (C.out, C.r_out) if last else (C.x2_d, C.r_x2)
                phase_moe(C, l, x_dst, r_xdst)
                x_src, r_xsrc = x_dst, r_xdst
        C.kb.barrier()
    C.ninst = C.kb.ninst
    return nc, C


def host_inputs(inputs, b):
    f = np.float32
    return {
        "x": np.ascontiguousarray(inputs["x"][b]),
        "c_col": np.ascontiguousarray(np.asarray(inputs["c"][b], f).reshape(8, 128).T),
        "w_ada": inputs["w_ada"], "b_ada": inputs["b_ada"], "w_in": inputs["w_in"],
        "w_uk_t": np.ascontiguousarray(np.transpose(inputs["w_uk"], (0, 2, 3, 1))),
        "g_kv": inputs["g_kv"],
        "w_a_out": inputs["w_a_out"], "w_b_out": inputs["w_b_out"], "w_o": inputs["w_o"],
        "ln1_g": inputs["ln1_g"], "ln1_b": inputs["ln1_b"], "ln2_g": inputs["ln2_g"], "ln2_b": inputs["ln2_b"],
        "w_router": inputs["w_router"], "b_router": inputs["b_router"],
        "w_gu": inputs["w_gu"], "w_dn": inputs["w_dn"], "b_dn": inputs["b_dn"],
        "b_gu_t": np.ascontiguousarray(np.asarray(inputs["b_gu"], f).reshape(DEPTH, E, 16, 128).transpose(0, 1, 3, 2)),
        "w_uv": inputs["w_uv"], "rel_bias": inputs["rel_bias"],
        "rel_bias_t": np.ascontiguousarray(np.asarray(inputs["rel_bias"], f).T),
        "tz": _tz_table(np.asarray(inputs["rel_bias"], f)),
        "ident_bf": np.eye(128, dtype=np.float32).astype(ml_dtypes.bfloat16),
        **CONSTS,
    }


def _make_consts():
    bf = ml_dtypes.bfloat16
    jj = np.arange(128)
    negU = -(jj[:, None] >= jj[None, :]).astype(np.float32)
    ones = np.ones((128, 128), np.float32)
    s_ = np.arange(128)[None, :, None]
    t_ = np.arange(512)[None, None, :]
    rel = np.arange(4)[:, None, None]
    cm = ((s_ + 128 * rel) < t_).astype(np.float32)
    dmask = np.where(jj[None, :] > jj[:, None], np.float32(NEG), np.float32(0)).astype(np.float32)
    irep = np.tile(np.eye(128, dtype=np.float32), (1, 4))
    bmask = np.zeros((8, 8, 128), np.float32)
    for h in range(8):
        bmask[h, h, :] = 1.0
    return {"negU_bf": negU.astype(bf), "ones_bf": ones.astype(bf), "cm_bf": cm.astype(bf),
            "dmask": dmask, "irep_bf": irep.astype(bf), "ident_f": np.eye(128, dtype=np.float32),
            "bmask": bmask.reshape(8, 1024)}


def _t5_bucket(n):
    import math
    max_exact = 16
    nf = np.maximum(n, 1).astype(np.float32)
    large = max_exact + (np.log(nf / np.float32(max_exact)) / np.float32(math.log(128 / max_exact))
                         * np.float32(32 - max_exact)).astype(np.int32)
    large = np.minimum(large, 31)
    return np.where(n < max_exact, n, large)


_TZ_IDX = None


def _tz_table(rel_bias):
    global _TZ_IDX
    if _TZ_IDX is None:
        s_ = np.arange(128)[None, :, None]
        t_ = np.arange(128)[None, None, :]
        rel = np.arange(2)[:, None, None]
        dist = np.maximum(128 * rel + t_ - s_, 0)
        _TZ_IDX = _t5_bucket(dist)
    g = rel_bias[_TZ_IDX]
    return np.ascontiguousarray(np.transpose(g, (0, 1, 3, 2)).reshape(2, 128, 1024))


CONSTS = _make_consts()


def kernel(**inputs):
    inputs = {k: np.asarray(v) for k, v in inputs.items()}
    nc, _ = build()
    n = 8
    in_maps = [host_inputs(inputs, b) for b in range(n)]
    res = run_bass_kernel_spmd(nc, in_maps, core_ids=list(range(n)))
    out = np.stack([np.asarray(res.results[b]["out"], dtype=np.float32) for b in range(n)], axis=0)
    return out
```

```python
import contextlib
import numpy as np
import ml_dtypes
import concourse.bass as bass
import concourse.mybir as mybir
from concourse.bass_utils import run_bass_kernel_spmd

F32 = mybir.dt.float32
BF16 = mybir.dt.bfloat16
AF = mybir.ActivationFunctionType
ALU = mybir.AluOpType
AX = mybir.AxisListType

D = 1024
S = 8192
DEPTH = 2
NT = S // 128
NST = S // 512
HD = 64
NCOLS = 4548
NW1 = 2500
E = 32
FF = 1024
LN_EPS = 1e-5
RMS_EPS = 1e-6
DN_ALPHA = (2 * DEPTH) ** 0.25
TOPK = 256
NEG = -1.0e30
MBIG = -30000.0


class Res:
    __slots__ = ("wc", "wd", "rc", "rd", "name", "psum")

    def __init__(self, name="", psum=False):
        self.psum = psum
        self.wc = {}
        self.wd = []
        self.rc = {}
        self.rd = []
        self.name = name


class KB:
    COMPUTE = ("pe", "act", "dve", "pool")

    def __init__(self, nc, es):
        self.nc = nc
        self.es = es
        self.eng = {"pe": nc.tensor, "act": nc.scalar, "dve": nc.vector, "pool": nc.gpsimd, "sp": nc.sync}
        self.sem = {e: es.enter_context(nc.semaphore("s_" + e)) for e in self.COMPUTE}
        self.cnt = {e: 0 for e in self.COMPUTE}
        self.known = {e: {} for e in self.eng}
        self.NSD = 8
        self.dq = {}
        self.ninst = 0

    def _dq(self, q):
        if q not in self.dq:
            self.dq[q] = {"sems": [self.es.enter_context(self.nc.semaphore(f"d_{q}_{i}")) for i in range(self.NSD)],
                          "n": 0}
        return self.dq[q]

    nosame = False

    def _wait(self, e, tok):
        if tok[0] == "c":
            _, e2, seq = tok
            if e == "pe" and e2 == "pe":
                return
            if self.nosame and e == e2:
                return
            if self.known[e].get(e2, 0) >= seq:
                return
            self.eng[e].wait_ge(self.sem[e2], seq)
            self.known[e][e2] = seq
        else:
            _, q, slot, val = tok
            key = (q, slot)
            if self.known[e].get(key, 0) >= val:
                return
            self.eng[e].wait_ge(self.dq[q]["sems"][slot], val)
            self.known[e][key] = val
        self.ninst += 1

    def _deps(self, e, reads, writes):
        for r in reads:
            for e2, seq in r.wc.items():
                self._wait(e, ("c", e2, seq))
            for tok in r.wd:
                self._wait(e, tok)
            if r.psum:
                for e2, seq in r.rc.items():
                    if e2 != e:
                        self._wait(e, ("c", e2, seq))
        for w in writes:
            for e2, seq in w.wc.items():
                self._wait(e, ("c", e2, seq))
            for tok in w.wd:
                self._wait(e, tok)
            for e2, seq in w.rc.items():
                self._wait(e, ("c", e2, seq))
            for tok in w.rd:
                self._wait(e, tok)

    def op(self, e, fn, reads=(), writes=(), nosame=False):
        self.nosame = nosame
        self._deps(e, reads, writes)
        self.nosame = False
        ins = fn(self.eng[e])
        self.cnt[e] += 1
        seq = self.cnt[e]
        ins.then_inc(self.sem[e], 1)
        self.ninst += 1
        for r in reads:
            r.rc[e] = seq
        for w in writes:
            w.wc = {e: seq}
            w.wd = []
            w.rc = {}
            w.rd = []
        return ins

    def dma(self, q, out, in_, reads=(), writes=(), **kw):
        d = self._dq(q)
        k = d["n"]
        slot = k % self.NSD
        val = 16 * (k // self.NSD + 1)
        if k >= self.NSD:
            self._wait(q, ("d", q, slot, val - 16))
        self._deps(q, reads, writes)
        ins = self.eng[q].dma_start(out=out, in_=in_, **kw)
        ins.then_inc(d["sems"][slot], 16)
        d["n"] += 1
        self.ninst += 1
        tok = ("d", q, slot, val)
        for r in reads:
            r.rd.append(tok)
        for w in writes:
            w.wc = {}
            w.wd = [tok]
            w.rc = {}
            w.rd = []
        return ins

    def barrier(self, engines=None):
        engines = engines or list(self.eng)
        for e in engines:
            for e2 in self.COMPUTE:
                if self.cnt[e2] > 0:
                    self._wait(e, ("c", e2, self.cnt[e2]))
            for q, d in self.dq.items():
                n = d["n"]
                for k in range(max(0, n - self.NSD), n):
                    self._wait(e, ("d", q, k % self.NSD, 16 * (k // self.NSD + 1)))


class Ctx:
    pass


_uid = [0]


def T(C, ph, name, shape, dt):
    _uid[0] += 1
    h = ph.enter_context(C.nc.sbuf_tensor(f"{name}_{_uid[0]}", list(shape), dt))
    return h, Res(name)


def dram(C, name, shape, dt):
    return C.nc.dram_tensor(name, list(shape), dt, kind="Internal").ap(), Res(name)


def phase_ada(C):
    nc, kb = C.nc, C.kb
    with contextlib.ExitStack() as ph:
        ccol, r_ccol = T(C, ph, "ccol", [128, 8], F32)
        cond, r_cond = T(C, ph, "cond", [128, 8], F32)
        condB, r_condB = T(C, ph, "condB", [128, 8, 128], F32)
        wb = [T(C, ph, f"wada{i}", [128, 8, 512], F32) for i in range(2)]
        bB, r_bB = T(C, ph, "badaB", [128, 6144], F32)
        mo = [T(C, ph, f"mo{i}", [128, 512], F32) for i in range(2)]
        kb.dma("sp", ccol[:], C.c_col[:, :], writes=[r_ccol])
        kb.op("act", lambda e: e.activation(out=cond[:], in_=ccol[:], func=AF.Silu), reads=[r_ccol], writes=[r_cond])
        for kc in range(8):
            kb.op("dve", lambda e: e.tensor_copy(out=condB[:, kc, :], in_=cond[:, kc:kc + 1].to_broadcast([128, 128])),
                  reads=[r_cond], writes=[r_condB])
        it = 0
        for l in range(DEPTH):
            kb.dma("sp", bB[:], C.b_ada[l:l + 1, :].to_broadcast([128, 6144]), writes=[r_bB])
            for j in range(12):
                w, r_w = wb[it % 2]
                m, r_m = mo[it % 2]
                ps, r_ps = C.ps[it % 2]
                kb.dma("sp", w[:], C.w_ada[l, :, j * 512:(j + 1) * 512].rearrange("(kc p) n -> p kc n", p=128),
                       writes=[r_w])
                for kc in range(8):
                    kb.op("pe", lambda e: e.matmul(ps[:], lhsT=condB[:, kc, :], rhs=w[:, kc, :],
                                                   start=(kc == 0), stop=(kc == 7)),
                          reads=[r_condB, r_w], writes=[r_ps])
                plus1 = 1.0 if j in (2, 3, 8, 9) else 0.0
                kb.op("dve", lambda e: e.scalar_tensor_tensor(out=m[:], in0=ps[:], scalar=plus1,
                                                              in1=bB[:, j * 512:(j + 1) * 512],
                                                              op0=ALU.add, op1=ALU.add),
                      reads=[r_ps, r_bB], writes=[r_m])
                kb.dma("sp", C.mod_d[l, :, j * 512:(j + 1) * 512], m[:], reads=[r_m], writes=[C.r_mod])
                it += 1
        kb.barrier()


def dram(C, name, shape, dt):
    kind = "ExternalOutput" if name in C.dbg else "Internal"
    return C.nc.dram_tensor(name, list(shape), dt, kind=kind).ap(), Res(name)


class Rot:
    def __init__(self, items):
        self.items = list(items)
        self.i = 0

    def next(self):
        it = self.items[self.i % len(self.items)]
        self.i += 1
        return it


def bfv(ps):
    return ps[:].bitcast(BF16)


def ln_modulate(C, W, xt, r_xt, out_ap, r_out, scB, r_scB, shB, r_shB):
    kb = C.kb
    st_, r_st = W["stats"]
    mv, r_mv = W["mv"]
    sd, r_sd = W["sd"]
    rstd, r_rstd = W["rstd"]
    nmr, r_nmr = W["nmr"]
    xn, r_xn = W["xn"]
    epsT, r_eps = W["eps"]
    for c in range(2):
        kb.op("dve", lambda e: e.bn_stats(out=st_[:, c * 6:(c + 1) * 6], in_=xt[:, c * 512:(c + 1) * 512]),
              reads=[r_xt], writes=[r_st])
    kb.op("dve", lambda e: e.bn_aggr(out=mv[:], in_=st_[:]), reads=[r_st], writes=[r_mv])
    kb.op("act", lambda e: e.activation(out=sd[:], in_=mv[:, 1:2], func=AF.Sqrt, bias=epsT[:, 0:1], scale=1.0),
          reads=[r_mv, r_eps], writes=[r_sd])
    kb.op("dve", lambda e: e.reciprocal(out=rstd[:], in_=sd[:]), reads=[r_sd], writes=[r_rstd])
    kb.op("dve", lambda e: e.tensor_scalar(out=nmr[:], in0=mv[:, 0:1], scalar1=rstd[:, 0:1], scalar2=-1.0,
                                           op0=ALU.mult, op1=ALU.mult),
          reads=[r_mv, r_rstd], writes=[r_nmr])
    kb.op("act", lambda e: e.activation(out=xn[:], in_=xt[:], func=AF.Identity, bias=nmr[:, 0:1], scale=rstd[:, 0:1]),
          reads=[r_xt, r_rstd, r_nmr], writes=[r_xn])
    kb.op("pool", lambda e: e.tensor_tensor(out=xn[:], in0=xn[:], in1=scB, op=ALU.mult),
          reads=[r_xn, r_scB], writes=[r_xn])
    kb.op("dve", lambda e: e.tensor_tensor(out=out_ap, in0=xn[:], in1=shB, op=ALU.add),
          reads=[r_xn, r_shB], writes=[r_out])


def ln_work(C, ph, tag, eps):
    W = {
        "stats": T(C, ph, tag + "stats", [128, 12], F32),
        "mv": T(C, ph, tag + "mv", [128, 2], F32),
        "sd": T(C, ph, tag + "sd", [128, 1], F32),
        "rstd": T(C, ph, tag + "rstd", [128, 1], F32),
        "nmr": T(C, ph, tag + "nmr", [128, 1], F32),
        "xn": T(C, ph, tag + "xn", [128, 1024], F32),
        "eps": T(C, ph, tag + "eps", [128, 1], F32),
    }
    e_, r_e = W["eps"]
    C.kb.op("pool", lambda e: e.memset(e_[:], eps), writes=[r_e])
    return W


def phase_proj(C, l, x_src, r_xsrc):
    nc, kb = C.nc, C.kb
    with contextlib.ExitStack() as ph:
        W1, r_W1 = T(C, ph, "W1", [128, 8, NW1], BF16)
        Wkk, r_Wkk = T(C, ph, "Wkk", [128, 8, 128], BF16)
        wuk, r_wuk = T(C, ph, "wuk", [128, 4, 128], BF16)
        gkvB, r_gkvB = T(C, ph, "gkvB", [128, 128], F32)
        scB, r_scB = T(C, ph, "scB", [128, 1024], F32)
        shB, r_shB = T(C, ph, "shB", [128, 1024], F32)
        ident, r_id = T(C, ph, "ident", [128, 128], BF16)
        reps, r_reps = T(C, ph, "reps", [128, 1], F32)
        kb.op("pool", lambda e: e.memset(reps[:], RMS_EPS), writes=[r_reps])
        kb.dma("pool", W1[:], C.w_in[l, :, 0:NW1].rearrange("(kc p) n -> p kc n", p=128), writes=[r_W1])
        for hh in range(2):
            kb.dma("pool", Wkk[:, :, hh * 64:(hh + 1) * 64],
                   C.w_in[l, :, 896:960].rearrange("(kc p) n -> p kc n", p=128), writes=[r_Wkk])
        kb.dma("pool", wuk[:], C.w_uk_t[l].rearrange("(hp two) d r -> (two d) hp r", two=2), writes=[r_wuk])
        kb.dma("sp", gkvB[:], C.g_kv[l:l + 1, :].to_broadcast([128, 128]), writes=[r_gkvB])
        kb.dma("sp", scB[:], C.mod_d[l, :, 1024:2048], reads=[C.r_mod], writes=[r_scB])
        kb.dma("sp", shB[:], C.mod_d[l, :, 0:1024], reads=[C.r_mod], writes=[r_shB])
        kb.dma("sp", ident[:], C.ident_bf[:, :], writes=[r_id])
        LW = [ln_work(C, ph, f"lw{i}", LN_EPS) for i in range(2)]
        xb = [T(C, ph, f"xb{i}", [128, 1024], F32) for i in range(2)]
        hb = [T(C, ph, f"hb{i}", [128, 1024], BF16) for i in range(2)]
        hT = [(T(C, ph, f"hT{i}", [128, 8, 512], BF16)[0], [Res(f"hT{i}_{j}") for j in range(4)]) for i in range(2)]
        qaT = [T(C, ph, f"qaT{i}", [128, 512], BF16) for i in range(2)]
        def stage(name, shape, n):
            return [(T(C, ph, f"{name}{i}", shape, BF16)[0], [Res(f"{name}{i}_{j}") for j in range(n)]) for i in range(2)]
        s_qlat = stage("s_qlat", [128, 8, 512], 8)
        s_qidx = stage("s_qidx", [128, 2, 512], 2)
        s_kidx = stage("s_kidx", [128, 1, 512], 1)
        s_qb = stage("s_qb", [128, 4, 512], 4)
        s_kb = stage("s_kb", [128, 4, 512], 4)
        s_ckvT = stage("s_ckvT", [128, 4, 128], 4)
        s_ckv = stage("s_ckv", [128, 4, 128], 4)
        s_vb = stage("s_vb", [128, 4, 512], 4)
        s_wi = [(T(C, ph, f"s_wi{i}", [128, 4, 4], F32)[0], [Res(f"s_wi{i}_{j}") for j in range(4)]) for i in range(2)]
        ss_t = [T(C, ph, f"ss{i}", [128, 1], F32) for i in range(2)]
        sd2_t = [T(C, ph, f"sd2{i}", [128, 1], F32) for i in range(2)]
        rs_t = [T(C, ph, f"rs{i}", [128, 1], F32) for i in range(2)]
        junk, r_junk = T(C, ph, "junk", [128, 128], F32)
        pp = Rot(C.ps[0:6])
        psT, r_psT = C.ps[7]
        psT2, r_psT2 = C.ps[6]
        ev = Rot(["act", "dve"])

        def evac(eng, out_ap, in_ap, reads, writes, scale=None):
            if eng == "act":
                if scale is None:
                    kb.op("act", lambda e: e.copy(out=out_ap, in_=in_ap), reads=reads, writes=writes)
                else:
                    kb.op("act", lambda e: e.mul(out=out_ap, in_=in_ap, mul=scale), reads=reads, writes=writes)
            else:
                if scale is None:
                    kb.op("dve", lambda e: e.tensor_copy(out=out_ap, in_=in_ap), reads=reads, writes=writes)
                else:
                    kb.op("dve", lambda e: e.tensor_scalar(out=out_ap, in0=in_ap, scalar1=scale, scalar2=None,
                                                           op0=ALU.mult), reads=reads, writes=writes)

        for st in range(NST):
            sp_ = st % 2
            hTt, r_hT = hT[sp_]
            t0 = st * 512
            for j in range(4):
                tt = st * 4 + j
                xt, r_xt = xb[tt % 2]
                hbt, r_hb = hb[tt % 2]
                kb.dma("sp", xt[:], x_src[tt * 128:(tt + 1) * 128, :], reads=[r_xsrc], writes=[r_xt])
                ln_modulate(C, LW[tt % 2], xt, r_xt, hbt[:], r_hb, scB[:], r_scB, shB[:], r_shB)
                for kc in range(8):
                    kb.op("pe", lambda e: e.transpose(out=bfv(psT)[:, kc * 128:(kc + 1) * 128],
                                                      in_=hbt[:, kc * 128:(kc + 1) * 128], identity=ident[:]),
                          reads=[r_hb, r_id], writes=[r_psT])
                evac(ev.next(), hTt[:, :, j * 128:(j + 1) * 128],
                     bfv(psT).rearrange("p (kc t) -> p kc t", kc=8), [r_psT], [r_hT[j]])
            for j in range(4):
                tt = st * 4 + j
                ps, r_ps = pp.next()
                for (c0, c1, o0) in ((512, 640, 0), (960, 964, 128)):
                    for kc in range(8):
                        kb.op("pe", lambda e: e.matmul(ps[:, o0:o0 + (c1 - c0)], lhsT=hTt[:, kc, j * 128:(j + 1) * 128],
                                                       rhs=W1[:, kc, c0:c1], start=(kc == 0), stop=(kc == 7)),
                              reads=[r_hT[j], r_W1], writes=[r_ps])
                ss, r_ss = ss_t[tt % 2]
                sd2, r_sd2 = sd2_t[tt % 2]
                rs, r_rs = rs_t[tt % 2]
                kb.op("act", lambda e: e.activation(out=junk[:], in_=ps[:, 0:128], func=AF.Square, accum_out=ss[:]),
                      reads=[r_ps], writes=[r_junk, r_ss])
                kb.op("act", lambda e: e.activation(out=sd2[:], in_=ss[:], func=AF.Sqrt, bias=reps[:, 0:1],
                                                    scale=1.0 / 128.0), reads=[r_ss, r_reps], writes=[r_sd2])
                kb.op("dve", lambda e: e.reciprocal(out=rs[:], in_=sd2[:]), reads=[r_sd2], writes=[r_rs])
                ckv_t, r_ckv = s_ckv[sp_]
                kb.op("dve", lambda e: e.scalar_tensor_tensor(out=ckv_t[:, j, :], in0=ps[:, 0:128], scalar=rs[:, 0:1],
                                                              in1=gkvB[:], op0=ALU.mult, op1=ALU.mult),
                      reads=[r_ps, r_rs, r_gkvB], writes=[r_ckv[j]])
                wi_t, r_wi = s_wi[sp_]
                kb.op("dve", lambda e: e.tensor_scalar(out=wi_t[:, j, :], in0=ps[:, 128:132], scalar1=1.0 / 16.0,
                                                       scalar2=None, op0=ALU.mult), reads=[r_ps], writes=[r_wi[j]])
                kb.op("pe", lambda e: e.transpose(out=bfv(psT2)[:, j * 128:(j + 1) * 128], in_=ckv_t[:, j, :],
                                                  identity=ident[:]), reads=[r_ckv[j], r_id], writes=[r_psT2])
                ps, r_ps = pp.next()
                for kc in range(8):
                    kb.op("pe", lambda e: e.matmul(ps[:], lhsT=hTt[:, kc, j * 128:(j + 1) * 128],
                                                   rhs=W1[:, kc, 1988:2500], start=(kc == 0), stop=(kc == 7)),
                          reads=[r_hT[j], r_W1], writes=[r_ps])
                vb_t, r_vb = s_vb[sp_]
                evac(ev.next(), vb_t[:, j, :], ps[:], [r_ps], [r_vb[j]])
            ckvT_t, r_ckvT = s_ckvT[sp_]
            evac(ev.next(), ckvT_t[:].rearrange("p j t -> p (j t)"), bfv(psT2)[:, 0:512], [r_psT2], r_ckvT)
            rows = slice(t0, t0 + 512)
            kb.dma("sp", C.ckv_d[rows, :].rearrange("(j p) r -> p j r", p=128), ckv_t[:], reads=r_ckv, writes=[C.r_ckv])
            kb.dma("sp", C.widx_d[rows, :].rearrange("(j p) r -> p j r", p=128), wi_t[:], reads=r_wi, writes=[C.r_widx])
            kb.dma("sp", C.vb_d[rows, :].rearrange("(j p) r -> p j r", p=128), vb_t[:], reads=r_vb, writes=[C.r_vb])
            kb.dma("sp", C.ckvT_d[:, rows], ckvT_t[:].rearrange("p j t -> p (j t)"), reads=r_ckvT, writes=[C.r_ckvT])
            allj = r_hT
            groups = [("qa", 0, 4), ("qidx", 640, 2), ("kidx", None, 1), ("qb", 964, 4), ("kb", 1476, 4)]
            for (gname, c0, nch) in groups:
                for c in range(nch):
                    ps, r_ps = pp.next()
                    for kc in range(8):
                        if gname == "kidx":
                            lw, rl = Wkk[:, kc, :], r_Wkk
                        else:
                            lw, rl = W1[:, kc, c0 + c * 128:c0 + (c + 1) * 128], r_W1
                        kb.op("pe", lambda e: e.matmul(ps[:], lhsT=lw, rhs=hTt[:, kc, :], start=(kc == 0), stop=(kc == 7)),
                              reads=allj + [rl], writes=[r_ps])
                    if gname == "qa":
                        qa, r_qa = qaT[c % 2]
                        evac(ev.next(), qa[:], ps[:], [r_ps], [r_qa])
                        for hp in range(2):
                            h = 2 * c + hp
                            ps2, r_ps2 = pp.next()
                            kb.op("pe", lambda e: e.matmul(ps2[:], lhsT=wuk[hp * 64:(hp + 1) * 64, c, :],
                                                           rhs=qa[hp * 64:(hp + 1) * 64, :], start=True, stop=True),
                                  reads=[r_wuk, r_qa], writes=[r_ps2])
                            stg, r_stg = s_qlat[sp_]
                            evac(ev.next(), stg[:, h, :], ps2[:], [r_ps2], [r_stg[h]], scale=0.125)
                    else:
                        stg, r_stg = {"qidx": s_qidx, "kidx": s_kidx, "qb": s_qb, "kb": s_kb}[gname][sp_]
                        evac(ev.next(), stg[:, c, :], ps[:], [r_ps], [r_stg[c]], scale=(0.125 if gname == "kb" else None))
            cols = slice(t0, t0 + 512)
            stg, r_stg = s_qlat[sp_]
            kb.dma("sp", C.qlatT_d[:, :, cols].rearrange("h p t -> p h t"), stg[:], reads=r_stg, writes=[C.r_qlatT])
            stg, r_stg = s_qidx[sp_]
            kb.dma("sp", C.qidxT_d[:, :, cols].rearrange("h p t -> p h t"), stg[:], reads=r_stg, writes=[C.r_qidxT])
            stg, r_stg = s_kidx[sp_]
            kb.dma("sp", C.kidxT_d[:, cols], stg[:, 0, :], reads=r_stg, writes=[C.r_kidxT])
            stg, r_stg = s_qb[sp_]
            kb.dma("sp", C.qbT_d[:, :, cols].rearrange("h p t -> p h t"), stg[:], reads=r_stg, writes=[C.r_qbT])
            stg, r_stg = s_kb[sp_]
            kb.dma("sp", C.kbT_d[:, :, cols].rearrange("h p t -> p h t"), stg[:], reads=r_stg, writes=[C.r_kbT])
        kb.barrier()


def phase_sb(C, l):
    nc, kb = C.nc, C.kb
    with contextlib.ExitStack() as ph:
        negU, r_negU = T(C, ph, "negU", [128, 128], BF16)
        ones, r_ones = T(C, ph, "ones", [128, 128], BF16)
        cm, r_cm = T(C, ph, "cm", [128, 4, 512], BF16)
        kb.dma("sp", negU[:], C.negU_bf[:, :], writes=[r_negU])
        kb.dma("sp", ones[:], C.ones_bf[:, :], writes=[r_ones])
        kb.dma("sp", cm[:], C.cm_bf.rearrange("r p t -> p r t"), writes=[r_cm])
        qT = [T(C, ph, f"sbq{i}", [128, S], BF16) for i in range(2)]
        kT = [T(C, ph, f"sbk{i}", [128, S], BF16) for i in range(2)]
        vv = [T(C, ph, f"sbv{i}", [128, NT, 128], BF16) for i in range(2)]
        et = [T(C, ph, f"et{i}", [128, 512], F32) for i in range(2)]
        spT = [T(C, ph, f"spT{i}", [128, 512], BF16) for i in range(4)]
        arg2 = [T(C, ph, f"arg2{i}", [128, 512], F32) for i in range(2)]
        AT = [T(C, ph, f"AT{i}", [128, 512], BF16) for i in range(4)]
        carry = [T(C, ph, f"carry{i}", [128, 512], F32) for i in range(2)]
        ostg = [T(C, ph, f"ostg{i}", [128, 512], BF16) for i in range(2)]
        psz = C.ps[0:2]
        psa = C.ps[2:4]
        psc = C.ps[4:5]
        pso = C.ps[5:7]

        items = []
        gid = 0
        for c in range(4):
            for qi in range(NST):
                for hp in range(2):
                    nb = 4 * qi + 4
                    for j in range(nb - 1, -1, -1):
                        items.append(dict(c=c, qi=qi, hp=hp, j=j, first=(j == nb - 1), last=(j == 0), g=gid,
                                          rel=(j - 4 * qi)))
                    gid += 1
        loaded = set()

        def load_pair(c):
            if c in loaded or c >= 4:
                return
            loaded.add(c)
            q, r_q = qT[c % 2]
            k, r_k = kT[c % 2]
            v, r_v = vv[c % 2]
            kb.dma("sp", q[:], C.qbT_d[c], reads=[C.r_qbT], writes=[r_q])
            kb.dma("sp", k[:], C.kbT_d[c], reads=[C.r_kbT], writes=[r_k])
            kb.dma("sp", v[:], C.vb_d[:, c * 128:(c + 1) * 128].rearrange("(j p) d -> p j d", p=128),
                   reads=[C.r_vb], writes=[r_v])

        def stA(n, it):
            c, qi, hp, j = it["c"], it["qi"], it["hp"], it["j"]
            q, r_q = qT[c % 2]
            k, r_k = kT[c % 2]
            P = slice(hp * 64, hp * 64 + 64)
            pz, r_pz = psz[n % 2]
            kb.op("pe", lambda e: e.matmul(pz[:], lhsT=k[P, j * 128:(j + 1) * 128], rhs=q[P, qi * 512:(qi + 1) * 512],
                                           start=True, stop=True), reads=[r_k, r_q], writes=[r_pz])
            e_, r_e = et[n % 2]
            kb.op("act", lambda e: e.activation(out=e_[:], in_=pz[:], func=AF.Exp), reads=[r_pz], writes=[r_e])

        def stA2(n, it):
            e_, r_e = et[n % 2]
            s_, r_s = spT[n % 4]
            kb.op("act", lambda e: e.activation(out=s_[:], in_=e_[:], func=AF.Ln, bias=1.0, scale=1.0),
                  reads=[r_e], writes=[r_s])
            if it["rel"] >= 0:
                kb.op("dve", lambda e: e.tensor_tensor(out=s_[:], in0=s_[:], in1=cm[:, it["rel"], :], op=ALU.mult),
                      reads=[r_s, r_cm], writes=[r_s])

        def stB(n, it):
            c, qi, hp, j = it["c"], it["qi"], it["hp"], it["j"]
            q, r_q = qT[c % 2]
            k, r_k = kT[c % 2]
            P = slice(hp * 64, hp * 64 + 64)
            s_, r_s = spT[n % 4]
            pa, r_pa = psa[n % 2]
            pc, r_pc = psc[0]
            kb.op("pe", lambda e: e.matmul(pa[:], lhsT=k[P, j * 128:(j + 1) * 128], rhs=q[P, qi * 512:(qi + 1) * 512],
                                           start=True, stop=False), reads=[r_k, r_q], writes=[r_pa])
            kb.op("pe", lambda e: e.matmul(pa[:], lhsT=negU[:], rhs=s_[:], start=False, stop=True),
                  reads=[r_negU, r_s], writes=[r_pa])
            kk = n % 2
            cb, r_cb = carry[kk]
            cbn, r_cbn = carry[1 - kk]
            if it["first"]:
                kb.op("dve", lambda e: e.memset(cb[:], 0.0), writes=[r_cb])
            if not it["last"]:
                kb.op("pe", lambda e: e.matmul(pc[:], lhsT=ones[:], rhs=s_[:], start=True, stop=True),
                      reads=[r_ones, r_s], writes=[r_pc])
                kb.op("dve", lambda e: e.tensor_tensor(out=cbn[:], in0=pc[:], in1=cb[:], op=ALU.add),
                      reads=[r_pc, r_cb], writes=[r_cbn])
            a2, r_a2 = arg2[n % 2]
            kb.op("dve", lambda e: e.tensor_tensor(out=a2[:], in0=pa[:], in1=cb[:], op=ALU.subtract),
                  reads=[r_pa, r_cb], writes=[r_a2])

        def stB2(n, it):
            a2, r_a2 = arg2[n % 2]
            a_, r_a = AT[n % 4]
            kb.op("act", lambda e: e.activation(out=a_[:], in_=a2[:], func=AF.Exp), reads=[r_a2], writes=[r_a])
            if it["rel"] >= 0:
                kb.op("pool", lambda e: e.tensor_tensor(out=a_[:], in0=a_[:], in1=cm[:, it["rel"], :], op=ALU.mult),
                      reads=[r_a, r_cm], writes=[r_a])

        def stC(n, it):
            c, qi, hp, j = it["c"], it["qi"], it["hp"], it["j"]
            v, r_v = vv[c % 2]
            a_, r_a = AT[n % 4]
            po, r_po = pso[(c * NST + qi) % 2]
            kb.op("pe", lambda e: e.matmul(po[hp * 64:(hp + 1) * 64, :], lhsT=v[:, j, hp * 64:(hp + 1) * 64], rhs=a_[:],
                                           start=it["first"], stop=it["last"]), reads=[r_v, r_a], writes=[r_po])
            if it["last"] and hp == 1:
                og, r_og = ostg[(c * NST + qi) % 2]
                kb.op("dve", lambda e: e.tensor_copy(out=og[:], in_=po[:]), reads=[r_po], writes=[r_og])
                kb.dma("sp", C.obT_d[c, :, qi * 512:(qi + 1) * 512], og[:], reads=[r_og], writes=[C.r_obT])
                if qi == NST - 1:
                    load_pair(c + 2)

        N = len(items)
        load_pair(0)
        load_pair(1)
        for n in range(N + 3):
            if n < N:
                stA(n, items[n])
            if 0 <= n - 2 < N:
                stB2(n - 2, items[n - 2])
            if n < N:
                stA2(n, items[n])
            if 0 <= n - 1 < N:
                stB(n - 1, items[n - 1])
            if 0 <= n - 3 < N:
                stC(n - 3, items[n - 3])
        kb.barrier()


def phase_dsa(C, l):
    nc, kb = C.nc, C.kb
    with contextlib.ExitStack() as ph:
        kidxT, r_kidxT = T(C, ph, "kidxT", [128, S], BF16)
        ckvT, r_ckvT = T(C, ph, "ckvT", [128, S], BF16)
        ckv, r_ckv = T(C, ph, "ckv", [128, NT, 128], BF16)
        wuv, r_wuv = T(C, ph, "wuv", [128, 8, 64], BF16)
        sc, r_sc = T(C, ph, "sc", [128, S], F32)
        mb, r_mb = T(C, ph, "mb", [128, S], BF16)
        dmask, r_dmask = T(C, ph, "dmask", [128, 128], F32)
        Irep, r_Irep = T(C, ph, "Irep", [128, 512], BF16)
        onesF, r_onesF = T(C, ph, "onesF", [128, 128], BF16)
        identf, r_identf = T(C, ph, "identf", [128, 128], F32)
        Tz, r_Tz = T(C, ph, "Tz", [128, 2, 1024], F32)
        b31B, r_b31B = T(C, ph, "b31B", [128, 8], F32)
        bmask, r_bmask = T(C, ph, "bmask", [8, 1024], F32)
        rbT, r_rbT = T(C, ph, "rbT", [8, 32], F32)
        c8, r_c8 = T(C, ph, "c8", [8, 1], F32)
        b31c, r_b31c = T(C, ph, "b31c", [40, 1], F32)
        b31hi, r_b31hi = T(C, ph, "b31hi", [40, 1], BF16)
        b31hif, r_b31hif = T(C, ph, "b31hif", [40, 1], F32)
        b31lo, r_b31lo = T(C, ph, "b31lo", [72, 1], F32)
        CB, r_CB = T(C, ph, "CB", [72, 1024], BF16)
        CBd = Res("CBdyn")
        kb.dma("sp", kidxT[:], C.kidxT_d[:, :], reads=[C.r_kidxT], writes=[r_kidxT])
        kb.dma("sp", ckvT[:], C.ckvT_d[:, :], reads=[C.r_ckvT], writes=[r_ckvT])
        kb.dma("sp", ckv[:], C.ckv_d.rearrange("(j p) r -> p j r", p=128), reads=[C.r_ckv], writes=[r_ckv])
        kb.dma("pool", wuv[:], C.w_uv[l], writes=[r_wuv])
        kb.dma("sp", dmask[:], C.dmask[:, :], writes=[r_dmask])
        kb.dma("sp", Irep[:], C.irep_bf[:, :], writes=[r_Irep])
        kb.dma("sp", onesF[:], C.ones_bf[:, :], writes=[r_onesF])
        kb.dma("sp", identf[:], C.ident_f[:, :], writes=[r_identf])
        kb.dma("sp", Tz[:], C.tz.rearrange("r p c -> p r c"), writes=[r_Tz])
        kb.dma("sp", b31B[:], C.rel_bias[31:32, :].to_broadcast([128, 8]), writes=[r_b31B])
        kb.dma("sp", bmask[:], C.bmask[:, :], writes=[r_bmask])
        kb.dma("sp", rbT[:], C.rel_bias_t[:, :], writes=[r_rbT])
        for r_ in range(2):
            kb.op("dve", lambda e: e.tensor_tensor(out=Tz[:, r_, :].rearrange("p (h t) -> p h t", h=8),
                                                   in0=Tz[:, r_, :].rearrange("p (h t) -> p h t", h=8),
                                                   in1=b31B[:].to_broadcast([128, 8, 128]) if False else
                                                   b31B[:].unsqueeze(2).to_broadcast([128, 8, 128]),
                                                   op=ALU.subtract), reads=[r_Tz, r_b31B], writes=[r_Tz])
        kb.op("dve", lambda e: e.reduce_max(out=c8[:], in_=rbT[:], axis=AX.X), reads=[r_rbT], writes=[r_c8])
        kb.op("dve", lambda e: e.tensor_scalar(out=c8[:], in0=c8[:], scalar1=-1.0, scalar2=None, op0=ALU.mult),
              reads=[r_c8], writes=[r_c8])
        kb.op("pool", lambda e: e.memset(CB[:], 0.0), writes=[r_CB])
        kb.dma("sp", b31c[32:40, :], C.rel_bias_t[:, 31:32], writes=[r_b31c], allow_slow_non_contiguous=True)
        kb.dma("sp", b31lo[64:72, :], C.rel_bias_t[:, 31:32], writes=[r_b31lo], allow_slow_non_contiguous=True)
        kb.op("dve", lambda e: e.tensor_copy(out=b31hi[32:40, :], in_=b31c[32:40, :]), reads=[r_b31c], writes=[r_b31hi])
        kb.op("dve", lambda e: e.tensor_copy(out=b31hif[32:40, :], in_=b31hi[32:40, :]), reads=[r_b31hi], writes=[r_b31hif])
        kb.dma("sp", b31lo[32:40, :], b31hif[32:40, :], reads=[r_b31hif], writes=[r_b31lo])
        bm32, r_bm32 = T(C, ph, "bm32", [72, 1024], F32)
        kb.dma("sp", bm32[32:40, :], C.bmask[:, :], writes=[r_bm32])
        kb.dma("sp", bm32[64:72, :], C.bmask[:, :], writes=[r_bm32])
        kb.op("dve", lambda e: e.tensor_scalar(out=CB[32:40, :], in0=bm32[32:40, :], scalar1=b31hif[32:40, 0:1],
                                               scalar2=None, op0=ALU.mult), reads=[r_bm32, r_b31hif], writes=[r_CB])
        hi64, r_hi64 = T(C, ph, "hi64", [72, 1], F32)
        kb.dma("sp", hi64[64:72, :], b31hif[32:40, :], reads=[r_b31hif], writes=[r_hi64])
        kb.op("dve", lambda e: e.tensor_tensor(out=b31lo[64:72, :], in0=b31lo[64:72, :], in1=hi64[64:72, :], op=ALU.subtract),
              reads=[r_b31lo, r_hi64], writes=[r_b31lo])
        kb.op("dve", lambda e: e.tensor_scalar(out=CB[64:72, :], in0=bm32[64:72, :], scalar1=b31lo[64:72, 0:1],
                                               scalar2=None, op0=ALU.mult), reads=[r_bm32, r_b31lo], writes=[r_CB])

        CB2, r_CB2 = T(C, ph, "CB2", [72, 1024], BF16)
        kb.op("dve", lambda e: e.tensor_copy(out=CB2[:], in_=CB[:]), reads=[r_CB], writes=[r_CB2])
        CB3, r_CB3 = T(C, ph, "CB3", [72, 1024], BF16)
        kb.op("dve", lambda e: e.tensor_copy(out=CB3[:], in_=CB[:]), reads=[r_CB], writes=[r_CB3])
        CBs = [(CB, r_CB, Res("CBdyn0")), (CB2, r_CB2, Res("CBdyn1")), (CB3, r_CB3, Res("CBdyn2"))]
        g8, r_g8 = T(C, ph, "g8", [8, 128], F32)
        negK, r_negK = T(C, ph, "negK", [8, 1], F32)
        kb.dma("sp", g8[:], C.g_kv[l:l + 1, :].to_broadcast([8, 128]), writes=[r_g8])
        kb.op("dve", lambda e: e.tensor_reduce(out=negK[:], in_=g8[:], axis=AX.X, op=ALU.max, apply_absolute_value=True),
              reads=[r_g8], writes=[r_negK])
        kb.op("dve", lambda e: e.tensor_scalar(out=negK[:], in0=negK[:], scalar1=-1.02 * (128.0 ** 0.5), scalar2=None,
                                               op0=ALU.mult), reads=[r_negK], writes=[r_negK])
        sc2, r_sc2 = T(C, ph, "sc2", [128, S], F32)
        mb2, r_mb2 = T(C, ph, "mb2", [128, S], BF16)
        scs = [(sc, r_sc), (sc2, r_sc2)]
        mbs = [(mb, r_mb), (mb2, r_mb2)]
        qi_t = [T(C, ph, f"qi{i}", [128, 2, 128], BF16) for i in range(3)]
        wi_t = [T(C, ph, f"wi{i}", [128, 4], F32) for i in range(3)]
        ql_t = [T(C, ph, f"ql{i}", [128, 1024], BF16) for i in range(3)]
        qsq, r_qsq = T(C, ph, "qsq", [128, 1024], BF16)
        sq8, r_sq8 = T(C, ph, "sq8", [8, 1024], F32)
        rl = [T(C, ph, f"rl{i}", [128, 512], F32) for i in range(4)]
        rtmp, r_rtmp = T(C, ph, "rtmp", [128, 512], F32)
        m8, r_m8 = T(C, ph, "m8", [128, 8], F32)
        zt = [T(C, ph, f"zt{i}", [128, 512], F32) for i in range(2)]
        pT = [T(C, ph, f"pT{i}", [128, 512], BF16) for i in range(3)]
        rden, r_rden = T(C, ph, "rden", [128, 512], F32)
        olT, r_olT = T(C, ph, "olT", [128, 1024], BF16)
        oast = [T(C, ph, f"oast{i}", [128, 4, 128], BF16) for i in range(2)]
        psA = Rot(C.ps[0:2])
        psZ = C.ps[2:4]
        psO = C.ps[4:6]
        psD = C.ps[6:8]
        nctr = [0]

        def chunks_of(i):
            nk = (i + 1) * 128
            return [(c0, min(512, nk - c0)) for c0 in range(0, nk, 512)]

        def stL(i):
            tcols = slice(i * 128, (i + 1) * 128)
            qi, r_qi = qi_t[i % 3]
            wi, r_wi = wi_t[i % 3]
            ql, r_ql = ql_t[i % 3]
            kb.dma("sp", qi[:], C.qidxT_d[:, :, tcols].rearrange("c p t -> p c t"), reads=[C.r_qidxT], writes=[r_qi])
            kb.dma("sp", wi[:], C.widx_d[tcols, :], reads=[C.r_widx], writes=[r_wi])
            kb.dma("sp", ql[:].rearrange("p (h t) -> p h t", h=8), C.qlatT_d[:, :, tcols].rearrange("h p t -> p h t"),
                   reads=[C.r_qlatT], writes=[r_ql])
            cb, r_cb, r_cbd = CBs[i % 3]
            kb.op("dve", lambda e: e.tensor_tensor(out=qsq[:], in0=ql[:], in1=ql[:], op=ALU.mult),
                  reads=[r_ql], writes=[r_qsq])
            ps, r_ps = psA.next()
            ps2, r_ps2 = psA.next()
            for half, (p_, r_p) in enumerate(((ps, r_ps), (ps2, r_ps2))):
                kb.op("pe", lambda e: e.matmul(p_[0:8, :], lhsT=onesF[:, 0:8], rhs=qsq[:, half * 512:(half + 1) * 512],
                                               start=True, stop=True), reads=[r_onesF, r_qsq], writes=[r_p])
                kb.op("act", lambda e: e.activation(out=sq8[:, half * 512:(half + 1) * 512], in_=p_[0:8, :], func=AF.Sqrt),
                      reads=[r_p], writes=[r_sq8])
            kb.op("dve", lambda e: e.tensor_scalar(out=sq8[:], in0=sq8[:], scalar1=negK[:, 0:1], scalar2=c8[:, 0:1],
                                                    op0=ALU.mult, op1=ALU.add), reads=[r_sq8, r_negK, r_c8], writes=[r_sq8])
            kb.op("dve", lambda e: e.tensor_tensor(out=cb[0:8, :], in0=sq8[:], in1=bmask[:], op=ALU.mult),
                  reads=[r_sq8, r_bmask, r_cb], writes=[r_cbd])

        def stA(i):
            tcols = slice(i * 128, (i + 1) * 128)
            qi, r_qi = qi_t[i % 3]
            wi, r_wi = wi_t[i % 3]
            sc_, r_sc_ = scs[i % 2]
            for (c0, w) in chunks_of(i):
                for h in range(4):
                    P = slice((h % 2) * 64, (h % 2) * 64 + 64)
                    ps, r_ps = psA.next()
                    kb.op("pe", lambda e: e.matmul(ps[:, 0:w], lhsT=qi[P, h // 2, :], rhs=kidxT[P, c0:c0 + w],
                                                   start=True, stop=True), reads=[r_qi, r_kidxT], writes=[r_ps])
                    r_, r_r = rl[h]
                    kb.op("act", lambda e: e.activation(out=r_[:, 0:w], in_=ps[:, 0:w], func=AF.Relu),
                          reads=[r_ps], writes=[r_r])
                    if h == 0:
                        kb.op("dve", lambda e: e.tensor_scalar(out=sc_[:, c0:c0 + w], in0=r_[:, 0:w], scalar1=wi[:, 0:1],
                                                               scalar2=None, op0=ALU.mult),
                              reads=[r_r, r_wi], writes=[r_sc_])
                    else:
                        kb.op("dve", lambda e: e.scalar_tensor_tensor(out=sc_[:, c0:c0 + w], in0=r_[:, 0:w],
                                                                      scalar=wi[:, h:h + 1], in1=sc_[:, c0:c0 + w],
                                                                      op0=ALU.mult, op1=ALU.add),
                              reads=[r_r, r_wi, r_sc_], writes=[r_sc_])
            kb.op("dve", lambda e: e.tensor_tensor(out=sc_[:, tcols], in0=sc_[:, tcols], in1=dmask[:], op=ALU.add),
                  reads=[r_sc_, r_dmask], writes=[r_sc_])

        def stB(i):
            nk = (i + 1) * 128
            sc_, r_sc_ = scs[i % 2]
            mb_, r_mb_ = mbs[i % 2]
            if i >= 2:
                for rnd in range(TOPK // 8):
                    kb.op("dve", lambda e: e.max(out=m8[:], in_=sc_[:, 0:nk]), reads=[r_sc_], writes=[r_m8],
                          nosame=(rnd > 0))
                    kb.op("dve", lambda e: e.match_replace(out=sc_[:, 0:nk], in_to_replace=m8[:], in_values=sc_[:, 0:nk],
                                                           imm_value=2.0 * NEG), reads=[r_sc_, r_m8], writes=[r_sc_])
                kb.op("dve", lambda e: e.tensor_scalar(out=mb_[:, 0:nk], in0=sc_[:, 0:nk], scalar1=1.5 * NEG, scalar2=MBIG,
                                                       op0=ALU.is_gt, op1=ALU.mult), reads=[r_sc_], writes=[r_mb_])
            else:
                kb.op("dve", lambda e: e.tensor_scalar(out=mb_[:, 0:nk], in0=sc_[:, 0:nk], scalar1=0.5 * NEG, scalar2=MBIG,
                                                       op0=ALU.is_le, op1=ALU.mult), reads=[r_sc_], writes=[r_mb_])

        def stF(i, jbs):
            ql, r_ql = ql_t[i % 3]
            mb_, r_mb_ = mbs[i % 2]
            cb, r_cb, r_cbd = CBs[i % 3]
            items = [(half, jb) for half in range(2) for jb in jbs]
            first_jb, last_jb = i, 0

            def fa(n, it):
                half, jb = it
                hc = slice(half * 512, (half + 1) * 512)
                pz, r_pz = psZ[n % 2]
                kb.op("pe", lambda e: e.matmul(pz[:], lhsT=ckvT[:, jb * 128:(jb + 1) * 128], rhs=ql[:, hc],
                                               start=True, stop=False), reads=[r_ckvT, r_ql], writes=[r_pz])
                kb.op("pe", lambda e: e.matmul(pz[:], lhsT=mb_[:, jb * 128:(jb + 1) * 128], rhs=Irep[:],
                                               start=False, stop=False), reads=[r_mb_, r_Irep], writes=[r_pz])
                kb.op("pe", lambda e: e.matmul(pz[:], lhsT=onesF[0:72, :], rhs=cb[0:72, hc], start=False, stop=True),
                      reads=[r_onesF, r_cb, r_cbd], writes=[r_pz])
                p_, r_p = pT[n % 3]
                rel = i - jb
                if rel <= 1:
                    z_, r_z = zt[n % 2]
                    kb.op("dve", lambda e: e.tensor_tensor(out=z_[:], in0=pz[:], in1=Tz[:, rel, hc], op=ALU.add),
                          reads=[r_pz, r_Tz], writes=[r_z])
                    kb.op("act", lambda e: e.activation(out=p_[:], in_=z_[:], func=AF.Exp), reads=[r_z], writes=[r_p])
                else:
                    kb.op("act", lambda e: e.activation(out=p_[:], in_=pz[:], func=AF.Exp), reads=[r_pz], writes=[r_p])

            def fb(n, it):
                half, jb = it
                p_, r_p = pT[n % 3]
                po, r_po = psO[half]
                pd, r_pd = psD[half]
                kb.op("pe", lambda e: e.matmul(po[:], lhsT=ckv[:, jb, :], rhs=p_[:], start=(jb == first_jb), stop=(jb == last_jb)),
                      reads=[r_ckv, r_p], writes=[r_po])
                kb.op("pe", lambda e: e.matmul(pd[:], lhsT=onesF[:], rhs=p_[:], start=(jb == first_jb), stop=(jb == last_jb)),
                      reads=[r_onesF, r_p], writes=[r_pd])

            N = len(items)
            base = nctr[0]
            for n in range(N + 1):
                if n < N:
                    fa(base + n, items[n])
                if n >= 1:
                    fb(base + n - 1, items[n - 1])
            nctr[0] += N

        def stT(i):
            tcols = slice(i * 128, (i + 1) * 128)
            for half in range(2):
                hc = slice(half * 512, (half + 1) * 512)
                po, r_po = psO[half]
                pd, r_pd = psD[half]
                kb.op("dve", lambda e: e.reciprocal(out=rden[:], in_=pd[:]), reads=[r_pd], writes=[r_rden])
                kb.op("dve", lambda e: e.tensor_tensor(out=olT[:, hc], in0=po[:], in1=rden[:], op=ALU.mult),
                      reads=[r_po, r_rden], writes=[r_olT])
            ps, r_ps = psA.next()
            for h in range(8):
                kb.op("pe", lambda e: e.matmul(ps[(h % 2) * 64:(h % 2) * 64 + 64, (h // 2) * 128:(h // 2 + 1) * 128],
                                               lhsT=wuv[:, h, :], rhs=olT[:, h * 128:(h + 1) * 128], start=True, stop=True),
                      reads=[r_wuv, r_olT], writes=[r_ps])
            og, r_og = oast[i % 2]
            kb.op("act", lambda e: e.copy(out=og[:].rearrange("p c t -> p (c t)"), in_=ps[:]), reads=[r_ps], writes=[r_og])
            kb.dma("sp", C.oaT_d[:, :, tcols].rearrange("c p t -> p c t"), og[:], reads=[r_og], writes=[C.r_oaT])

        stL(0)
        stL(1)
        stA(0)
        stB(0)
        stA(1)
        for i in range(NT):
            if i + 2 < NT:
                stL(i + 2)
            stF(i, [jb for jb in (i, i - 1) if jb >= 0])
            if i + 2 < NT:
                stA(i + 2)
            if i + 1 < NT:
                stB(i + 1)
            if i >= 2:
                stF(i, list(range(i - 2, -1, -1)))
            stT(i)
        kb.barrier()


def bload(C, ph, name, src_ap, n, reads=()):
    t, r = T(C, ph, name, [128, n], F32)
    C.kb.dma("sp", t[:], src_ap, reads=list(reads), writes=[r])
    return t, r


def phase_out(C, l, x_src, r_xsrc):
    nc, kb = C.nc, C.kb
    with contextlib.ExitStack() as ph:
        Wg, r_Wg = T(C, ph, "Wg", [128, 8, 2048], BF16)
        wao, r_wao = T(C, ph, "wao", [128, 4, 1024], BF16)
        wbo, r_wbo = T(C, ph, "wbo", [128, 4, 1024], BF16)
        wo, r_wo = T(C, ph, "wo", [128, 8, 1024], BF16)
        wr, r_wr = T(C, ph, "wr", [128, 8, 32], F32)
        kb.dma("pool", Wg[:], C.w_in[l, :, NW1:NCOLS].rearrange("(kc p) n -> p kc n", p=128), writes=[r_Wg])
        kb.dma("pool", wao[:], C.w_a_out[l].rearrange("(kc p) n -> p kc n", p=128), writes=[r_wao])
        kb.dma("pool", wbo[:], C.w_b_out[l].rearrange("(kc p) n -> p kc n", p=128), writes=[r_wbo])
        kb.dma("pool", wo[:], C.w_o[l].rearrange("(kc p) n -> p kc n", p=128), writes=[r_wo])
        kb.dma("sp", wr[:], C.w_router[l].rearrange("(kc p) n -> p kc n", p=128), writes=[r_wr])
        sc1, r_sc1 = bload(C, ph, "sc1", C.mod_d[l, :, 1024:2048], 1024, [C.r_mod])
        sh1, r_sh1 = bload(C, ph, "sh1", C.mod_d[l, :, 0:1024], 1024, [C.r_mod])
        g1, r_g1 = bload(C, ph, "g1", C.mod_d[l, :, 2048:3072], 1024, [C.r_mod])
        sh2, r_sh2 = bload(C, ph, "sh2", C.mod_d[l, :, 3072:4096], 1024, [C.r_mod])
        sc2, r_sc2 = bload(C, ph, "sc2", C.mod_d[l, :, 4096:5120], 1024, [C.r_mod])
        lg, r_lg = bload(C, ph, "lg", C.ln1_g[l:l + 1, :].to_broadcast([128, 1024]), 1024)
        lb, r_lb = bload(C, ph, "lb", C.ln1_b[l:l + 1, :].to_broadcast([128, 1024]), 1024)
        brB, r_brB = bload(C, ph, "brB", C.b_router[l:l + 1, :].to_broadcast([128, 32]), 32)
        ident, r_id = T(C, ph, "identb", [128, 128], BF16)
        identf, r_idf = T(C, ph, "identf2", [128, 128], F32)
        kb.dma("sp", ident[:], C.ident_bf[:, :], writes=[r_id])
        kb.dma("sp", identf[:], C.ident_f[:, :], writes=[r_idf])
        LW = ln_work(C, ph, "olw", LN_EPS)
        xres = [T(C, ph, "xres0", [128, 4, 1024], F32)[0]] * 2
        r_xres = [[Res(f"xres_{j}") for j in range(4)]] * 2
        hb, r_hb = T(C, ph, "ohb", [128, 1024], BF16)
        hT, _ = T(C, ph, "ohT", [128, 8, 512], BF16)
        r_hT = [Res(f"ohT_{j}") for j in range(4)]
        oaT, r_oaT = T(C, ph, "ooaT", [128, 4, 512], BF16)
        obT, r_obT = T(C, ph, "oobT", [128, 4, 512], BF16)
        sga = [T(C, ph, f"sga{i}", [128, 512], F32) for i in range(2)]
        sgb = [T(C, ph, f"sgb{i}", [128, 512], F32) for i in range(2)]
        t1 = [T(C, ph, f"t1{i}", [128, 512], F32) for i in range(2)]
        t2 = [T(C, ph, f"t2{i}", [128, 512], F32) for i in range(2)]
        mT, _ = T(C, ph, "mergT", [128, 8, 512], BF16)
        r_mT = [Res(f"mergT_{j}") for j in range(8)]
        yt, r_yt = T(C, ph, "yt", [128, 1024], F32)
        zt, r_zt = T(C, ph, "zt_o", [128, 1024], F32)
        x1t = [T(C, ph, f"x1t{i}", [128, 1024], F32) for i in range(2)]
        h2f, r_h2f = T(C, ph, "h2f", [128, 1024], F32)
        h2T32, r_h2T32 = T(C, ph, "h2T32", [128, 8, 128], F32)
        h2Ts = [T(C, ph, f"h2Ts{i}", [128, 8, 512], BF16)[0] for i in range(2)]
        r_h2Ts = [[Res(f"h2Ts{i}_{j}") for j in range(4)] for i in range(2)]
        lgt, r_lgt = T(C, ph, "lgt", [128, 32], F32)
        m8, r_m8 = T(C, ph, "om8", [128, 8], F32)
        nmx, r_nmx = T(C, ph, "nmx", [128, 1], F32)
        msk, r_msk = T(C, ph, "msk", [128, 32], F32)
        ex, r_ex = T(C, ph, "ex", [128, 32], F32)
        rs, r_rs = T(C, ph, "ors", [128, 1], F32)
        gst = [T(C, ph, f"gst{i}", [128, 4, 32], F32)[0] for i in range(2)]
        r_gst = [[Res(f"gst{i}_{j}") for j in range(4)] for i in range(2)]
        gTs = [T(C, ph, f"gTs{i}", [32, 512], F32)[0] for i in range(2)]
        r_gTs = [[Res(f"gTs{i}_{j}") for j in range(4)] for i in range(2)]
        pp = Rot(C.ps[0:4])
        psT, r_psT = C.ps[7]
        psT32 = C.ps[5:7]
        psY = C.ps[4:5]

        for st in range(getattr(C, 'out_nst', NST)):
            stage = getattr(C, 'out_stage', 99)
            sp_ = st % 2
            t0 = st * 512
            cols = slice(t0, t0 + 512)
            xr = xres[sp_]
            for j in range(4):
                tt = st * 4 + j
                kb.dma("sp", xr[:, j, :], x_src[tt * 128:(tt + 1) * 128, :], reads=[r_xsrc], writes=[r_xres[sp_][j]])
                ln_modulate(C, LW, xr[:, j, :], r_xres[sp_][j], hb[:], r_hb, sc1[:], r_sc1, sh1[:], r_sh1)
                for kc in range(8):
                    kb.op("pe", lambda e: e.transpose(out=bfv(psT)[:, kc * 128:(kc + 1) * 128],
                                                      in_=hb[:, kc * 128:(kc + 1) * 128], identity=ident[:]),
                          reads=[r_hb, r_id], writes=[r_psT])
                kb.op("act", lambda e: e.copy(out=hT[:, :, j * 128:(j + 1) * 128],
                                              in_=bfv(psT).rearrange("p (kc t) -> p kc t", kc=8)),
                      reads=[r_psT], writes=[r_hT[j]])
            kb.dma("sp", oaT[:], C.oaT_d[:, :, cols].rearrange("c p t -> p c t"), reads=[C.r_oaT], writes=[r_oaT])
            kb.dma("sp", obT[:], C.obT_d[:, :, cols].rearrange("c p t -> p c t"), reads=[C.r_obT], writes=[r_obT])
            for n_ in range(8 if stage >= 1 else 0):
                ncs = slice(n_ * 128, (n_ + 1) * 128)
                pga, r_pga = pp.next()
                pgb, r_pgb = pp.next()
                pa, r_pa = pp.next()
                pb, r_pb = pp.next()
                for kc in range(8):
                    kb.op("pe", lambda e: e.matmul(pga[:], lhsT=Wg[:, kc, n_ * 128:(n_ + 1) * 128], rhs=hT[:, kc, :],
                                                   start=(kc == 0), stop=(kc == 7)), reads=r_hT + [r_Wg], writes=[r_pga])
                for kc in range(8):
                    kb.op("pe", lambda e: e.matmul(pgb[:], lhsT=Wg[:, kc, 1024 + n_ * 128:1024 + (n_ + 1) * 128],
                                                   rhs=hT[:, kc, :], start=(kc == 0), stop=(kc == 7)),
                          reads=r_hT + [r_Wg], writes=[r_pgb])
                for c in range(4):
                    kb.op("pe", lambda e: e.matmul(pa[:], lhsT=wao[:, c, ncs], rhs=oaT[:, c, :], start=(c == 0), stop=(c == 3)),
                          reads=[r_wao, r_oaT], writes=[r_pa])
                for c in range(4):
                    kb.op("pe", lambda e: e.matmul(pb[:], lhsT=wbo[:, c, ncs], rhs=obT[:, c, :], start=(c == 0), stop=(c == 3)),
                          reads=[r_wbo, r_obT], writes=[r_pb])
                sa, r_sa = sga[n_ % 2]
                sb_, r_sb = sgb[n_ % 2]
                a1, r_a1 = t1[n_ % 2]
                a2, r_a2 = t2[n_ % 2]
                kb.op("act", lambda e: e.activation(out=sa[:], in_=pga[:], func=AF.Sigmoid), reads=[r_pga], writes=[r_sa])
                kb.op("act", lambda e: e.activation(out=sb_[:], in_=pgb[:], func=AF.Sigmoid), reads=[r_pgb], writes=[r_sb])
                kb.op("dve", lambda e: e.tensor_tensor(out=a1[:], in0=pa[:], in1=sa[:], op=ALU.mult),
                      reads=[r_pa, r_sa], writes=[r_a1])
                kb.op("dve", lambda e: e.tensor_tensor(out=a2[:], in0=pb[:], in1=sb_[:], op=ALU.mult),
                      reads=[r_pb, r_sb], writes=[r_a2])
                kb.op("pool", lambda e: e.tensor_tensor(out=mT[:, n_, :], in0=a1[:], in1=a2[:], op=ALU.add),
                      reads=[r_a1, r_a2], writes=[r_mT[n_]])
            for j in range(4 if stage >= 2 else 0):
                tt = st * 4 + j
                for nh in range(2):
                    py, r_py = psY[0]
                    for n_ in range(8):
                        kb.op("pe", lambda e: e.matmul(py[:], lhsT=mT[:, n_, j * 128:(j + 1) * 128],
                                                       rhs=wo[:, n_, nh * 512:(nh + 1) * 512], start=(n_ == 0), stop=(n_ == 7)),
                              reads=r_mT + [r_wo], writes=[r_py])
                    kb.op("dve", lambda e: e.tensor_tensor(out=yt[:, nh * 512:(nh + 1) * 512], in0=py[:],
                                                           in1=g1[:, nh * 512:(nh + 1) * 512], op=ALU.mult),
                          reads=[r_py, r_g1], writes=[r_yt])
                kb.op("dve", lambda e: e.scalar_tensor_tensor(out=zt[:], in0=xr[:, j, :], scalar=float(DN_ALPHA), in1=yt[:],
                                                              op0=ALU.mult, op1=ALU.add),
                      reads=[r_xres[sp_][j], r_yt], writes=[r_zt])
                x1, r_x1 = x1t[tt % 2]
                ln_modulate(C, LW, zt, r_zt, x1[:], r_x1, lg[:], r_lg, lb[:], r_lb)
                kb.dma("sp", C.x1_d[tt * 128:(tt + 1) * 128, :], x1[:], reads=[r_x1], writes=[C.r_x1])
                if stage < 3:
                    continue
                ln_modulate(C, LW, x1, r_x1, h2f[:], r_h2f, sc2[:], r_sc2, sh2[:], r_sh2)
                sub = getattr(C, 'out_sub', 99)
                for half in range(2 if sub >= 1 else 0):
                    p32, r_p32 = psT32[half]
                    for k4 in range(4):
                        kc = half * 4 + k4
                        kb.op("pe", lambda e: e.transpose(out=p32[:, k4 * 128:(k4 + 1) * 128],
                                                          in_=h2f[:, kc * 128:(kc + 1) * 128], identity=identf[:]),
                              reads=[r_h2f, r_idf], writes=[r_p32])
                    if sub < 2:
                        continue
                    kb.op("act", lambda e: e.copy(out=h2T32[:, half * 4:half * 4 + 4, :],
                                                  in_=p32[:].rearrange("p (k t) -> p k t", k=4)),
                          reads=[r_p32], writes=[r_h2T32])
                    if sub < 3:
                        continue
                    kb.op("dve", lambda e: e.tensor_copy(out=h2Ts[sp_][:, half * 4:half * 4 + 4, j * 128:(j + 1) * 128],
                                                         in_=p32[:].rearrange("p (k t) -> p k t", k=4)),
                          reads=[r_p32], writes=[r_h2Ts[sp_][j]])
                if stage < 4:
                    continue
                pl, r_pl = pp.next()
                for kc in range(8):
                    kb.op("pe", lambda e: e.matmul(pl[:, 0:32], lhsT=h2T32[:, kc, :], rhs=wr[:, kc, :],
                                                   start=(kc == 0), stop=(kc == 7)), reads=[r_h2T32, r_wr], writes=[r_pl])
                kb.op("dve", lambda e: e.tensor_tensor(out=lgt[:], in0=pl[:, 0:32], in1=brB[:], op=ALU.add),
                      reads=[r_pl, r_brB], writes=[r_lgt])
                kb.op("dve", lambda e: e.max(out=m8[:], in_=lgt[:]), reads=[r_lgt], writes=[r_m8])
                kb.op("dve", lambda e: e.tensor_scalar(out=nmx[:], in0=m8[:, 0:1], scalar1=-1.0, scalar2=None, op0=ALU.mult),
                      reads=[r_m8], writes=[r_nmx])
                kb.op("dve", lambda e: e.tensor_scalar(out=msk[:], in0=lgt[:], scalar1=m8[:, 3:4], scalar2=None, op0=ALU.is_ge),
                      reads=[r_lgt, r_m8], writes=[r_msk])
                kb.op("act", lambda e: e.activation(out=ex[:], in_=lgt[:], func=AF.Exp, bias=nmx[:, 0:1], scale=1.0),
                      reads=[r_lgt, r_nmx], writes=[r_ex])
                kb.op("dve", lambda e: e.tensor_tensor(out=ex[:], in0=ex[:], in1=msk[:], op=ALU.mult),
                      reads=[r_ex, r_msk], writes=[r_ex])
                kb.op("dve", lambda e: e.reduce_sum(out=rs[:], in_=ex[:], axis=AX.X), reads=[r_ex], writes=[r_rs])
                kb.op("dve", lambda e: e.reciprocal(out=rs[:], in_=rs[:]), reads=[r_rs], writes=[r_rs])
                kb.op("dve", lambda e: e.tensor_scalar(out=gst[sp_][:, j, :], in0=ex[:], scalar1=rs[:, 0:1], scalar2=None,
                                                       op0=ALU.mult), reads=[r_ex, r_rs], writes=[r_gst[sp_][j]])
                pg, r_pg = pp.next()
                kb.op("pe", lambda e: e.transpose(out=pg[0:32, 0:128], in_=gst[sp_][:, j, :], identity=identf[:]),
                      reads=[r_gst[sp_][j], r_idf], writes=[r_pg])
                kb.op("act", lambda e: e.copy(out=gTs[sp_][:, j * 128:(j + 1) * 128], in_=pg[0:32, 0:128]),
                      reads=[r_pg], writes=[r_gTs[sp_][j]])
            if stage < 4:
                continue
            kb.dma("sp", C.h2T_d[:, :, cols].rearrange("k p t -> p k t"), h2Ts[sp_][:], reads=r_h2Ts[sp_], writes=[C.r_h2T])
            kb.dma("sp", C.gates_d[cols, :].rearrange("(j p) e -> p j e", p=128), gst[sp_][:], reads=r_gst[sp_],
                   writes=[C.r_gates])
            kb.dma("sp", C.gatesT_d[:, cols], gTs[sp_][:], reads=r_gTs[sp_], writes=[C.r_gatesT])
        kb.barrier()


TS = 1024


def phase_moe(C, l, x_dst, r_xdst):
    nc, kb = C.nc, C.kb
    with contextlib.ExitStack() as ph:
        wgu = [T(C, ph, f"wgu{i}", [128, 8, 2048], BF16) for i in range(2)]
        wdn = [T(C, ph, f"wdn{i}", [128, 8, 1024], BF16) for i in range(2)]
        bgu, r_bgu = T(C, ph, "bgu", [128, E, 16], F32)
        bdn, r_bdn = T(C, ph, "bdn", [32, 1024], F32)
        kb.dma("sp", bgu[:], C.b_gu_t[l].rearrange("e p c -> p e c"), writes=[r_bgu])
        kb.dma("sp", bdn[:], C.b_dn[l], writes=[r_bdn])
        kb.op("pool", lambda e: e.tensor_scalar(out=bgu[:, :, 8:16], in0=bgu[:, :, 8:16], scalar1=1.0, scalar2=None,
                                                op0=ALU.add), reads=[r_bgu], writes=[r_bgu])
        g2, r_g2 = bload(C, ph, "g2", C.mod_d[l, :, 5120:6144], 1024, [C.r_mod])
        lg, r_lg = bload(C, ph, "lg2", C.ln2_g[l:l + 1, :].to_broadcast([128, 1024]), 1024)
        lb, r_lb = bload(C, ph, "lb2", C.ln2_b[l:l + 1, :].to_broadcast([128, 1024]), 1024)
        LW = ln_work(C, ph, "mlw", LN_EPS)
        h2T, r_h2T = T(C, ph, "mh2T", [128, 8, TS], BF16)
        gts, r_gts = T(C, ph, "mgts", [128, TS // 128, 32], F32)
        gT, r_gT = T(C, ph, "mgT", [32, TS], F32)
        acc, _ = T(C, ph, "macc", [128, TS // 128, 1024], F32)
        r_acc = [[Res(f"acc{j}_{nh}") for nh in range(2)] for j in range(TS // 128)]
        a_sb = [T(C, ph, f"a_sb{i}", [128, 512], F32) for i in range(2)]
        sg = [T(C, ph, f"sg{i}", [128, 512], BF16) for i in range(2)]
        gg = [T(C, ph, f"gg{i}", [128, 512], F32) for i in range(2)]
        u_sb = [T(C, ph, f"u_sb{i}", [128, 512], F32) for i in range(2)]
        actT = [T(C, ph, f"actT{i}", [128, 8, 512], BF16)[0] for i in range(2)]
        r_actT = [[Res(f"actT{i}_{f}") for f in range(8)] for i in range(2)]
        x1t, r_x1t = T(C, ph, "mx1t", [128, 1024], F32)
        xo = [(x1t, r_x1t)] * 2
        ppA = Rot(C.ps[0:2])
        ppU = Rot(C.ps[2:4])
        ppY = Rot(C.ps[4:8])
        wloaded = {}

        def load_w(idx):
            if idx in wloaded or idx >= (S // TS) * E:
                return
            wloaded[idx] = True
            e_ = idx % E
            g_, r_g = wgu[idx % 2]
            d_, r_d = wdn[idx % 2]
            kb.dma("pool", g_[:], C.w_gu[l, e_].rearrange("(kc p) n -> p kc n", p=128), writes=[r_g])
            kb.dma("pool", d_[:], C.w_dn[l, e_].rearrange("(kc p) n -> p kc n", p=128), writes=[r_d])

        load_w(0)
        load_w(1)

        def loads_h(ts):
            tcols = slice(ts * TS, (ts + 1) * TS)
            kb.dma("sp", h2T[:], C.h2T_d[:, :, tcols].rearrange("k p t -> p k t"), reads=[C.r_h2T], writes=[r_h2T])

        def loads_g(ts):
            tcols = slice(ts * TS, (ts + 1) * TS)
            kb.dma("sp", gts[:], C.gates_d[tcols, :].rearrange("(j p) e -> p j e", p=128), reads=[C.r_gates], writes=[r_gts])
            kb.dma("sp", gT[:], C.gatesT_d[:, tcols], reads=[C.r_gatesT], writes=[r_gT])

        def acc_init(ts):
            for j in range(TS // 128):
                for nh in range(2):
                    py, r_py = ppY.next()
                    kb.op("pe", lambda e: e.matmul(py[:], lhsT=gT[:, j * 128:(j + 1) * 128], rhs=bdn[:, nh * 512:(nh + 1) * 512],
                                                   start=True, stop=True), reads=[r_gT, r_bdn], writes=[r_py])
                    kb.op("act", lambda e: e.copy(out=acc[:, j, nh * 512:(nh + 1) * 512], in_=py[:]),
                          reads=[r_py], writes=[r_acc[j][nh]])

        def AU(ts, e_, t2):
            idx = ts * E + e_
            g_, r_g = wgu[idx % 2]
            aT = actT[t2 % 2]
            r_aT = r_actT[t2 % 2]
            hc = slice(t2 * 512, (t2 + 1) * 512)
            for fc in range(8):
                pa, r_pa = ppA.next()
                pu, r_pu = ppU.next()
                for kc in range(8):
                    kb.op("pe", lambda e: e.matmul(pa[:], lhsT=g_[:, kc, fc * 128:(fc + 1) * 128], rhs=h2T[:, kc, hc],
                                                   start=(kc == 0), stop=(kc == 7)), reads=[r_g, r_h2T], writes=[r_pa])
                for kc in range(8):
                    kb.op("pe", lambda e: e.matmul(pu[:], lhsT=g_[:, kc, 1024 + fc * 128:1024 + (fc + 1) * 128],
                                                   rhs=h2T[:, kc, hc], start=(kc == 0), stop=(kc == 7)),
                          reads=[r_g, r_h2T], writes=[r_pu])
                a_, r_a = a_sb[fc % 2]
                s_, r_s = sg[fc % 2]
                q_, r_q = gg[fc % 2]
                u_, r_u = u_sb[fc % 2]
                kb.op("dve", lambda e: e.tensor_scalar(out=a_[:], in0=pa[:], scalar1=bgu[:, e_, fc:fc + 1], scalar2=7.0,
                                                       op0=ALU.add, op1=ALU.min), reads=[r_pa, r_bgu], writes=[r_a])
                kb.op("act", lambda e: e.activation(out=s_[:], in_=a_[:], func=AF.Sigmoid, scale=1.702),
                      reads=[r_a], writes=[r_s])
                kb.op("dve", lambda e: e.tensor_scalar(out=u_[:], in0=pu[:], scalar1=bgu[:, e_, 8 + fc:9 + fc], scalar2=8.0,
                                                       op0=ALU.add, op1=ALU.min), reads=[r_pu, r_bgu], writes=[r_u])
                kb.op("pool", lambda e: e.tensor_tensor(out=q_[:], in0=a_[:], in1=s_[:], op=ALU.mult),
                      reads=[r_a, r_s], writes=[r_q])
                kb.op("dve", lambda e: e.scalar_tensor_tensor(out=aT[:, fc, :], in0=u_[:], scalar=-6.0, in1=q_[:],
                                                              op0=ALU.max, op1=ALU.mult),
                      reads=[r_u, r_q], writes=[r_aT[fc]])

        def YY(ts, e_, t2):
            idx = ts * E + e_
            d_, r_d = wdn[idx % 2]
            aT = actT[t2 % 2]
            r_aT = r_actT[t2 % 2]
            for j4 in range(4):
                j = t2 * 4 + j4
                for nh in range(2):
                    py, r_py = ppY.next()
                    for fc in range(8):
                        kb.op("pe", lambda e: e.matmul(py[:], lhsT=aT[:, fc, j4 * 128:(j4 + 1) * 128],
                                                       rhs=d_[:, fc, nh * 512:(nh + 1) * 512],
                                                       start=(fc == 0), stop=(fc == 7)), reads=r_aT + [r_d], writes=[r_py])
                    kb.op("dve", lambda e: e.scalar_tensor_tensor(out=acc[:, j, nh * 512:(nh + 1) * 512], in0=py[:],
                                                                  scalar=gts[:, j, e_:e_ + 1],
                                                                  in1=acc[:, j, nh * 512:(nh + 1) * 512],
                                                                  op0=ALU.mult, op1=ALU.add),
                          reads=[r_py, r_gts, r_acc[j][nh]], writes=[r_acc[j][nh]])
            if t2 == TS // 512 - 1:
                load_w(idx + 2)

        def postnorm(ts):
            for j in range(TS // 128):
                tt = ts * (TS // 128) + j
                kb.dma("sp", x1t[:], C.x1_d[tt * 128:(tt + 1) * 128, :], reads=[C.r_x1], writes=[r_x1t])
                kb.op("pool", lambda e: e.tensor_tensor(out=acc[:, j, :], in0=acc[:, j, :], in1=g2[:], op=ALU.mult),
                      reads=r_acc[j] + [r_g2], writes=r_acc[j])
                kb.op("dve", lambda e: e.scalar_tensor_tensor(out=x1t[:], in0=x1t[:], scalar=float(DN_ALPHA), in1=acc[:, j, :],
                                                              op0=ALU.mult, op1=ALU.add), reads=[r_x1t] + r_acc[j], writes=[r_x1t])
                xo_, r_xo = xo[tt % 2]
                ln_modulate(C, LW, x1t, r_x1t, xo_[:], r_xo, lg[:], r_lg, lb[:], r_lb)
                kb.dma("sp", x_dst[tt * 128:(tt + 1) * 128, :], xo_[:], reads=[r_xo], writes=[r_xdst])

        units = [(ts, e_, t2) for ts in range(S // TS) for e_ in range(E) for t2 in range(TS // 512)]
        loads_h(0)
        loads_g(0)
        acc_init(0)
        for ui, un in enumerate(units):
            ts, e_, t2 = un
            first = (e_ == 0 and t2 == 0 and ts > 0)
            if first:
                loads_h(ts)
            AU(*un)
            if ui >= 1:
                YY(*units[ui - 1])
            if first:
                postnorm(ts - 1)
                loads_g(ts)
                acc_init(ts)
        YY(*units[-1])
        postnorm(S // TS - 1)
        kb.barrier()

def alloc_scratch(C):
    C.mod_d, C.r_mod = dram(C, "mod_d", [DEPTH, 128, 6 * D], F32)
    C.qlatT_d, C.r_qlatT = dram(C, "qlatT_d", [8, 128, S], BF16)
    C.qidxT_d, C.r_qidxT = dram(C, "qidxT_d", [2, 128, S], BF16)
    C.kidxT_d, C.r_kidxT = dram(C, "kidxT_d", [128, S], BF16)
    C.widx_d, C.r_widx = dram(C, "widx_d", [S, 4], F32)
    C.ckv_d, C.r_ckv = dram(C, "ckv_d", [S, 128], BF16)
    C.ckvT_d, C.r_ckvT = dram(C, "ckvT_d", [128, S], BF16)
    C.qbT_d, C.r_qbT = dram(C, "qbT_d", [4, 128, S], BF16)
    C.kbT_d, C.r_kbT = dram(C, "kbT_d", [4, 128, S], BF16)
    C.vb_d, C.r_vb = dram(C, "vb_d", [S, 512], BF16)
    C.obT_d, C.r_obT = dram(C, "obT_d", [4, 128, S], BF16)
    C.oaT_d, C.r_oaT = dram(C, "oaT_d", [4, 128, S], BF16)
    C.x1_d, C.r_x1 = dram(C, "x1_d", [S, D], F32)
    C.x2_d, C.r_x2 = dram(C, "x2_d", [S, D], F32)
    C.h2T_d, C.r_h2T = dram(C, "h2T_d", [8, 128, S], BF16)
    C.gates_d, C.r_gates = dram(C, "gates_d", [S, 32], F32)
    C.gatesT_d, C.r_gatesT = dram(C, "gatesT_d", [32, S], F32)


def build(dbg=(), upto=99, skip_sb=False, skip_dsa=False, nlayers=DEPTH):
    nc = bass.Bass("TRN2", target_bir_lowering=False)
    C = Ctx()
    C.skip_sb = skip_sb
    C.skip_dsa = skip_dsa
    C.nc = nc
    C.dbg = set(dbg)

    def inp(name, shape, dt=F32):
        return nc.dram_tensor(name, list(shape), dt, kind="ExternalInput").ap()

    C.x = inp("x", [S, D])
    C.c_col = inp("c_col", [128, 8])
    C.w_ada = inp("w_ada", [DEPTH, D, 6 * D])
    C.b_ada = inp("b_ada", [DEPTH, 6 * D])
    C.w_in = inp("w_in", [DEPTH, D, NCOLS])
    C.w_uk_t = inp("w_uk_t", [DEPTH, 8, 64, 128])
    C.g_kv = inp("g_kv", [DEPTH, 128])
    C.ident_bf = inp("ident_bf", [128, 128], BF16)
    C.negU_bf = inp("negU_bf", [128, 128], BF16)
    C.ones_bf = inp("ones_bf", [128, 128], BF16)
    C.cm_bf = inp("cm_bf", [4, 128, 512], BF16)
    C.w_uv = inp("w_uv", [DEPTH, 128, 8, 64])
    C.rel_bias = inp("rel_bias", [32, 8])
    C.rel_bias_t = inp("rel_bias_t", [8, 32])
    C.dmask = inp("dmask", [128, 128])
    C.irep_bf = inp("irep_bf", [128, 512], BF16)
    C.ident_f = inp("ident_f", [128, 128])
    C.tz = inp("tz", [2, 128, 1024])
    C.bmask = inp("bmask", [8, 1024])
    C.w_a_out = inp("w_a_out", [DEPTH, 512, D])
    C.w_b_out = inp("w_b_out", [DEPTH, 512, D])
    C.w_o = inp("w_o", [DEPTH, D, D])
    C.ln1_g = inp("ln1_g", [DEPTH, D])
    C.ln1_b = inp("ln1_b", [DEPTH, D])
    C.ln2_g = inp("ln2_g", [DEPTH, D])
    C.ln2_b = inp("ln2_b", [DEPTH, D])
    C.w_router = inp("w_router", [DEPTH, D, E])
    C.b_router = inp("b_router", [DEPTH, E])
    C.w_gu = inp("w_gu", [DEPTH, E, D, 2 * FF])
    C.b_gu_t = inp("b_gu_t", [DEPTH, E, 128, 16])
    C.w_dn = inp("w_dn", [DEPTH, E, FF, D])
    C.b_dn = inp("b_dn", [DEPTH, E, D])
    C.out = nc.dram_tensor("out", [S, D], F32, kind="ExternalOutput").ap()
    C.r_out = Res("out")
    C.r_x = Res("x")
    with contextlib.ExitStack() as es:
        C.es = es
        C.kb = KB(nc, es)
        C.ps = [(es.enter_context(nc.psum_tensor(f"ps{i}", [128, 512], F32)), Res(f"ps{i}", psum=True)) for i in range(8)]
        alloc_scratch(C)
        phase_ada(C)
        x_src, r_xsrc = C.x, C.r_x
        for l in range(nlayers):
            if upto >= 1:
                phase_proj(C, l, x_src, r_xsrc)
            if upto >= 2 and not C.skip_sb:
                phase_sb(C, l)
            if upto >= 3 and not C.skip_dsa:
                phase_dsa(C, l)
            if upto >= 4:
                phase_out(C, l, x_src, r_xsrc)
            if upto >= 5:
                last = (l == DEPTH - 1)
                x_dst, r_xdst = (C.out, C.r_out) if last else (C.x2_d, C.r_x2)
                phase_moe(C, l, x_dst, r_xdst)
                x_src, r_xsrc = x_dst, r_xdst
        C.kb.barrier()
    C.ninst = C.kb.ninst
    return nc, C


def host_inputs(inputs, b):
    f = np.float32
    return {
        "x": np.ascontiguousarray(inputs["x"][b]),
        "c_col": np.ascontiguousarray(np.asarray(inputs["c"][b], f).reshape(8, 128).T),
        "w_ada": inputs["w_ada"], "b_ada": inputs["b_ada"], "w_in": inputs["w_in"],
        "w_uk_t": np.ascontiguousarray(np.transpose(inputs["w_uk"], (0, 2, 3, 1))),
        "g_kv": inputs["g_kv"],
        "w_a_out": inputs["w_a_out"], "w_b_out": inputs["w_b_out"], "w_o": inputs["w_o"],
        "ln1_g": inputs["ln1_g"], "ln1_b": inputs["ln1_b"], "ln2_g": inputs["ln2_g"], "ln2_b": inputs["ln2_b"],
        "w_router": inputs["w_router"], "b_router": inputs["b_router"],
        "w_gu": inputs["w_gu"], "w_dn": inputs["w_dn"], "b_dn": inputs["b_dn"],
        "b_gu_t": np.ascontiguousarray(np.asarray(inputs["b_gu"], f).reshape(DEPTH, E, 16, 128).transpose(0, 1, 3, 2)),
        "w_uv": inputs["w_uv"], "rel_bias": inputs["rel_bias"],
        "rel_bias_t": np.ascontiguousarray(np.asarray(inputs["rel_bias"], f).T),
        "tz": _tz_table(np.asarray(inputs["rel_bias"], f)),
        "ident_bf": np.eye(128, dtype=np.float32).astype(ml_dtypes.bfloat16),
        **CONSTS,
    }


def _make_consts():
    bf = ml_dtypes.bfloat16
    jj = np.arange(128)
    negU = -(jj[:, None] >= jj[None, :]).astype(np.float32)
    ones = np.ones((128, 128), np.float32)
    s_ = np.arange(128)[None, :, None]
    t_ = np.arange(512)[None, None, :]
    rel = np.arange(4)[:, None, None]
    cm = ((s_ + 128 * rel) < t_).astype(np.float32)
    dmask = np.where(jj[None, :] > jj[:, None], np.float32(NEG), np.float32(0)).astype(np.float32)
    irep = np.tile(np.eye(128, dtype=np.float32), (1, 4))
    bmask = np.zeros((8, 8, 128), np.float32)
    for h in range(8):
        bmask[h, h, :] = 1.0
    return {"negU_bf": negU.astype(bf), "ones_bf": ones.astype(bf), "cm_bf": cm.astype(bf),
            "dmask": dmask, "irep_bf": irep.astype(bf), "ident_f": np.eye(128, dtype=np.float32),
            "bmask": bmask.reshape(8, 1024)}


def _t5_bucket(n):
    import math
    max_exact = 16
    nf = np.maximum(n, 1).astype(np.float32)
    large = max_exact + (np.log(nf / np.float32(max_exact)) / np.float32(math.log(128 / max_exact))
                         * np.float32(32 - max_exact)).astype(np.int32)
    large = np.minimum(large, 31)
    return np.where(n < max_exact, n, large)


_TZ_IDX = None


def _tz_table(rel_bias):
    global _TZ_IDX
    if _TZ_IDX is None:
        s_ = np.arange(128)[None, :, None]
        t_ = np.arange(128)[None, None, :]
        rel = np.arange(2)[:, None, None]
        dist = np.maximum(128 * rel + t_ - s_, 0)
        _TZ_IDX = _t5_bucket(dist)
    g = rel_bias[_TZ_IDX]
    return np.ascontiguousarray(np.transpose(g, (0, 1, 3, 2)).reshape(2, 128, 1024))


CONSTS = _make_consts()


def kernel(**inputs):
    inputs = {k: np.asarray(v) for k, v in inputs.items()}
    nc, _ = build()
    n = 8
    in_maps = [host_inputs(inputs, b) for b in range(n)]
    res = run_bass_kernel_spmd(nc, in_maps, core_ids=list(range(n)))
    out = np.stack([np.asarray(res.results[b]["out"], dtype=np.float32) for b in range(n)], axis=0)
    return out
```
